# Optimizing a Trainium2 kernel written in Bass

```python
import math
import jax, jax.numpy as jnp
from jax import lax
import numpy as np

D_MODEL = 2048
BATCH = 4
SEQ = 4096
DEPTH = 2

GRID_W = 64
CTX_LEN = 256
N_MIXERS = 2
N_HYENA_LAYERS = (DEPTH + 1) // 2
N_S5_LAYERS = DEPTH // 2
HY_EMB = 33
HY_BANDS = (HY_EMB - 1) // 2
HY_ORDER = 64
HY_DECAY_TARGET = 1e-2
HY_FAST_PCT = 0.3
HY_SLOW_PCT = 1.5
S5_H = 16
S5_P = 64
S5_G = D_MODEL // S5_H
S5_DT_MIN = 1e-3
S5_DT_MAX = 1e-1
N_GROUPS = 4
EXPERTS_PER_GROUP = 8
N_EXPERTS = N_GROUPS * EXPERTS_PER_GROUP
TOP_K = 2
D_EXPERT = D_MODEL // 2
MOE_BLOCK = 128
NORM_EPS = 1e-6

kernel_name = "hybrid_hyena_s5_hmoe_dit"


def _rmsnorm(x, g):
    x32 = x.astype(jnp.float32)
    y = x32 * lax.rsqrt(jnp.mean(x32 * x32, axis=-1, keepdims=True) + NORM_EPS)
    return y.astype(x.dtype) * g


def _modulate(h, shift, scale):
    return h * (1 + scale) + shift


def _hyena_filter(L, fw1, fb1, fw2, fb2, fw3, freq):
    f32 = jnp.float32
    t = jnp.linspace(0.0, 1.0, L, dtype=f32)[:, None]
    w = (2.0 * math.pi / L) * jnp.arange(L, dtype=f32)[:, None]
    bands = jnp.linspace(1e-4, HY_BANDS - 1, HY_BANDS, dtype=f32)[None, :]
    z = jnp.concatenate([t, jnp.cos(bands * w), -jnp.sin(bands * w)], axis=-1)
    fr = freq.astype(f32)
    h = jnp.sin(fr * (z @ fw1.astype(f32) + fb1.astype(f32)))
    h = jnp.sin(fr * (h @ fw2.astype(f32) + fb2.astype(f32)))
    h = (h @ fw3.astype(f32)).reshape(L, 2, D_MODEL)
    max_decay = math.log(HY_DECAY_TARGET) / HY_FAST_PCT
    min_decay = math.log(HY_DECAY_TARGET) / HY_SLOW_PCT
    deltas = jnp.abs(jnp.linspace(min_decay, max_decay, D_MODEL, dtype=f32))
    h = h * jnp.exp(-t * deltas)[:, None, :]
    k = jnp.concatenate([h[:, 0], jnp.zeros((1, D_MODEL), f32), h[:0:-1, 1]], axis=0)
    return k / jnp.sum(jnp.abs(k), axis=0, keepdims=True)


def _hyena_mix(u, w_in, b_in, conv_w, conv_b, fw1, fb1, fw2, fb2, fw3, freq, skip, w_out, b_out):
    L = u.shape[1]
    z = u @ w_in + b_in
    zp = jnp.pad(z, ((0, 0), (1, 1), (0, 0)))
    z = zp[:, :-2] * conv_w[0] + zp[:, 1:-1] * conv_w[1] + zp[:, 2:] * conv_w[2] + conv_b
    x0, x1, v = jnp.split(z, 3, axis=-1)
    v = (v * x1).astype(jnp.float32)
    k = _hyena_filter(L, fw1, fb1, fw2, fb2, fw3, freq)
    n = 2 * L
    y = jnp.fft.irfft(jnp.fft.rfft(v, n=n, axis=1) * jnp.fft.rfft(k, n=n, axis=0)[None], n=n, axis=1)[:, :L]
    y = y + skip.astype(jnp.float32) * v
    y = y.astype(u.dtype) * x0
    return y @ w_out + b_out


def _ssm_op(e1, e2):
    a1, b1 = e1
    a2, b2 = e2
    return a1 * a2, a2 * b1 + b2


def _diag_scan(bu, lam_bar, h0):
    if h0 is not None:
        bu = bu.at[:, 0].add(lam_bar * h0)
    a = jnp.broadcast_to(lam_bar, (1, bu.shape[1]) + lam_bar.shape)
    _, h = lax.associative_scan(_ssm_op, (a, bu), axis=1)
    return h


def _s5_mix(u_lat, u_ctx, a_re, a_im, log_step, b_re, b_im, c_re, c_im, d_skip, w1, b1, w2, b2, ctx_out):
    f32 = jnp.float32
    Bn, L, _ = u_lat.shape
    Lc = u_ctx.shape[1]
    ul = u_lat.astype(f32)
    uc = u_ctx.astype(f32)
    ulg = ul.reshape(Bn, L, S5_G, S5_H)
    ucg = uc.reshape(Bn, Lc, S5_G, S5_H)
    y_lat = d_skip.astype(f32) * ul
    y_ctx = d_skip.astype(f32) * uc if ctx_out else None
    for d in range(2):
        lam = lax.complex(a_re[d].astype(f32), a_im[d].astype(f32))
        step = jnp.exp(log_step[d].astype(f32))[:, None]
        lam_bar = jnp.exp(lam * step)
        b_bar = ((lam_bar - 1.0) / lam)[..., None] * lax.complex(b_re[d].astype(f32), b_im[d].astype(f32))
        c_mat = lax.complex(c_re[d].astype(f32), c_im[d].astype(f32))
        orient = (lambda t: jnp.flip(t, axis=1)) if d == 1 else (lambda t: t)
        h_ctx = _diag_scan(orient(jnp.einsum('blgh,gph->blgp', ucg, b_bar)), lam_bar, None)
        h_lat = _diag_scan(orient(jnp.einsum('blgh,gph->blgp', ulg, b_bar)), lam_bar, h_ctx[:, -1])
        y_lat = y_lat + orient(jnp.real(jnp.einsum('blgp,ghp->blgh', h_lat, c_mat))).reshape(Bn, L, D_MODEL)
        if ctx_out:
            y_ctx = y_ctx + orient(jnp.real(jnp.einsum('blgp,ghp->blgh', h_ctx, c_mat))).reshape(Bn, Lc, D_MODEL)

    def glu(y, dtype):
        y = jax.nn.gelu(y).astype(dtype)
        return (y @ w1 + b1) * jax.nn.sigmoid(y @ w2 + b2)

    o_lat = glu(y_lat, u_lat.dtype)
    o_ctx = glu(y_ctx, u_ctx.dtype) if ctx_out else None
    return o_lat, o_ctx


def _hier_moe(t, wg, bg, we, be, w_gate, w_up, w_down):
    T = t.shape[0]
    pg = jax.nn.softmax((t @ wg + bg).astype(jnp.float32), axis=-1)
    p_top, g_idx = lax.top_k(pg, 1)
    le = (t @ we + be).astype(jnp.float32).reshape(T, N_GROUPS, EXPERTS_PER_GROUP)
    le_sel = jnp.take_along_axis(le, g_idx[:, :, None], axis=1)[:, 0]
    vals, e_idx = lax.top_k(le_sel, TOP_K)
    gate = p_top * jax.nn.softmax(vals, axis=-1)
    expert = g_idx * EXPERTS_PER_GROUP + e_idx

    A = T * TOP_K
    e_flat = expert.reshape(-1)
    order = jnp.argsort(e_flat)
    e_sorted = e_flat[order]
    tok_sorted = order // TOP_K
    gate_sorted = gate.reshape(-1)[order].astype(t.dtype)
    counts = jax.ops.segment_sum(jnp.ones((A,), jnp.int32), e_flat, num_segments=N_EXPERTS)
    start = jnp.cumsum(counts) - counts
    padded = (counts + MOE_BLOCK - 1) // MOE_BLOCK * MOE_BLOCK
    pad_end = jnp.cumsum(padded)
    pad_start = pad_end - padded
    dest = pad_start[e_sorted] + jnp.arange(A, dtype=jnp.int32) - start[e_sorted]
    n_blocks = -(-A // MOE_BLOCK) + N_EXPERTS
    buf = jnp.zeros((n_blocks * MOE_BLOCK, t.shape[1]), t.dtype).at[dest].set(t[tok_sorted])
    block_expert = jnp.minimum(
        jnp.searchsorted(pad_end, jnp.arange(n_blocks, dtype=jnp.int32) * MOE_BLOCK, side='right'),
        N_EXPERTS - 1)

    def expert_block(args):
        xb, e = args
        h = jax.nn.silu(xb @ w_gate[e]) * (xb @ w_up[e])
        return h @ w_down[e]

    out = lax.map(expert_block, (buf.reshape(n_blocks, MOE_BLOCK, -1), block_expert))
    y = out.reshape(n_blocks * MOE_BLOCK, -1)[dest] * gate_sorted[:, None]
    return jax.ops.segment_sum(y, tok_sorted, num_segments=T)


def setup_inputs(seed: int = 0) -> dict:
    key = jax.random.key(seed)
    ks = iter(jax.random.split(key, 64))
    f32 = jnp.float32
    D = D_MODEL
    NH, NS = N_HYENA_LAYERS, N_S5_LAYERS

    def nrm(shape, scale):
        return scale * jax.random.normal(next(ks), shape, f32)

    return {
        "x": nrm((BATCH, SEQ, D), 1.0),
        "c": nrm((BATCH, D), 1.0),
        "ctx": nrm((BATCH, CTX_LEN, D), 1.0),
        "c_ctx": nrm((D,), 1.0),
        "ada_w": nrm((DEPTH, D, 6 * D), 0.5 * D ** -0.5),
        "ada_b": nrm((DEPTH, 6 * D), 0.02),
        "norm_g": 1.0 + nrm((DEPTH, 2, D), 0.05),
        "final_g": 1.0 + nrm((D,), 0.05),
        "hy_w_in": nrm((NH, D, 3 * D), D ** -0.5),
        "hy_b_in": nrm((NH, 3 * D), 0.02),
        "hy_conv_w": nrm((NH, 3, 3 * D), 3 ** -0.5),
        "hy_conv_b": nrm((NH, 3 * D), 0.02),
        "hy_fw1": nrm((NH, HY_EMB, HY_ORDER), HY_EMB ** -0.5),
        "hy_fb1": nrm((NH, HY_ORDER), 0.5),
        "hy_fw2": nrm((NH, HY_ORDER, HY_ORDER), HY_ORDER ** -0.5),
        "hy_fb2": nrm((NH, HY_ORDER), 0.5),
        "hy_fw3": nrm((NH, HY_ORDER, 2 * D), HY_ORDER ** -0.5),
        "hy_freq": 1.0 + nrm((NH, HY_ORDER), 0.1),
        "hy_skip": nrm((NH, D), 0.5),
        "hy_w_out": nrm((NH, D, D), D ** -0.5),
        "hy_b_out": nrm((NH, D), 0.02),
        "s5_a_re": -0.5 + nrm((NS, 2, S5_G, S5_P), 0.01),
        "s5_a_im": math.pi * jnp.arange(S5_P, dtype=f32) + nrm((NS, 2, S5_G, S5_P), 0.01),
        "s5_log_step": jax.random.uniform(next(ks), (NS, 2, S5_G), f32,
                                          math.log(S5_DT_MIN), math.log(S5_DT_MAX)),
        "s5_b_re": nrm((NS, 2, S5_G, S5_P, S5_H), (2 * S5_H) ** -0.5),
        "s5_b_im": nrm((NS, 2, S5_G, S5_P, S5_H), (2 * S5_H) ** -0.5),
        "s5_c_re": nrm((NS, 2, S5_G, S5_H, S5_P), S5_P ** -0.5),
        "s5_c_im": nrm((NS, 2, S5_G, S5_H, S5_P), S5_P ** -0.5),
        "s5_d": nrm((NS, D), 1.0),
        "s5_w1": nrm((NS, D, D), D ** -0.5),
        "s5_b1": nrm((NS, D), 0.02),
        "s5_w2": nrm((NS, D, D), D ** -0.5),
        "s5_b2": nrm((NS, D), 0.02),
        "moe_wg": nrm((DEPTH, D, N_GROUPS), D ** -0.5),
        "moe_bg": nrm((DEPTH, N_GROUPS), 0.01),
        "moe_we": nrm((DEPTH, D, N_EXPERTS), D ** -0.5),
        "moe_be": nrm((DEPTH, N_EXPERTS), 0.01),
        "moe_w_gate": nrm((DEPTH, N_EXPERTS, D, D_EXPERT), D ** -0.5),
        "moe_w_up": nrm((DEPTH, N_EXPERTS, D, D_EXPERT), D ** -0.5),
        "moe_w_down": nrm((DEPTH, N_EXPERTS, D_EXPERT, D), D_EXPERT ** -0.5),
    }


def reference(x, c, ctx, c_ctx, ada_w, ada_b, norm_g, final_g,
              hy_w_in, hy_b_in, hy_conv_w, hy_conv_b, hy_fw1, hy_fb1, hy_fw2, hy_fb2, hy_fw3,
              hy_freq, hy_skip, hy_w_out, hy_b_out,
              s5_a_re, s5_a_im, s5_log_step, s5_b_re, s5_b_im, s5_c_re, s5_c_im, s5_d,
              s5_w1, s5_b1, s5_w2, s5_b2,
              moe_wg, moe_bg, moe_we, moe_be, moe_w_gate, moe_w_up, moe_w_down):
    xl, xc = x, ctx
    for i in range(DEPTH):
        last = i == DEPTH - 1
        j = i // N_MIXERS
        sh_a, sc_a, gt_a, sh_f, sc_f, gt_f = [m[:, None, :] for m in
                                             jnp.split(jax.nn.silu(c) @ ada_w[i] + ada_b[i], 6, axis=-1)]
        csh_a, csc_a, cgt_a, csh_f, csc_f, cgt_f = jnp.split(jax.nn.silu(c_ctx) @ ada_w[i] + ada_b[i], 6, axis=-1)

        hl = _modulate(_rmsnorm(xl, norm_g[i, 0]), sh_a, sc_a)
        hc = _modulate(_rmsnorm(xc, norm_g[i, 0]), csh_a, csc_a)
        if i % N_MIXERS == 0:
            ol = _hyena_mix(hl, hy_w_in[j], hy_b_in[j], hy_conv_w[j], hy_conv_b[j], hy_fw1[j], hy_fb1[j],
                            hy_fw2[j], hy_fb2[j], hy_fw3[j], hy_freq[j], hy_skip[j], hy_w_out[j], hy_b_out[j])
            oc = None if last else _hyena_mix(hc, hy_w_in[j], hy_b_in[j], hy_conv_w[j], hy_conv_b[j], hy_fw1[j],
                                              hy_fb1[j], hy_fw2[j], hy_fb2[j], hy_fw3[j], hy_freq[j],
                                              hy_skip[j], hy_w_out[j], hy_b_out[j])
        else:
            ol, oc = _s5_mix(hl, hc, s5_a_re[j], s5_a_im[j], s5_log_step[j], s5_b_re[j], s5_b_im[j],
                             s5_c_re[j], s5_c_im[j], s5_d[j], s5_w1[j], s5_b1[j], s5_w2[j], s5_b2[j],
                             not last)
        xl = xl + gt_a * ol
        if not last:
            xc = xc + cgt_a * oc

        n_lat = xl.shape[0] * xl.shape[1]
        tok = _modulate(_rmsnorm(xl, norm_g[i, 1]), sh_f, sc_f).reshape(n_lat, D_MODEL)
        if not last:
            tok_c = _modulate(_rmsnorm(xc, norm_g[i, 1]), csh_f, csc_f).reshape(-1, D_MODEL)
            tok = jnp.concatenate([tok, tok_c], axis=0)
        mo = _hier_moe(tok, moe_wg[i], moe_bg[i], moe_we[i], moe_be[i],
                       moe_w_gate[i], moe_w_up[i], moe_w_down[i])
        xl = xl + gt_f * mo[:n_lat].reshape(xl.shape)
        if not last:
            xc = xc + cgt_f * mo[n_lat:].reshape(xc.shape)
    return _rmsnorm(xl, final_g)
```

```python
import contextlib
import numpy as np
import concourse.bass as bass
import concourse.mybir as mybir
from concourse.bass_utils import run_bass_kernel_spmd

F32 = mybir.dt.float32
BF16 = mybir.dt.bfloat16
I32 = mybir.dt.int32
ALU = mybir.AluOpType
AF = mybir.ActivationFunctionType
AX = mybir.AxisListType

COMPUTE = ("pe", "act", "dve", "pool")


class Prog:
    def __init__(self):
        self.nc = bass.Bass("TRN2", target_bir_lowering=False)
        self.stack = contextlib.ExitStack()
        self.ops = []
        self.lastw = {}
        self.readers = {}
        self.out_names = []
        self.n_sb = 0
        self.barrier_idx = None

    def din(self, name, shape, dt=F32):
        return self.nc.dram_tensor(name, list(shape), dt, kind="ExternalInput").ap()

    def dout(self, name, shape, dt=F32):
        self.out_names.append(name)
        return self.nc.dram_tensor(name, list(shape), dt, kind="ExternalOutput").ap()

    def dtmp(self, name, shape, dt=F32):
        return self.nc.dram_tensor(name, list(shape), dt, kind="Internal").ap()

    def sb(self, name, shape, dt=F32):
        return self.stack.enter_context(self.nc.sbuf_tensor(name, list(shape), dt))

    def ps(self, name, shape, dt=F32):
        return self.stack.enter_context(self.nc.psum_tensor(name, list(shape), dt))

    def barrier(self):
        deps = set()
        last = {}
        for i, o in enumerate(self.ops):
            key = ("dma", o["grp"]) if o["dma"] else ("eng", o["eng"])
            last[key] = i
        deps = set(last.values())
        idx = len(self.ops)
        d = self.sb("bar%d" % idx, [128, 1])
        self.ops.append(dict(eng="dve", fn=lambda e: e.memset(d[:], 0.0), deps=deps, dma=False))
        self.barrier_idx = idx

    def _deps(self, r, w):
        deps = set()
        if self.barrier_idx is not None:
            deps.add(self.barrier_idx)
        for k in r:
            if k in self.lastw:
                deps.add(self.lastw[k])
        for k in w:
            if k in self.lastw:
                deps.add(self.lastw[k])
            for o in self.readers.get(k, ()):
                deps.add(o)
        return deps

    def _commit(self, idx, r, w):
        for k in r:
            self.readers.setdefault(k, []).append(idx)
        for k in w:
            self.lastw[k] = idx
            self.readers[k] = []

    def op(self, eng, fn, r=(), w=()):
        assert eng in COMPUTE
        idx = len(self.ops)
        deps = self._deps(r, w)
        self.ops.append(dict(eng=eng, fn=fn, deps=deps, dma=False))
        self._commit(idx, r, w)
        return idx

    def dma(self, q, out, in_, r=(), w=(), grp=None, **kw):
        idx = len(self.ops)
        deps = self._deps(r, w)
        if grp is None:
            grp = ("g", tuple(w)[0] if len(w) else tuple(r)[0])
        self.ops.append(dict(eng=q, fn=lambda e: e.dma_start(out=out, in_=in_, **kw),
                             deps=deps, dma=True, grp=grp))
        self._commit(idx, r, w)
        return idx

    def build(self):
        nc = self.nc
        ops = self.ops
        cnt = {}
        semkeys = []
        for o in ops:
            key = ("dma", o["grp"]) if o["dma"] else ("eng", o["eng"])
            if key not in cnt:
                cnt[key] = 0
                semkeys.append(key)
            cnt[key] += 16 if o["dma"] else 1
            o["sem"] = key
            o["val"] = cnt[key]
        sems = {}
        for i, key in enumerate(semkeys):
            sems[key] = self.stack.enter_context(nc.semaphore("s%d" % i))
        streams = {}
        for i, o in enumerate(ops):
            streams.setdefault(o["eng"], []).append(i)
        final_waits = [(sems[k], cnt[k]) for k in semkeys if k[0] == "dma"]
        blk = self.stack.enter_context(nc.Block())

        def emit_stream(eng_name, e, last=False):
            waited = {}
            for i in streams.get(eng_name, []):
                o = ops[i]
                need = {}
                for d in o["deps"]:
                    od = ops[d]
                    if od["eng"] == "pe" and o["eng"] == "pe" and not od["dma"] and not o["dma"]:
                        continue
                    k = od["sem"]
                    need[k] = max(need.get(k, 0), od["val"])
                for k, v in need.items():
                    if waited.get(k, 0) < v:
                        e.wait_ge(sems[k], v)
                        waited[k] = v
                ins = o["fn"](e)
                ins.then_inc(sems[o["sem"]], 16 if o["dma"] else 1)
            if last:
                for s, v in final_waits:
                    e.wait_ge(s, v)

        @blk.tensor
        def _(e):
            emit_stream("pe", e)

        @blk.scalar
        def _(e):
            emit_stream("act", e)

        @blk.vector
        def _(e):
            emit_stream("dve", e)

        @blk.gpsimd
        def _(e):
            emit_stream("pool", e)

        @blk.sync
        def _(e):
            emit_stream("sync", e, last=True)

        self.stack.close()
        return nc


def run(prog_nc, in_maps, n=8):
    res = run_bass_kernel_spmd(prog_nc, in_maps, core_ids=list(range(n)))
    return res.results


class Cfg:
    def __init__(self, D=2048, B=4, L=4096, LC=256):
        self.D, self.B, self.L, self.LC = D, B, L, LC
        self.NCORE = 8
        self.ND = D // 128
        self.G = D // 16
        self.DE = D // 2
        self.CPB = self.NCORE // B
        self.TL = L // self.CPB
        self.TC = LC // self.CPB
        self.Cc = D // self.NCORE
        self.NE = 32
        self.EPC = self.NE // self.NCORE


FULL = Cfg()
EPS = 1e-6
MAGIC = 12582912.0
TWO_PI = 6.283185307179586
PI_LO = 3.1415925


def fm(a, ND):
    T, D = a.shape
    return np.ascontiguousarray(a.T.reshape(ND, 128, T).transpose(1, 0, 2))


def unfm(a):
    P, ND, T = a.shape
    return np.ascontiguousarray(a.transpose(1, 0, 2).reshape(ND * P, T).T)


def vfm(v, ND):
    return np.ascontiguousarray(v.reshape(ND, 128).T)


def tiles(n, t):
    return [(s, min(t, n - s)) for s in range(0, n, t)]


def build_mods(cfg):
    p = Prog()
    ND, NB1 = cfg.ND, cfg.B + 1
    W = 6 * cfg.D // cfg.NCORE
    ccT = p.din("ccT", [128, ND, NB1])
    aw = p.din("aw", [2, 128, ND, W])
    ab = p.din("ab", [2, 1, W])
    out = p.dout("mods", [2, NB1, W])
    cs = p.sb("cs", [128, ND, 128])
    p.op("dve", lambda e: e.memset(cs[:], 0.0), w=["cs"])
    p.dma("sync", cs[:, :, :NB1], ccT[:, :, :], w=["cs"])
    p.op("act", lambda e: e.activation(out=cs[:, :, :NB1], in_=cs[:, :, :NB1], func=AF.Silu), r=["cs"], w=["cs"])
    WT = 512
    wt_sb = [p.sb("wt%d" % i, [128, ND, WT]) for i in range(2)]
    bt = [p.sb("bt%d" % i, [NB1, WT]) for i in range(2)]
    ot = [p.sb("ot%d" % i, [NB1, WT]) for i in range(2)]
    pm = [p.ps("pm%d" % i, [128, WT]) for i in range(2)]
    it = 0
    for l in range(2):
        for (c0, cw) in tiles(W, WT):
            s = it % 2
            it += 1
            p.dma("sync", wt_sb[s][:, :, :cw], aw[l, :, :, c0:c0 + cw], w=[("wt", s)])
            p.dma("pool", bt[s][:, :cw], ab[l, 0:1, c0:c0 + cw].broadcast_to([NB1, cw]), w=[("bt", s)])
            for k in range(ND):
                p.op("pe", lambda e, s=s, k=k, cw=cw: e.matmul(pm[s][:, :cw], lhsT=cs[:, k, :], rhs=wt_sb[s][:, k, :cw],
                                                               start=(k == 0), stop=(k == ND - 1)),
                     r=["cs", ("wt", s)], w=[("pm", s)])
            p.op("dve", lambda e, s=s, cw=cw: e.tensor_tensor(out=ot[s][:, :cw], in0=pm[s][:NB1, :cw], in1=bt[s][:, :cw], op=ALU.add),
                 r=[("pm", s), ("bt", s)], w=[("ot", s)])
            p.dma("sync", out[l, :, c0:c0 + cw], ot[s][:, :cw], r=[("ot", s)], w=[("out", l, c0)], grp=("st", s))
    return p.build()


def run_mods(cfg, I):
    ND, NC = cfg.ND, cfg.NCORE
    W = 6 * cfg.D // NC
    cc = np.concatenate([I["c"], I["c_ctx"][None]], 0).astype(np.float32)
    ccT = fm(cc, ND)
    nc = build_mods(cfg)
    ims = []
    for c in range(NC):
        aw = I["ada_w"][:, :, c * W:(c + 1) * W]
        aw = np.ascontiguousarray(aw.reshape(2, ND, 128, W).transpose(0, 2, 1, 3))
        ab = np.ascontiguousarray(I["ada_b"][:, None, c * W:(c + 1) * W])
        ims.append({"ccT": ccT, "aw": aw, "ab": ab})
    res = run(nc, ims)
    mods = np.concatenate([r["mods"] for r in res], axis=2)
    return mods


def emit_rstd(p, cfg, xt, xkey, ncols, ones, sq, sqkey, pss, psskey, rstd, rkey):
    ND = cfg.ND
    p.op("pool", lambda e: e.tensor_tensor(out=sq[:, :, :ncols], in0=xt[:, :, :ncols], in1=xt[:, :, :ncols], op=ALU.mult),
         r=[xkey], w=[sqkey])
    for k in range(ND):
        p.op("pe", lambda e, k=k: e.matmul(pss[:, :ncols], lhsT=ones[:], rhs=sq[:, k, :ncols], start=(k == 0), stop=(k == ND - 1)),
             r=[sqkey, "ones"], w=[psskey])
    p.op("act", lambda e: e.activation(out=rstd[:, :ncols], in_=pss[:, :ncols], func=AF.Sqrt, bias=EPS, scale=1.0 / cfg.D),
         r=[psskey], w=[rkey])
    p.op("dve", lambda e: e.reciprocal(out=rstd[:, :ncols], in_=rstd[:, :ncols]), r=[rkey], w=[rkey])


def build_h1(cfg):
    p = Prog()
    ND, D, TL, TC = cfg.ND, cfg.D, cfg.TL, cfg.TC
    TT = TL + TC + 4
    NT = TL + TC
    xT = p.din("xT", [128, ND, TT])
    hm = p.din("hm", [128, 4])
    mv = p.din("mv", [128, 5, ND])
    w_in = p.din("w_in", [128, ND, 3 * D])
    fv = p.din("fv", [128, 5, 3 * ND])
    vT = p.dout("vT", [128, ND, NT], BF16)
    x0T = p.dout("x0T", [128, ND, NT], BF16)

    ones = p.sb("ones", [128, 128])
    p.op("dve", lambda e: e.memset(ones[:], 1.0), w=["ones"])
    hms = p.sb("hms", [128, 4]); p.dma("sync", hms[:], hm[:, :], w=["hms"])
    mvs = p.sb("mvs", [128, 5, ND]); p.dma("sync", mvs[:], mv[:, :, :], w=["mvs"])
    fvs = p.sb("fvs", [128, 5, 3 * ND]); p.dma("sync", fvs[:], fv[:, :, :], w=["fvs"])
    ml = p.sb("ml", [128, 2, ND])
    for i, j in ((0, 1), (1, 3)):
        p.op("dve", lambda e, i=i, j=j: e.scalar_tensor_tensor(out=ml[:, i, :], in0=mvs[:, j, :], scalar=1.0, in1=mvs[:, 0, :],
                                                               op0=ALU.add, op1=ALU.mult), r=["mvs"], w=["ml"])
    u = p.sb("u", [128, ND, TT], BF16)
    TK = 256
    xt = p.sb("xt", [128, ND, TK]); sq = p.sb("sq", [128, ND, TK]); rstd = p.sb("rstd", [128, TK])
    pss = p.ps("pss", [128, TK])
    segs = [(0, TL + 2, 0), (TL + 2, TC + 2, 1)]
    for (s0, sl, mi) in segs:
        for (c0, cw) in tiles(sl, TK):
            a = s0 + c0
            p.dma("sync", xt[:, :, :cw], xT[:, :, a:a + cw], w=["xt"])
            emit_rstd(p, cfg, xt, "xt", cw, ones, sq, "sq", pss, "pss", rstd, "rstd")
            for k in range(ND):
                p.op("dve", lambda e, k=k, cw=cw: e.tensor_tensor(out=sq[:, k, :cw], in0=xt[:, k, :cw], in1=rstd[:, :cw], op=ALU.mult),
                     r=["xt", "rstd"], w=["sq"])
                p.op("dve", lambda e, k=k, cw=cw, a=a, mi=mi: e.tensor_scalar(
                    out=u[:, k, a:a + cw], in0=sq[:, k, :cw], scalar1=ml[:, mi, k:k + 1], scalar2=mvs[:, 2 + 2 * mi, k:k + 1],
                    op0=ALU.mult, op1=ALU.add), r=["sq", "ml", "mvs"], w=["u"])
    wf = [p.sb("wf%d" % i, [128, ND, 128]) for i in range(2)]
    wb = [p.sb("wb%d" % i, [128, ND, 128], BF16) for i in range(2)]
    z = p.sb("z", [128, TT])
    zc = [p.sb("zc%d" % i, [128, TT]) for i in range(3)]
    ob = [p.sb("ob%d" % i, [128, NT], BF16) for i in range(2)]
    pz = [p.ps("pz%d" % i, [128, 512]) for i in range(2)]
    wi = 0
    pi = 0
    for c in range(ND):
        for which, blk in ((1, ND + c), (2, 2 * ND + c), (0, c)):
            s = wi % 2
            wi += 1
            p.dma("sync", wf[s][:], w_in[:, :, blk * 128:(blk + 1) * 128], w=[("wf", s)])
            p.op("pool", lambda e, s=s: e.tensor_copy(out=wb[s][:], in_=wf[s][:]), r=[("wf", s)], w=[("wb", s)])
            for (c0, cw) in tiles(TT, 512):
                q = pi % 2
                pi += 1
                for k in range(ND):
                    p.op("pe", lambda e, s=s, q=q, k=k, c0=c0, cw=cw: e.matmul(
                        pz[q][:, :cw], lhsT=wb[s][:, k, :], rhs=u[:, k, c0:c0 + cw], start=(k == 0), stop=(k == ND - 1)),
                        r=[("wb", s), "u"], w=[("pz", q)])
                p.op("act", lambda e, q=q, c0=c0, cw=cw, blk=blk: e.activation(
                    out=z[:, c0:c0 + cw], in_=pz[q][:, :cw], func=AF.Identity, bias=fvs[:, 0, blk:blk + 1], scale=1.0),
                    r=[("pz", q), "fvs"], w=["z"])
            for hi, col in enumerate((0, TL + 1, TL + 2, TL + TC + 3)):
                p.op("dve", lambda e, hi=hi, col=col: e.tensor_scalar(out=z[:, col:col + 1], in0=z[:, col:col + 1],
                                                                     scalar1=hms[:, hi:hi + 1], scalar2=None, op0=ALU.mult),
                     r=["z", "hms"], w=["z"])
            zo = zc[which]
            zk = ("zc", which)
            for (a, n) in ((1, TL), (TL + 3, TC)):
                p.op("dve", lambda e, a=a, n=n, blk=blk, zo=zo: e.tensor_scalar(
                    out=zo[:, a:a + n], in0=z[:, a - 1:a - 1 + n], scalar1=fvs[:, 1, blk:blk + 1], scalar2=fvs[:, 4, blk:blk + 1],
                    op0=ALU.mult, op1=ALU.add), r=["z", "fvs"], w=[zk])
                p.op("dve", lambda e, a=a, n=n, blk=blk, zo=zo: e.scalar_tensor_tensor(
                    out=zo[:, a:a + n], in0=z[:, a:a + n], scalar=fvs[:, 2, blk:blk + 1], in1=zo[:, a:a + n],
                    op0=ALU.mult, op1=ALU.add), r=["z", "fvs", zk], w=[zk])
                p.op("dve", lambda e, a=a, n=n, blk=blk, zo=zo: e.scalar_tensor_tensor(
                    out=zo[:, a:a + n], in0=z[:, a + 1:a + 1 + n], scalar=fvs[:, 3, blk:blk + 1], in1=zo[:, a:a + n],
                    op0=ALU.mult, op1=ALU.add), r=["z", "fvs", zk], w=[zk])
            if which == 2:
                for (a, n, o0) in ((1, TL, 0), (TL + 3, TC, TL)):
                    p.op("pool", lambda e, a=a, n=n, o0=o0: e.tensor_tensor(out=ob[0][:, o0:o0 + n], in0=zc[2][:, a:a + n],
                                                                           in1=zc[1][:, a:a + n], op=ALU.mult),
                         r=[("zc", 1), ("zc", 2)], w=[("ob", 0)])
                p.dma("pool", vT[:, c, :], ob[0][:], r=[("ob", 0)], w=[("vT", c)], grp=("st", 0))
            if which == 0:
                for (a, n, o0) in ((1, TL, 0), (TL + 3, TC, TL)):
                    p.op("pool", lambda e, a=a, n=n, o0=o0: e.tensor_copy(out=ob[1][:, o0:o0 + n], in_=zc[0][:, a:a + n]),
                         r=[("zc", 0)], w=[("ob", 1)])
                p.dma("pool", x0T[:, c, :], ob[1][:], r=[("ob", 1)], w=[("x0T", c)], grp=("st", 1))
    return p.build()


def tok_layout(cfg, lat, ctx):
    outs = []
    for c in range(cfg.NCORE):
        b, h = c // cfg.CPB, c % cfg.CPB
        outs.append(np.concatenate([lat[b, h * cfg.TL:(h + 1) * cfg.TL], ctx[b, h * cfg.TC:(h + 1) * cfg.TC]], 0))
    return outs


def tok_unlayout(cfg, per_core):
    Dd = per_core[0].shape[1]
    lat = np.zeros((cfg.B, cfg.L, Dd), per_core[0].dtype)
    ctx = np.zeros((cfg.B, cfg.LC, Dd), per_core[0].dtype)
    for c in range(cfg.NCORE):
        b, h = c // cfg.CPB, c % cfg.CPB
        lat[b, h * cfg.TL:(h + 1) * cfg.TL] = per_core[c][:cfg.TL]
        ctx[b, h * cfg.TC:(h + 1) * cfg.TC] = per_core[c][cfg.TL:]
    return lat, ctx


def mod_vecs(cfg, mods_l, b):
    D = cfg.D
    names = ["sh_a", "sc_a", "gt_a", "sh_f", "sc_f", "gt_f"]
    out = {}
    for i, n in enumerate(names):
        out[n] = mods_l[b, i * D:(i + 1) * D]
        out["c" + n] = mods_l[cfg.B, i * D:(i + 1) * D]
    return out


def run_h1(cfg, I, mods):
    ND, D, TL, TC, NC = cfg.ND, cfg.D, cfg.TL, cfg.TC, cfg.NCORE
    nc = build_h1(cfg)
    x, ctx = I["x"], I["ctx"]
    w_in = np.ascontiguousarray(I["hy_w_in"][0].reshape(ND, 128, 3 * D).transpose(1, 0, 2))
    fvec = np.stack([vfm(v, 3 * ND) for v in (I["hy_b_in"][0], I["hy_conv_w"][0, 0], I["hy_conv_w"][0, 1],
                                               I["hy_conv_w"][0, 2], I["hy_conv_b"][0])], axis=1)
    fvec = np.ascontiguousarray(fvec)
    ims = []
    zrow = np.zeros((1, D), np.float32)
    for c in range(NC):
        b, h = c // cfg.CPB, c % cfg.CPB
        l0, l1 = h * TL, (h + 1) * TL
        c0, c1 = h * TC, (h + 1) * TC
        hl = x[b, l0 - 1:l0] if l0 > 0 else zrow
        hr = x[b, l1:l1 + 1] if l1 < cfg.L else zrow
        chl = ctx[b, c0 - 1:c0] if c0 > 0 else zrow
        chr_ = ctx[b, c1:c1 + 1] if c1 < cfg.LC else zrow
        cols = np.concatenate([hl, x[b, l0:l1], hr, chl, ctx[b, c0:c1], chr_], 0)
        hm = np.array([l0 > 0, l1 < cfg.L, c0 > 0, c1 < cfg.LC], np.float32)
        mvd = mod_vecs(cfg, mods[0], b)
        mv = np.stack([vfm(v, ND) for v in (I["norm_g"][0, 0], mvd["sc_a"], mvd["sh_a"], mvd["csc_a"], mvd["csh_a"])], axis=1)
        ims.append({"xT": fm(cols, ND), "hm": np.ascontiguousarray(np.broadcast_to(hm, (128, 4))),
                    "mv": np.ascontiguousarray(mv), "w_in": w_in, "fv": fvec})
    res = run(nc, ims)
    v = [unfm(np.asarray(r["vT"]).astype(np.float32)) for r in res]
    x0 = [unfm(np.asarray(r["x0T"]).astype(np.float32)) for r in res]
    return tok_unlayout(cfg, v), tok_unlayout(cfg, x0)


def hy_consts(Lx, D):
    f32 = np.float32
    t = np.linspace(0.0, 1.0, Lx, dtype=f32)[:, None]
    w = (f32(2.0 * np.pi / Lx) * np.arange(Lx, dtype=f32))[:, None]
    bands = np.linspace(1e-4, 15, 16, dtype=f32)[None, :]
    z = np.concatenate([t, np.cos(bands * w), -np.sin(bands * w)], axis=-1).astype(f32)
    max_decay = np.log(1e-2) / 0.3
    min_decay = np.log(1e-2) / 1.5
    deltas = np.abs(np.linspace(min_decay, max_decay, D, dtype=f32))
    win = np.exp(-t * deltas[None, :]).astype(f32)
    return z, win


def emit_sin(p, e_out, okey, src, skey, ncols, tmp, tkey, scale_ap, bias_ap, extra_r=()):
    t0, t1 = tmp
    p.op("dve", lambda e: e.tensor_scalar(out=t0[:, :ncols], in0=src, scalar1=scale_ap, scalar2=bias_ap, op0=ALU.mult, op1=ALU.add),
         r=[skey] + list(extra_r), w=[(tkey, 0)])
    p.op("dve", lambda e: e.tensor_scalar(out=t1[:, :ncols], in0=t0[:, :ncols], scalar1=1.0 / TWO_PI, scalar2=MAGIC, op0=ALU.mult, op1=ALU.add),
         r=[(tkey, 0)], w=[(tkey, 1)])
    p.op("dve", lambda e: e.tensor_scalar(out=t1[:, :ncols], in0=t1[:, :ncols], scalar1=MAGIC, scalar2=-TWO_PI, op0=ALU.subtract, op1=ALU.mult),
         r=[(tkey, 1)], w=[(tkey, 1)])
    p.op("dve", lambda e: e.tensor_tensor(out=t0[:, :ncols], in0=t0[:, :ncols], in1=t1[:, :ncols], op=ALU.add),
         r=[(tkey, 0), (tkey, 1)], w=[(tkey, 0)])
    p.op("dve", lambda e: e.tensor_scalar(out=t0[:, :ncols], in0=t0[:, :ncols], scalar1=PI_LO, scalar2=-PI_LO, op0=ALU.min, op1=ALU.max),
         r=[(tkey, 0)], w=[(tkey, 0)])
    p.op("act", lambda e: e.activation(out=e_out, in_=t0[:, :ncols], func=AF.Sin), r=[(tkey, 0)], w=[okey])


def build_filt(cfg):
    p = Prog()
    Cc = cfg.Cc
    CP = min(Cc, 128)
    NCH = Cc // CP
    fw1 = p.din("fw1", [128, 128]); fw2 = p.din("fw2", [128, 128]); fw3 = p.din("fw3", [128, 2, NCH, 128])
    pv = p.din("pv", [128, 3])
    w1s = p.sb("w1s", [128, 128]); w2s = p.sb("w2s", [128, 128]); w3s = p.sb("w3s", [128, 2, NCH, 128]); pvs = p.sb("pvs", [128, 3])
    p.dma("sync", w1s[:], fw1[:, :], w=["w1s"]); p.dma("sync", w2s[:], fw2[:, :], w=["w2s"])
    p.dma("sync", w3s[:], fw3[:, :, :, :], w=["w3s"]); p.dma("sync", pvs[:], pv[:, :], w=["pvs"])
    fb = p.sb("fb", [128, 2])
    p.op("dve", lambda e: e.tensor_scalar(out=fb[:, 0:2], in0=pvs[:, 0:2], scalar1=pvs[:, 2:3], scalar2=None, op0=ALU.mult),
         r=["pvs"], w=["fb"])
    Lmax = max(cfg.L, cfg.LC)
    CT = 512
    zt = p.sb("zt", [128, CT]); h1 = p.sb("h1", [128, CT]); h2 = p.sb("h2", [128, Lmax])
    tmp = [p.sb("tmpa", [128, CT]), p.sb("tmpb", [128, CT])]
    hw = [p.sb("hw%d" % i, [128, Lmax]) for i in range(2)]
    wn = p.sb("wn", [128, Lmax])
    ab = p.sb("ab", [128, Lmax]); nr = p.sb("nr", [128, 2]); rn = p.sb("rn", [128, 1])
    ho = [p.sb("ho%d" % i, [128, Lmax], BF16) for i in range(2)]
    pa = p.ps("pa", [128, CT]); pb = p.ps("pb", [128, CT]); pc = p.ps("pc", [128, CT])
    for li, Lx in enumerate((cfg.L, cfg.LC)):
        zT = p.din("zT%d" % li, [128, Lx]); winT = p.din("winT%d" % li, [CP, NCH, Lx])
        hsT = p.dout("hsT%d" % li, [CP, NCH, Lx], BF16); hdT = p.dout("hdT%d" % li, [CP, NCH, Lx], BF16)
        for (c0, cw) in tiles(Lx, CT):
            p.dma("sync", zt[:, :cw], zT[:, c0:c0 + cw], w=["zt"])
            p.op("pe", lambda e, cw=cw: e.matmul(pa[:, :cw], lhsT=w1s[:], rhs=zt[:, :cw], start=True, stop=True), r=["w1s", "zt"], w=["pa"])
            emit_sin(p, h1[:, :cw], "h1", pa[:, :cw], "pa", cw, tmp, "tmp", pvs[:, 2:3], fb[:, 0:1], extra_r=["pvs", "fb"])
            p.op("pe", lambda e, cw=cw: e.matmul(pb[:, :cw], lhsT=w2s[:], rhs=h1[:, :cw], start=True, stop=True), r=["w2s", "h1"], w=["pb"])
            emit_sin(p, h2[:, c0:c0 + cw], "h2", pb[:, :cw], "pb", cw, tmp, "tmp", pvs[:, 2:3], fb[:, 1:2], extra_r=["pvs", "fb"])
        for ch in range(NCH):
            p.dma("sync", wn[:CP, :Lx], winT[:, ch, :], w=["wn"])
            for d in range(2):
                for (c0, cw) in tiles(Lx, CT):
                    p.op("pe", lambda e, d=d, ch=ch, c0=c0, cw=cw: e.matmul(pc[:, :cw], lhsT=w3s[:, d, ch, :], rhs=h2[:, c0:c0 + cw], start=True, stop=True),
                         r=["w3s", "h2"], w=["pc"])
                    p.op("dve", lambda e, d=d, c0=c0, cw=cw: e.tensor_tensor(out=hw[d][:CP, c0:c0 + cw], in0=pc[:CP, :cw], in1=wn[:CP, c0:c0 + cw], op=ALU.mult),
                         r=["pc", "wn"], w=[("hw", d)])
            p.op("dve", lambda e: e.memset(hw[1][:CP, 0:1], 0.0), r=[("hw", 1)], w=[("hw", 1)])
            for d in range(2):
                p.op("dve", lambda e, d=d, Lx=Lx: e.scalar_tensor_tensor(out=ab[:CP, :Lx], in0=hw[d][:CP, :Lx], scalar=-1.0, in1=hw[d][:CP, :Lx], op0=ALU.mult, op1=ALU.max),
                     r=[("hw", d)], w=["ab"])
                p.op("dve", lambda e, d=d, Lx=Lx: e.tensor_reduce(out=nr[:CP, d:d + 1], in_=ab[:CP, :Lx], axis=AX.X, op=ALU.add),
                     r=["ab"], w=["nr"])
            p.op("dve", lambda e: e.tensor_tensor(out=rn[:CP, :], in0=nr[:CP, 0:1], in1=nr[:CP, 1:2], op=ALU.add), r=["nr"], w=["rn"])
            p.op("dve", lambda e: e.reciprocal(out=rn[:CP, :], in_=rn[:CP, :]), r=["rn"], w=["rn"])
            p.op("dve", lambda e, Lx=Lx: e.tensor_tensor(out=ab[:CP, :Lx], in0=hw[0][:CP, :Lx], in1=hw[1][:CP, :Lx], op=ALU.add),
                 r=[("hw", 0), ("hw", 1)], w=["ab"])
            p.op("dve", lambda e, Lx=Lx: e.tensor_scalar(out=ho[0][:CP, :Lx], in0=ab[:CP, :Lx], scalar1=rn[:CP, 0:1], scalar2=None, op0=ALU.mult),
                 r=["ab", "rn"], w=[("ho", 0)])
            p.op("dve", lambda e, Lx=Lx: e.tensor_tensor(out=ab[:CP, :Lx], in0=hw[0][:CP, :Lx], in1=hw[1][:CP, :Lx], op=ALU.subtract),
                 r=[("hw", 0), ("hw", 1), ("ho", 0)], w=["ab"])
            p.op("dve", lambda e, Lx=Lx: e.tensor_scalar(out=ho[1][:CP, :Lx], in0=ab[:CP, :Lx], scalar1=rn[:CP, 0:1], scalar2=None, op0=ALU.mult),
                 r=["ab", "rn"], w=[("ho", 1)])
            p.dma("pool", hsT[:, ch, :], ho[0][:CP, :Lx], r=[("ho", 0)], w=[("hs", li, ch)], grp=("st", 0))
            p.dma("pool", hdT[:, ch, :], ho[1][:CP, :Lx], r=[("ho", 1)], w=[("hd", li, ch)], grp=("st", 1))
    return p.build()


def pad128(a):
    out = np.zeros((128,) + a.shape[1:], np.float32)
    out[:a.shape[0]] = a
    return out


def run_filt(cfg, I):
    Cc, D, NC = cfg.Cc, cfg.D, cfg.NCORE
    CP = min(Cc, 128); NCH = Cc // CP
    nc = build_filt(cfg)
    fw1 = np.zeros((128, 128), np.float32); fw1[:33, :64] = I["hy_fw1"][0]
    fw2 = np.zeros((128, 128), np.float32); fw2[:64, :64] = I["hy_fw2"][0]
    pv = np.zeros((128, 3), np.float32)
    pv[:64, 0] = I["hy_fb1"][0]; pv[:64, 1] = I["hy_fb2"][0]; pv[:64, 2] = I["hy_freq"][0]
    consts = [hy_consts(Lx, D) for Lx in (cfg.L, cfg.LC)]
    ims = []
    for c in range(NC):
        fw3 = np.zeros((128, 2, NCH, 128), np.float32)
        for d in range(2):
            blk = I["hy_fw3"][0][:, d * D + c * Cc: d * D + (c + 1) * Cc]
            fw3[:64, d, :, :CP] = blk.reshape(64, NCH, CP)
        m = {"fw1": fw1, "fw2": fw2, "fw3": fw3, "pv": pv}
        for li, (z, win) in enumerate(consts):
            m["zT%d" % li] = pad128(np.ascontiguousarray(z.T))
            wc = win[:, c * Cc:(c + 1) * Cc].T
            m["winT%d" % li] = np.ascontiguousarray(wc.reshape(NCH, CP, -1).transpose(1, 0, 2))
        ims.append(m)
    res = run(nc, ims)
    outs = []
    for li, Lx in enumerate((cfg.L, cfg.LC)):
        hs = np.zeros((Lx, D), np.float32); hd = np.zeros((Lx, D), np.float32)
        for c in range(NC):
            a = np.asarray(res[c]["hsT%d" % li]).astype(np.float32).transpose(1, 0, 2).reshape(Cc, Lx)
            b = np.asarray(res[c]["hdT%d" % li]).astype(np.float32).transpose(1, 0, 2).reshape(Cc, Lx)
            hs[:, c * Cc:(c + 1) * Cc] = a.T
            hd[:, c * Cc:(c + 1) * Cc] = b.T
        outs.append((hs, hd))
    return outs


def dft_tables(Lx):
    NS = Lx // 128
    M = 4 * Lx
    p = np.arange(128)[:, None, None]
    i = np.arange(NS)[None, :, None]
    q = np.arange(128)[None, None, :]

    def cis(k):
        ang = -2.0 * np.pi * (np.asarray(k, np.int64) % M).astype(np.float64) / M
        return np.stack([np.cos(ang), np.sin(ang)]).astype(np.float32)

    B2 = cis((2 * q + 1) * (128 * i + p))
    j = np.arange(NS)[None, :, None]
    ii = np.arange(NS)[None, None, :]
    A2 = cis(256 * j * (128 * ii + np.arange(128)[:, None, None]))
    qq = np.arange(128)[:, None, None]
    jj = np.arange(NS)[None, :, None]
    pp = np.arange(128)[None, None, :]
    B3 = cis((2 * (128 * jj + qq) + 1) * pp)
    i3 = np.arange(NS)[None, :, None]
    j3 = np.arange(NS)[None, None, :]
    A3 = cis((2 * (128 * j3 + qq) + 1) * 128 * i3)
    return [np.ascontiguousarray(t) for t in (A2, B2, A3, B3)]


def build_h2(cfg):
    p = Prog()
    B, Cc = cfg.B, cfg.Cc
    ncol = B * Cc
    CW = min(512, ncol)
    nbp = CW // Cc
    NSmax = max(cfg.L, cfg.LC) // 128
    RAWN = 4 * NSmax * 128
    raw = p.sb("raw", [128, RAWN])
    es = [[p.sb("es%d%d" % (a, b), [128, NSmax, 128], BF16) for b in range(2)] for a in range(2)]
    a_s = p.sb("a_s", [128, 2, NSmax, NSmax])
    vs = p.sb("vs", [128, NSmax, CW], BF16)
    hsd = p.sb("hsd", [128, 2, NSmax, Cc], BF16)
    skb = p.sb("skb", [128, ncol])
    kk = p.sb("kk", [128, 2, Cc])
    tt = [p.sb("tt%d" % i, [128, CW]) for i in range(3)]
    yo = p.sb("yo", [128, CW], BF16)
    pV = [p.ps("pV%d" % i, [128, 512]) for i in range(2)]
    pK = [p.ps("pK%d" % i, [128, 512]) for i in range(2)]
    pY = p.ps("pY", [128, 512])
    def conv_li(li, Lx, skip):
        NS = Lx // 128
        NF = NS
        Mv = NS * 128
        v = p.din("v%d" % li, [128, NS, ncol], BF16)
        hsdi = p.din("hsd%d" % li, [128, 2, NS, Cc], BF16)
        A2 = p.din("A2_%d" % li, [2, 128, NF, NS]); B2 = p.din("B2_%d" % li, [2, 128, NS, 128])
        A3 = p.din("A3_%d" % li, [2, 128, NS, NF]); B3 = p.din("B3_%d" % li, [2, 128, NF, 128])
        if li == 0:
            skip = p.din("skip0", [128, ncol])
        y = p.dout("y%d" % li, [128, NS, ncol], BF16)
        Es = p.dtmp("Es%d" % li, [NF, 2, 128, NS, 128], BF16)
        Gs = p.dtmp("Gs%d" % li, [NS, 2, 128, NF, 128], BF16)
        p.barrier()
        bre = raw[:, 0:Mv].rearrange("p (i q) -> p i q", q=128)
        bim = raw[:, Mv:2 * Mv].rearrange("p (i q) -> p i q", q=128)
        t1 = raw[:, 2 * Mv:3 * Mv].rearrange("p (i q) -> p i q", q=128)
        t2 = raw[:, 3 * Mv:4 * Mv].rearrange("p (i q) -> p i q", q=128)
        for (At, Bt, dst, nout) in ((A2, B2, Es, NF), (A3, B3, Gs, NS)):
            p.dma("sync", raw[:, 0:Mv], Bt[0].rearrange("p i q -> p (i q)"), w=["bre"])
            p.dma("sync", raw[:, Mv:2 * Mv], Bt[1].rearrange("p i q -> p (i q)"), w=["bim"])
            p.dma("sync", a_s[:, 0, :nout, :NS], At[0], w=["a_s0"])
            p.dma("sync", a_s[:, 1, :nout, :NS], At[1], w=["a_s1"])
            for j in range(nout):
                s = j % 2
                are = a_s[:, 0, j, :NS].unsqueeze(2).broadcast_to([128, NS, 128])
                aim = a_s[:, 1, j, :NS].unsqueeze(2).broadcast_to([128, NS, 128])
                ere = es[s][0][:, :NS, :]
                eim = es[s][1][:, :NS, :]
                p.op("dve", lambda e, are=are: e.tensor_tensor(out=t1, in0=bre, in1=are, op=ALU.mult), r=["bre", "a_s0"], w=["t1"])
                p.op("pool", lambda e, aim=aim: e.tensor_tensor(out=t2, in0=bim, in1=aim, op=ALU.mult), r=["bim", "a_s1"], w=["t2"])
                p.op("dve", lambda e, ere=ere: e.tensor_tensor(out=ere, in0=t1, in1=t2, op=ALU.subtract), r=["t1", "t2"], w=[("es", s, 0)])
                p.op("dve", lambda e, are=are: e.tensor_tensor(out=t1, in0=bim, in1=are, op=ALU.mult), r=["bim", "a_s0"], w=["t1"])
                p.op("pool", lambda e, aim=aim: e.tensor_tensor(out=t2, in0=bre, in1=aim, op=ALU.mult), r=["bre", "a_s1"], w=["t2"])
                p.op("dve", lambda e, eim=eim: e.tensor_tensor(out=eim, in0=t1, in1=t2, op=ALU.add), r=["t1", "t2"], w=[("es", s, 1)])
                p.dma("sync", dst[j, 0], ere, r=[("es", s, 0)], w=[("tab", li, j, 0)], grp=("tst", s, 0))
                p.dma("sync", dst[j, 1], eim, r=[("es", s, 1)], w=[("tab", li, j, 1)], grp=("tst", s, 1))
        p.barrier()
        Yv = raw[:, 0:NF * CW].bitcast(BF16).rearrange("p (c j w) -> p c j w", c=2, j=NF)
        p.dma("sync", hsd[:, :, :NS, :], hsdi[:, :, :, :], w=["hsd"])
        if li == 0:
            p.dma("sync", skb[:], skip[:, :], w=["skb"])
        for c0 in range(0, ncol, CW):
            p.dma("sync", vs[:, :NS, :], v[:, :, c0:c0 + CW], w=["vs"])
            for j in range(NF):
                s = j % 2
                for c in range(2):
                    p.dma("pool" if c else "sync", es[s][c][:, :NS, :], Es[j, c], w=[("es", s, c)])
                for i in range(NS):
                    fl = dict(start=(i == 0), stop=(i == NS - 1))
                    p.op("pe", lambda e, s=s, i=i, fl=fl: e.matmul(pV[0][:, :CW], lhsT=es[s][0][:, i, :], rhs=vs[:, i, :], **fl),
                         r=[("es", s, 0), "vs"], w=["pV0"])
                    p.op("pe", lambda e, s=s, i=i, fl=fl: e.matmul(pV[1][:, :CW], lhsT=es[s][1][:, i, :], rhs=vs[:, i, :], **fl),
                         r=[("es", s, 1), "vs"], w=["pV1"])
                    p.op("pe", lambda e, s=s, i=i, fl=fl: e.matmul(pK[0][:, :Cc], lhsT=es[s][0][:, i, :], rhs=hsd[:, 0, i, :], **fl),
                         r=[("es", s, 0), "hsd"], w=["pK0"])
                    p.op("pe", lambda e, s=s, i=i, fl=fl: e.matmul(pK[1][:, :Cc], lhsT=es[s][1][:, i, :], rhs=hsd[:, 1, i, :], **fl),
                         r=[("es", s, 1), "hsd"], w=["pK1"])
                for c in range(2):
                    p.op("act", lambda e, c=c: e.activation(out=kk[:, c, :], in_=pK[c][:, :Cc], func=AF.Copy), r=["pK%d" % c], w=[("kk", c)])
                kre = kk[:, 0, :].unsqueeze(1).broadcast_to([128, nbp, Cc])
                kim = kk[:, 1, :].unsqueeze(1).broadcast_to([128, nbp, Cc])
                vre = pV[0][:, :CW].rearrange("p (b c) -> p b c", c=Cc)
                vim = pV[1][:, :CW].rearrange("p (b c) -> p b c", c=Cc)
                t3 = [t[:, :].rearrange("p (b c) -> p b c", c=Cc) for t in tt]
                yre = Yv[:, 0, j, :].rearrange("p (b c) -> p b c", c=Cc)
                yim = Yv[:, 1, j, :].rearrange("p (b c) -> p b c", c=Cc)
                p.op("dve", lambda e, vre=vre, kre=kre, t3=t3: e.tensor_tensor(out=t3[0], in0=vre, in1=kre, op=ALU.mult), r=["pV0", ("kk", 0)], w=["tt0"])
                p.op("dve", lambda e, vim=vim, kim=kim, t3=t3: e.tensor_tensor(out=t3[1], in0=vim, in1=kim, op=ALU.mult), r=["pV1", ("kk", 1)], w=["tt1"])
                p.op("pool", lambda e, yre=yre, t3=t3: e.tensor_tensor(out=yre, in0=t3[0], in1=t3[1], op=ALU.subtract), r=["tt0", "tt1"], w=[("Y", j)])
                p.op("dve", lambda e, vre=vre, kim=kim, t3=t3: e.tensor_tensor(out=t3[2], in0=vre, in1=kim, op=ALU.mult), r=["pV0", ("kk", 1)], w=["tt2"])
                p.op("dve", lambda e, vim=vim, kre=kre, t3=t3: e.tensor_tensor(out=t3[0], in0=vim, in1=kre, op=ALU.mult), r=["pV1", ("kk", 0), "tt0"], w=["tt0"])
                p.op("pool", lambda e, yim=yim, t3=t3: e.tensor_tensor(out=yim, in0=t3[2], in1=t3[0], op=ALU.add), r=["tt0", "tt2"], w=[("Y", j)])
            for i in range(NS):
                s = i % 2
                for c in range(2):
                    p.dma("pool" if c else "sync", es[s][c][:, :NF, :], Gs[i, c], w=[("es", s, c)])
                n = 0
                for j in range(NF):
                    for c in range(2):
                        p.op("pe", lambda e, s=s, j=j, c=c, n=n: e.matmul(pY[:, :CW], lhsT=es[s][c][:, j, :], rhs=Yv[:, c, j, :],
                                                                         start=(n == 0), stop=(n == 2 * NF - 1)),
                             r=[("es", s, c), ("Y", j)], w=["pY"])
                        n += 1
                p.op("pool", lambda e, i=i, c0=c0: e.tensor_tensor(out=tt[0][:, :], in0=vs[:, i, :], in1=skb[:, c0:c0 + CW], op=ALU.mult),
                     r=["vs", "skb"], w=["tt0"])
                p.op("dve", lambda e, Lx=Lx: e.scalar_tensor_tensor(out=yo[:, :], in0=pY[:, :CW], scalar=1.0 / Lx, in1=tt[0][:, :],
                                                                   op0=ALU.mult, op1=ALU.add), r=["pY", "tt0"], w=["yo"])
                p.dma("sync", y[:, i, c0:c0 + CW], yo[:, :], r=["yo"], w=[("y", li, i, c0)], grp=("yst",))
        return skip

    skip = None
    for li, Lx in enumerate((cfg.L, cfg.LC)):
        skip = conv_li(li, Lx, skip)
    return p.build()


def run_h2(cfg, I, v_lat, v_ctx, filt):
    import ml_dtypes
    bf = ml_dtypes.bfloat16
    B, Cc, D, NC = cfg.B, cfg.Cc, cfg.D, cfg.NCORE
    ncol = B * Cc
    nc = build_h2(cfg)
    tabs = [dft_tables(Lx) for Lx in (cfg.L, cfg.LC)]
    ims = []
    for c in range(NC):
        m = {}
        for li, (Lx, vv) in enumerate(((cfg.L, v_lat), (cfg.LC, v_ctx))):
            NS = Lx // 128
            vc = vv[:, :, c * Cc:(c + 1) * Cc].transpose(1, 0, 2).reshape(Lx, ncol)
            m["v%d" % li] = np.ascontiguousarray(vc.reshape(NS, 128, ncol).transpose(1, 0, 2)).astype(bf)
            hs, hd = filt[li]
            hh = np.stack([hs[:, c * Cc:(c + 1) * Cc], hd[:, c * Cc:(c + 1) * Cc]])
            m["hsd%d" % li] = np.ascontiguousarray(hh.reshape(2, NS, 128, Cc).transpose(2, 0, 1, 3)).astype(bf)
            A2, B2, A3, B3 = tabs[li]
            m["A2_%d" % li], m["B2_%d" % li], m["A3_%d" % li], m["B3_%d" % li] = A2, B2, A3, B3
        sk = np.tile(I["hy_skip"][0][c * Cc:(c + 1) * Cc], B)
        m["skip0"] = np.ascontiguousarray(np.broadcast_to(sk[None, :], (128, ncol))).astype(np.float32)
        ims.append(m)
    res = run(nc, ims)
    outs = []
    for li, Lx in enumerate((cfg.L, cfg.LC)):
        NS = Lx // 128
        yy = np.zeros((B, Lx, D), bf)
        for c in range(NC):
            a = np.asarray(res[c]["y%d" % li]).transpose(1, 0, 2).reshape(Lx, B, Cc)
            yy[:, :, c * Cc:(c + 1) * Cc] = a.transpose(1, 0, 2)
        outs.append(yy)
    return outs


class PostCtx:
    def __init__(self, p, cfg, TK):
        ND = cfg.ND
        self.TK = TK
        self.ones = p.sb("ones", [128, 128]); p.op("dve", lambda e: e.memset(self.ones[:], 1.0), w=["ones"])
        self.ident = p.sb("ident", [128, 128])
        self.xt = p.sb("xt", [128, ND, TK]); self.xl = p.sb("xl", [128, ND, TK]); self.sq = p.sb("sq", [128, ND, TK])
        self.tokf = p.sb("tokf", [128, ND, TK]); self.tokb = p.sb("tokb", [128, ND, TK], BF16)
        self.rstd = p.sb("rstd", [128, TK]); self.olt = p.sb("olt", [128, TK])
        self.wr = p.sb("wr", [128, ND, 128]); self.br = p.sb("br", [128, 1])
        self.lgT = p.sb("lgT", [128, TK]); self.lg = p.sb("lg", [128, 128])
        self.sm = p.sb("sm", [128, 16]); self.pen = p.sb("pen", [128, 4]); self.lem = p.sb("lem", [128, 32]); self.lem2 = p.sb("lem2", [128, 32])
        self.mk = p.sb("mk", [128, 2, 32]); self.gt = p.sb("gt", [128, 32])
        self.pss = p.ps("pss", [128, TK]); self.pz = [p.ps("pz%d" % i, [128, TK]) for i in range(2)]
        self.plg = p.ps("plg", [128, TK]); self.ptr = p.ps("ptr", [128, 128])
        self.pzi = 0


def emit_post(p, cfg, C, cw, a_tile, akey, Wb, wkey, bvec_ap_fn, gt_fn, m2_fn, sh_fn, xl_out_ap, tok_out_ap, gates_out_fn, want_router=True):
    ND = cfg.ND
    for blk in range(ND):
        q = C.pzi % 2
        C.pzi += 1
        for k in range(ND):
            p.op("pe", lambda e, q=q, k=k, blk=blk: e.matmul(C.pz[q][:, :cw], lhsT=Wb[:, k, blk * 128:(blk + 1) * 128], rhs=a_tile[:, k, :cw],
                                                           start=(k == 0), stop=(k == ND - 1)), r=[wkey, akey], w=[("pz", q)])
        p.op("act", lambda e, q=q, blk=blk: e.activation(out=C.olt[:, :cw], in_=C.pz[q][:, :cw], func=AF.Identity, bias=bvec_ap_fn(blk), scale=1.0),
             r=[("pz", q), "vecs"], w=["olt"])
        p.op("dve", lambda e, blk=blk: e.scalar_tensor_tensor(out=C.xl[:, blk, :cw], in0=C.olt[:, :cw], scalar=gt_fn(blk), in1=C.xt[:, blk, :cw],
                                                            op0=ALU.mult, op1=ALU.add), r=["olt", "xt", "vecs"], w=["xl"])
    p.dma("sync", xl_out_ap, C.xl[:, :, :cw], r=["xl"], w=[("xlo", id(xl_out_ap))], grp=("xlst",))
    emit_norm_router(p, cfg, C, cw, m2_fn, sh_fn, tok_out_ap, gates_out_fn, want_router)


def emit_norm_router(p, cfg, C, cw, m2_fn, sh_fn, tok_out_ap, gates_out_fn, want_router=True):
    ND = cfg.ND
    emit_rstd(p, cfg, C.xl, "xl", cw, C.ones, C.sq, "sq", C.pss, "pss", C.rstd, "rstd")
    for k in range(ND):
        p.op("dve", lambda e, k=k: e.tensor_tensor(out=C.sq[:, k, :cw], in0=C.xl[:, k, :cw], in1=C.rstd[:, :cw], op=ALU.mult),
             r=["xl", "rstd"], w=["sq"])
        p.op("dve", lambda e, k=k: e.tensor_scalar(out=C.tokf[:, k, :cw], in0=C.sq[:, k, :cw], scalar1=m2_fn(k), scalar2=sh_fn(k),
                                                  op0=ALU.mult, op1=ALU.add), r=["sq", "vecs"], w=["tokf"])
    p.op("pool", lambda e: e.tensor_copy(out=C.tokb[:, :, :cw], in_=C.tokf[:, :, :cw]), r=["tokf"], w=["tokb"])
    p.dma("sync", tok_out_ap, C.tokb[:, :, :cw], r=["tokb"], w=[("toko", id(tok_out_ap))], grp=("tokst",))
    if not want_router:
        return
    for k in range(ND):
        p.op("pe", lambda e, k=k: e.matmul(C.plg[:, :cw], lhsT=C.wr[:, k, :], rhs=C.tokf[:, k, :cw], start=(k == 0), stop=(k == ND - 1)),
             r=["wr", "tokf"], w=["plg"])
    p.op("act", lambda e: e.activation(out=C.lgT[:, :cw], in_=C.plg[:, :cw], func=AF.Identity, bias=C.br[:, 0:1], scale=1.0),
         r=["plg", "br"], w=["lgT"])
    for (t0, tw) in tiles(cw, 128):
        p.op("pe", lambda e, t0=t0, tw=tw: e.transpose(out=C.ptr[:tw, :], in_=C.lgT[:, t0:t0 + tw], identity=C.ident[:]),
             r=["lgT", "ident"], w=["ptr"])
        p.op("act", lambda e, tw=tw: e.activation(out=C.lg[:tw, :], in_=C.ptr[:tw, :], func=AF.Copy), r=["ptr"], w=["lg"])
        lg, sm = C.lg, C.sm
        R = lambda *k: list(k)
        p.op("dve", lambda e, tw=tw: e.tensor_reduce(out=sm[:tw, 0:1], in_=lg[:tw, 0:4], axis=AX.X, op=ALU.max), r=["lg"], w=["sm0"])
        p.op("dve", lambda e, tw=tw: e.tensor_scalar(out=sm[:tw, 4:8], in0=lg[:tw, 0:4], scalar1=sm[:tw, 0:1], scalar2=None, op0=ALU.subtract),
             r=["lg", "sm0"], w=["sm4"])
        p.op("act", lambda e, tw=tw: e.activation(out=sm[:tw, 4:8], in_=sm[:tw, 4:8], func=AF.Exp), r=["sm4"], w=["sm4"])
        p.op("dve", lambda e, tw=tw: e.tensor_reduce(out=sm[:tw, 1:2], in_=sm[:tw, 4:8], axis=AX.X, op=ALU.add), r=["sm4"], w=["sm1"])
        p.op("dve", lambda e, tw=tw: e.reciprocal(out=sm[:tw, 1:2], in_=sm[:tw, 1:2]), r=["sm1"], w=["sm1"])
        p.op("dve", lambda e, tw=tw: e.tensor_scalar(out=C.pen[:tw, :], in0=lg[:tw, 0:4], scalar1=sm[:tw, 0:1], scalar2=None, op0=ALU.is_equal),
             r=["lg", "sm0"], w=["pen"])
        p.op("dve", lambda e, tw=tw: e.tensor_scalar(out=C.pen[:tw, :], in0=C.pen[:tw, :], scalar1=-1.0, scalar2=1e30, op0=ALU.add, op1=ALU.mult),
             r=["pen"], w=["pen"])
        le = lg[:tw, 4:36].rearrange("p (g e) -> p g e", e=8)
        penb = C.pen[:tw, :].unsqueeze(2).broadcast_to([tw, 4, 8])
        lem3 = C.lem[:tw, :].rearrange("p (g e) -> p g e", e=8)
        p.op("dve", lambda e, le=le, penb=penb, lem3=lem3: e.tensor_tensor(out=lem3, in0=le, in1=penb, op=ALU.add), r=["lg", "pen"], w=["lem"])
        p.op("dve", lambda e, tw=tw: e.tensor_reduce(out=sm[:tw, 2:3], in_=C.lem[:tw, :], axis=AX.X, op=ALU.max), r=["lem"], w=["sm2"])
        p.op("dve", lambda e, tw=tw: e.tensor_scalar(out=C.mk[:tw, 0, :], in0=C.lem[:tw, :], scalar1=sm[:tw, 2:3], scalar2=None, op0=ALU.is_equal),
             r=["lem", "sm2"], w=["mk0"])
        p.op("dve", lambda e, tw=tw: e.scalar_tensor_tensor(out=C.lem2[:tw, :], in0=C.mk[:tw, 0, :], scalar=-1e30, in1=C.lem[:tw, :],
                                                           op0=ALU.mult, op1=ALU.add), r=["mk0", "lem"], w=["lem2"])
        p.op("dve", lambda e, tw=tw: e.tensor_reduce(out=sm[:tw, 3:4], in_=C.lem2[:tw, :], axis=AX.X, op=ALU.max), r=["lem2"], w=["sm3"])
        p.op("dve", lambda e, tw=tw: e.tensor_scalar(out=C.mk[:tw, 1, :], in0=C.lem2[:tw, :], scalar1=sm[:tw, 3:4], scalar2=None, op0=ALU.is_equal),
             r=["lem2", "sm3"], w=["mk1"])
        p.op("dve", lambda e, tw=tw: e.tensor_tensor(out=sm[:tw, 8:9], in0=sm[:tw, 3:4], in1=sm[:tw, 2:3], op=ALU.subtract), r=["sm2", "sm3"], w=["sm8"])
        p.op("act", lambda e, tw=tw: e.activation(out=sm[:tw, 8:9], in_=sm[:tw, 8:9], func=AF.Exp), r=["sm8"], w=["sm8"])
        p.op("dve", lambda e, tw=tw: e.tensor_scalar(out=sm[:tw, 9:10], in0=sm[:tw, 8:9], scalar1=1.0, scalar2=None, op0=ALU.add), r=["sm8"], w=["sm9"])
        p.op("dve", lambda e, tw=tw: e.reciprocal(out=sm[:tw, 9:10], in_=sm[:tw, 9:10]), r=["sm9"], w=["sm9"])
        p.op("dve", lambda e, tw=tw: e.tensor_tensor(out=sm[:tw, 10:11], in0=sm[:tw, 8:9], in1=sm[:tw, 9:10], op=ALU.mult), r=["sm8", "sm9"], w=["sm10"])
        p.op("dve", lambda e, tw=tw: e.tensor_scalar(out=sm[:tw, 9:11], in0=sm[:tw, 9:11], scalar1=sm[:tw, 1:2], scalar2=None, op0=ALU.mult),
             r=["sm9", "sm10", "sm1"], w=["sm9", "sm10"])
        p.op("dve", lambda e, tw=tw: e.tensor_scalar(out=C.gt[:tw, :], in0=C.mk[:tw, 0, :], scalar1=sm[:tw, 9:10], scalar2=None, op0=ALU.mult),
             r=["mk0", "sm9"], w=["gt"])
        p.op("dve", lambda e, tw=tw: e.scalar_tensor_tensor(out=C.gt[:tw, :], in0=C.mk[:tw, 1, :], scalar=sm[:tw, 10:11], in1=C.gt[:tw, :],
                                                           op0=ALU.mult, op1=ALU.add), r=["mk1", "sm10", "gt"], w=["gt"])
        go = gates_out_fn(t0, tw)
        p.dma("sync", go, C.gt[:tw, :], r=["gt"], w=[("go", id(go))], grp=("gst",))


def build_h3(cfg):
    p = Prog()
    ND, D, TL, TC = cfg.ND, cfg.D, cfg.TL, cfg.TC
    NT = TL + TC
    TK = 256
    ycT = p.din("ycT", [128, ND, NT], BF16); x0T = p.din("x0T", [128, ND, NT], BF16); xT = p.din("xT", [128, ND, NT])
    w_out = p.din("w_out", [128, ND, D]); vecs = p.din("vecsD", [128, 8, ND])
    wr_d = p.din("wrD", [128, ND, 128]); br_d = p.din("brD", [128, 1]); ident_d = p.din("identD", [128, 128])
    xlT = p.dout("xlT", [128, ND, NT]); tokT = p.dout("tokT", [128, ND, NT], BF16); gates = p.dout("gates", [NT, 32])
    C = PostCtx(p, cfg, TK)
    p.dma("sync", C.ident[:], ident_d[:, :], w=["ident"]); p.dma("sync", C.wr[:], wr_d[:, :, :], w=["wr"]); p.dma("sync", C.br[:], br_d[:, :], w=["br"])
    vs_ = p.sb("vecs", [128, 8, ND]); p.dma("sync", vs_[:], vecs[:, :, :], w=["vecs"])
    m2 = p.sb("m2", [128, 2, ND])
    for i, j in ((0, 3), (1, 6)):
        p.op("dve", lambda e, i=i, j=j: e.scalar_tensor_tensor(out=m2[:, i, :], in0=vs_[:, j, :], scalar=1.0, in1=vs_[:, 1, :], op0=ALU.add, op1=ALU.mult),
             r=["vecs"], w=["vecs"])
    Wb = p.sb("Wb", [128, ND, D], BF16)
    wf = [p.sb("wf%d" % i, [128, ND, 128]) for i in range(2)]
    for blk in range(ND):
        s = blk % 2
        p.dma("sync", wf[s][:], w_out[:, :, blk * 128:(blk + 1) * 128], w=[("wf", s)])
        p.op("pool", lambda e, s=s, blk=blk: e.tensor_copy(out=Wb[:, :, blk * 128:(blk + 1) * 128], in_=wf[s][:]), r=[("wf", s)], w=["Wb"])
    yc = p.sb("yc", [128, ND, TK], BF16); x0 = p.sb("x0", [128, ND, TK], BF16); a = p.sb("a", [128, ND, TK], BF16)
    for (s0, sl, mi) in ((0, TL, 0), (TL, TC, 1)):
        for (c0, cw) in tiles(sl, TK):
            o = s0 + c0
            p.dma("sync", yc[:, :, :cw], ycT[:, :, o:o + cw], w=["yc"])
            p.dma("pool", x0[:, :, :cw], x0T[:, :, o:o + cw], w=["x0"])
            p.dma("sync", C.xt[:, :, :cw], xT[:, :, o:o + cw], w=["xt"])
            p.op("pool", lambda e, cw=cw: e.tensor_tensor(out=a[:, :, :cw], in0=yc[:, :, :cw], in1=x0[:, :, :cw], op=ALU.mult), r=["yc", "x0"], w=["a"])
            emit_post(p, cfg, C, cw, a, "a", Wb, "Wb",
                      lambda blk: vs_[:, 0, blk:blk + 1],
                      lambda blk, mi=mi: vs_[:, 2 + 3 * mi, blk:blk + 1],
                      lambda k, mi=mi: m2[:, mi, k:k + 1],
                      lambda k, mi=mi: vs_[:, 4 + 3 * mi, k:k + 1],
                      xlT[:, :, o:o + cw], tokT[:, :, o:o + cw],
                      lambda t0, tw, o=o: gates[o + t0:o + t0 + tw, :])
    return p.build()


def router_inputs(cfg, I, layer):
    ND = cfg.ND
    wr = np.zeros((cfg.D, 128), np.float32)
    wr[:, 0:4] = I["moe_wg"][layer]; wr[:, 4:36] = I["moe_we"][layer]
    br = np.zeros((128, 1), np.float32)
    br[0:4, 0] = I["moe_bg"][layer]; br[4:36, 0] = I["moe_be"][layer]
    wr = np.ascontiguousarray(wr.reshape(ND, 128, 128).transpose(1, 0, 2))
    return wr, br


def wfm(w, ND):
    return np.ascontiguousarray(w.reshape(ND, 128, w.shape[1]).transpose(1, 0, 2))


def run_h3(cfg, I, mods, y_lat, y_ctx, x0_lat, x0_ctx):
    import ml_dtypes
    bf = ml_dtypes.bfloat16
    ND, NC = cfg.ND, cfg.NCORE
    nc = build_h3(cfg)
    yc = tok_layout(cfg, y_lat, y_ctx); x0 = tok_layout(cfg, x0_lat, x0_ctx); xx = tok_layout(cfg, I["x"], I["ctx"])
    wr, br = router_inputs(cfg, I, 0)
    w_out = wfm(I["hy_w_out"][0], ND)
    ims = []
    for c in range(NC):
        b = c // cfg.CPB
        mvd = mod_vecs(cfg, mods[0], b)
        vecs = np.stack([vfm(v, ND) for v in (I["hy_b_out"][0], I["norm_g"][0, 1], mvd["gt_a"], mvd["sc_f"], mvd["sh_f"],
                                              mvd["cgt_a"], mvd["csc_f"], mvd["csh_f"])], axis=1)
        ims.append({"ycT": fm(yc[c], ND).astype(bf), "x0T": fm(x0[c], ND).astype(bf), "xT": fm(xx[c].astype(np.float32), ND),
                    "w_out": w_out, "vecsD": np.ascontiguousarray(vecs), "wrD": wr, "brD": br, "identD": np.eye(128, dtype=np.float32)})
    res = run(nc, ims)
    xl = tok_unlayout(cfg, [unfm(r["xlT"]) for r in res])
    tok = tok_unlayout(cfg, [unfm(np.asarray(r["tokT"])) for r in res])
    gates = tok_unlayout(cfg, [r["gates"] for r in res])
    return xl, tok, gates


def build_moe(cfg, CAP):
    p = Prog()
    ND, D, DE, EPC = cfg.ND, cfg.D, cfg.DE, cfg.EPC
    NDE = DE // 128
    xe = p.din("xe", [EPC, 128, ND, CAP], BF16)
    wg = p.din("wg", [EPC, 128, ND, DE]); wu = p.din("wu", [EPC, 128, ND, DE]); wd = p.din("wd", [EPC, 128, NDE, D])
    ye = p.dout("ye", [EPC, 128, ND, CAP], BF16)
    Wg = p.sb("Wg", [128, ND, DE], BF16); Wu = p.sb("Wu", [128, ND, DE], BF16); Wd = p.sb("Wd", [128, NDE, D], BF16)
    SW = 512
    st = [p.sb("st%d" % i, [128, max(ND, NDE), SW]) for i in range(2)]
    CT = min(512, CAP)
    xs = p.sb("xs", [128, ND, CT], BF16); h = p.sb("h", [128, NDE, CT], BF16); yo = p.sb("yo", [128, ND, CT], BF16)
    tg = p.sb("tg", [128, CT])
    pg = p.ps("pg", [128, CT]); pu = p.ps("pu", [128, CT]); py = [p.ps("py%d" % i, [128, CT]) for i in range(2)]
    si = 0
    for ex in range(EPC):
        for (src, dst, key, nk, ncols) in ((wg, Wg, "Wg", ND, DE), (wu, Wu, "Wu", ND, DE), (wd, Wd, "Wd", NDE, D)):
            for (c0, cw) in tiles(ncols, SW):
                s = si % 2
                si += 1
                p.dma("sync" if s else "pool", st[s][:, :nk, :cw], src[ex, :, :, c0:c0 + cw], w=[("st", s)])
                p.op("dve" if s else "act", (lambda e, s=s, dst=dst, nk=nk, c0=c0, cw=cw: e.tensor_copy(out=dst[:, :, c0:c0 + cw], in_=st[s][:, :nk, :cw]))
                     if s else (lambda e, s=s, dst=dst, nk=nk, c0=c0, cw=cw: e.activation(out=dst[:, :, c0:c0 + cw], in_=st[s][:, :nk, :cw], func=AF.Copy)),
                     r=[("st", s)], w=[key])
        for (t0, tw) in tiles(CAP, CT):
            p.dma("sync", xs[:, :, :tw], xe[ex, :, :, t0:t0 + tw], w=["xs"])
            for fb in range(NDE):
                for k in range(ND):
                    p.op("pe", lambda e, fb=fb, k=k, tw=tw: e.matmul(pg[:, :tw], lhsT=Wg[:, k, fb * 128:(fb + 1) * 128], rhs=xs[:, k, :tw],
                                                                    start=(k == 0), stop=(k == ND - 1)), r=["Wg", "xs"], w=["pg"])
                for k in range(ND):
                    p.op("pe", lambda e, fb=fb, k=k, tw=tw: e.matmul(pu[:, :tw], lhsT=Wu[:, k, fb * 128:(fb + 1) * 128], rhs=xs[:, k, :tw],
                                                                    start=(k == 0), stop=(k == ND - 1)), r=["Wu", "xs"], w=["pu"])
                p.op("act", lambda e, tw=tw: e.activation(out=tg[:, :tw], in_=pg[:, :tw], func=AF.Silu), r=["pg"], w=["tg"])
                p.op("dve", lambda e, fb=fb, tw=tw: e.tensor_tensor(out=h[:, fb, :tw], in0=pu[:, :tw], in1=tg[:, :tw], op=ALU.mult),
                     r=["pu", "tg"], w=["h"])
            for ob in range(ND):
                q = ob % 2
                for f in range(NDE):
                    p.op("pe", lambda e, ob=ob, f=f, q=q, tw=tw: e.matmul(py[q][:, :tw], lhsT=Wd[:, f, ob * 128:(ob + 1) * 128], rhs=h[:, f, :tw],
                                                                         start=(f == 0), stop=(f == NDE - 1)), r=["Wd", "h"], w=[("py", q)])
                if q:
                    p.op("act", lambda e, ob=ob, q=q, tw=tw: e.activation(out=yo[:, ob, :tw], in_=py[q][:, :tw], func=AF.Copy), r=[("py", q)], w=["yo"])
                else:
                    p.op("dve", lambda e, ob=ob, q=q, tw=tw: e.tensor_copy(out=yo[:, ob, :tw], in_=py[q][:, :tw]), r=[("py", q)], w=["yo"])
            p.dma("sync", ye[ex, :, :, t0:t0 + tw], yo[:, :, :tw], r=["yo"], w=[("ye", ex, t0)], grp=("yst",))
    return p.build()


_MOE_NC = {}


def run_moe(cfg, I, layer, tok, gates, CAP):
    import ml_dtypes
    bf = ml_dtypes.bfloat16
    ND, NC, EPC, D, DE = cfg.ND, cfg.NCORE, cfg.EPC, cfg.D, cfg.DE
    NDE = DE // 128
    key = (cfg.D, CAP)
    if key not in _MOE_NC:
        _MOE_NC[key] = build_moe(cfg, CAP)
    nc = _MOE_NC[key]
    T = tok.shape[0]
    sel = gates > 0
    idx = [np.nonzero(sel[:, e])[0] for e in range(cfg.NE)]
    rounds = max(1, max((len(ix) + CAP - 1) // CAP for ix in idx))
    slot = np.cumsum(sel, axis=1) - 1
    y01 = np.zeros((2, T, D), bf)
    g01 = np.zeros((2, T), np.float32)
    wgl = [wfm(I["moe_w_gate"][layer][e], ND) for e in range(cfg.NE)]
    wul = [wfm(I["moe_w_up"][layer][e], ND) for e in range(cfg.NE)]
    wdl = [wfm(I["moe_w_down"][layer][e], NDE) for e in range(cfg.NE)]
    for r in range(rounds):
        ims = []
        for c in range(NC):
            xe = np.zeros((EPC, 128, ND, CAP), bf)
            for j in range(EPC):
                ix = idx[c * EPC + j][r * CAP:(r + 1) * CAP]
                if len(ix):
                    xe[j, :, :, :len(ix)] = fm(tok[ix], ND)
            ims.append({"xe": xe, "wg": np.stack(wgl[c * EPC:(c + 1) * EPC]), "wu": np.stack(wul[c * EPC:(c + 1) * EPC]),
                        "wd": np.stack(wdl[c * EPC:(c + 1) * EPC])})
        res = run(nc, ims)
        for c in range(NC):
            ye = np.asarray(res[c]["ye"])
            for j in range(EPC):
                e = c * EPC + j
                ix = idx[e][r * CAP:(r + 1) * CAP]
                if len(ix):
                    yy = unfm(ye[j][:, :, :len(ix)])
                    sl = slot[ix, e]
                    for s in (0, 1):
                        m = sl == s
                        y01[s, ix[m]] = yy[m]
                        g01[s, ix[m]] = gates[ix[m], e]
    return y01, g01


def build_comb(cfg, final):
    p = Prog()
    ND, TL, TC = cfg.ND, cfg.TL, cfg.TC
    NT = TL if final else TL + TC
    TK = 256
    xlT = p.din("xlT", [128, ND, NT]); y0T = p.din("y0T", [128, ND, NT], BF16); y1T = p.din("y1T", [128, ND, NT], BF16)
    gb = p.din("gb", [128, 2, NT]); vecs = p.din("vecsD", [128, 7, ND])
    xoT = p.dout("xoT", [128, ND, NT]); uT = p.dout("uT", [128, ND, NT], F32 if final else BF16)
    vs_ = p.sb("vecs", [128, 7, ND]); p.dma("sync", vs_[:], vecs[:, :, :], w=["vecs"])
    m2 = p.sb("m2", [128, 2, ND])
    for i, j in ((0, 3), (1, 5)):
        p.op("dve", lambda e, i=i, j=j: e.scalar_tensor_tensor(out=m2[:, i, :], in0=vs_[:, j, :], scalar=1.0, in1=vs_[:, 2, :], op0=ALU.add, op1=ALU.mult),
             r=["vecs"], w=["vecs"])
    ones = p.sb("ones", [128, 128]); p.op("dve", lambda e: e.memset(ones[:], 1.0), w=["ones"])
    xt = p.sb("xt", [128, ND, TK]); y0 = p.sb("y0", [128, ND, TK], BF16); y1 = p.sb("y1", [128, ND, TK], BF16); gs = p.sb("gs", [128, 2, TK])
    mo = p.sb("mo", [128, ND, TK]); mo2 = p.sb("mo2", [128, ND, TK]); xl = p.sb("xl", [128, ND, TK]); sq = p.sb("sq", [128, ND, TK])
    rstd = p.sb("rstd", [128, TK]); uo = p.sb("uo", [128, ND, TK], F32 if final else BF16)
    pss = p.ps("pss", [128, TK])
    segs = ((0, TL, 0),) if final else ((0, TL, 0), (TL, TC, 1))
    for (s0, sl, mi) in segs:
        for (c0, cw) in tiles(sl, TK):
            o = s0 + c0
            p.dma("sync", xt[:, :, :cw], xlT[:, :, o:o + cw], w=["xt"])
            p.dma("pool", y0[:, :, :cw], y0T[:, :, o:o + cw], w=["y0"])
            p.dma("pool", y1[:, :, :cw], y1T[:, :, o:o + cw], w=["y1"])
            p.dma("sync", gs[:, :, :cw], gb[:, :, o:o + cw], w=["gs"])
            g0 = gs[:, 0, :cw].unsqueeze(1).broadcast_to([128, ND, cw])
            g1 = gs[:, 1, :cw].unsqueeze(1).broadcast_to([128, ND, cw])
            p.op("dve", lambda e, cw=cw, g0=g0: e.tensor_tensor(out=mo[:, :, :cw], in0=y0[:, :, :cw], in1=g0, op=ALU.mult), r=["y0", "gs"], w=["mo"])
            p.op("pool", lambda e, cw=cw, g1=g1: e.tensor_tensor(out=mo2[:, :, :cw], in0=y1[:, :, :cw], in1=g1, op=ALU.mult), r=["y1", "gs"], w=["mo2"])
            p.op("dve", lambda e, cw=cw: e.tensor_tensor(out=mo[:, :, :cw], in0=mo[:, :, :cw], in1=mo2[:, :, :cw], op=ALU.add), r=["mo", "mo2"], w=["mo"])
            for k in range(ND):
                p.op("dve", lambda e, k=k, cw=cw, mi=mi: e.scalar_tensor_tensor(out=xl[:, k, :cw], in0=mo[:, k, :cw], scalar=vs_[:, mi, k:k + 1],
                                                                              in1=xt[:, k, :cw], op0=ALU.mult, op1=ALU.add), r=["mo", "xt", "vecs"], w=["xl"])
            p.dma("sync", xoT[:, :, o:o + cw], xl[:, :, :cw], r=["xl"], w=[("xo", o)], grp=("xst",))
            emit_rstd(p, cfg, xl, "xl", cw, ones, sq, "sq", pss, "pss", rstd, "rstd")
            for k in range(ND):
                p.op("dve", lambda e, k=k, cw=cw: e.tensor_tensor(out=sq[:, k, :cw], in0=xl[:, k, :cw], in1=rstd[:, :cw], op=ALU.mult),
                     r=["xl", "rstd"], w=["sq"])
                if final:
                    p.op("dve", lambda e, k=k, cw=cw: e.tensor_scalar(out=uo[:, k, :cw], in0=sq[:, k, :cw], scalar1=vs_[:, 2, k:k + 1], scalar2=None, op0=ALU.mult),
                         r=["sq", "vecs"], w=["uo"])
                else:
                    p.op("dve", lambda e, k=k, cw=cw, mi=mi: e.tensor_scalar(out=uo[:, k, :cw], in0=sq[:, k, :cw], scalar1=m2[:, mi, k:k + 1],
                                                                           scalar2=vs_[:, 4 + 2 * mi, k:k + 1], op0=ALU.mult, op1=ALU.add),
                         r=["sq", "vecs"], w=["uo"])
            p.dma("sync", uT[:, :, o:o + cw], uo[:, :, :cw], r=["uo"], w=[("uo_", o)], grp=("ust",))
    return p.build()


def run_comb(cfg, I, mods, layer, xl_lat, xl_ctx, y01, g01, final):
    import ml_dtypes
    bf = ml_dtypes.bfloat16
    ND, NC, B, L, LC, D = cfg.ND, cfg.NCORE, cfg.B, cfg.L, cfg.LC, cfg.D
    nc = build_comb(cfg, final)
    nl = B * L
    def split(a, last):
        lat = a[:nl].reshape((B, L) + last)
        ctx = a[nl:].reshape((B, LC) + last) if not final else np.zeros((B, LC) + last, a.dtype)
        return lat, ctx
    y0 = split(y01[0], (D,)); y1 = split(y01[1], (D,))
    g0 = split(g01[0][:, None], (1,)); g1 = split(g01[1][:, None], (1,))
    xs = tok_layout(cfg, xl_lat, xl_ctx if xl_ctx is not None else np.zeros((B, LC, D), np.float32))
    y0s = tok_layout(cfg, *y0); y1s = tok_layout(cfg, *y1); g0s = tok_layout(cfg, *g0); g1s = tok_layout(cfg, *g1)
    NT = cfg.TL if final else cfg.TL + cfg.TC
    ims = []
    for c in range(NC):
        b = c // cfg.CPB
        mvd = mod_vecs(cfg, mods[layer], b)
        if final:
            z = np.zeros(D, np.float32)
            vl = (mvd["gt_f"], z, I["final_g"], z, z, z, z)
        else:
            nm = mod_vecs(cfg, mods[layer + 1], b)
            vl = (mvd["gt_f"], mvd["cgt_f"], I["norm_g"][layer + 1, 0], nm["sc_a"], nm["sh_a"], nm["csc_a"], nm["csh_a"])
        vecs = np.ascontiguousarray(np.stack([vfm(np.asarray(v, np.float32), ND) for v in vl], axis=1))
        gbv = np.stack([g0s[c][:NT, 0], g1s[c][:NT, 0]])
        ims.append({"xlT": fm(xs[c][:NT].astype(np.float32), ND), "y0T": fm(y0s[c][:NT], ND).astype(bf), "y1T": fm(y1s[c][:NT], ND).astype(bf),
                    "gb": np.ascontiguousarray(np.broadcast_to(gbv[None], (128, 2, NT))).astype(np.float32), "vecsD": vecs})
    res = run(nc, ims)
    def un(name):
        per = []
        for r in res:
            a = unfm(np.asarray(r[name]))
            if final:
                a = np.concatenate([a, np.zeros((cfg.TC, D), a.dtype)], 0)
            per.append(a)
        return tok_unlayout(cfg, per)
    xo = un("xoT"); u = un("uT")
    return xo[0], xo[1], u[0], u[1]


S5P, S5H = 64, 16


def emit_exp_poly(p, out_ap, okey, in_ap, ikey, shape, center, degree, name):
    import math
    t = p.sb(name + "_t", shape); r = p.sb(name + "_r", shape)
    p.op("dve", lambda e: e.tensor_scalar(out=t[:], in0=in_ap, scalar1=-center, scalar2=None, op0=ALU.add), r=[ikey], w=[name + "t"])
    p.op("dve", lambda e: e.memset(r[:], 1.0 / math.factorial(degree)), w=[name + "r"])
    for n in range(degree - 1, -1, -1):
        p.op("dve", lambda e: e.tensor_tensor(out=r[:], in0=r[:], in1=t[:], op=ALU.mult), r=[name + "t", name + "r"], w=[name + "r"])
        p.op("dve", lambda e, n=n: e.tensor_scalar(out=r[:], in0=r[:], scalar1=1.0 / math.factorial(n), scalar2=None, op0=ALU.add), r=[name + "r"], w=[name + "r"])
    p.op("dve", lambda e: e.tensor_scalar(out=out_ap, in0=r[:], scalar1=float(math.exp(center)), scalar2=None, op0=ALU.mult), r=[name + "r"], w=[okey])

def build_s5prep(cfg, CH):
    p = Prog()
    G = cfg.G
    P_, H = S5P, S5H
    NLc = max(1, CH // cfg.NCORE)
    NPW = CH + 1
    a_d = p.din("a_d", [G, 2, 2, P_])
    ls_d = p.din("ls_d", [G, 2])
    b_d = p.din("b_d", [G, 2, 2, P_, H])
    c_d = p.din("c_d", [G, 2, 2, H, P_])
    lsel = p.din("lsel", [G, NLc, NPW])
    XB = p.dout("XB", [G, NLc, 2, 2, P_, H])
    OC = p.dout("OC", [G, NLc, 2, 2, H, P_])
    Mo = p.dout("Mo", [G, NLc, 2, H, H])
    PC = p.dout("PC", [G, 3, 2, P_])
    DP = 2 * P_
    a = p.sb("a", [G, 2, DP]); ls = p.sb("ls", [G, 2]); b = p.sb("b", [G, 2, 2 * P_ * H]); c = p.sb("c", [G, 2, 2 * H * P_])
    p.dma("sync", a[:], a_d.rearrange("g r d p -> g r (d p)"), w=["a"]); p.dma("sync", ls[:], ls_d[:, :], w=["ls"])
    p.dma("sync", b[:], b_d.rearrange("g r d p h -> g r (d p h)"), w=["b"]); p.dma("pool", c[:], c_d.rearrange("g r d h p -> g r (d h p)"), w=["c"])
    sel = p.sb("sel", [G, NLc, NPW]); p.dma("sync", sel[:], lsel[:, :, :], w=["sel"])
    st = p.sb("stp", [G, 2])
    emit_exp_poly(p, st[:], "st", ls[:], "ls", [G, 2], float(np.log(0.01)), 20, "ep1")
    stb = st[:, :].unsqueeze(2).broadcast_to([G, 2, P_])
    lr = p.sb("lr", [G, DP]); li = p.sb("li", [G, DP]); mag = p.sb("mag", [G, DP]); cs = p.sb("cs", [G, 2, DP])
    v3 = lambda t: t.rearrange("g (d p) -> g d p", p=P_)
    p.op("dve", lambda e: e.tensor_tensor(out=v3(lr[:, :]), in0=v3(a[:, 0, :]), in1=stb, op=ALU.mult), r=["a", "st"], w=["lr"])
    p.op("dve", lambda e: e.tensor_tensor(out=v3(li[:, :]), in0=v3(a[:, 1, :]), in1=stb, op=ALU.mult), r=["a", "st"], w=["li"])
    emit_exp_poly(p, mag[:], "mag", lr[:], "lr", [G, DP], 0.0, 7, "ep2")
    tmp = [p.sb("tmpa", [G, DP]), p.sb("tmpb", [G, DP])]
    one = p.sb("one1", [G, 1]); p.op("dve", lambda e: e.memset(one[:], 1.0), w=["one1"])
    hp = p.sb("hpi", [G, 1]); p.op("dve", lambda e: e.memset(hp[:], float(np.pi / 2)), w=["hpi"])
    zr = p.sb("zr", [G, 1]); p.op("dve", lambda e: e.memset(zr[:], 0.0), w=["zr"])
    emit_sin(p, cs[:, 1, :], ("cs", 1), li[:, :], "li", DP, tmp, "tmp", one[:, 0:1], zr[:, 0:1], extra_r=["one1", "zr"])
    emit_sin(p, cs[:, 0, :], ("cs", 0), li[:, :], "li", DP, tmp, "tmp", one[:, 0:1], hp[:, 0:1], extra_r=["one1", "hpi"])
    pw = p.sb("pw", [G, NPW, 2, DP])
    p.op("dve", lambda e: e.memset(pw[:, 0, 0, :], 1.0), w=["pw"])
    p.op("dve", lambda e: e.memset(pw[:, 0, 1, :], 0.0), w=["pw"])
    p.op("dve", lambda e: e.tensor_tensor(out=pw[:, 1, 0, :], in0=mag[:], in1=cs[:, 0, :], op=ALU.mult), r=["mag", ("cs", 0)], w=["pw"])
    p.op("dve", lambda e: e.tensor_tensor(out=pw[:, 1, 1, :], in0=mag[:], in1=cs[:, 1, :], op=ALU.mult), r=["mag", ("cs", 1)], w=["pw"])
    t0, t1 = tmp

    def cmul(out_re, out_im, are, aim, bre, bim, rk, wk, neg_im_out=None):
        raise NotImplementedError

    for l in range(1, CH):
        p.op("dve", lambda e, l=l: e.tensor_tensor(out=t0[:], in0=pw[:, l, 0, :], in1=pw[:, 1, 0, :], op=ALU.mult), r=["pw"], w=[("tmp", 0)])
        p.op("dve", lambda e, l=l: e.tensor_tensor(out=t1[:], in0=pw[:, l, 1, :], in1=pw[:, 1, 1, :], op=ALU.mult), r=["pw"], w=[("tmp", 1)])
        p.op("dve", lambda e, l=l: e.tensor_tensor(out=pw[:, l + 1, 0, :], in0=t0[:], in1=t1[:], op=ALU.subtract), r=[("tmp", 0), ("tmp", 1)], w=["pw"])
        p.op("dve", lambda e, l=l: e.tensor_tensor(out=t0[:], in0=pw[:, l, 0, :], in1=pw[:, 1, 1, :], op=ALU.mult), r=["pw"], w=[("tmp", 0)])
        p.op("dve", lambda e, l=l: e.tensor_tensor(out=t1[:], in0=pw[:, l, 1, :], in1=pw[:, 1, 0, :], op=ALU.mult), r=["pw"], w=[("tmp", 1)])
        p.op("dve", lambda e, l=l: e.tensor_tensor(out=pw[:, l + 1, 1, :], in0=t0[:], in1=t1[:], op=ALU.add), r=[("tmp", 0), ("tmp", 1)], w=["pw"])
    pc = p.sb("pc", [G, 3, DP])
    p.op("dve", lambda e: e.tensor_copy(out=pc[:, 0:2, :], in_=pw[:, CH, :, :]), r=["pw"], w=["pc"])
    p.op("dve", lambda e: e.tensor_scalar(out=pc[:, 2, :], in0=pw[:, CH, 1, :], scalar1=-1.0, scalar2=None, op0=ALU.mult), r=["pw"], w=["pc"])
    p.dma("sync", PC.rearrange("g t d p -> g t (d p)"), pc[:], r=["pc"], w=["PC"])
    q = p.sb("q", [G, 2, DP]); dd = p.sb("dd", [G, DP]); nr = p.sb("nr", [G, DP])
    p.op("dve", lambda e: e.tensor_scalar(out=nr[:], in0=pw[:, 1, 0, :], scalar1=-1.0, scalar2=None, op0=ALU.add), r=["pw"], w=["nr"])
    p.op("dve", lambda e: e.tensor_tensor(out=t0[:], in0=a[:, 0, :], in1=a[:, 0, :], op=ALU.mult), r=["a"], w=[("tmp", 0)])
    p.op("dve", lambda e: e.tensor_tensor(out=t1[:], in0=a[:, 1, :], in1=a[:, 1, :], op=ALU.mult), r=["a"], w=[("tmp", 1)])
    p.op("dve", lambda e: e.tensor_tensor(out=dd[:], in0=t0[:], in1=t1[:], op=ALU.add), r=[("tmp", 0), ("tmp", 1)], w=["dd"])
    p.op("dve", lambda e: e.reciprocal(out=dd[:], in_=dd[:]), r=["dd"], w=["dd"])
    p.op("dve", lambda e: e.tensor_tensor(out=t0[:], in0=nr[:], in1=a[:, 0, :], op=ALU.mult), r=["nr", "a"], w=[("tmp", 0)])
    p.op("dve", lambda e: e.tensor_tensor(out=t1[:], in0=pw[:, 1, 1, :], in1=a[:, 1, :], op=ALU.mult), r=["pw", "a"], w=[("tmp", 1)])
    p.op("dve", lambda e: e.tensor_tensor(out=t0[:], in0=t0[:], in1=t1[:], op=ALU.add), r=[("tmp", 0), ("tmp", 1)], w=[("tmp", 0)])
    p.op("dve", lambda e: e.tensor_tensor(out=q[:, 0, :], in0=t0[:], in1=dd[:], op=ALU.mult), r=[("tmp", 0), "dd"], w=["q"])
    p.op("dve", lambda e: e.tensor_tensor(out=t0[:], in0=pw[:, 1, 1, :], in1=a[:, 0, :], op=ALU.mult), r=["pw", "a"], w=[("tmp", 0)])
    p.op("dve", lambda e: e.tensor_tensor(out=t1[:], in0=nr[:], in1=a[:, 1, :], op=ALU.mult), r=["nr", "a"], w=[("tmp", 1)])
    p.op("dve", lambda e: e.tensor_tensor(out=t0[:], in0=t0[:], in1=t1[:], op=ALU.subtract), r=[("tmp", 0), ("tmp", 1)], w=[("tmp", 0)])
    p.op("dve", lambda e: e.tensor_tensor(out=q[:, 1, :], in0=t0[:], in1=dd[:], op=ALU.mult), r=[("tmp", 0), "dd"], w=["q"])
    NB_ = 2 * P_ * H
    bb = p.sb("bb", [G, 2, NB_]); w0 = p.sb("w0", [G, NB_]); w1 = p.sb("w1", [G, NB_])
    v_ph = lambda t: t.rearrange("g (dp h) -> g dp h", h=H)
    bc_h = lambda t: t.unsqueeze(2).broadcast_to([G, DP, H])

    def cmul_ph(out_re, out_im, sre, sim, xre, xim, rk, wk, neg_im=False):
        p.op("dve", lambda e: e.tensor_tensor(out=v_ph(w0[:, :]), in0=v_ph(xre), in1=bc_h(sre), op=ALU.mult), r=rk, w=["w0"])
        p.op("pool", lambda e: e.tensor_tensor(out=v_ph(w1[:, :]), in0=v_ph(xim), in1=bc_h(sim), op=ALU.mult), r=rk, w=["w1"])
        p.op("dve", lambda e: e.tensor_tensor(out=out_re, in0=w0[:, :], in1=w1[:, :], op=ALU.subtract), r=["w0", "w1"], w=wk)
        p.op("dve", lambda e: e.tensor_tensor(out=v_ph(w0[:, :]), in0=v_ph(xim), in1=bc_h(sre), op=ALU.mult), r=rk, w=["w0"])
        p.op("pool", lambda e: e.tensor_tensor(out=v_ph(w1[:, :]), in0=v_ph(xre), in1=bc_h(sim), op=ALU.mult), r=rk, w=["w1"])
        if neg_im:
            p.op("dve", lambda e: e.scalar_tensor_tensor(out=out_im, in0=w0[:, :], scalar=-1.0, in1=w1[:, :], op0=ALU.mult, op1=ALU.subtract),
                 r=["w0", "w1"], w=wk)
        else:
            p.op("dve", lambda e: e.tensor_tensor(out=out_im, in0=w0[:, :], in1=w1[:, :], op=ALU.add), r=["w0", "w1"], w=wk)

    cmul_ph(bb[:, 0, :], bb[:, 1, :], q[:, 0, :], q[:, 1, :], b[:, 0, :], b[:, 1, :], ["q", "b"], ["bb"])
    pl = p.sb("pl", [G, 2, DP]); pl1 = p.sb("pl1", [G, 2, DP])
    xb = p.sb("xb", [G, 2, NB_]); oc = p.sb("oc", [G, 2, NB_]); mo = p.sb("mo", [G, 2, H * H])
    HH = H * H
    big0 = p.sb("big0", [G, (H // 2) * H * P_]); big1 = p.sb("big1", [G, (H // 2) * H * P_])
    for n in range(NLc):
        for (dst, off, key) in ((pl, 0, "pl"), (pl1, 1, "pl1")):
            for comp in range(2):
                first = True
                for l in range(CH):
                    src = pw[:, l + off, comp, :]
                    if first:
                        p.op("dve", lambda e, dst=dst, comp=comp, src=src, n=n, l=l: e.tensor_scalar(
                            out=dst[:, comp, :], in0=src, scalar1=sel[:, n, l:l + 1], scalar2=None, op0=ALU.mult), r=["pw", "sel"], w=[key])
                        first = False
                    else:
                        p.op("dve", lambda e, dst=dst, comp=comp, src=src, n=n, l=l: e.scalar_tensor_tensor(
                            out=dst[:, comp, :], in0=src, scalar=sel[:, n, l:l + 1], in1=dst[:, comp, :], op0=ALU.mult, op1=ALU.add),
                            r=["pw", "sel", key], w=[key])
        cmul_ph(xb[:, 0, :], xb[:, 1, :], pl[:, 0, :], pl[:, 1, :], bb[:, 0, :], bb[:, 1, :], ["pl", "bb"], ["xb"])
        p.dma("sync", XB[:, n].rearrange("g r d p h -> g r (d p h)"), xb[:], r=["xb"], w=[("XB", n)], grp=("xbst",))
        cv = lambda t: t.rearrange("g (d h p) -> g d h p", d=2, h=H)
        bc_hp = lambda t: t.rearrange("g (d p) -> g d p", d=2).unsqueeze(2).broadcast_to([G, 2, H, P_])
        NC_ = 2 * H * P_
        p.op("dve", lambda e: e.tensor_tensor(out=cv(w0[:, :NC_]), in0=cv(c[:, 0, :]), in1=bc_hp(pl1[:, 0, :]), op=ALU.mult), r=["c", "pl1"], w=["w0"])
        p.op("pool", lambda e: e.tensor_tensor(out=cv(w1[:, :NC_]), in0=cv(c[:, 1, :]), in1=bc_hp(pl1[:, 1, :]), op=ALU.mult), r=["c", "pl1"], w=["w1"])
        p.op("dve", lambda e: e.tensor_tensor(out=oc[:, 0, :], in0=w0[:, :NC_], in1=w1[:, :NC_], op=ALU.subtract), r=["w0", "w1"], w=["oc"])
        p.op("dve", lambda e: e.tensor_tensor(out=cv(w0[:, :NC_]), in0=cv(c[:, 1, :]), in1=bc_hp(pl1[:, 0, :]), op=ALU.mult), r=["c", "pl1"], w=["w0"])
        p.op("pool", lambda e: e.tensor_tensor(out=cv(w1[:, :NC_]), in0=cv(c[:, 0, :]), in1=bc_hp(pl1[:, 1, :]), op=ALU.mult), r=["c", "pl1"], w=["w1"])
        p.op("dve", lambda e: e.scalar_tensor_tensor(out=oc[:, 1, :], in0=w0[:, :NC_], scalar=-1.0, in1=w1[:, :NC_], op0=ALU.mult, op1=ALU.subtract),
             r=["w0", "w1"], w=["oc"])
        p.dma("sync", OC[:, n].rearrange("g r d h p -> g r (d h p)"), oc[:], r=["oc"], w=[("OC", n)], grp=("ocst",))
        for d in range(2):
            for hh in range(2):
                h0 = hh * (H // 2)
                def cview(comp, d=d, h0=h0):
                    t = c[:, comp, d * H * P_:(d + 1) * H * P_].rearrange("g (h p) -> g h p", p=P_)[:, h0:h0 + H // 2, :]
                    return t.unsqueeze(2).broadcast_to([G, H // 2, H, P_])
                def xview(comp, d=d):
                    t = xb[:, comp, d * P_ * H:(d + 1) * P_ * H].rearrange("g (p h) -> g h p", h=H)
                    return t.unsqueeze(1).broadcast_to([G, H // 2, H, P_])
                b0v = big0[:, :].rearrange("g (a h p) -> g a h p", a=H // 2, h=H)
                b1v = big1[:, :].rearrange("g (a h p) -> g a h p", a=H // 2, h=H)
                p.op("dve", lambda e, cview=cview, xview=xview, b0v=b0v: e.tensor_tensor(out=b0v, in0=cview(0), in1=xview(0), op=ALU.mult), r=["c", "xb"], w=["big0"])
                p.op("pool", lambda e, cview=cview, xview=xview, b1v=b1v: e.tensor_tensor(out=b1v, in0=cview(1), in1=xview(1), op=ALU.mult), r=["c", "xb"], w=["big1"])
                p.op("dve", lambda e: e.tensor_tensor(out=big0[:, :], in0=big0[:, :], in1=big1[:, :], op=ALU.subtract), r=["big0", "big1"], w=["big0"])
                p.op("dve", lambda e, d=d, h0=h0: e.tensor_reduce(out=mo[:, d, h0 * H:(h0 + H // 2) * H],
                                                                 in_=big0[:, :].rearrange("g (a p) -> g a p", p=P_), axis=AX.X, op=ALU.add),
                     r=["big0"], w=["mo"])
        p.dma("sync", Mo[:, n].rearrange("g d a b -> g d (a b)"), mo[:], r=["mo"], w=[("Mo", n)], grp=("most",))
    return p.build()


def run_s5prep(cfg, I, CH):
    G, NC = cfg.G, cfg.NCORE
    P_, H = S5P, S5H
    NLc = max(1, CH // NC)
    nc = build_s5prep(cfg, CH)
    a_d = np.ascontiguousarray(np.stack([I["s5_a_re"][0], I["s5_a_im"][0]]).transpose(2, 0, 1, 3)).astype(np.float32)
    ls_d = np.ascontiguousarray(I["s5_log_step"][0].T).astype(np.float32)
    b_d = np.ascontiguousarray(np.stack([I["s5_b_re"][0], I["s5_b_im"][0]]).transpose(2, 0, 1, 3, 4)).astype(np.float32)
    c_d = np.ascontiguousarray(np.stack([I["s5_c_re"][0], I["s5_c_im"][0]]).transpose(2, 0, 1, 3, 4)).astype(np.float32)
    ims = []
    lags = []
    for c in range(NC):
        ls = [c + NC * n for n in range(NLc)] if CH >= NC else [c % CH]
        lags.append(ls)
        sel = np.zeros((G, NLc, CH + 1), np.float32)
        for n, l in enumerate(ls):
            sel[:, n, l] = 1.0
        ims.append({"a_d": a_d, "ls_d": ls_d, "b_d": b_d, "c_d": c_d, "lsel": sel})
    res = run(nc, ims)
    XB = np.zeros((CH, G, 2, 2, P_, H), np.float32); OC = np.zeros((CH, G, 2, 2, H, P_), np.float32); Mo = np.zeros((CH, G, 2, H, H), np.float32)
    for c in range(NC):
        for n, l in enumerate(lags[c]):
            XB[l] = res[c]["XB"][:, n]; OC[l] = res[c]["OC"][:, n]; Mo[l] = res[c]["Mo"][:, n]
    PC = res[0]["PC"]
    return dict(XB=XB, OC=OC, Mo=Mo, PC=PC)


def build_s5(cfg, CH, PG):
    p = Prog()
    B = cfg.B
    GPC = cfg.G // cfg.NCORE
    NPr = 2 * GPC
    KR = CH * S5H
    KP = KR // 128
    LT = cfg.LC + cfg.L
    NCK = LT // CH
    NCOL = NCK * B
    CL0 = (cfg.LC // CH) * B
    NCOLL = NCOL - CL0
    U = p.din("U", [NPr, 128, KP, NCOL], BF16)
    Tm = p.din("Tm", [NPr, 128, KP, KR]); Xm = p.din("Xm", [NPr, 128, 2, KP, 128]); Om = p.din("Om", [NPr, 128, KR])
    CAd = p.din("CA", [128, NPr]); CBd = p.din("CB", [128, NPr])
    Y = p.dout("Y", [NPr, 128, KP, NCOLL], BF16)
    ca = p.sb("ca", [128, NPr]); cb = p.sb("cb", [128, NPr])
    p.dma("sync", ca[:], CAd[:, :], w=["ca"]); p.dma("sync", cb[:], CBd[:, :], w=["cb"])
    SA = p.sb("SA", [128, PG, NCK, B]); SW = p.sb("SW", [128, PG, NCK, B]); SAb = p.sb("SAb", [128, PG, NCK, B], BF16)
    Ub = p.sb("Ub", [128, PG, KP, NCOL], BF16)
    Tb = p.sb("Tb", [128, PG, KP, KR], BF16); Xb = p.sb("Xb", [128, PG, 2, KP, 128], BF16); Ob = p.sb("Ob", [128, PG, KR], BF16)
    stT = p.sb("stT", [128, KP, KR]); stX = p.sb("stX", [128, 2, KP, 128]); stO = p.sb("stO", [128, KR])
    tq = [p.sb("tq%d" % i, [128, PG, B]) for i in range(4)]
    yo = [p.sb("yo%d" % i, [128, 512], BF16) for i in range(2)]
    px = [p.ps("px%d" % i, [128, 512]) for i in range(2)]
    py = [p.ps("py%d" % i, [128, 512]) for i in range(2)]
    yi = 0
    for pg0 in range(0, NPr, PG):
        for pi in range(PG):
            pr = pg0 + pi
            p.dma("sync", Ub[:, pi], U[pr], w=[("Ub", pi)])
            p.dma("pool", stT[:], Tm[pr], w=["stT"]); p.dma("pool", stX[:], Xm[pr], w=["stX"]); p.dma("pool", stO[:], Om[pr], w=["stO"])
            p.op("dve", lambda e, pi=pi: e.tensor_copy(out=Tb[:, pi], in_=stT[:]), r=["stT"], w=[("Tb", pi)])
            p.op("act", lambda e, pi=pi: e.activation(out=Xb[:, pi], in_=stX[:], func=AF.Copy), r=["stX"], w=[("Xb", pi)])
            p.op("dve", lambda e, pi=pi: e.tensor_copy(out=Ob[:, pi], in_=stO[:]), r=["stO"], w=[("Ob", pi)])
            SAf = SA[:, pi].rearrange("p k b -> p (k b)")
            SWf = SW[:, pi].rearrange("p k b -> p (k b)")
            for (c0, cw) in tiles(NCOL, 512):
                for w_, dstf, key in ((0, SAf, "SA"), (1, SWf, "SW")):
                    for qk in range(KP):
                        p.op("pe", lambda e, pi=pi, w_=w_, qk=qk, c0=c0, cw=cw: e.matmul(px[w_][:, :cw], lhsT=Xb[:, pi, w_, qk, :], rhs=Ub[:, pi, qk, c0:c0 + cw],
                                                                                     start=(qk == 0), stop=(qk == KP - 1)),
                             r=[("Xb", pi), ("Ub", pi)], w=[("px", w_)])
                    if w_ == 0:
                        p.op("act", lambda e, dstf=dstf, c0=c0, cw=cw: e.activation(out=dstf[:, c0:c0 + cw], in_=px[0][:, :cw], func=AF.Copy),
                             r=[("px", 0)], w=[("SA", pi)])
                    else:
                        p.op("dve", lambda e, dstf=dstf, c0=c0, cw=cw: e.tensor_copy(out=dstf[:, c0:c0 + cw], in_=px[1][:, :cw]),
                             r=[("px", 1)], w=[("SW", pi)])
        cab = ca[:, pg0:pg0 + PG].unsqueeze(2).broadcast_to([128, PG, B])
        cbb = cb[:, pg0:pg0 + PG].unsqueeze(2).broadcast_to([128, PG, B])
        allSA = [("SA", pi) for pi in range(PG)]
        allSW = [("SW", pi) for pi in range(PG)]
        for k in range(1, NCK):
            rA = allSA if k == 1 else [("SAk", k - 1)]
            rW = allSW if k == 1 else [("SWk", k - 1)]
            wA = (allSA if k == 1 else []) + [("SAk", k)]
            wW = (allSW if k == 1 else []) + [("SWk", k)]
            Sp = SA[:, :, k - 1, :]; Wp = SW[:, :, k - 1, :]; Sk = SA[:, :, k, :]; Wk = SW[:, :, k, :]
            p.op("dve", lambda e, Sp=Sp, cab=cab: e.tensor_tensor(out=tq[0][:], in0=Sp, in1=cab, op=ALU.mult), r=rA + ["ca"], w=["tq0"])
            p.op("dve", lambda e, Wp=Wp, cbb=cbb: e.tensor_tensor(out=tq[1][:], in0=Wp, in1=cbb, op=ALU.mult), r=rW + ["cb"], w=["tq1"])
            p.op("dve", lambda e: e.tensor_tensor(out=tq[0][:], in0=tq[0][:], in1=tq[1][:], op=ALU.add), r=["tq0", "tq1"], w=["tq0"])
            p.op("dve", lambda e, Sk=Sk: e.tensor_tensor(out=Sk, in0=Sk, in1=tq[0][:], op=ALU.add), r=["tq0"] + rA, w=wA)
            p.op("pool", lambda e, Wp=Wp, cab=cab: e.tensor_tensor(out=tq[2][:], in0=Wp, in1=cab, op=ALU.mult), r=rW + ["ca"], w=["tq2"])
            p.op("pool", lambda e, Sp=Sp, cbb=cbb: e.tensor_tensor(out=tq[3][:], in0=Sp, in1=cbb, op=ALU.mult), r=rA + ["cb"], w=["tq3"])
            p.op("pool", lambda e: e.tensor_tensor(out=tq[2][:], in0=tq[2][:], in1=tq[3][:], op=ALU.subtract), r=["tq2", "tq3"], w=["tq2"])
            p.op("pool", lambda e, Wk=Wk: e.tensor_tensor(out=Wk, in0=Wk, in1=tq[2][:], op=ALU.add), r=["tq2"] + rW, w=wW)
        fin = [("SAk", NCK - 1), ("SWk", NCK - 1)] + allSA + allSW
        p.op("dve", lambda e: e.memset(SAb[:, :, 0, :], 0.0), r=fin, w=["SAb"])
        p.op("act", lambda e: e.activation(out=SAb[:, :, 1:NCK, :], in_=SA[:, :, 0:NCK - 1, :], func=AF.Copy), r=fin + [("SAk", k) for k in range(1, NCK)], w=["SAb"])
        for pi in range(PG):
            pr = pg0 + pi
            SAbf = SAb[:, pi].rearrange("p k b -> p (k b)")
            for mb in range(KP):
                for (c0, cw) in tiles(NCOLL, 512):
                    q = yi % 2
                    yi += 1
                    a0 = CL0 + c0
                    for qk in range(mb + 1):
                        p.op("pe", lambda e, pi=pi, mb=mb, qk=qk, q=q, a0=a0, cw=cw: e.matmul(
                            py[q][:, :cw], lhsT=Tb[:, pi, qk, mb * 128:(mb + 1) * 128], rhs=Ub[:, pi, qk, a0:a0 + cw], start=(qk == 0), stop=False),
                            r=[("Tb", pi), ("Ub", pi)], w=[("py", q)])
                    p.op("pe", lambda e, pi=pi, mb=mb, q=q, a0=a0, cw=cw, SAbf=SAbf: e.matmul(
                        py[q][:, :cw], lhsT=Ob[:, pi, mb * 128:(mb + 1) * 128], rhs=SAbf[:, a0:a0 + cw], start=False, stop=True),
                        r=[("Ob", pi), "SAb"], w=[("py", q)])
                    if q:
                        p.op("act", lambda e, q=q, cw=cw: e.activation(out=yo[q][:, :cw], in_=py[q][:, :cw], func=AF.Copy), r=[("py", q)], w=[("yo", q)])
                    else:
                        p.op("dve", lambda e, q=q, cw=cw: e.tensor_copy(out=yo[q][:, :cw], in_=py[q][:, :cw]), r=[("py", q)], w=[("yo", q)])
                    p.dma("sync", Y[pr, :, mb, c0:c0 + cw], yo[q][:, :cw], r=[("yo", q)], w=[("Y", pr, mb, c0)], grp=("yst", q))
        for k in range(1, NCK):
            for nm in ("SAk", "SWk"):
                pass
        p.barrier()
    return p.build()


def s5_matrices(cfg, prep, CH):
    G = cfg.G
    P_, H = S5P, S5H
    KR = CH * H
    KP = KR // 128
    XB, OC, Mo, PC = prep["XB"], prep["OC"], prep["Mo"], prep["PC"]
    NPall = G * 2
    Tm = np.zeros((NPall, KR, KR), np.float32); Xm = np.zeros((NPall, 2, KR, 128), np.float32); Om = np.zeros((NPall, 128, KR), np.float32)
    CA = np.zeros((128, NPall), np.float32); CB = np.zeros((128, NPall), np.float32)
    for g in range(G):
        for d in range(2):
            pr = g * 2 + d
            for j in range(CH):
                for j2 in range(j, CH):
                    Tm[pr, j * H:(j + 1) * H, j2 * H:(j2 + 1) * H] = Mo[j2 - j][g, d].T
                xb = XB[CH - 1 - j][g, :, d]
                Xm[pr, 0, j * H:(j + 1) * H, 0:P_] = xb[0].T; Xm[pr, 0, j * H:(j + 1) * H, P_:] = xb[1].T
                Xm[pr, 1, j * H:(j + 1) * H, 0:P_] = xb[1].T; Xm[pr, 1, j * H:(j + 1) * H, P_:] = xb[0].T
                oc = OC[j][g, :, d]
                Om[pr, 0:P_, j * H:(j + 1) * H] = oc[0].T; Om[pr, P_:, j * H:(j + 1) * H] = oc[1].T
            CA[0:P_, pr] = PC[g, 0, d]; CA[P_:, pr] = PC[g, 0, d]
            CB[0:P_, pr] = PC[g, 2, d]; CB[P_:, pr] = PC[g, 1, d]
    Tm = np.ascontiguousarray(Tm.reshape(NPall, KP, 128, KR).transpose(0, 2, 1, 3))
    Xm = np.ascontiguousarray(Xm.reshape(NPall, 2, KP, 128, 128).transpose(0, 3, 1, 2, 4))
    return Tm, Xm, Om, CA, CB


def run_s5(cfg, mats, u_lat, u_ctx, CH, PG):
    import ml_dtypes
    bf = ml_dtypes.bfloat16
    B, L, LC, D, NC = cfg.B, cfg.L, cfg.LC, cfg.D, cfg.NCORE
    GPC = cfg.G // NC
    NPr = 2 * GPC
    H = S5H
    KR = CH * H; KP = KR // 128
    LT = LC + L; NCK = LT // CH; NCOL = NCK * B
    CL0 = (LC // CH) * B
    Tm, Xm, Om, CA, CB = mats
    nc = build_s5(cfg, CH, PG)
    seqs = [np.concatenate([u_ctx, u_lat], 1), np.concatenate([u_ctx[:, ::-1], u_lat[:, ::-1]], 1)]
    ims = []
    for c in range(NC):
        U = np.zeros((NPr, 128, KP, NCOL), bf)
        for gi in range(GPC):
            g = c * GPC + gi
            for d in range(2):
                s = seqs[d][:, :, g * H:(g + 1) * H]
                s = s.reshape(B, NCK, CH, H).transpose(2, 3, 1, 0)
                U[gi * 2 + d] = s.reshape(KP, 128, NCOL).transpose(1, 0, 2)
        sl = slice(c * NPr, (c + 1) * NPr)
        ims.append({"U": U, "Tm": Tm[sl], "Xm": Xm[sl], "Om": Om[sl], "CA": np.ascontiguousarray(CA[:, sl]), "CB": np.ascontiguousarray(CB[:, sl])})
    res = run(nc, ims)
    ys = [np.zeros((B, L, D), bf), np.zeros((B, L, D), bf)]
    for c in range(NC):
        Yc = np.asarray(res[c]["Y"])
        for gi in range(GPC):
            g = c * GPC + gi
            for d in range(2):
                a = Yc[gi * 2 + d].transpose(1, 0, 2).reshape(CH, H, L // CH, B)
                a = a.transpose(3, 2, 0, 1).reshape(B, L, H)
                if d == 1:
                    a = a[:, ::-1]
                ys[d][:, :, g * H:(g + 1) * H] = a
    return ys


def build_glu(cfg):
    p = Prog()
    ND, D, TL = cfg.ND, cfg.D, cfg.TL
    TK = 64
    yfT = p.din("yfT", [128, ND, TL], BF16); ybT = p.din("ybT", [128, ND, TL], BF16); uT = p.din("uT", [128, ND, TL], BF16)
    xT = p.din("xT", [128, ND, TL]); w1 = p.din("w1", [128, ND, D]); w2 = p.din("w2", [128, ND, D]); vecs = p.din("vecsD", [128, 7, ND])
    wr_d = p.din("wrD", [128, ND, 128]); br_d = p.din("brD", [128, 1]); ident_d = p.din("identD", [128, 128])
    xlT = p.dout("xlT", [128, ND, TL]); tokT = p.dout("tokT", [128, ND, TL], BF16); gates = p.dout("gates", [TL, 32])
    C = PostCtx(p, cfg, TK)
    p.dma("sync", C.ident[:], ident_d[:, :], w=["ident"]); p.dma("sync", C.wr[:], wr_d[:, :, :], w=["wr"]); p.dma("sync", C.br[:], br_d[:, :], w=["br"])
    vs_ = p.sb("vecs", [128, 7, ND]); p.dma("sync", vs_[:], vecs[:, :, :], w=["vecs"])
    m2 = p.sb("m2", [128, ND])
    p.op("dve", lambda e: e.scalar_tensor_tensor(out=m2[:, :], in0=vs_[:, 5, :], scalar=1.0, in1=vs_[:, 4, :], op0=ALU.add, op1=ALU.mult), r=["vecs"], w=["vecs"])
    W1b = p.sb("W1b", [128, ND, D], BF16); W2b = p.sb("W2b", [128, ND, D], BF16)
    wf = [p.sb("wf%d" % i, [128, ND, 128]) for i in range(2)]
    wi = 0
    for (src, dst, key) in ((w1, W1b, "W1b"), (w2, W2b, "W2b")):
        for blk in range(ND):
            s = wi % 2
            wi += 1
            p.dma("sync", wf[s][:], src[:, :, blk * 128:(blk + 1) * 128], w=[("wf", s)])
            p.op("pool", lambda e, s=s, blk=blk, dst=dst: e.tensor_copy(out=dst[:, :, blk * 128:(blk + 1) * 128], in_=wf[s][:]), r=[("wf", s)], w=[key])
    yf = p.sb("yf", [128, ND, TK], BF16); yb = p.sb("yb", [128, ND, TK], BF16); uu = p.sb("uu", [128, ND, TK], BF16)
    ys = p.sb("ys", [128, ND, TK]); yt = p.sb("yt", [128, ND, TK]); a = p.sb("a", [128, ND, TK], BF16)
    sg = p.sb("sg", [128, TK])
    pz2 = [p.ps("pzb%d" % i, [128, TK]) for i in range(2)]
    GC = 2.0 * float(np.sqrt(2.0 / np.pi))
    qi = 0
    for (c0, cw) in tiles(TL, TK):
        p.dma("sync", yf[:, :, :cw], yfT[:, :, c0:c0 + cw], w=["yf"])
        p.dma("pool", yb[:, :, :cw], ybT[:, :, c0:c0 + cw], w=["yb"])
        p.dma("pool", uu[:, :, :cw], uT[:, :, c0:c0 + cw], w=["uu"])
        p.dma("sync", C.xt[:, :, :cw], xT[:, :, c0:c0 + cw], w=["xt"])
        p.op("dve", lambda e, cw=cw: e.tensor_tensor(out=ys[:, :, :cw], in0=yf[:, :, :cw], in1=yb[:, :, :cw], op=ALU.add), r=["yf", "yb"], w=["ys"])
        for k in range(ND):
            p.op("dve", lambda e, k=k, cw=cw: e.scalar_tensor_tensor(out=ys[:, k, :cw], in0=uu[:, k, :cw], scalar=vs_[:, 0, k:k + 1], in1=ys[:, k, :cw],
                                                                   op0=ALU.mult, op1=ALU.add), r=["uu", "ys", "vecs"], w=["ys"])
        p.op("pool", lambda e, cw=cw: e.tensor_tensor(out=yt[:, :, :cw], in0=ys[:, :, :cw], in1=ys[:, :, :cw], op=ALU.mult), r=["ys"], w=["yt"])
        p.op("dve", lambda e, cw=cw: e.tensor_scalar(out=yt[:, :, :cw], in0=yt[:, :, :cw], scalar1=0.044715, scalar2=1.0, op0=ALU.mult, op1=ALU.add), r=["yt"], w=["yt"])
        p.op("pool", lambda e, cw=cw: e.tensor_tensor(out=yt[:, :, :cw], in0=yt[:, :, :cw], in1=ys[:, :, :cw], op=ALU.mult), r=["yt", "ys"], w=["yt"])
        p.op("act", lambda e, cw=cw: e.activation(out=yt[:, :, :cw], in_=yt[:, :, :cw], func=AF.Sigmoid, scale=GC), r=["yt"], w=["yt"])
        p.op("dve", lambda e, cw=cw: e.tensor_tensor(out=a[:, :, :cw], in0=yt[:, :, :cw], in1=ys[:, :, :cw], op=ALU.mult), r=["yt", "ys"], w=["a"])
        for blk in range(ND):
            q = qi % 2
            qi += 1
            for k in range(ND):
                p.op("pe", lambda e, q=q, k=k, blk=blk, cw=cw: e.matmul(C.pz[q][:, :cw], lhsT=W1b[:, k, blk * 128:(blk + 1) * 128], rhs=a[:, k, :cw],
                                                                       start=(k == 0), stop=(k == ND - 1)), r=["W1b", "a"], w=[("pz", q)])
            for k in range(ND):
                p.op("pe", lambda e, q=q, k=k, blk=blk, cw=cw: e.matmul(pz2[q][:, :cw], lhsT=W2b[:, k, blk * 128:(blk + 1) * 128], rhs=a[:, k, :cw],
                                                                       start=(k == 0), stop=(k == ND - 1)), r=["W2b", "a"], w=[("pzb", q)])
            p.op("act", lambda e, q=q, blk=blk, cw=cw: e.activation(out=sg[:, :cw], in_=pz2[q][:, :cw], func=AF.Sigmoid, bias=vs_[:, 2, blk:blk + 1], scale=1.0),
                 r=[("pzb", q), "vecs"], w=["sg"])
            p.op("dve", lambda e, q=q, blk=blk, cw=cw: e.scalar_tensor_tensor(out=C.olt[:, :cw], in0=C.pz[q][:, :cw], scalar=vs_[:, 1, blk:blk + 1], in1=sg[:, :cw],
                                                                            op0=ALU.add, op1=ALU.mult), r=[("pz", q), "sg", "vecs"], w=["olt"])
            p.op("dve", lambda e, blk=blk, cw=cw: e.scalar_tensor_tensor(out=C.xl[:, blk, :cw], in0=C.olt[:, :cw], scalar=vs_[:, 3, blk:blk + 1], in1=C.xt[:, blk, :cw],
                                                                       op0=ALU.mult, op1=ALU.add), r=["olt", "xt", "vecs"], w=["xl"])
        p.dma("sync", xlT[:, :, c0:c0 + cw], C.xl[:, :, :cw], r=["xl"], w=[("xlo", c0)], grp=("xlst",))
        emit_norm_router(p, cfg, C, cw, lambda k: m2[:, k:k + 1], lambda k: vs_[:, 6, k:k + 1], tokT[:, :, c0:c0 + cw],
                         lambda t0, tw, c0=c0: gates[c0 + t0:c0 + t0 + tw, :])
    return p.build()


def lat_layout(cfg, lat):
    return [lat[c // cfg.CPB, (c % cfg.CPB) * cfg.TL:(c % cfg.CPB + 1) * cfg.TL] for c in range(cfg.NCORE)]


def lat_unlayout(cfg, per):
    out = np.zeros((cfg.B, cfg.L, per[0].shape[1]), per[0].dtype)
    for c in range(cfg.NCORE):
        out[c // cfg.CPB, (c % cfg.CPB) * cfg.TL:(c % cfg.CPB + 1) * cfg.TL] = per[c]
    return out


def run_glu(cfg, I, mods, yf, yb, u_lat, xl_lat):
    import ml_dtypes
    bf = ml_dtypes.bfloat16
    ND, NC = cfg.ND, cfg.NCORE
    nc = build_glu(cfg)
    wr, br = router_inputs(cfg, I, 1)
    w1 = wfm(I["s5_w1"][0], ND); w2 = wfm(I["s5_w2"][0], ND)
    yfs, ybs, us, xs = lat_layout(cfg, yf), lat_layout(cfg, yb), lat_layout(cfg, u_lat), lat_layout(cfg, xl_lat)
    ims = []
    for c in range(NC):
        mvd = mod_vecs(cfg, mods[1], c // cfg.CPB)
        vecs = np.stack([vfm(np.asarray(v, np.float32), ND) for v in (I["s5_d"][0], I["s5_b1"][0], I["s5_b2"][0], mvd["gt_a"], I["norm_g"][1, 1],
                                                                        mvd["sc_f"], mvd["sh_f"])], axis=1)
        ims.append({"yfT": fm(yfs[c], ND).astype(bf), "ybT": fm(ybs[c], ND).astype(bf), "uT": fm(us[c], ND).astype(bf),
                    "xT": fm(xs[c].astype(np.float32), ND), "w1": w1, "w2": w2, "vecsD": np.ascontiguousarray(vecs),
                    "wrD": wr, "brD": br, "identD": np.eye(128, dtype=np.float32)})
    res = run(nc, ims)
    xl = lat_unlayout(cfg, [unfm(r["xlT"]) for r in res])
    tok = lat_unlayout(cfg, [unfm(np.asarray(r["tokT"])) for r in res])
    gates = lat_unlayout(cfg, [r["gates"] for r in res])
    return xl, tok, gates


def forward(cfg, I, CH=16, PG=8, CAP=1536, log=None):
    import time
    t0 = time.time()

    def lg(msg):
        if log:
            print("[fwd %.1fs] %s" % (time.time() - t0, msg), flush=True)
    D = cfg.D
    mods = run_mods(cfg, I); lg("mods")
    filt = run_filt(cfg, I); lg("filt")
    (v_l, v_c), (x0_l, x0_c) = run_h1(cfg, I, mods); lg("h1")
    y_l, y_c = run_h2(cfg, I, v_l, v_c, filt); lg("h2")
    xl, tok, gates = run_h3(cfg, I, mods, y_l, y_c, x0_l, x0_c); lg("h3")
    tok_all = np.concatenate([tok[0].reshape(-1, D), tok[1].reshape(-1, D)], 0)
    gates_all = np.concatenate([gates[0].reshape(-1, 32), gates[1].reshape(-1, 32)], 0)
    y01, g01 = run_moe(cfg, I, 0, tok_all, gates_all, CAP); lg("moe0")
    xl_lat, xl_ctx, u_lat, u_ctx = run_comb(cfg, I, mods, 0, xl[0], xl[1], y01, g01, False); lg("comb0")
    prep = run_s5prep(cfg, I, CH); lg("s5prep")
    mats = s5_matrices(cfg, prep, CH); lg("s5mats")
    yf, yb = run_s5(cfg, mats, u_lat, u_ctx, CH, PG); lg("s5")
    xl3, tok1, gates1 = run_glu(cfg, I, mods, yf, yb, u_lat, xl_lat); lg("glu")
    y01, g01 = run_moe(cfg, I, 1, tok1.reshape(-1, D), gates1.reshape(-1, 32), CAP); lg("moe1")
    _, _, out, _ = run_comb(cfg, I, mods, 1, xl3, None, y01, g01, True); lg("final")
    return np.ascontiguousarray(out.astype(np.float32))


def kernel(**inputs):
    I = {k: np.asarray(v) for k, v in inputs.items()}
    return forward(FULL, I, CH=16, PG=8, CAP=1536, log=True)
```

```python
import contextlib
import numpy as np
import concourse.bass as bass
import concourse.mybir as mybir
from concourse.bass_utils import run_bass_kernel_spmd

F32 = mybir.dt.float32
BF16 = mybir.dt.bfloat16
I32 = mybir.dt.int32
ALU = mybir.AluOpType
AF = mybir.ActivationFunctionType
AX = mybir.AxisListType

COMPUTE = ("pe", "act", "dve", "pool")


class Prog:
    def __init__(self):
        self.nc = bass.Bass("TRN2", target_bir_lowering=False)
        self.stack = contextlib.ExitStack()
        self.ops = []
        self.lastw = {}
        self.readers = {}
        self.out_names = []
        self.n_sb = 0
        self.barrier_idx = None

    def din(self, name, shape, dt=F32):
        return self.nc.dram_tensor(name, list(shape), dt, kind="ExternalInput").ap()

    def dout(self, name, shape, dt=F32):
        self.out_names.append(name)
        return self.nc.dram_tensor(name, list(shape), dt, kind="ExternalOutput").ap()

    def dtmp(self, name, shape, dt=F32):
        return self.nc.dram_tensor(name, list(shape), dt, kind="Internal").ap()

    def sb(self, name, shape, dt=F32):
        return self.stack.enter_context(self.nc.sbuf_tensor(name, list(shape), dt))

    def ps(self, name, shape, dt=F32):
        return self.stack.enter_context(self.nc.psum_tensor(name, list(shape), dt))

    def barrier(self):
        deps = set()
        last = {}
        for i, o in enumerate(self.ops):
            key = ("dma", o["grp"]) if o["dma"] else ("eng", o["eng"])
            last[key] = i
        deps = set(last.values())
        idx = len(self.ops)
        d = self.sb("bar%d" % idx, [128, 1])
        self.ops.append(dict(eng="dve", fn=lambda e: e.memset(d[:], 0.0), deps=deps, dma=False))
        self.barrier_idx = idx

    def _deps(self, r, w):
        deps = set()
        if self.barrier_idx is not None:
            deps.add(self.barrier_idx)
        for k in r:
            if k in self.lastw:
                deps.add(self.lastw[k])
        for k in w:
            if k in self.lastw:
                deps.add(self.lastw[k])
            for o in self.readers.get(k, ()):
                deps.add(o)
        return deps

    def _commit(self, idx, r, w):
        for k in r:
            self.readers.setdefault(k, []).append(idx)
        for k in w:
            self.lastw[k] = idx
            self.readers[k] = []

    def op(self, eng, fn, r=(), w=()):
        assert eng in COMPUTE
        idx = len(self.ops)
        deps = self._deps(r, w)
        self.ops.append(dict(eng=eng, fn=fn, deps=deps, dma=False))
        self._commit(idx, r, w)
        return idx

    def dma(self, q, out, in_, r=(), w=(), grp=None, **kw):
        idx = len(self.ops)
        deps = self._deps(r, w)
        if grp is None:
            grp = ("g", tuple(w)[0] if len(w) else tuple(r)[0])
        self.ops.append(dict(eng=q, fn=lambda e: e.dma_start(out=out, in_=in_, **kw),
                             deps=deps, dma=True, grp=grp))
        self._commit(idx, r, w)
        return idx

    def build(self):
        nc = self.nc
        ops = self.ops
        cnt = {}
        semkeys = []
        for o in ops:
            key = ("dma", o["grp"]) if o["dma"] else ("eng", o["eng"])
            if key not in cnt:
                cnt[key] = 0
                semkeys.append(key)
            cnt[key] += 16 if o["dma"] else 1
            o["sem"] = key
            o["val"] = cnt[key]
        sems = {}
        for i, key in enumerate(semkeys):
            sems[key] = self.stack.enter_context(nc.semaphore("s%d" % i))
        streams = {}
        for i, o in enumerate(ops):
            streams.setdefault(o["eng"], []).append(i)
        final_waits = [(sems[k], cnt[k]) for k in semkeys if k[0] == "dma"]
        blk = self.stack.enter_context(nc.Block())

        def emit_stream(eng_name, e, last=False):
            waited = {}
            for i in streams.get(eng_name, []):
                o = ops[i]
                need = {}
                for d in o["deps"]:
                    od = ops[d]
                    if od["eng"] == "pe" and o["eng"] == "pe" and not od["dma"] and not o["dma"]:
                        continue
                    k = od["sem"]
                    need[k] = max(need.get(k, 0), od["val"])
                for k, v in need.items():
                    if waited.get(k, 0) < v:
                        e.wait_ge(sems[k], v)
                        waited[k] = v
                ins = o["fn"](e)
                ins.then_inc(sems[o["sem"]], 16 if o["dma"] else 1)
            if last:
                for s, v in final_waits:
                    e.wait_ge(s, v)

        @blk.tensor
        def _(e):
            emit_stream("pe", e)

        @blk.scalar
        def _(e):
            emit_stream("act", e)

        @blk.vector
        def _(e):
            emit_stream("dve", e)

        @blk.gpsimd
        def _(e):
            emit_stream("pool", e)

        @blk.sync
        def _(e):
            emit_stream("sync", e, last=True)

        self.stack.close()
        return nc


def run(prog_nc, in_maps, n=8):
    res = run_bass_kernel_spmd(prog_nc, in_maps, core_ids=list(range(n)))
    return res.results


class Cfg:
    def __init__(self, D=2048, B=4, L=4096, LC=256):
        self.D, self.B, self.L, self.LC = D, B, L, LC
        self.NCORE = 8
        self.ND = D // 128
        self.G = D // 16
        self.DE = D // 2
        self.CPB = self.NCORE // B
        self.TL = L // self.CPB
        self.TC = LC // self.CPB
        self.Cc = D // self.NCORE
        self.NE = 32
        self.EPC = self.NE // self.NCORE


FULL = Cfg()
EPS = 1e-6
MAGIC = 12582912.0
TWO_PI = 6.283185307179586
PI_LO = 3.1415925


def fm(a, ND):
    T, D = a.shape
    return np.ascontiguousarray(a.T.reshape(ND, 128, T).transpose(1, 0, 2))


def unfm(a):
    P, ND, T = a.shape
    return np.ascontiguousarray(a.transpose(1, 0, 2).reshape(ND * P, T).T)


def vfm(v, ND):
    return np.ascontiguousarray(v.reshape(ND, 128).T)


def tiles(n, t):
    return [(s, min(t, n - s)) for s in range(0, n, t)]


def build_mods(cfg):
    p = Prog()
    ND, NB1 = cfg.ND, cfg.B + 1
    W = 6 * cfg.D // cfg.NCORE
    ccT = p.din("ccT", [128, ND, NB1])
    aw = p.din("aw", [2, 128, ND, W])
    ab = p.din("ab", [2, 1, W])
    out = p.dout("mods", [2, NB1, W])
    cs = p.sb("cs", [128, ND, 128])
    p.op("dve", lambda e: e.memset(cs[:], 0.0), w=["cs"])
    p.dma("sync", cs[:, :, :NB1], ccT[:, :, :], w=["cs"])
    p.op("act", lambda e: e.activation(out=cs[:, :, :NB1], in_=cs[:, :, :NB1], func=AF.Silu), r=["cs"], w=["cs"])
    WT = 512
    wt_sb = [p.sb("wt%d" % i, [128, ND, WT]) for i in range(2)]
    bt = [p.sb("bt%d" % i, [NB1, WT]) for i in range(2)]
    ot = [p.sb("ot%d" % i, [NB1, WT]) for i in range(2)]
    pm = [p.ps("pm%d" % i, [128, WT]) for i in range(2)]
    it = 0
    for l in range(2):
        for (c0, cw) in tiles(W, WT):
            s = it % 2
            it += 1
            p.dma("sync", wt_sb[s][:, :, :cw], aw[l, :, :, c0:c0 + cw], w=[("wt", s)])
            p.dma("pool", bt[s][:, :cw], ab[l, 0:1, c0:c0 + cw].broadcast_to([NB1, cw]), w=[("bt", s)])
            for k in range(ND):
                p.op("pe", lambda e, s=s, k=k, cw=cw: e.matmul(pm[s][:, :cw], lhsT=cs[:, k, :], rhs=wt_sb[s][:, k, :cw],
                                                               start=(k == 0), stop=(k == ND - 1)),
                     r=["cs", ("wt", s)], w=[("pm", s)])
            p.op("dve", lambda e, s=s, cw=cw: e.tensor_tensor(out=ot[s][:, :cw], in0=pm[s][:NB1, :cw], in1=bt[s][:, :cw], op=ALU.add),
                 r=[("pm", s), ("bt", s)], w=[("ot", s)])
            p.dma("sync", out[l, :, c0:c0 + cw], ot[s][:, :cw], r=[("ot", s)], w=[("out", l, c0)], grp=("st", s))
    return p.build()


def run_mods(cfg, I):
    ND, NC = cfg.ND, cfg.NCORE
    W = 6 * cfg.D // NC
    cc = np.concatenate([I["c"], I["c_ctx"][None]], 0).astype(np.float32)
    ccT = fm(cc, ND)
    nc = build_mods(cfg)
    ims = []
    for c in range(NC):
        aw = I["ada_w"][:, :, c * W:(c + 1) * W]
        aw = np.ascontiguousarray(aw.reshape(2, ND, 128, W).transpose(0, 2, 1, 3))
        ab = np.ascontiguousarray(I["ada_b"][:, None, c * W:(c + 1) * W])
        ims.append({"ccT": ccT, "aw": aw, "ab": ab})
    res = run(nc, ims)
    mods = np.concatenate([r["mods"] for r in res], axis=2)
    return mods


def emit_rstd(p, cfg, xt, xkey, ncols, ones, sq, sqkey, pss, psskey, rstd, rkey):
    ND = cfg.ND
    p.op("pool", lambda e: e.tensor_tensor(out=sq[:, :, :ncols], in0=xt[:, :, :ncols], in1=xt[:, :, :ncols], op=ALU.mult),
         r=[xkey], w=[sqkey])
    for k in range(ND):
        p.op("pe", lambda e, k=k: e.matmul(pss[:, :ncols], lhsT=ones[:], rhs=sq[:, k, :ncols], start=(k == 0), stop=(k == ND - 1)),
             r=[sqkey, "ones"], w=[psskey])
    p.op("act", lambda e: e.activation(out=rstd[:, :ncols], in_=pss[:, :ncols], func=AF.Sqrt, bias=EPS, scale=1.0 / cfg.D),
         r=[psskey], w=[rkey])
    p.op("dve", lambda e: e.reciprocal(out=rstd[:, :ncols], in_=rstd[:, :ncols]), r=[rkey], w=[rkey])


def build_h1(cfg):
    p = Prog()
    ND, D, TL, TC = cfg.ND, cfg.D, cfg.TL, cfg.TC
    TT = TL + TC + 4
    NT = TL + TC
    xT = p.din("xT", [128, ND, TT])
    hm = p.din("hm", [128, 4])
    mv = p.din("mv", [128, 5, ND])
    w_in = p.din("w_in", [128, ND, 3 * D])
    fv = p.din("fv", [128, 5, 3 * ND])
    vT = p.dout("vT", [128, ND, NT], BF16)
    x0T = p.dout("x0T", [128, ND, NT], BF16)

    ones = p.sb("ones", [128, 128])
    p.op("dve", lambda e: e.memset(ones[:], 1.0), w=["ones"])
    hms = p.sb("hms", [128, 4]); p.dma("sync", hms[:], hm[:, :], w=["hms"])
    mvs = p.sb("mvs", [128, 5, ND]); p.dma("sync", mvs[:], mv[:, :, :], w=["mvs"])
    fvs = p.sb("fvs", [128, 5, 3 * ND]); p.dma("sync", fvs[:], fv[:, :, :], w=["fvs"])
    ml = p.sb("ml", [128, 2, ND])
    for i, j in ((0, 1), (1, 3)):
        p.op("dve", lambda e, i=i, j=j: e.scalar_tensor_tensor(out=ml[:, i, :], in0=mvs[:, j, :], scalar=1.0, in1=mvs[:, 0, :],
                                                               op0=ALU.add, op1=ALU.mult), r=["mvs"], w=["ml"])
    u = p.sb("u", [128, ND, TT], BF16)
    TK = 256
    xt = p.sb("xt", [128, ND, TK]); sq = p.sb("sq", [128, ND, TK]); rstd = p.sb("rstd", [128, TK])
    pss = p.ps("pss", [128, TK])
    segs = [(0, TL + 2, 0), (TL + 2, TC + 2, 1)]
    for (s0, sl, mi) in segs:
        for (c0, cw) in tiles(sl, TK):
            a = s0 + c0
            p.dma("sync", xt[:, :, :cw], xT[:, :, a:a + cw], w=["xt"])
            emit_rstd(p, cfg, xt, "xt", cw, ones, sq, "sq", pss, "pss", rstd, "rstd")
            for k in range(ND):
                p.op("dve", lambda e, k=k, cw=cw: e.tensor_tensor(out=sq[:, k, :cw], in0=xt[:, k, :cw], in1=rstd[:, :cw], op=ALU.mult),
                     r=["xt", "rstd"], w=["sq"])
                p.op("dve", lambda e, k=k, cw=cw, a=a, mi=mi: e.tensor_scalar(
                    out=u[:, k, a:a + cw], in0=sq[:, k, :cw], scalar1=ml[:, mi, k:k + 1], scalar2=mvs[:, 2 + 2 * mi, k:k + 1],
                    op0=ALU.mult, op1=ALU.add), r=["sq", "ml", "mvs"], w=["u"])
    wf = [p.sb("wf%d" % i, [128, ND, 128]) for i in range(2)]
    wb = [p.sb("wb%d" % i, [128, ND, 128], BF16) for i in range(2)]
    z = p.sb("z", [128, TT])
    zc = [p.sb("zc%d" % i, [128, TT]) for i in range(3)]
    ob = [p.sb("ob%d" % i, [128, NT], BF16) for i in range(2)]
    pz = [p.ps("pz%d" % i, [128, 512]) for i in range(2)]
    wi = 0
    pi = 0
    for c in range(ND):
        for which, blk in ((1, ND + c), (2, 2 * ND + c), (0, c)):
            s = wi % 2
            wi += 1
            p.dma("sync", wf[s][:], w_in[:, :, blk * 128:(blk + 1) * 128], w=[("wf", s)])
            p.op("pool", lambda e, s=s: e.tensor_copy(out=wb[s][:], in_=wf[s][:]), r=[("wf", s)], w=[("wb", s)])
            for (c0, cw) in tiles(TT, 512):
                q = pi % 2
                pi += 1
                for k in range(ND):
                    p.op("pe", lambda e, s=s, q=q, k=k, c0=c0, cw=cw: e.matmul(
                        pz[q][:, :cw], lhsT=wb[s][:, k, :], rhs=u[:, k, c0:c0 + cw], start=(k == 0), stop=(k == ND - 1)),
                        r=[("wb", s), "u"], w=[("pz", q)])
                p.op("act", lambda e, q=q, c0=c0, cw=cw, blk=blk: e.activation(
                    out=z[:, c0:c0 + cw], in_=pz[q][:, :cw], func=AF.Identity, bias=fvs[:, 0, blk:blk + 1], scale=1.0),
                    r=[("pz", q), "fvs"], w=["z"])
            for hi, col in enumerate((0, TL + 1, TL + 2, TL + TC + 3)):
                p.op("dve", lambda e, hi=hi, col=col: e.tensor_scalar(out=z[:, col:col + 1], in0=z[:, col:col + 1],
                                                                     scalar1=hms[:, hi:hi + 1], scalar2=None, op0=ALU.mult),
                     r=["z", "hms"], w=["z"])
            zo = zc[which]
            zk = ("zc", which)
            for (a, n) in ((1, TL), (TL + 3, TC)):
                p.op("dve", lambda e, a=a, n=n, blk=blk, zo=zo: e.tensor_scalar(
                    out=zo[:, a:a + n], in0=z[:, a - 1:a - 1 + n], scalar1=fvs[:, 1, blk:blk + 1], scalar2=fvs[:, 4, blk:blk + 1],
                    op0=ALU.mult, op1=ALU.add), r=["z", "fvs"], w=[zk])
                p.op("dve", lambda e, a=a, n=n, blk=blk, zo=zo: e.scalar_tensor_tensor(
                    out=zo[:, a:a + n], in0=z[:, a:a + n], scalar=fvs[:, 2, blk:blk + 1], in1=zo[:, a:a + n],
                    op0=ALU.mult, op1=ALU.add), r=["z", "fvs", zk], w=[zk])
                p.op("dve", lambda e, a=a, n=n, blk=blk, zo=zo: e.scalar_tensor_tensor(
                    out=zo[:, a:a + n], in0=z[:, a + 1:a + 1 + n], scalar=fvs[:, 3, blk:blk + 1], in1=zo[:, a:a + n],
                    op0=ALU.mult, op1=ALU.add), r=["z", "fvs", zk], w=[zk])
            if which == 2:
                for (a, n, o0) in ((1, TL, 0), (TL + 3, TC, TL)):
                    p.op("pool", lambda e, a=a, n=n, o0=o0: e.tensor_tensor(out=ob[0][:, o0:o0 + n], in0=zc[2][:, a:a + n],
                                                                           in1=zc[1][:, a:a + n], op=ALU.mult),
                         r=[("zc", 1), ("zc", 2)], w=[("ob", 0)])
                p.dma("pool", vT[:, c, :], ob[0][:], r=[("ob", 0)], w=[("vT", c)], grp=("st", 0))
            if which == 0:
                for (a, n, o0) in ((1, TL, 0), (TL + 3, TC, TL)):
                    p.op("pool", lambda e, a=a, n=n, o0=o0: e.tensor_copy(out=ob[1][:, o0:o0 + n], in_=zc[0][:, a:a + n]),
                         r=[("zc", 0)], w=[("ob", 1)])
                p.dma("pool", x0T[:, c, :], ob[1][:], r=[("ob", 1)], w=[("x0T", c)], grp=("st", 1))
    return p.build()


def tok_layout(cfg, lat, ctx):
    outs = []
    for c in range(cfg.NCORE):
        b, h = c // cfg.CPB, c % cfg.CPB
        outs.append(np.concatenate([lat[b, h * cfg.TL:(h + 1) * cfg.TL], ctx[b, h * cfg.TC:(h + 1) * cfg.TC]], 0))
    return outs


def tok_unlayout(cfg, per_core):
    Dd = per_core[0].shape[1]
    lat = np.zeros((cfg.B, cfg.L, Dd), per_core[0].dtype)
    ctx = np.zeros((cfg.B, cfg.LC, Dd), per_core[0].dtype)
    for c in range(cfg.NCORE):
        b, h = c // cfg.CPB, c % cfg.CPB
        lat[b, h * cfg.TL:(h + 1) * cfg.TL] = per_core[c][:cfg.TL]
        ctx[b, h * cfg.TC:(h + 1) * cfg.TC] = per_core[c][cfg.TL:]
    return lat, ctx


def mod_vecs(cfg, mods_l, b):
    D = cfg.D
    names = ["sh_a", "sc_a", "gt_a", "sh_f", "sc_f", "gt_f"]
    out = {}
    for i, n in enumerate(names):
        out[n] = mods_l[b, i * D:(i + 1) * D]
        out["c" + n] = mods_l[cfg.B, i * D:(i + 1) * D]
    return out


def run_h1(cfg, I, mods):
    ND, D, TL, TC, NC = cfg.ND, cfg.D, cfg.TL, cfg.TC, cfg.NCORE
    nc = build_h1(cfg)
    x, ctx = I["x"], I["ctx"]
    w_in = np.ascontiguousarray(I["hy_w_in"][0].reshape(ND, 128, 3 * D).transpose(1, 0, 2))
    fvec = np.stack([vfm(v, 3 * ND) for v in (I["hy_b_in"][0], I["hy_conv_w"][0, 0], I["hy_conv_w"][0, 1],
                                               I["hy_conv_w"][0, 2], I["hy_conv_b"][0])], axis=1)
    fvec = np.ascontiguousarray(fvec)
    ims = []
    zrow = np.zeros((1, D), np.float32)
    for c in range(NC):
        b, h = c // cfg.CPB, c % cfg.CPB
        l0, l1 = h * TL, (h + 1) * TL
        c0, c1 = h * TC, (h + 1) * TC
        hl = x[b, l0 - 1:l0] if l0 > 0 else zrow
        hr = x[b, l1:l1 + 1] if l1 < cfg.L else zrow
        chl = ctx[b, c0 - 1:c0] if c0 > 0 else zrow
        chr_ = ctx[b, c1:c1 + 1] if c1 < cfg.LC else zrow
        cols = np.concatenate([hl, x[b, l0:l1], hr, chl, ctx[b, c0:c1], chr_], 0)
        hm = np.array([l0 > 0, l1 < cfg.L, c0 > 0, c1 < cfg.LC], np.float32)
        mvd = mod_vecs(cfg, mods[0], b)
        mv = np.stack([vfm(v, ND) for v in (I["norm_g"][0, 0], mvd["sc_a"], mvd["sh_a"], mvd["csc_a"], mvd["csh_a"])], axis=1)
        ims.append({"xT": fm(cols, ND), "hm": np.ascontiguousarray(np.broadcast_to(hm, (128, 4))),
                    "mv": np.ascontiguousarray(mv), "w_in": w_in, "fv": fvec})
    res = run(nc, ims)
    v = [unfm(np.asarray(r["vT"]).astype(np.float32)) for r in res]
    x0 = [unfm(np.asarray(r["x0T"]).astype(np.float32)) for r in res]
    return tok_unlayout(cfg, v), tok_unlayout(cfg, x0)


def hy_consts(Lx, D):
    f32 = np.float32
    t = np.linspace(0.0, 1.0, Lx, dtype=f32)[:, None]
    w = (f32(2.0 * np.pi / Lx) * np.arange(Lx, dtype=f32))[:, None]
    bands = np.linspace(1e-4, 15, 16, dtype=f32)[None, :]
    z = np.concatenate([t, np.cos(bands * w), -np.sin(bands * w)], axis=-1).astype(f32)
    max_decay = np.log(1e-2) / 0.3
    min_decay = np.log(1e-2) / 1.5
    deltas = np.abs(np.linspace(min_decay, max_decay, D, dtype=f32))
    win = np.exp(-t * deltas[None, :]).astype(f32)
    return z, win


def emit_sin(p, e_out, okey, src, skey, ncols, tmp, tkey, scale_ap, bias_ap, extra_r=()):
    t0, t1 = tmp
    p.op("dve", lambda e: e.tensor_scalar(out=t0[:, :ncols], in0=src, scalar1=scale_ap, scalar2=bias_ap, op0=ALU.mult, op1=ALU.add),
         r=[skey] + list(extra_r), w=[(tkey, 0)])
    p.op("dve", lambda e: e.tensor_scalar(out=t1[:, :ncols], in0=t0[:, :ncols], scalar1=1.0 / TWO_PI, scalar2=MAGIC, op0=ALU.mult, op1=ALU.add),
         r=[(tkey, 0)], w=[(tkey, 1)])
    p.op("dve", lambda e: e.tensor_scalar(out=t1[:, :ncols], in0=t1[:, :ncols], scalar1=MAGIC, scalar2=-TWO_PI, op0=ALU.subtract, op1=ALU.mult),
         r=[(tkey, 1)], w=[(tkey, 1)])
    p.op("dve", lambda e: e.tensor_tensor(out=t0[:, :ncols], in0=t0[:, :ncols], in1=t1[:, :ncols], op=ALU.add),
         r=[(tkey, 0), (tkey, 1)], w=[(tkey, 0)])
    p.op("dve", lambda e: e.tensor_scalar(out=t0[:, :ncols], in0=t0[:, :ncols], scalar1=PI_LO, scalar2=-PI_LO, op0=ALU.min, op1=ALU.max),
         r=[(tkey, 0)], w=[(tkey, 0)])
    p.op("act", lambda e: e.activation(out=e_out, in_=t0[:, :ncols], func=AF.Sin), r=[(tkey, 0)], w=[okey])


def build_filt(cfg):
    p = Prog()
    Cc = cfg.Cc
    CP = min(Cc, 128)
    NCH = Cc // CP
    fw1 = p.din("fw1", [128, 128]); fw2 = p.din("fw2", [128, 128]); fw3 = p.din("fw3", [128, 2, NCH, 128])
    pv = p.din("pv", [128, 3])
    w1s = p.sb("w1s", [128, 128]); w2s = p.sb("w2s", [128, 128]); w3s = p.sb("w3s", [128, 2, NCH, 128]); pvs = p.sb("pvs", [128, 3])
    p.dma("sync", w1s[:], fw1[:, :], w=["w1s"]); p.dma("sync", w2s[:], fw2[:, :], w=["w2s"])
    p.dma("sync", w3s[:], fw3[:, :, :, :], w=["w3s"]); p.dma("sync", pvs[:], pv[:, :], w=["pvs"])
    fb = p.sb("fb", [128, 2])
    p.op("dve", lambda e: e.tensor_scalar(out=fb[:, 0:2], in0=pvs[:, 0:2], scalar1=pvs[:, 2:3], scalar2=None, op0=ALU.mult),
         r=["pvs"], w=["fb"])
    Lmax = max(cfg.L, cfg.LC)
    CT = 512
    zt = p.sb("zt", [128, CT]); h1 = p.sb("h1", [128, CT]); h2 = p.sb("h2", [128, Lmax])
    tmp = [p.sb("tmpa", [128, CT]), p.sb("tmpb", [128, CT])]
    hw = [p.sb("hw%d" % i, [128, Lmax]) for i in range(2)]
    wn = p.sb("wn", [128, Lmax])
    ab = p.sb("ab", [128, Lmax]); nr = p.sb("nr", [128, 2]); rn = p.sb("rn", [128, 1])
    ho = [p.sb("ho%d" % i, [128, Lmax], BF16) for i in range(2)]
    pa = p.ps("pa", [128, CT]); pb = p.ps("pb", [128, CT]); pc = p.ps("pc", [128, CT])
    for li, Lx in enumerate((cfg.L, cfg.LC)):
        zT = p.din("zT%d" % li, [128, Lx]); winT = p.din("winT%d" % li, [CP, NCH, Lx])
        hsT = p.dout("hsT%d" % li, [CP, NCH, Lx], BF16); hdT = p.dout("hdT%d" % li, [CP, NCH, Lx], BF16)
        for (c0, cw) in tiles(Lx, CT):
            p.dma("sync", zt[:, :cw], zT[:, c0:c0 + cw], w=["zt"])
            p.op("pe", lambda e, cw=cw: e.matmul(pa[:, :cw], lhsT=w1s[:], rhs=zt[:, :cw], start=True, stop=True), r=["w1s", "zt"], w=["pa"])
            emit_sin(p, h1[:, :cw], "h1", pa[:, :cw], "pa", cw, tmp, "tmp", pvs[:, 2:3], fb[:, 0:1], extra_r=["pvs", "fb"])
            p.op("pe", lambda e, cw=cw: e.matmul(pb[:, :cw], lhsT=w2s[:], rhs=h1[:, :cw], start=True, stop=True), r=["w2s", "h1"], w=["pb"])
            emit_sin(p, h2[:, c0:c0 + cw], "h2", pb[:, :cw], "pb", cw, tmp, "tmp", pvs[:, 2:3], fb[:, 1:2], extra_r=["pvs", "fb"])
        for ch in range(NCH):
            p.dma("sync", wn[:CP, :Lx], winT[:, ch, :], w=["wn"])
            for d in range(2):
                for (c0, cw) in tiles(Lx, CT):
                    p.op("pe", lambda e, d=d, ch=ch, c0=c0, cw=cw: e.matmul(pc[:, :cw], lhsT=w3s[:, d, ch, :], rhs=h2[:, c0:c0 + cw], start=True, stop=True),
                         r=["w3s", "h2"], w=["pc"])
                    p.op("dve", lambda e, d=d, c0=c0, cw=cw: e.tensor_tensor(out=hw[d][:CP, c0:c0 + cw], in0=pc[:CP, :cw], in1=wn[:CP, c0:c0 + cw], op=ALU.mult),
                         r=["pc", "wn"], w=[("hw", d)])
            p.op("dve", lambda e: e.memset(hw[1][:CP, 0:1], 0.0), r=[("hw", 1)], w=[("hw", 1)])
            for d in range(2):
                p.op("dve", lambda e, d=d, Lx=Lx: e.scalar_tensor_tensor(out=ab[:CP, :Lx], in0=hw[d][:CP, :Lx], scalar=-1.0, in1=hw[d][:CP, :Lx], op0=ALU.mult, op1=ALU.max),
                     r=[("hw", d)], w=["ab"])
                p.op("dve", lambda e, d=d, Lx=Lx: e.tensor_reduce(out=nr[:CP, d:d + 1], in_=ab[:CP, :Lx], axis=AX.X, op=ALU.add),
                     r=["ab"], w=["nr"])
            p.op("dve", lambda e: e.tensor_tensor(out=rn[:CP, :], in0=nr[:CP, 0:1], in1=nr[:CP, 1:2], op=ALU.add), r=["nr"], w=["rn"])
            p.op("dve", lambda e: e.reciprocal(out=rn[:CP, :], in_=rn[:CP, :]), r=["rn"], w=["rn"])
            p.op("dve", lambda e, Lx=Lx: e.tensor_tensor(out=ab[:CP, :Lx], in0=hw[0][:CP, :Lx], in1=hw[1][:CP, :Lx], op=ALU.add),
                 r=[("hw", 0), ("hw", 1)], w=["ab"])
            p.op("dve", lambda e, Lx=Lx: e.tensor_scalar(out=ho[0][:CP, :Lx], in0=ab[:CP, :Lx], scalar1=rn[:CP, 0:1], scalar2=None, op0=ALU.mult),
                 r=["ab", "rn"], w=[("ho", 0)])
            p.op("dve", lambda e, Lx=Lx: e.tensor_tensor(out=ab[:CP, :Lx], in0=hw[0][:CP, :Lx], in1=hw[1][:CP, :Lx], op=ALU.subtract),
                 r=[("hw", 0), ("hw", 1), ("ho", 0)], w=["ab"])
            p.op("dve", lambda e, Lx=Lx: e.tensor_scalar(out=ho[1][:CP, :Lx], in0=ab[:CP, :Lx], scalar1=rn[:CP, 0:1], scalar2=None, op0=ALU.mult),
                 r=["ab", "rn"], w=[("ho", 1)])
            p.dma("pool", hsT[:, ch, :], ho[0][:CP, :Lx], r=[("ho", 0)], w=[("hs", li, ch)], grp=("st", 0))
            p.dma("pool", hdT[:, ch, :], ho[1][:CP, :Lx], r=[("ho", 1)], w=[("hd", li, ch)], grp=("st", 1))
    return p.build()


def pad128(a):
    out = np.zeros((128,) + a.shape[1:], np.float32)
    out[:a.shape[0]] = a
    return out


def run_filt(cfg, I):
    Cc, D, NC = cfg.Cc, cfg.D, cfg.NCORE
    CP = min(Cc, 128); NCH = Cc // CP
    nc = build_filt(cfg)
    fw1 = np.zeros((128, 128), np.float32); fw1[:33, :64] = I["hy_fw1"][0]
    fw2 = np.zeros((128, 128), np.float32); fw2[:64, :64] = I["hy_fw2"][0]
    pv = np.zeros((128, 3), np.float32)
    pv[:64, 0] = I["hy_fb1"][0]; pv[:64, 1] = I["hy_fb2"][0]; pv[:64, 2] = I["hy_freq"][0]
    consts = [hy_consts(Lx, D) for Lx in (cfg.L, cfg.LC)]
    ims = []
    for c in range(NC):
        fw3 = np.zeros((128, 2, NCH, 128), np.float32)
        for d in range(2):
            blk = I["hy_fw3"][0][:, d * D + c * Cc: d * D + (c + 1) * Cc]
            fw3[:64, d, :, :CP] = blk.reshape(64, NCH, CP)
        m = {"fw1": fw1, "fw2": fw2, "fw3": fw3, "pv": pv}
        for li, (z, win) in enumerate(consts):
            m["zT%d" % li] = pad128(np.ascontiguousarray(z.T))
            wc = win[:, c * Cc:(c + 1) * Cc].T
            m["winT%d" % li] = np.ascontiguousarray(wc.reshape(NCH, CP, -1).transpose(1, 0, 2))
        ims.append(m)
    res = run(nc, ims)
    outs = []
    for li, Lx in enumerate((cfg.L, cfg.LC)):
        hs = np.zeros((Lx, D), np.float32); hd = np.zeros((Lx, D), np.float32)
        for c in range(NC):
            a = np.asarray(res[c]["hsT%d" % li]).astype(np.float32).transpose(1, 0, 2).reshape(Cc, Lx)
            b = np.asarray(res[c]["hdT%d" % li]).astype(np.float32).transpose(1, 0, 2).reshape(Cc, Lx)
            hs[:, c * Cc:(c + 1) * Cc] = a.T
            hd[:, c * Cc:(c + 1) * Cc] = b.T
        outs.append((hs, hd))
    return outs


def dft_tables(Lx):
    NS = Lx // 128
    M = 4 * Lx
    p = np.arange(128)[:, None, None]
    i = np.arange(NS)[None, :, None]
    q = np.arange(128)[None, None, :]

    def cis(k):
        ang = -2.0 * np.pi * (np.asarray(k, np.int64) % M).astype(np.float64) / M
        return np.stack([np.cos(ang), np.sin(ang)]).astype(np.float32)

    B2 = cis((2 * q + 1) * (128 * i + p))
    j = np.arange(NS)[None, :, None]
    ii = np.arange(NS)[None, None, :]
    A2 = cis(256 * j * (128 * ii + np.arange(128)[:, None, None]))
    qq = np.arange(128)[:, None, None]
    jj = np.arange(NS)[None, :, None]
    pp = np.arange(128)[None, None, :]
    B3 = cis((2 * (128 * jj + qq) + 1) * pp)
    i3 = np.arange(NS)[None, :, None]
    j3 = np.arange(NS)[None, None, :]
    A3 = cis((2 * (128 * j3 + qq) + 1) * 128 * i3)
    return [np.ascontiguousarray(t) for t in (A2, B2, A3, B3)]


def build_h2(cfg):
    p = Prog()
    B, Cc = cfg.B, cfg.Cc
    ncol = B * Cc
    CW = min(getattr(cfg, 'H2_CW', 512), ncol)
    nbp = CW // Cc
    NSmax = max(cfg.L, cfg.LC) // 128
    RAWN = 4 * NSmax * 128
    raw = p.sb("raw", [128, RAWN])
    es = [[p.sb("es%d%d" % (a, b), [128, NSmax, 128], BF16) for b in range(2)] for a in range(2)]
    a_s = p.sb("a_s", [128, 2, NSmax, NSmax])
    vs = p.sb("vs", [128, NSmax, CW], BF16)
    hsd = p.sb("hsd", [128, 2, NSmax, Cc], BF16)
    skb = p.sb("skb", [128, ncol])
    kk = p.sb("kk", [128, 2, Cc])
    tt = [p.sb("tt%d" % i, [128, CW]) for i in range(3)]
    yo = p.sb("yo", [128, CW], BF16)
    pV = [p.ps("pV%d" % i, [128, 512]) for i in range(2)]
    pK = [p.ps("pK%d" % i, [128, 512]) for i in range(2)]
    pY = p.ps("pY", [128, 512])
    def conv_li(li, Lx, skip):
        NS = Lx // 128
        NF = NS
        Mv = NS * 128
        v = p.din("v%d" % li, [128, NS, ncol], BF16)
        hsdi = p.din("hsd%d" % li, [128, 2, NS, Cc], BF16)
        A2 = p.din("A2_%d" % li, [2, 128, NF, NS]); B2 = p.din("B2_%d" % li, [2, 128, NS, 128])
        A3 = p.din("A3_%d" % li, [2, 128, NS, NF]); B3 = p.din("B3_%d" % li, [2, 128, NF, 128])
        if li == 0:
            skip = p.din("skip0", [128, ncol])
        y = p.dout("y%d" % li, [128, NS, ncol], BF16)
        Es = p.dtmp("Es%d" % li, [NF, 2, 128, NS, 128], BF16)
        Gs = p.dtmp("Gs%d" % li, [NS, 2, 128, NF, 128], BF16)
        Ks = p.dtmp("Ks%d" % li, [NF, 128, 2, Cc])
        p.barrier()
        bre = raw[:, 0:Mv].rearrange("p (i q) -> p i q", q=128)
        bim = raw[:, Mv:2 * Mv].rearrange("p (i q) -> p i q", q=128)
        t1 = raw[:, 2 * Mv:3 * Mv].rearrange("p (i q) -> p i q", q=128)
        t2 = raw[:, 3 * Mv:4 * Mv].rearrange("p (i q) -> p i q", q=128)
        for (At, Bt, dst, nout) in ((A2, B2, Es, NF), (A3, B3, Gs, NS)):
            p.dma("sync", raw[:, 0:Mv], Bt[0].rearrange("p i q -> p (i q)"), w=["bre"])
            p.dma("sync", raw[:, Mv:2 * Mv], Bt[1].rearrange("p i q -> p (i q)"), w=["bim"])
            p.dma("sync", a_s[:, 0, :nout, :NS], At[0], w=["a_s0"])
            p.dma("sync", a_s[:, 1, :nout, :NS], At[1], w=["a_s1"])
            for j in range(nout):
                s = j % 2
                are = a_s[:, 0, j, :NS].unsqueeze(2).broadcast_to([128, NS, 128])
                aim = a_s[:, 1, j, :NS].unsqueeze(2).broadcast_to([128, NS, 128])
                ere = es[s][0][:, :NS, :]
                eim = es[s][1][:, :NS, :]
                p.op("dve", lambda e, are=are: e.tensor_tensor(out=t1, in0=bre, in1=are, op=ALU.mult), r=["bre", "a_s0"], w=["t1"])
                p.op("pool", lambda e, aim=aim: e.tensor_tensor(out=t2, in0=bim, in1=aim, op=ALU.mult), r=["bim", "a_s1"], w=["t2"])
                p.op("dve", lambda e, ere=ere: e.tensor_tensor(out=ere, in0=t1, in1=t2, op=ALU.subtract), r=["t1", "t2"], w=[("es", s, 0)])
                p.op("dve", lambda e, are=are: e.tensor_tensor(out=t1, in0=bim, in1=are, op=ALU.mult), r=["bim", "a_s0"], w=["t1"])
                p.op("pool", lambda e, aim=aim: e.tensor_tensor(out=t2, in0=bre, in1=aim, op=ALU.mult), r=["bre", "a_s1"], w=["t2"])
                p.op("dve", lambda e, eim=eim: e.tensor_tensor(out=eim, in0=t1, in1=t2, op=ALU.add), r=["t1", "t2"], w=[("es", s, 1)])
                p.dma("sync", dst[j, 0], ere, r=[("es", s, 0)], w=[("tab", li, j, 0)], grp=("tst", s, 0))
                p.dma("sync", dst[j, 1], eim, r=[("es", s, 1)], w=[("tab", li, j, 1)], grp=("tst", s, 1))
        p.barrier()
        Yv = raw[:, 0:NF * CW].bitcast(BF16).rearrange("p (c j w) -> p c j w", c=2, j=NF)
        p.dma("sync", hsd[:, :, :NS, :], hsdi[:, :, :, :], w=["hsd"])
        if li == 0:
            p.dma("sync", skb[:], skip[:, :], w=["skb"])
        for c0 in range(0, ncol, CW):
            p.dma("sync", vs[:, :NS, :], v[:, :, c0:c0 + CW], w=["vs"])
            for j in range(NF):
                s = j % 2
                for c in range(2):
                    p.dma("pool" if c else "sync", es[s][c][:, :NS, :], Es[j, c], w=[("es", s, c)])
                first_pass = (c0 == 0)
                for i in range(NS):
                    fl = dict(start=(i == 0), stop=(i == NS - 1))
                    p.op("pe", lambda e, s=s, i=i, fl=fl: e.matmul(pV[0][:, :CW], lhsT=es[s][0][:, i, :], rhs=vs[:, i, :], **fl),
                         r=[("es", s, 0), "vs"], w=["pV0"])
                    p.op("pe", lambda e, s=s, i=i, fl=fl: e.matmul(pV[1][:, :CW], lhsT=es[s][1][:, i, :], rhs=vs[:, i, :], **fl),
                         r=[("es", s, 1), "vs"], w=["pV1"])
                    if first_pass:
                        p.op("pe", lambda e, s=s, i=i, fl=fl: e.matmul(pK[0][:, :Cc], lhsT=es[s][0][:, i, :], rhs=hsd[:, 0, i, :], **fl),
                             r=[("es", s, 0), "hsd"], w=["pK0"])
                        p.op("pe", lambda e, s=s, i=i, fl=fl: e.matmul(pK[1][:, :Cc], lhsT=es[s][1][:, i, :], rhs=hsd[:, 1, i, :], **fl),
                             r=[("es", s, 1), "hsd"], w=["pK1"])
                if first_pass:
                    for c in range(2):
                        p.op("act", lambda e, c=c: e.activation(out=kk[:, c, :], in_=pK[c][:, :Cc], func=AF.Copy), r=["pK%d" % c], w=[("kk", c)])
                    if ncol > CW:
                        p.dma("pool", Ks[j], kk[:, :, :], r=[("kk", 0), ("kk", 1)], w=[("Ks", li, j)], grp=("kst",))
                else:
                    p.dma("pool", kk[:, :, :], Ks[j], r=[("Ks", li, j)], w=[("kk", 0), ("kk", 1)], grp=("kld",))
                kre = kk[:, 0, :].unsqueeze(1).broadcast_to([128, nbp, Cc])
                kim = kk[:, 1, :].unsqueeze(1).broadcast_to([128, nbp, Cc])
                vre = pV[0][:, :CW].rearrange("p (b c) -> p b c", c=Cc)
                vim = pV[1][:, :CW].rearrange("p (b c) -> p b c", c=Cc)
                t3 = [t[:, :].rearrange("p (b c) -> p b c", c=Cc) for t in tt]
                yre = Yv[:, 0, j, :].rearrange("p (b c) -> p b c", c=Cc)
                yim = Yv[:, 1, j, :].rearrange("p (b c) -> p b c", c=Cc)
                p.op("dve", lambda e, vre=vre, kre=kre, t3=t3: e.tensor_tensor(out=t3[0], in0=vre, in1=kre, op=ALU.mult), r=["pV0", ("kk", 0)], w=["tt0"])
                p.op("dve", lambda e, vim=vim, kim=kim, t3=t3: e.tensor_tensor(out=t3[1], in0=vim, in1=kim, op=ALU.mult), r=["pV1", ("kk", 1)], w=["tt1"])
                p.op("pool", lambda e, yre=yre, t3=t3: e.tensor_tensor(out=yre, in0=t3[0], in1=t3[1], op=ALU.subtract), r=["tt0", "tt1"], w=[("Y", j)])
                p.op("dve", lambda e, vre=vre, kim=kim, t3=t3: e.tensor_tensor(out=t3[2], in0=vre, in1=kim, op=ALU.mult), r=["pV0", ("kk", 1)], w=["tt2"])
                p.op("dve", lambda e, vim=vim, kre=kre, t3=t3: e.tensor_tensor(out=t3[0], in0=vim, in1=kre, op=ALU.mult), r=["pV1", ("kk", 0), "tt0"], w=["tt0"])
                p.op("pool", lambda e, yim=yim, t3=t3: e.tensor_tensor(out=yim, in0=t3[2], in1=t3[0], op=ALU.add), r=["tt0", "tt2"], w=[("Y", j)])
            for i in range(NS):
                s = i % 2
                for c in range(2):
                    p.dma("pool" if c else "sync", es[s][c][:, :NF, :], Gs[i, c], w=[("es", s, c)])
                n = 0
                for j in range(NF):
                    for c in range(2):
                        p.op("pe", lambda e, s=s, j=j, c=c, n=n: e.matmul(pY[:, :CW], lhsT=es[s][c][:, j, :], rhs=Yv[:, c, j, :],
                                                                         start=(n == 0), stop=(n == 2 * NF - 1)),
                             r=[("es", s, c), ("Y", j)], w=["pY"])
                        n += 1
                p.op("pool", lambda e, i=i, c0=c0: e.tensor_tensor(out=tt[0][:, :], in0=vs[:, i, :], in1=skb[:, c0:c0 + CW], op=ALU.mult),
                     r=["vs", "skb"], w=["tt0"])
                p.op("dve", lambda e, Lx=Lx: e.scalar_tensor_tensor(out=yo[:, :], in0=pY[:, :CW], scalar=1.0 / Lx, in1=tt[0][:, :],
                                                                   op0=ALU.mult, op1=ALU.add), r=["pY", "tt0"], w=["yo"])
                p.dma("sync", y[:, i, c0:c0 + CW], yo[:, :], r=["yo"], w=[("y", li, i, c0)], grp=("yst",))
        return skip

    skip = None
    for li, Lx in enumerate((cfg.L, cfg.LC)):
        skip = conv_li(li, Lx, skip)
    return p.build()


def run_h2(cfg, I, v_lat, v_ctx, filt):
    import ml_dtypes
    bf = ml_dtypes.bfloat16
    B, Cc, D, NC = cfg.B, cfg.Cc, cfg.D, cfg.NCORE
    ncol = B * Cc
    nc = build_h2(cfg)
    tabs = [dft_tables(Lx) for Lx in (cfg.L, cfg.LC)]
    ims = []
    for c in range(NC):
        m = {}
        for li, (Lx, vv) in enumerate(((cfg.L, v_lat), (cfg.LC, v_ctx))):
            NS = Lx // 128
            vc = vv[:, :, c * Cc:(c + 1) * Cc].transpose(1, 0, 2).reshape(Lx, ncol)
            m["v%d" % li] = np.ascontiguousarray(vc.reshape(NS, 128, ncol).transpose(1, 0, 2)).astype(bf)
            hs, hd = filt[li]
            hh = np.stack([hs[:, c * Cc:(c + 1) * Cc], hd[:, c * Cc:(c + 1) * Cc]])
            m["hsd%d" % li] = np.ascontiguousarray(hh.reshape(2, NS, 128, Cc).transpose(2, 0, 1, 3)).astype(bf)
            A2, B2, A3, B3 = tabs[li]
            m["A2_%d" % li], m["B2_%d" % li], m["A3_%d" % li], m["B3_%d" % li] = A2, B2, A3, B3
        sk = np.tile(I["hy_skip"][0][c * Cc:(c + 1) * Cc], B)
        m["skip0"] = np.ascontiguousarray(np.broadcast_to(sk[None, :], (128, ncol))).astype(np.float32)
        ims.append(m)
    res = run(nc, ims)
    outs = []
    for li, Lx in enumerate((cfg.L, cfg.LC)):
        NS = Lx // 128
        yy = np.zeros((B, Lx, D), bf)
        for c in range(NC):
            a = np.asarray(res[c]["y%d" % li]).transpose(1, 0, 2).reshape(Lx, B, Cc)
            yy[:, :, c * Cc:(c + 1) * Cc] = a.transpose(1, 0, 2)
        outs.append(yy)
    return outs


class PostCtx:
    def __init__(self, p, cfg, TK):
        ND = cfg.ND
        self.TK = TK
        self.ones = p.sb("ones", [128, 128]); p.op("dve", lambda e: e.memset(self.ones[:], 1.0), w=["ones"])
        self.ident = p.sb("ident", [128, 128])
        self.xt = p.sb("xt", [128, ND, TK]); self.xl = p.sb("xl", [128, ND, TK]); self.sq = p.sb("sq", [128, ND, TK])
        self.tokf = p.sb("tokf", [128, ND, TK]); self.tokb = p.sb("tokb", [128, ND, TK], BF16)
        self.rstd = p.sb("rstd", [128, TK]); self.olt = p.sb("olt", [128, TK])
        self.wr = p.sb("wr", [128, ND, 128]); self.br = p.sb("br", [128, 1])
        self.lgT = p.sb("lgT", [128, TK]); self.lg = p.sb("lg", [128, 128])
        self.sm = p.sb("sm", [128, 16]); self.pen = p.sb("pen", [128, 4]); self.lem = p.sb("lem", [128, 32]); self.lem2 = p.sb("lem2", [128, 32])
        self.mk = p.sb("mk", [128, 2, 32]); self.gt = p.sb("gt", [128, 32])
        self.pss = p.ps("pss", [128, TK]); self.pz = [p.ps("pz%d" % i, [128, TK]) for i in range(2)]
        self.plg = p.ps("plg", [128, TK]); self.ptr = p.ps("ptr", [128, 128])
        self.pzi = 0


def emit_post(p, cfg, C, cw, a_tile, akey, Wb, wkey, bvec_ap_fn, gt_fn, m2_fn, sh_fn, xl_out_ap, tok_out_ap, gates_out_fn, want_router=True):
    ND = cfg.ND
    for blk in range(ND):
        q = C.pzi % 2
        C.pzi += 1
        for k in range(ND):
            p.op("pe", lambda e, q=q, k=k, blk=blk: e.matmul(C.pz[q][:, :cw], lhsT=Wb[:, k, blk * 128:(blk + 1) * 128], rhs=a_tile[:, k, :cw],
                                                           start=(k == 0), stop=(k == ND - 1)), r=[wkey, akey], w=[("pz", q)])
        p.op("act", lambda e, q=q, blk=blk: e.activation(out=C.olt[:, :cw], in_=C.pz[q][:, :cw], func=AF.Identity, bias=bvec_ap_fn(blk), scale=1.0),
             r=[("pz", q), "vecs"], w=["olt"])
        p.op("dve", lambda e, blk=blk: e.scalar_tensor_tensor(out=C.xl[:, blk, :cw], in0=C.olt[:, :cw], scalar=gt_fn(blk), in1=C.xt[:, blk, :cw],
                                                            op0=ALU.mult, op1=ALU.add), r=["olt", "xt", "vecs"], w=["xl"])
    p.dma("sync", xl_out_ap, C.xl[:, :, :cw], r=["xl"], w=[("xlo", id(xl_out_ap))], grp=("xlst",))
    emit_norm_router(p, cfg, C, cw, m2_fn, sh_fn, tok_out_ap, gates_out_fn, want_router)


def emit_norm_router(p, cfg, C, cw, m2_fn, sh_fn, tok_out_ap, gates_out_fn, want_router=True):
    ND = cfg.ND
    emit_rstd(p, cfg, C.xl, "xl", cw, C.ones, C.sq, "sq", C.pss, "pss", C.rstd, "rstd")
    for k in range(ND):
        p.op("dve", lambda e, k=k: e.tensor_tensor(out=C.sq[:, k, :cw], in0=C.xl[:, k, :cw], in1=C.rstd[:, :cw], op=ALU.mult),
             r=["xl", "rstd"], w=["sq"])
        p.op("dve", lambda e, k=k: e.tensor_scalar(out=C.tokf[:, k, :cw], in0=C.sq[:, k, :cw], scalar1=m2_fn(k), scalar2=sh_fn(k),
                                                  op0=ALU.mult, op1=ALU.add), r=["sq", "vecs"], w=["tokf"])
    p.op("pool", lambda e: e.tensor_copy(out=C.tokb[:, :, :cw], in_=C.tokf[:, :, :cw]), r=["tokf"], w=["tokb"])
    p.dma("sync", tok_out_ap, C.tokb[:, :, :cw], r=["tokb"], w=[("toko", id(tok_out_ap))], grp=("tokst",))
    if not want_router:
        return
    for k in range(ND):
        p.op("pe", lambda e, k=k: e.matmul(C.plg[:, :cw], lhsT=C.wr[:, k, :], rhs=C.tokf[:, k, :cw], start=(k == 0), stop=(k == ND - 1)),
             r=["wr", "tokf"], w=["plg"])
    p.op("act", lambda e: e.activation(out=C.lgT[:, :cw], in_=C.plg[:, :cw], func=AF.Identity, bias=C.br[:, 0:1], scale=1.0),
         r=["plg", "br"], w=["lgT"])
    for (t0, tw) in tiles(cw, 128):
        p.op("pe", lambda e, t0=t0, tw=tw: e.transpose(out=C.ptr[:tw, :], in_=C.lgT[:, t0:t0 + tw], identity=C.ident[:]),
             r=["lgT", "ident"], w=["ptr"])
        p.op("act", lambda e, tw=tw: e.activation(out=C.lg[:tw, :], in_=C.ptr[:tw, :], func=AF.Copy), r=["ptr"], w=["lg"])
        lg, sm = C.lg, C.sm
        R = lambda *k: list(k)
        p.op("dve", lambda e, tw=tw: e.tensor_reduce(out=sm[:tw, 0:1], in_=lg[:tw, 0:4], axis=AX.X, op=ALU.max), r=["lg"], w=["sm0"])
        p.op("dve", lambda e, tw=tw: e.tensor_scalar(out=sm[:tw, 4:8], in0=lg[:tw, 0:4], scalar1=sm[:tw, 0:1], scalar2=None, op0=ALU.subtract),
             r=["lg", "sm0"], w=["sm4"])
        p.op("act", lambda e, tw=tw: e.activation(out=sm[:tw, 4:8], in_=sm[:tw, 4:8], func=AF.Exp), r=["sm4"], w=["sm4"])
        p.op("dve", lambda e, tw=tw: e.tensor_reduce(out=sm[:tw, 1:2], in_=sm[:tw, 4:8], axis=AX.X, op=ALU.add), r=["sm4"], w=["sm1"])
        p.op("dve", lambda e, tw=tw: e.reciprocal(out=sm[:tw, 1:2], in_=sm[:tw, 1:2]), r=["sm1"], w=["sm1"])
        p.op("dve", lambda e, tw=tw: e.tensor_scalar(out=C.pen[:tw, :], in0=lg[:tw, 0:4], scalar1=sm[:tw, 0:1], scalar2=None, op0=ALU.is_equal),
             r=["lg", "sm0"], w=["pen"])
        p.op("dve", lambda e, tw=tw: e.tensor_scalar(out=C.pen[:tw, :], in0=C.pen[:tw, :], scalar1=-1.0, scalar2=1e30, op0=ALU.add, op1=ALU.mult),
             r=["pen"], w=["pen"])
        le = lg[:tw, 4:36].rearrange("p (g e) -> p g e", e=8)
        penb = C.pen[:tw, :].unsqueeze(2).broadcast_to([tw, 4, 8])
        lem3 = C.lem[:tw, :].rearrange("p (g e) -> p g e", e=8)
        p.op("dve", lambda e, le=le, penb=penb, lem3=lem3: e.tensor_tensor(out=lem3, in0=le, in1=penb, op=ALU.add), r=["lg", "pen"], w=["lem"])
        p.op("dve", lambda e, tw=tw: e.tensor_reduce(out=sm[:tw, 2:3], in_=C.lem[:tw, :], axis=AX.X, op=ALU.max), r=["lem"], w=["sm2"])
        p.op("dve", lambda e, tw=tw: e.tensor_scalar(out=C.mk[:tw, 0, :], in0=C.lem[:tw, :], scalar1=sm[:tw, 2:3], scalar2=None, op0=ALU.is_equal),
             r=["lem", "sm2"], w=["mk0"])
        p.op("dve", lambda e, tw=tw: e.scalar_tensor_tensor(out=C.lem2[:tw, :], in0=C.mk[:tw, 0, :], scalar=-1e30, in1=C.lem[:tw, :],
                                                           op0=ALU.mult, op1=ALU.add), r=["mk0", "lem"], w=["lem2"])
        p.op("dve", lambda e, tw=tw: e.tensor_reduce(out=sm[:tw, 3:4], in_=C.lem2[:tw, :], axis=AX.X, op=ALU.max), r=["lem2"], w=["sm3"])
        p.op("dve", lambda e, tw=tw: e.tensor_scalar(out=C.mk[:tw, 1, :], in0=C.lem2[:tw, :], scalar1=sm[:tw, 3:4], scalar2=None, op0=ALU.is_equal),
             r=["lem2", "sm3"], w=["mk1"])
        p.op("dve", lambda e, tw=tw: e.tensor_tensor(out=sm[:tw, 8:9], in0=sm[:tw, 3:4], in1=sm[:tw, 2:3], op=ALU.subtract), r=["sm2", "sm3"], w=["sm8"])
        p.op("act", lambda e, tw=tw: e.activation(out=sm[:tw, 8:9], in_=sm[:tw, 8:9], func=AF.Exp), r=["sm8"], w=["sm8"])
        p.op("dve", lambda e, tw=tw: e.tensor_scalar(out=sm[:tw, 9:10], in0=sm[:tw, 8:9], scalar1=1.0, scalar2=None, op0=ALU.add), r=["sm8"], w=["sm9"])
        p.op("dve", lambda e, tw=tw: e.reciprocal(out=sm[:tw, 9:10], in_=sm[:tw, 9:10]), r=["sm9"], w=["sm9"])
        p.op("dve", lambda e, tw=tw: e.tensor_tensor(out=sm[:tw, 10:11], in0=sm[:tw, 8:9], in1=sm[:tw, 9:10], op=ALU.mult), r=["sm8", "sm9"], w=["sm10"])
        p.op("dve", lambda e, tw=tw: e.tensor_scalar(out=sm[:tw, 9:11], in0=sm[:tw, 9:11], scalar1=sm[:tw, 1:2], scalar2=None, op0=ALU.mult),
             r=["sm9", "sm10", "sm1"], w=["sm9", "sm10"])
        p.op("dve", lambda e, tw=tw: e.tensor_scalar(out=C.gt[:tw, :], in0=C.mk[:tw, 0, :], scalar1=sm[:tw, 9:10], scalar2=None, op0=ALU.mult),
             r=["mk0", "sm9"], w=["gt"])
        p.op("dve", lambda e, tw=tw: e.scalar_tensor_tensor(out=C.gt[:tw, :], in0=C.mk[:tw, 1, :], scalar=sm[:tw, 10:11], in1=C.gt[:tw, :],
                                                           op0=ALU.mult, op1=ALU.add), r=["mk1", "sm10", "gt"], w=["gt"])
        go = gates_out_fn(t0, tw)
        p.dma("sync", go, C.gt[:tw, :], r=["gt"], w=[("go", id(go))], grp=("gst",))


def build_h3(cfg):
    p = Prog()
    ND, D, TL, TC = cfg.ND, cfg.D, cfg.TL, cfg.TC
    NT = TL + TC
    TK = 256
    ycT = p.din("ycT", [128, ND, NT], BF16); x0T = p.din("x0T", [128, ND, NT], BF16); xT = p.din("xT", [128, ND, NT])
    w_out = p.din("w_out", [128, ND, D]); vecs = p.din("vecsD", [128, 8, ND])
    wr_d = p.din("wrD", [128, ND, 128]); br_d = p.din("brD", [128, 1]); ident_d = p.din("identD", [128, 128])
    xlT = p.dout("xlT", [128, ND, NT]); tokT = p.dout("tokT", [128, ND, NT], BF16); gates = p.dout("gates", [NT, 32])
    C = PostCtx(p, cfg, TK)
    p.dma("sync", C.ident[:], ident_d[:, :], w=["ident"]); p.dma("sync", C.wr[:], wr_d[:, :, :], w=["wr"]); p.dma("sync", C.br[:], br_d[:, :], w=["br"])
    vs_ = p.sb("vecs", [128, 8, ND]); p.dma("sync", vs_[:], vecs[:, :, :], w=["vecs"])
    m2 = p.sb("m2", [128, 2, ND])
    for i, j in ((0, 3), (1, 6)):
        p.op("dve", lambda e, i=i, j=j: e.scalar_tensor_tensor(out=m2[:, i, :], in0=vs_[:, j, :], scalar=1.0, in1=vs_[:, 1, :], op0=ALU.add, op1=ALU.mult),
             r=["vecs"], w=["vecs"])
    Wb = p.sb("Wb", [128, ND, D], BF16)
    wf = [p.sb("wf%d" % i, [128, ND, 128]) for i in range(2)]
    for blk in range(ND):
        s = blk % 2
        p.dma("sync", wf[s][:], w_out[:, :, blk * 128:(blk + 1) * 128], w=[("wf", s)])
        p.op("pool", lambda e, s=s, blk=blk: e.tensor_copy(out=Wb[:, :, blk * 128:(blk + 1) * 128], in_=wf[s][:]), r=[("wf", s)], w=["Wb"])
    yc = p.sb("yc", [128, ND, TK], BF16); x0 = p.sb("x0", [128, ND, TK], BF16); a = p.sb("a", [128, ND, TK], BF16)
    for (s0, sl, mi) in ((0, TL, 0), (TL, TC, 1)):
        for (c0, cw) in tiles(sl, TK):
            o = s0 + c0
            p.dma("sync", yc[:, :, :cw], ycT[:, :, o:o + cw], w=["yc"])
            p.dma("pool", x0[:, :, :cw], x0T[:, :, o:o + cw], w=["x0"])
            p.dma("sync", C.xt[:, :, :cw], xT[:, :, o:o + cw], w=["xt"])
            p.op("pool", lambda e, cw=cw: e.tensor_tensor(out=a[:, :, :cw], in0=yc[:, :, :cw], in1=x0[:, :, :cw], op=ALU.mult), r=["yc", "x0"], w=["a"])
            emit_post(p, cfg, C, cw, a, "a", Wb, "Wb",
                      lambda blk: vs_[:, 0, blk:blk + 1],
                      lambda blk, mi=mi: vs_[:, 2 + 3 * mi, blk:blk + 1],
                      lambda k, mi=mi: m2[:, mi, k:k + 1],
                      lambda k, mi=mi: vs_[:, 4 + 3 * mi, k:k + 1],
                      xlT[:, :, o:o + cw], tokT[:, :, o:o + cw],
                      lambda t0, tw, o=o: gates[o + t0:o + t0 + tw, :])
    return p.build()


def router_inputs(cfg, I, layer):
    ND = cfg.ND
    wr = np.zeros((cfg.D, 128), np.float32)
    wr[:, 0:4] = I["moe_wg"][layer]; wr[:, 4:36] = I["moe_we"][layer]
    br = np.zeros((128, 1), np.float32)
    br[0:4, 0] = I["moe_bg"][layer]; br[4:36, 0] = I["moe_be"][layer]
    wr = np.ascontiguousarray(wr.reshape(ND, 128, 128).transpose(1, 0, 2))
    return wr, br


def wfm(w, ND):
    return np.ascontiguousarray(w.reshape(ND, 128, w.shape[1]).transpose(1, 0, 2))


def run_h3(cfg, I, mods, y_lat, y_ctx, x0_lat, x0_ctx):
    import ml_dtypes
    bf = ml_dtypes.bfloat16
    ND, NC = cfg.ND, cfg.NCORE
    nc = build_h3(cfg)
    yc = tok_layout(cfg, y_lat, y_ctx); x0 = tok_layout(cfg, x0_lat, x0_ctx); xx = tok_layout(cfg, I["x"], I["ctx"])
    wr, br = router_inputs(cfg, I, 0)
    w_out = wfm(I["hy_w_out"][0], ND)
    ims = []
    for c in range(NC):
        b = c // cfg.CPB
        mvd = mod_vecs(cfg, mods[0], b)
        vecs = np.stack([vfm(v, ND) for v in (I["hy_b_out"][0], I["norm_g"][0, 1], mvd["gt_a"], mvd["sc_f"], mvd["sh_f"],
                                              mvd["cgt_a"], mvd["csc_f"], mvd["csh_f"])], axis=1)
        ims.append({"ycT": fm(yc[c], ND).astype(bf), "x0T": fm(x0[c], ND).astype(bf), "xT": fm(xx[c].astype(np.float32), ND),
                    "w_out": w_out, "vecsD": np.ascontiguousarray(vecs), "wrD": wr, "brD": br, "identD": np.eye(128, dtype=np.float32)})
    res = run(nc, ims)
    xl = tok_unlayout(cfg, [unfm(r["xlT"]) for r in res])
    tok = tok_unlayout(cfg, [unfm(np.asarray(r["tokT"])) for r in res])
    gates = tok_unlayout(cfg, [r["gates"] for r in res])
    return xl, tok, gates


def build_moe(cfg, CAP):
    p = Prog()
    ND, D, DE, EPC = cfg.ND, cfg.D, cfg.DE, cfg.EPC
    NDE = DE // 128
    xe = p.din("xe", [EPC, 128, ND, CAP], BF16)
    wg = p.din("wg", [EPC, 128, ND, DE]); wu = p.din("wu", [EPC, 128, ND, DE]); wd = p.din("wd", [EPC, 128, NDE, D])
    ye = p.dout("ye", [EPC, 128, ND, CAP], BF16)
    Wg = p.sb("Wg", [128, ND, DE], BF16); Wu = p.sb("Wu", [128, ND, DE], BF16); Wd = p.sb("Wd", [128, NDE, D], BF16)
    SW = 512
    st = [p.sb("st%d" % i, [128, max(ND, NDE), SW]) for i in range(2)]
    CT = min(512, CAP)
    xs = p.sb("xs", [128, ND, CT], BF16); h = p.sb("h", [128, NDE, CT], BF16); yo = p.sb("yo", [128, ND, CT], BF16)
    tg = p.sb("tg", [128, CT])
    pg = p.ps("pg", [128, CT]); pu = p.ps("pu", [128, CT]); py = [p.ps("py%d" % i, [128, CT]) for i in range(2)]
    si = 0
    for ex in range(EPC):
        for (src, dst, key, nk, ncols) in ((wg, Wg, "Wg", ND, DE), (wu, Wu, "Wu", ND, DE), (wd, Wd, "Wd", NDE, D)):
            for (c0, cw) in tiles(ncols, SW):
                s = si % 2
                si += 1
                p.dma("sync" if s else "pool", st[s][:, :nk, :cw], src[ex, :, :, c0:c0 + cw], w=[("st", s)])
                p.op("dve" if s else "act", (lambda e, s=s, dst=dst, nk=nk, c0=c0, cw=cw: e.tensor_copy(out=dst[:, :, c0:c0 + cw], in_=st[s][:, :nk, :cw]))
                     if s else (lambda e, s=s, dst=dst, nk=nk, c0=c0, cw=cw: e.activation(out=dst[:, :, c0:c0 + cw], in_=st[s][:, :nk, :cw], func=AF.Copy)),
                     r=[("st", s)], w=[key])
        for (t0, tw) in tiles(CAP, CT):
            p.dma("sync", xs[:, :, :tw], xe[ex, :, :, t0:t0 + tw], w=["xs"])
            for fb in range(NDE):
                for k in range(ND):
                    p.op("pe", lambda e, fb=fb, k=k, tw=tw: e.matmul(pg[:, :tw], lhsT=Wg[:, k, fb * 128:(fb + 1) * 128], rhs=xs[:, k, :tw],
                                                                    start=(k == 0), stop=(k == ND - 1)), r=["Wg", "xs"], w=["pg"])
                for k in range(ND):
                    p.op("pe", lambda e, fb=fb, k=k, tw=tw: e.matmul(pu[:, :tw], lhsT=Wu[:, k, fb * 128:(fb + 1) * 128], rhs=xs[:, k, :tw],
                                                                    start=(k == 0), stop=(k == ND - 1)), r=["Wu", "xs"], w=["pu"])
                p.op("act", lambda e, tw=tw: e.activation(out=tg[:, :tw], in_=pg[:, :tw], func=AF.Silu), r=["pg"], w=["tg"])
                p.op("dve", lambda e, fb=fb, tw=tw: e.tensor_tensor(out=h[:, fb, :tw], in0=pu[:, :tw], in1=tg[:, :tw], op=ALU.mult),
                     r=["pu", "tg"], w=["h"])
            for ob in range(ND):
                q = ob % 2
                for f in range(NDE):
                    p.op("pe", lambda e, ob=ob, f=f, q=q, tw=tw: e.matmul(py[q][:, :tw], lhsT=Wd[:, f, ob * 128:(ob + 1) * 128], rhs=h[:, f, :tw],
                                                                         start=(f == 0), stop=(f == NDE - 1)), r=["Wd", "h"], w=[("py", q)])
                if q:
                    p.op("act", lambda e, ob=ob, q=q, tw=tw: e.activation(out=yo[:, ob, :tw], in_=py[q][:, :tw], func=AF.Copy), r=[("py", q)], w=["yo"])
                else:
                    p.op("dve", lambda e, ob=ob, q=q, tw=tw: e.tensor_copy(out=yo[:, ob, :tw], in_=py[q][:, :tw]), r=[("py", q)], w=["yo"])
            p.dma("sync", ye[ex, :, :, t0:t0 + tw], yo[:, :, :tw], r=["yo"], w=[("ye", ex, t0)], grp=("yst",))
    return p.build()


_MOE_NC = {}


def run_moe(cfg, I, layer, tok, gates, CAP):
    import ml_dtypes
    bf = ml_dtypes.bfloat16
    ND, NC, EPC, D, DE = cfg.ND, cfg.NCORE, cfg.EPC, cfg.D, cfg.DE
    NDE = DE // 128
    T = tok.shape[0]
    sel = gates > 0
    idx = [np.nonzero(sel[:, e])[0] for e in range(cfg.NE)]
    if CAP is None:
        mx = max(len(ix) for ix in idx)
        CAP = max(512, min(4096, ((mx + 127) // 128) * 128))
    key = (cfg.D, CAP)
    if key not in _MOE_NC:
        _MOE_NC[key] = build_moe(cfg, CAP)
    nc = _MOE_NC[key]
    print("[moe] counts min/mean/max", min(len(i) for i in idx), T * 2 // cfg.NE, max(len(i) for i in idx), "CAP", CAP, flush=True)
    rounds = max(1, max((len(ix) + CAP - 1) // CAP for ix in idx))
    slot = np.cumsum(sel, axis=1) - 1
    y01 = np.zeros((2, T, D), bf)
    g01 = np.zeros((2, T), np.float32)
    wgl = [wfm(I["moe_w_gate"][layer][e], ND) for e in range(cfg.NE)]
    wul = [wfm(I["moe_w_up"][layer][e], ND) for e in range(cfg.NE)]
    wdl = [wfm(I["moe_w_down"][layer][e], NDE) for e in range(cfg.NE)]
    for r in range(rounds):
        ims = []
        for c in range(NC):
            xe = np.zeros((EPC, 128, ND, CAP), bf)
            for j in range(EPC):
                ix = idx[c * EPC + j][r * CAP:(r + 1) * CAP]
                if len(ix):
                    xe[j, :, :, :len(ix)] = fm(tok[ix], ND)
            ims.append({"xe": xe, "wg": np.stack(wgl[c * EPC:(c + 1) * EPC]), "wu": np.stack(wul[c * EPC:(c + 1) * EPC]),
                        "wd": np.stack(wdl[c * EPC:(c + 1) * EPC])})
        res = run(nc, ims)
        for c in range(NC):
            ye = np.asarray(res[c]["ye"])
            for j in range(EPC):
                e = c * EPC + j
                ix = idx[e][r * CAP:(r + 1) * CAP]
                if len(ix):
                    yy = unfm(ye[j][:, :, :len(ix)])
                    sl = slot[ix, e]
                    for s in (0, 1):
                        m = sl == s
                        y01[s, ix[m]] = yy[m]
                        g01[s, ix[m]] = gates[ix[m], e]
    return y01, g01


def build_comb(cfg, final):
    p = Prog()
    ND, TL, TC = cfg.ND, cfg.TL, cfg.TC
    NT = TL if final else TL + TC
    TK = 256
    xlT = p.din("xlT", [128, ND, NT]); y0T = p.din("y0T", [128, ND, NT], BF16); y1T = p.din("y1T", [128, ND, NT], BF16)
    gb = p.din("gb", [128, 2, NT]); vecs = p.din("vecsD", [128, 7, ND])
    xoT = p.dout("xoT", [128, ND, NT]); uT = p.dout("uT", [128, ND, NT], F32 if final else BF16)
    vs_ = p.sb("vecs", [128, 7, ND]); p.dma("sync", vs_[:], vecs[:, :, :], w=["vecs"])
    m2 = p.sb("m2", [128, 2, ND])
    for i, j in ((0, 3), (1, 5)):
        p.op("dve", lambda e, i=i, j=j: e.scalar_tensor_tensor(out=m2[:, i, :], in0=vs_[:, j, :], scalar=1.0, in1=vs_[:, 2, :], op0=ALU.add, op1=ALU.mult),
             r=["vecs"], w=["vecs"])
    ones = p.sb("ones", [128, 128]); p.op("dve", lambda e: e.memset(ones[:], 1.0), w=["ones"])
    xt = p.sb("xt", [128, ND, TK]); y0 = p.sb("y0", [128, ND, TK], BF16); y1 = p.sb("y1", [128, ND, TK], BF16); gs = p.sb("gs", [128, 2, TK])
    mo = p.sb("mo", [128, ND, TK]); mo2 = p.sb("mo2", [128, ND, TK]); xl = p.sb("xl", [128, ND, TK]); sq = p.sb("sq", [128, ND, TK])
    rstd = p.sb("rstd", [128, TK]); uo = p.sb("uo", [128, ND, TK], F32 if final else BF16)
    pss = p.ps("pss", [128, TK])
    segs = ((0, TL, 0),) if final else ((0, TL, 0), (TL, TC, 1))
    for (s0, sl, mi) in segs:
        for (c0, cw) in tiles(sl, TK):
            o = s0 + c0
            p.dma("sync", xt[:, :, :cw], xlT[:, :, o:o + cw], w=["xt"])
            p.dma("pool", y0[:, :, :cw], y0T[:, :, o:o + cw], w=["y0"])
            p.dma("pool", y1[:, :, :cw], y1T[:, :, o:o + cw], w=["y1"])
            p.dma("sync", gs[:, :, :cw], gb[:, :, o:o + cw], w=["gs"])
            g0 = gs[:, 0, :cw].unsqueeze(1).broadcast_to([128, ND, cw])
            g1 = gs[:, 1, :cw].unsqueeze(1).broadcast_to([128, ND, cw])
            p.op("dve", lambda e, cw=cw, g0=g0: e.tensor_tensor(out=mo[:, :, :cw], in0=y0[:, :, :cw], in1=g0, op=ALU.mult), r=["y0", "gs"], w=["mo"])
            p.op("pool", lambda e, cw=cw, g1=g1: e.tensor_tensor(out=mo2[:, :, :cw], in0=y1[:, :, :cw], in1=g1, op=ALU.mult), r=["y1", "gs"], w=["mo2"])
            p.op("dve", lambda e, cw=cw: e.tensor_tensor(out=mo[:, :, :cw], in0=mo[:, :, :cw], in1=mo2[:, :, :cw], op=ALU.add), r=["mo", "mo2"], w=["mo"])
            for k in range(ND):
                p.op("dve", lambda e, k=k, cw=cw, mi=mi: e.scalar_tensor_tensor(out=xl[:, k, :cw], in0=mo[:, k, :cw], scalar=vs_[:, mi, k:k + 1],
                                                                              in1=xt[:, k, :cw], op0=ALU.mult, op1=ALU.add), r=["mo", "xt", "vecs"], w=["xl"])
            p.dma("sync", xoT[:, :, o:o + cw], xl[:, :, :cw], r=["xl"], w=[("xo", o)], grp=("xst",))
            emit_rstd(p, cfg, xl, "xl", cw, ones, sq, "sq", pss, "pss", rstd, "rstd")
            for k in range(ND):
                p.op("dve", lambda e, k=k, cw=cw: e.tensor_tensor(out=sq[:, k, :cw], in0=xl[:, k, :cw], in1=rstd[:, :cw], op=ALU.mult),
                     r=["xl", "rstd"], w=["sq"])
                if final:
                    p.op("dve", lambda e, k=k, cw=cw: e.tensor_scalar(out=uo[:, k, :cw], in0=sq[:, k, :cw], scalar1=vs_[:, 2, k:k + 1], scalar2=None, op0=ALU.mult),
                         r=["sq", "vecs"], w=["uo"])
                else:
                    p.op("dve", lambda e, k=k, cw=cw, mi=mi: e.tensor_scalar(out=uo[:, k, :cw], in0=sq[:, k, :cw], scalar1=m2[:, mi, k:k + 1],
                                                                           scalar2=vs_[:, 4 + 2 * mi, k:k + 1], op0=ALU.mult, op1=ALU.add),
                         r=["sq", "vecs"], w=["uo"])
            p.dma("sync", uT[:, :, o:o + cw], uo[:, :, :cw], r=["uo"], w=[("uo_", o)], grp=("ust",))
    return p.build()


def run_comb(cfg, I, mods, layer, xl_lat, xl_ctx, y01, g01, final):
    import ml_dtypes
    bf = ml_dtypes.bfloat16
    ND, NC, B, L, LC, D = cfg.ND, cfg.NCORE, cfg.B, cfg.L, cfg.LC, cfg.D
    nc = build_comb(cfg, final)
    nl = B * L
    def split(a, last):
        lat = a[:nl].reshape((B, L) + last)
        ctx = a[nl:].reshape((B, LC) + last) if not final else np.zeros((B, LC) + last, a.dtype)
        return lat, ctx
    y0 = split(y01[0], (D,)); y1 = split(y01[1], (D,))
    g0 = split(g01[0][:, None], (1,)); g1 = split(g01[1][:, None], (1,))
    xs = tok_layout(cfg, xl_lat, xl_ctx if xl_ctx is not None else np.zeros((B, LC, D), np.float32))
    y0s = tok_layout(cfg, *y0); y1s = tok_layout(cfg, *y1); g0s = tok_layout(cfg, *g0); g1s = tok_layout(cfg, *g1)
    NT = cfg.TL if final else cfg.TL + cfg.TC
    ims = []
    for c in range(NC):
        b = c // cfg.CPB
        mvd = mod_vecs(cfg, mods[layer], b)
        if final:
            z = np.zeros(D, np.float32)
            vl = (mvd["gt_f"], z, I["final_g"], z, z, z, z)
        else:
            nm = mod_vecs(cfg, mods[layer + 1], b)
            vl = (mvd["gt_f"], mvd["cgt_f"], I["norm_g"][layer + 1, 0], nm["sc_a"], nm["sh_a"], nm["csc_a"], nm["csh_a"])
        vecs = np.ascontiguousarray(np.stack([vfm(np.asarray(v, np.float32), ND) for v in vl], axis=1))
        gbv = np.stack([g0s[c][:NT, 0], g1s[c][:NT, 0]])
        ims.append({"xlT": fm(xs[c][:NT].astype(np.float32), ND), "y0T": fm(y0s[c][:NT], ND).astype(bf), "y1T": fm(y1s[c][:NT], ND).astype(bf),
                    "gb": np.ascontiguousarray(np.broadcast_to(gbv[None], (128, 2, NT))).astype(np.float32), "vecsD": vecs})
    res = run(nc, ims)
    def un(name):
        per = []
        for r in res:
            a = unfm(np.asarray(r[name]))
            if final:
                a = np.concatenate([a, np.zeros((cfg.TC, D), a.dtype)], 0)
            per.append(a)
        return tok_unlayout(cfg, per)
    xo = un("xoT"); u = un("uT")
    return xo[0], xo[1], u[0], u[1]


S5P, S5H = 64, 16


def emit_exp_poly(p, out_ap, okey, in_ap, ikey, shape, center, degree, name):
    import math
    t = p.sb(name + "_t", shape); r = p.sb(name + "_r", shape)
    p.op("dve", lambda e: e.tensor_scalar(out=t[:], in0=in_ap, scalar1=-center, scalar2=None, op0=ALU.add), r=[ikey], w=[name + "t"])
    p.op("dve", lambda e: e.memset(r[:], 1.0 / math.factorial(degree)), w=[name + "r"])
    for n in range(degree - 1, -1, -1):
        p.op("dve", lambda e: e.tensor_tensor(out=r[:], in0=r[:], in1=t[:], op=ALU.mult), r=[name + "t", name + "r"], w=[name + "r"])
        p.op("dve", lambda e, n=n: e.tensor_scalar(out=r[:], in0=r[:], scalar1=1.0 / math.factorial(n), scalar2=None, op0=ALU.add), r=[name + "r"], w=[name + "r"])
    p.op("dve", lambda e: e.tensor_scalar(out=out_ap, in0=r[:], scalar1=float(math.exp(center)), scalar2=None, op0=ALU.mult), r=[name + "r"], w=[okey])

def build_s5prep(cfg, CH):
    p = Prog()
    G = cfg.G
    P_, H = S5P, S5H
    NLc = max(1, CH // cfg.NCORE)
    NPW = CH + 1
    a_d = p.din("a_d", [G, 2, 2, P_])
    ls_d = p.din("ls_d", [G, 2])
    b_d = p.din("b_d", [G, 2, 2, P_, H])
    c_d = p.din("c_d", [G, 2, 2, H, P_])
    lsel = p.din("lsel", [G, NLc, NPW])
    XB = p.dout("XB", [G, NLc, 2, 2, P_, H])
    OC = p.dout("OC", [G, NLc, 2, 2, H, P_])
    Mo = p.dout("Mo", [G, NLc, 2, H, H])
    PC = p.dout("PC", [G, 3, 2, P_])
    DP = 2 * P_
    a = p.sb("a", [G, 2, DP]); ls = p.sb("ls", [G, 2]); b = p.sb("b", [G, 2, 2 * P_ * H]); c = p.sb("c", [G, 2, 2 * H * P_])
    p.dma("sync", a[:], a_d.rearrange("g r d p -> g r (d p)"), w=["a"]); p.dma("sync", ls[:], ls_d[:, :], w=["ls"])
    p.dma("sync", b[:], b_d.rearrange("g r d p h -> g r (d p h)"), w=["b"]); p.dma("pool", c[:], c_d.rearrange("g r d h p -> g r (d h p)"), w=["c"])
    sel = p.sb("sel", [G, NLc, NPW]); p.dma("sync", sel[:], lsel[:, :, :], w=["sel"])
    st = p.sb("stp", [G, 2])
    emit_exp_poly(p, st[:], "st", ls[:], "ls", [G, 2], float(np.log(0.01)), 20, "ep1")
    stb = st[:, :].unsqueeze(2).broadcast_to([G, 2, P_])
    lr = p.sb("lr", [G, DP]); li = p.sb("li", [G, DP]); mag = p.sb("mag", [G, DP]); cs = p.sb("cs", [G, 2, DP])
    v3 = lambda t: t.rearrange("g (d p) -> g d p", p=P_)
    p.op("dve", lambda e: e.tensor_tensor(out=v3(lr[:, :]), in0=v3(a[:, 0, :]), in1=stb, op=ALU.mult), r=["a", "st"], w=["lr"])
    p.op("dve", lambda e: e.tensor_tensor(out=v3(li[:, :]), in0=v3(a[:, 1, :]), in1=stb, op=ALU.mult), r=["a", "st"], w=["li"])
    emit_exp_poly(p, mag[:], "mag", lr[:], "lr", [G, DP], 0.0, 7, "ep2")
    tmp = [p.sb("tmpa", [G, DP]), p.sb("tmpb", [G, DP])]
    one = p.sb("one1", [G, 1]); p.op("dve", lambda e: e.memset(one[:], 1.0), w=["one1"])
    hp = p.sb("hpi", [G, 1]); p.op("dve", lambda e: e.memset(hp[:], float(np.pi / 2)), w=["hpi"])
    zr = p.sb("zr", [G, 1]); p.op("dve", lambda e: e.memset(zr[:], 0.0), w=["zr"])
    emit_sin(p, cs[:, 1, :], ("cs", 1), li[:, :], "li", DP, tmp, "tmp", one[:, 0:1], zr[:, 0:1], extra_r=["one1", "zr"])
    emit_sin(p, cs[:, 0, :], ("cs", 0), li[:, :], "li", DP, tmp, "tmp", one[:, 0:1], hp[:, 0:1], extra_r=["one1", "hpi"])
    pw = p.sb("pw", [G, NPW, 2, DP])
    p.op("dve", lambda e: e.memset(pw[:, 0, 0, :], 1.0), w=["pw"])
    p.op("dve", lambda e: e.memset(pw[:, 0, 1, :], 0.0), w=["pw"])
    p.op("dve", lambda e: e.tensor_tensor(out=pw[:, 1, 0, :], in0=mag[:], in1=cs[:, 0, :], op=ALU.mult), r=["mag", ("cs", 0)], w=["pw"])
    p.op("dve", lambda e: e.tensor_tensor(out=pw[:, 1, 1, :], in0=mag[:], in1=cs[:, 1, :], op=ALU.mult), r=["mag", ("cs", 1)], w=["pw"])
    t0, t1 = tmp

    def cmul(out_re, out_im, are, aim, bre, bim, rk, wk, neg_im_out=None):
        raise NotImplementedError

    for l in range(1, CH):
        p.op("dve", lambda e, l=l: e.tensor_tensor(out=t0[:], in0=pw[:, l, 0, :], in1=pw[:, 1, 0, :], op=ALU.mult), r=["pw"], w=[("tmp", 0)])
        p.op("dve", lambda e, l=l: e.tensor_tensor(out=t1[:], in0=pw[:, l, 1, :], in1=pw[:, 1, 1, :], op=ALU.mult), r=["pw"], w=[("tmp", 1)])
        p.op("dve", lambda e, l=l: e.tensor_tensor(out=pw[:, l + 1, 0, :], in0=t0[:], in1=t1[:], op=ALU.subtract), r=[("tmp", 0), ("tmp", 1)], w=["pw"])
        p.op("dve", lambda e, l=l: e.tensor_tensor(out=t0[:], in0=pw[:, l, 0, :], in1=pw[:, 1, 1, :], op=ALU.mult), r=["pw"], w=[("tmp", 0)])
        p.op("dve", lambda e, l=l: e.tensor_tensor(out=t1[:], in0=pw[:, l, 1, :], in1=pw[:, 1, 0, :], op=ALU.mult), r=["pw"], w=[("tmp", 1)])
        p.op("dve", lambda e, l=l: e.tensor_tensor(out=pw[:, l + 1, 1, :], in0=t0[:], in1=t1[:], op=ALU.add), r=[("tmp", 0), ("tmp", 1)], w=["pw"])
    pc = p.sb("pc", [G, 3, DP])
    p.op("dve", lambda e: e.tensor_copy(out=pc[:, 0:2, :], in_=pw[:, CH, :, :]), r=["pw"], w=["pc"])
    p.op("dve", lambda e: e.tensor_scalar(out=pc[:, 2, :], in0=pw[:, CH, 1, :], scalar1=-1.0, scalar2=None, op0=ALU.mult), r=["pw"], w=["pc"])
    p.dma("sync", PC.rearrange("g t d p -> g t (d p)"), pc[:], r=["pc"], w=["PC"])
    q = p.sb("q", [G, 2, DP]); dd = p.sb("dd", [G, DP]); nr = p.sb("nr", [G, DP])
    p.op("dve", lambda e: e.tensor_scalar(out=nr[:], in0=pw[:, 1, 0, :], scalar1=-1.0, scalar2=None, op0=ALU.add), r=["pw"], w=["nr"])
    p.op("dve", lambda e: e.tensor_tensor(out=t0[:], in0=a[:, 0, :], in1=a[:, 0, :], op=ALU.mult), r=["a"], w=[("tmp", 0)])
    p.op("dve", lambda e: e.tensor_tensor(out=t1[:], in0=a[:, 1, :], in1=a[:, 1, :], op=ALU.mult), r=["a"], w=[("tmp", 1)])
    p.op("dve", lambda e: e.tensor_tensor(out=dd[:], in0=t0[:], in1=t1[:], op=ALU.add), r=[("tmp", 0), ("tmp", 1)], w=["dd"])
    p.op("dve", lambda e: e.reciprocal(out=dd[:], in_=dd[:]), r=["dd"], w=["dd"])
    p.op("dve", lambda e: e.tensor_tensor(out=t0[:], in0=nr[:], in1=a[:, 0, :], op=ALU.mult), r=["nr", "a"], w=[("tmp", 0)])
    p.op("dve", lambda e: e.tensor_tensor(out=t1[:], in0=pw[:, 1, 1, :], in1=a[:, 1, :], op=ALU.mult), r=["pw", "a"], w=[("tmp", 1)])
    p.op("dve", lambda e: e.tensor_tensor(out=t0[:], in0=t0[:], in1=t1[:], op=ALU.add), r=[("tmp", 0), ("tmp", 1)], w=[("tmp", 0)])
    p.op("dve", lambda e: e.tensor_tensor(out=q[:, 0, :], in0=t0[:], in1=dd[:], op=ALU.mult), r=[("tmp", 0), "dd"], w=["q"])
    p.op("dve", lambda e: e.tensor_tensor(out=t0[:], in0=pw[:, 1, 1, :], in1=a[:, 0, :], op=ALU.mult), r=["pw", "a"], w=[("tmp", 0)])
    p.op("dve", lambda e: e.tensor_tensor(out=t1[:], in0=nr[:], in1=a[:, 1, :], op=ALU.mult), r=["nr", "a"], w=[("tmp", 1)])
    p.op("dve", lambda e: e.tensor_tensor(out=t0[:], in0=t0[:], in1=t1[:], op=ALU.subtract), r=[("tmp", 0), ("tmp", 1)], w=[("tmp", 0)])
    p.op("dve", lambda e: e.tensor_tensor(out=q[:, 1, :], in0=t0[:], in1=dd[:], op=ALU.mult), r=[("tmp", 0), "dd"], w=["q"])
    NB_ = 2 * P_ * H
    bb = p.sb("bb", [G, 2, NB_]); w0 = p.sb("w0", [G, NB_]); w1 = p.sb("w1", [G, NB_])
    v_ph = lambda t: t.rearrange("g (dp h) -> g dp h", h=H)
    bc_h = lambda t: t.unsqueeze(2).broadcast_to([G, DP, H])

    def cmul_ph(out_re, out_im, sre, sim, xre, xim, rk, wk, neg_im=False):
        p.op("dve", lambda e: e.tensor_tensor(out=v_ph(w0[:, :]), in0=v_ph(xre), in1=bc_h(sre), op=ALU.mult), r=rk, w=["w0"])
        p.op("pool", lambda e: e.tensor_tensor(out=v_ph(w1[:, :]), in0=v_ph(xim), in1=bc_h(sim), op=ALU.mult), r=rk, w=["w1"])
        p.op("dve", lambda e: e.tensor_tensor(out=out_re, in0=w0[:, :], in1=w1[:, :], op=ALU.subtract), r=["w0", "w1"], w=wk)
        p.op("dve", lambda e: e.tensor_tensor(out=v_ph(w0[:, :]), in0=v_ph(xim), in1=bc_h(sre), op=ALU.mult), r=rk, w=["w0"])
        p.op("pool", lambda e: e.tensor_tensor(out=v_ph(w1[:, :]), in0=v_ph(xre), in1=bc_h(sim), op=ALU.mult), r=rk, w=["w1"])
        if neg_im:
            p.op("dve", lambda e: e.scalar_tensor_tensor(out=out_im, in0=w0[:, :], scalar=-1.0, in1=w1[:, :], op0=ALU.mult, op1=ALU.subtract),
                 r=["w0", "w1"], w=wk)
        else:
            p.op("dve", lambda e: e.tensor_tensor(out=out_im, in0=w0[:, :], in1=w1[:, :], op=ALU.add), r=["w0", "w1"], w=wk)

    cmul_ph(bb[:, 0, :], bb[:, 1, :], q[:, 0, :], q[:, 1, :], b[:, 0, :], b[:, 1, :], ["q", "b"], ["bb"])
    pl = p.sb("pl", [G, 2, DP]); pl1 = p.sb("pl1", [G, 2, DP])
    xb = p.sb("xb", [G, 2, NB_]); oc = p.sb("oc", [G, 2, NB_]); mo = p.sb("mo", [G, 2, H * H])
    HH = H * H
    big0 = p.sb("big0", [G, (H // 2) * H * P_]); big1 = p.sb("big1", [G, (H // 2) * H * P_])
    for n in range(NLc):
        for (dst, off, key) in ((pl, 0, "pl"), (pl1, 1, "pl1")):
            for comp in range(2):
                first = True
                for l in range(CH):
                    src = pw[:, l + off, comp, :]
                    if first:
                        p.op("dve", lambda e, dst=dst, comp=comp, src=src, n=n, l=l: e.tensor_scalar(
                            out=dst[:, comp, :], in0=src, scalar1=sel[:, n, l:l + 1], scalar2=None, op0=ALU.mult), r=["pw", "sel"], w=[key])
                        first = False
                    else:
                        p.op("dve", lambda e, dst=dst, comp=comp, src=src, n=n, l=l: e.scalar_tensor_tensor(
                            out=dst[:, comp, :], in0=src, scalar=sel[:, n, l:l + 1], in1=dst[:, comp, :], op0=ALU.mult, op1=ALU.add),
                            r=["pw", "sel", key], w=[key])
        cmul_ph(xb[:, 0, :], xb[:, 1, :], pl[:, 0, :], pl[:, 1, :], bb[:, 0, :], bb[:, 1, :], ["pl", "bb"], ["xb"])
        p.dma("sync", XB[:, n].rearrange("g r d p h -> g r (d p h)"), xb[:], r=["xb"], w=[("XB", n)], grp=("xbst",))
        cv = lambda t: t.rearrange("g (d h p) -> g d h p", d=2, h=H)
        bc_hp = lambda t: t.rearrange("g (d p) -> g d p", d=2).unsqueeze(2).broadcast_to([G, 2, H, P_])
        NC_ = 2 * H * P_
        p.op("dve", lambda e: e.tensor_tensor(out=cv(w0[:, :NC_]), in0=cv(c[:, 0, :]), in1=bc_hp(pl1[:, 0, :]), op=ALU.mult), r=["c", "pl1"], w=["w0"])
        p.op("pool", lambda e: e.tensor_tensor(out=cv(w1[:, :NC_]), in0=cv(c[:, 1, :]), in1=bc_hp(pl1[:, 1, :]), op=ALU.mult), r=["c", "pl1"], w=["w1"])
        p.op("dve", lambda e: e.tensor_tensor(out=oc[:, 0, :], in0=w0[:, :NC_], in1=w1[:, :NC_], op=ALU.subtract), r=["w0", "w1"], w=["oc"])
        p.op("dve", lambda e: e.tensor_tensor(out=cv(w0[:, :NC_]), in0=cv(c[:, 1, :]), in1=bc_hp(pl1[:, 0, :]), op=ALU.mult), r=["c", "pl1"], w=["w0"])
        p.op("pool", lambda e: e.tensor_tensor(out=cv(w1[:, :NC_]), in0=cv(c[:, 0, :]), in1=bc_hp(pl1[:, 1, :]), op=ALU.mult), r=["c", "pl1"], w=["w1"])
        p.op("dve", lambda e: e.scalar_tensor_tensor(out=oc[:, 1, :], in0=w0[:, :NC_], scalar=-1.0, in1=w1[:, :NC_], op0=ALU.mult, op1=ALU.subtract),
             r=["w0", "w1"], w=["oc"])
        p.dma("sync", OC[:, n].rearrange("g r d h p -> g r (d h p)"), oc[:], r=["oc"], w=[("OC", n)], grp=("ocst",))
        for d in range(2):
            for hh in range(2):
                h0 = hh * (H // 2)
                def cview(comp, d=d, h0=h0):
                    t = c[:, comp, d * H * P_:(d + 1) * H * P_].rearrange("g (h p) -> g h p", p=P_)[:, h0:h0 + H // 2, :]
                    return t.unsqueeze(2).broadcast_to([G, H // 2, H, P_])
                def xview(comp, d=d):
                    t = xb[:, comp, d * P_ * H:(d + 1) * P_ * H].rearrange("g (p h) -> g h p", h=H)
                    return t.unsqueeze(1).broadcast_to([G, H // 2, H, P_])
                b0v = big0[:, :].rearrange("g (a h p) -> g a h p", a=H // 2, h=H)
                b1v = big1[:, :].rearrange("g (a h p) -> g a h p", a=H // 2, h=H)
                p.op("dve", lambda e, cview=cview, xview=xview, b0v=b0v: e.tensor_tensor(out=b0v, in0=cview(0), in1=xview(0), op=ALU.mult), r=["c", "xb"], w=["big0"])
                p.op("pool", lambda e, cview=cview, xview=xview, b1v=b1v: e.tensor_tensor(out=b1v, in0=cview(1), in1=xview(1), op=ALU.mult), r=["c", "xb"], w=["big1"])
                p.op("dve", lambda e: e.tensor_tensor(out=big0[:, :], in0=big0[:, :], in1=big1[:, :], op=ALU.subtract), r=["big0", "big1"], w=["big0"])
                p.op("dve", lambda e, d=d, h0=h0: e.tensor_reduce(out=mo[:, d, h0 * H:(h0 + H // 2) * H],
                                                                 in_=big0[:, :].rearrange("g (a p) -> g a p", p=P_), axis=AX.X, op=ALU.add),
                     r=["big0"], w=["mo"])
        p.dma("sync", Mo[:, n].rearrange("g d a b -> g d (a b)"), mo[:], r=["mo"], w=[("Mo", n)], grp=("most",))
    return p.build()


def run_s5prep(cfg, I, CH):
    G, NC = cfg.G, cfg.NCORE
    P_, H = S5P, S5H
    NLc = max(1, CH // NC)
    nc = build_s5prep(cfg, CH)
    a_d = np.ascontiguousarray(np.stack([I["s5_a_re"][0], I["s5_a_im"][0]]).transpose(2, 0, 1, 3)).astype(np.float32)
    ls_d = np.ascontiguousarray(I["s5_log_step"][0].T).astype(np.float32)
    b_d = np.ascontiguousarray(np.stack([I["s5_b_re"][0], I["s5_b_im"][0]]).transpose(2, 0, 1, 3, 4)).astype(np.float32)
    c_d = np.ascontiguousarray(np.stack([I["s5_c_re"][0], I["s5_c_im"][0]]).transpose(2, 0, 1, 3, 4)).astype(np.float32)
    ims = []
    lags = []
    for c in range(NC):
        ls = [c + NC * n for n in range(NLc)] if CH >= NC else [c % CH]
        lags.append(ls)
        sel = np.zeros((G, NLc, CH + 1), np.float32)
        for n, l in enumerate(ls):
            sel[:, n, l] = 1.0
        ims.append({"a_d": a_d, "ls_d": ls_d, "b_d": b_d, "c_d": c_d, "lsel": sel})
    res = run(nc, ims)
    XB = np.zeros((CH, G, 2, 2, P_, H), np.float32); OC = np.zeros((CH, G, 2, 2, H, P_), np.float32); Mo = np.zeros((CH, G, 2, H, H), np.float32)
    for c in range(NC):
        for n, l in enumerate(lags[c]):
            XB[l] = res[c]["XB"][:, n]; OC[l] = res[c]["OC"][:, n]; Mo[l] = res[c]["Mo"][:, n]
    PC = res[0]["PC"]
    return dict(XB=XB, OC=OC, Mo=Mo, PC=PC)


def build_s5(cfg, CH, PG):
    p = Prog()
    B = cfg.B
    GPC = cfg.G // cfg.NCORE
    NPr = 2 * GPC
    KR = CH * S5H
    KP = KR // 128
    LT = cfg.LC + cfg.L
    NCK = LT // CH
    NCOL = NCK * B
    CL0 = (cfg.LC // CH) * B
    NCOLL = NCOL - CL0
    U = p.din("U", [NPr, 128, KP, NCOL], BF16)
    Tm = p.din("Tm", [NPr, 128, KP, KR]); Xm = p.din("Xm", [NPr, 128, 2, KP, 128]); Om = p.din("Om", [NPr, 128, KR])
    CAd = p.din("CA", [128, NPr]); CBd = p.din("CB", [128, NPr])
    Y = p.dout("Y", [NPr, 128, KP, NCOLL], BF16)
    ca = p.sb("ca", [128, NPr]); cb = p.sb("cb", [128, NPr])
    p.dma("sync", ca[:], CAd[:, :], w=["ca"]); p.dma("sync", cb[:], CBd[:, :], w=["cb"])
    SA = p.sb("SA", [128, PG, NCK, B]); SW = p.sb("SW", [128, PG, NCK, B]); SAb = p.sb("SAb", [128, PG, NCK, B], BF16)
    Ub = p.sb("Ub", [128, PG, KP, NCOL], BF16)
    Tb = p.sb("Tb", [128, PG, KP, KR], BF16); Xb = p.sb("Xb", [128, PG, 2, KP, 128], BF16); Ob = p.sb("Ob", [128, PG, KR], BF16)
    stT = p.sb("stT", [128, KP, KR]); stX = p.sb("stX", [128, 2, KP, 128]); stO = p.sb("stO", [128, KR])
    tq = [p.sb("tq%d" % i, [128, PG, B]) for i in range(4)]
    yo = [p.sb("yo%d" % i, [128, 512], BF16) for i in range(2)]
    px = [p.ps("px%d" % i, [128, 512]) for i in range(2)]
    py = [p.ps("py%d" % i, [128, 512]) for i in range(2)]
    yi = 0
    for pg0 in range(0, NPr, PG):
        for pi in range(PG):
            pr = pg0 + pi
            p.dma("sync", Ub[:, pi], U[pr], w=[("Ub", pi)])
            p.dma("pool", stT[:], Tm[pr], w=["stT"]); p.dma("pool", stX[:], Xm[pr], w=["stX"]); p.dma("pool", stO[:], Om[pr], w=["stO"])
            p.op("dve", lambda e, pi=pi: e.tensor_copy(out=Tb[:, pi], in_=stT[:]), r=["stT"], w=[("Tb", pi)])
            p.op("act", lambda e, pi=pi: e.activation(out=Xb[:, pi], in_=stX[:], func=AF.Copy), r=["stX"], w=[("Xb", pi)])
            p.op("dve", lambda e, pi=pi: e.tensor_copy(out=Ob[:, pi], in_=stO[:]), r=["stO"], w=[("Ob", pi)])
            SAf = SA[:, pi].rearrange("p k b -> p (k b)")
            SWf = SW[:, pi].rearrange("p k b -> p (k b)")
            for (c0, cw) in tiles(NCOL, 512):
                for w_, dstf, key in ((0, SAf, "SA"), (1, SWf, "SW")):
                    for qk in range(KP):
                        p.op("pe", lambda e, pi=pi, w_=w_, qk=qk, c0=c0, cw=cw: e.matmul(px[w_][:, :cw], lhsT=Xb[:, pi, w_, qk, :], rhs=Ub[:, pi, qk, c0:c0 + cw],
                                                                                     start=(qk == 0), stop=(qk == KP - 1)),
                             r=[("Xb", pi), ("Ub", pi)], w=[("px", w_)])
                    if w_ == 0:
                        p.op("act", lambda e, dstf=dstf, c0=c0, cw=cw: e.activation(out=dstf[:, c0:c0 + cw], in_=px[0][:, :cw], func=AF.Copy),
                             r=[("px", 0)], w=[("SA", pi)])
                    else:
                        p.op("dve", lambda e, dstf=dstf, c0=c0, cw=cw: e.tensor_copy(out=dstf[:, c0:c0 + cw], in_=px[1][:, :cw]),
                             r=[("px", 1)], w=[("SW", pi)])
        cab = ca[:, pg0:pg0 + PG].unsqueeze(2).broadcast_to([128, PG, B])
        cbb = cb[:, pg0:pg0 + PG].unsqueeze(2).broadcast_to([128, PG, B])
        allSA = [("SA", pi) for pi in range(PG)]
        allSW = [("SW", pi) for pi in range(PG)]
        for k in range(1, NCK):
            rA = allSA if k == 1 else [("SAk", k - 1)]
            rW = allSW if k == 1 else [("SWk", k - 1)]
            wA = (allSA if k == 1 else []) + [("SAk", k)]
            wW = (allSW if k == 1 else []) + [("SWk", k)]
            Sp = SA[:, :, k - 1, :]; Wp = SW[:, :, k - 1, :]; Sk = SA[:, :, k, :]; Wk = SW[:, :, k, :]
            p.op("dve", lambda e, Sp=Sp, cab=cab: e.tensor_tensor(out=tq[0][:], in0=Sp, in1=cab, op=ALU.mult), r=rA + ["ca"], w=["tq0"])
            p.op("dve", lambda e, Wp=Wp, cbb=cbb: e.tensor_tensor(out=tq[1][:], in0=Wp, in1=cbb, op=ALU.mult), r=rW + ["cb"], w=["tq1"])
            p.op("dve", lambda e: e.tensor_tensor(out=tq[0][:], in0=tq[0][:], in1=tq[1][:], op=ALU.add), r=["tq0", "tq1"], w=["tq0"])
            p.op("dve", lambda e, Sk=Sk: e.tensor_tensor(out=Sk, in0=Sk, in1=tq[0][:], op=ALU.add), r=["tq0"] + rA, w=wA)
            p.op("pool", lambda e, Wp=Wp, cab=cab: e.tensor_tensor(out=tq[2][:], in0=Wp, in1=cab, op=ALU.mult), r=rW + ["ca"], w=["tq2"])
            p.op("pool", lambda e, Sp=Sp, cbb=cbb: e.tensor_tensor(out=tq[3][:], in0=Sp, in1=cbb, op=ALU.mult), r=rA + ["cb"], w=["tq3"])
            p.op("pool", lambda e: e.tensor_tensor(out=tq[2][:], in0=tq[2][:], in1=tq[3][:], op=ALU.subtract), r=["tq2", "tq3"], w=["tq2"])
            p.op("pool", lambda e, Wk=Wk: e.tensor_tensor(out=Wk, in0=Wk, in1=tq[2][:], op=ALU.add), r=["tq2"] + rW, w=wW)
        fin = [("SAk", NCK - 1), ("SWk", NCK - 1)] + allSA + allSW
        p.op("dve", lambda e: e.memset(SAb[:, :, 0, :], 0.0), r=fin, w=["SAb"])
        p.op("act", lambda e: e.activation(out=SAb[:, :, 1:NCK, :], in_=SA[:, :, 0:NCK - 1, :], func=AF.Copy), r=fin + [("SAk", k) for k in range(1, NCK)], w=["SAb"])
        for pi in range(PG):
            pr = pg0 + pi
            SAbf = SAb[:, pi].rearrange("p k b -> p (k b)")
            for mb in range(KP):
                for (c0, cw) in tiles(NCOLL, 512):
                    q = yi % 2
                    yi += 1
                    a0 = CL0 + c0
                    for qk in range(mb + 1):
                        p.op("pe", lambda e, pi=pi, mb=mb, qk=qk, q=q, a0=a0, cw=cw: e.matmul(
                            py[q][:, :cw], lhsT=Tb[:, pi, qk, mb * 128:(mb + 1) * 128], rhs=Ub[:, pi, qk, a0:a0 + cw], start=(qk == 0), stop=False),
                            r=[("Tb", pi), ("Ub", pi)], w=[("py", q)])
                    p.op("pe", lambda e, pi=pi, mb=mb, q=q, a0=a0, cw=cw, SAbf=SAbf: e.matmul(
                        py[q][:, :cw], lhsT=Ob[:, pi, mb * 128:(mb + 1) * 128], rhs=SAbf[:, a0:a0 + cw], start=False, stop=True),
                        r=[("Ob", pi), "SAb"], w=[("py", q)])
                    if q:
                        p.op("act", lambda e, q=q, cw=cw: e.activation(out=yo[q][:, :cw], in_=py[q][:, :cw], func=AF.Copy), r=[("py", q)], w=[("yo", q)])
                    else:
                        p.op("dve", lambda e, q=q, cw=cw: e.tensor_copy(out=yo[q][:, :cw], in_=py[q][:, :cw]), r=[("py", q)], w=[("yo", q)])
                    p.dma("sync", Y[pr, :, mb, c0:c0 + cw], yo[q][:, :cw], r=[("yo", q)], w=[("Y", pr, mb, c0)], grp=("yst", q))
        for k in range(1, NCK):
            for nm in ("SAk", "SWk"):
                pass
        p.barrier()
    return p.build()


def s5_matrices(cfg, prep, CH):
    G = cfg.G
    P_, H = S5P, S5H
    KR = CH * H
    KP = KR // 128
    XB, OC, Mo, PC = prep["XB"], prep["OC"], prep["Mo"], prep["PC"]
    NPall = G * 2
    Tm = np.zeros((NPall, KR, KR), np.float32); Xm = np.zeros((NPall, 2, KR, 128), np.float32); Om = np.zeros((NPall, 128, KR), np.float32)
    CA = np.zeros((128, NPall), np.float32); CB = np.zeros((128, NPall), np.float32)
    for g in range(G):
        for d in range(2):
            pr = g * 2 + d
            for j in range(CH):
                for j2 in range(j, CH):
                    Tm[pr, j * H:(j + 1) * H, j2 * H:(j2 + 1) * H] = Mo[j2 - j][g, d].T
                xb = XB[CH - 1 - j][g, :, d]
                Xm[pr, 0, j * H:(j + 1) * H, 0:P_] = xb[0].T; Xm[pr, 0, j * H:(j + 1) * H, P_:] = xb[1].T
                Xm[pr, 1, j * H:(j + 1) * H, 0:P_] = xb[1].T; Xm[pr, 1, j * H:(j + 1) * H, P_:] = xb[0].T
                oc = OC[j][g, :, d]
                Om[pr, 0:P_, j * H:(j + 1) * H] = oc[0].T; Om[pr, P_:, j * H:(j + 1) * H] = oc[1].T
            CA[0:P_, pr] = PC[g, 0, d]; CA[P_:, pr] = PC[g, 0, d]
            CB[0:P_, pr] = PC[g, 2, d]; CB[P_:, pr] = PC[g, 1, d]
    Tm = np.ascontiguousarray(Tm.reshape(NPall, KP, 128, KR).transpose(0, 2, 1, 3))
    Xm = np.ascontiguousarray(Xm.reshape(NPall, 2, KP, 128, 128).transpose(0, 3, 1, 2, 4))
    return Tm, Xm, Om, CA, CB


def run_s5(cfg, mats, u_lat, u_ctx, CH, PG):
    import ml_dtypes
    bf = ml_dtypes.bfloat16
    B, L, LC, D, NC = cfg.B, cfg.L, cfg.LC, cfg.D, cfg.NCORE
    GPC = cfg.G // NC
    NPr = 2 * GPC
    H = S5H
    KR = CH * H; KP = KR // 128
    LT = LC + L; NCK = LT // CH; NCOL = NCK * B
    CL0 = (LC // CH) * B
    Tm, Xm, Om, CA, CB = mats
    nc = build_s5(cfg, CH, PG)
    seqs = [np.concatenate([u_ctx, u_lat], 1), np.concatenate([u_ctx[:, ::-1], u_lat[:, ::-1]], 1)]
    ims = []
    for c in range(NC):
        U = np.zeros((NPr, 128, KP, NCOL), bf)
        for gi in range(GPC):
            g = c * GPC + gi
            for d in range(2):
                s = seqs[d][:, :, g * H:(g + 1) * H]
                s = s.reshape(B, NCK, CH, H).transpose(2, 3, 1, 0)
                U[gi * 2 + d] = s.reshape(KP, 128, NCOL).transpose(1, 0, 2)
        sl = slice(c * NPr, (c + 1) * NPr)
        ims.append({"U": U, "Tm": Tm[sl], "Xm": Xm[sl], "Om": Om[sl], "CA": np.ascontiguousarray(CA[:, sl]), "CB": np.ascontiguousarray(CB[:, sl])})
    res = run(nc, ims)
    ys = [np.zeros((B, L, D), bf), np.zeros((B, L, D), bf)]
    for c in range(NC):
        Yc = np.asarray(res[c]["Y"])
        for gi in range(GPC):
            g = c * GPC + gi
            for d in range(2):
                a = Yc[gi * 2 + d].transpose(1, 0, 2).reshape(CH, H, L // CH, B)
                a = a.transpose(3, 2, 0, 1).reshape(B, L, H)
                if d == 1:
                    a = a[:, ::-1]
                ys[d][:, :, g * H:(g + 1) * H] = a
    return ys


def build_glu(cfg):
    p = Prog()
    ND, D, TL = cfg.ND, cfg.D, cfg.TL
    TK = 128
    TB = 512
    yfT = p.din("yfT", [128, ND, TL], BF16); ybT = p.din("ybT", [128, ND, TL], BF16); uT = p.din("uT", [128, ND, TL], BF16)
    xT = p.din("xT", [128, ND, TL]); w1 = p.din("w1", [128, ND, D]); w2 = p.din("w2", [128, ND, D]); vecs = p.din("vecsD", [128, 7, ND])
    wr_d = p.din("wrD", [128, ND, 128]); br_d = p.din("brD", [128, 1]); ident_d = p.din("identD", [128, 128])
    xlT = p.dout("xlT", [128, ND, TL]); tokT = p.dout("tokT", [128, ND, TL], BF16); gates = p.dout("gates", [TL, 32])
    C = PostCtx(p, cfg, TK)
    p.dma("sync", C.ident[:], ident_d[:, :], w=["ident"]); p.dma("sync", C.wr[:], wr_d[:, :, :], w=["wr"]); p.dma("sync", C.br[:], br_d[:, :], w=["br"])
    vs_ = p.sb("vecs", [128, 7, ND]); p.dma("sync", vs_[:], vecs[:, :, :], w=["vecs"])
    m2 = p.sb("m2", [128, ND])
    p.op("dve", lambda e: e.scalar_tensor_tensor(out=m2[:, :], in0=vs_[:, 5, :], scalar=1.0, in1=vs_[:, 4, :], op0=ALU.add, op1=ALU.mult), r=["vecs"], w=["vecs"])
    a = p.sb("a_all", [128, ND, TL], BF16)
    yf = p.sb("yf", [128, ND, TK], BF16); yb = p.sb("yb", [128, ND, TK], BF16); uu = p.sb("uu", [128, ND, TK], BF16)
    ys, yt = C.xt, C.sq
    GC = 2.0 * float(np.sqrt(2.0 / np.pi))
    for (c0, cw) in tiles(TL, TK):
        p.dma("sync", yf[:, :, :cw], yfT[:, :, c0:c0 + cw], w=["yf"])
        p.dma("pool", yb[:, :, :cw], ybT[:, :, c0:c0 + cw], w=["yb"])
        p.dma("sync", uu[:, :, :cw], uT[:, :, c0:c0 + cw], w=["uu"])
        p.op("dve", lambda e, cw=cw: e.tensor_tensor(out=ys[:, :, :cw], in0=yf[:, :, :cw], in1=yb[:, :, :cw], op=ALU.add), r=["yf", "yb"], w=["xt"])
        for k in range(ND):
            p.op("dve", lambda e, k=k, cw=cw: e.scalar_tensor_tensor(out=ys[:, k, :cw], in0=uu[:, k, :cw], scalar=vs_[:, 0, k:k + 1], in1=ys[:, k, :cw],
                                                                   op0=ALU.mult, op1=ALU.add), r=["uu", "xt", "vecs"], w=["xt"])
        p.op("pool", lambda e, cw=cw: e.tensor_tensor(out=yt[:, :, :cw], in0=ys[:, :, :cw], in1=ys[:, :, :cw], op=ALU.mult), r=["xt"], w=["sq"])
        p.op("dve", lambda e, cw=cw: e.tensor_scalar(out=yt[:, :, :cw], in0=yt[:, :, :cw], scalar1=0.044715, scalar2=1.0, op0=ALU.mult, op1=ALU.add), r=["sq"], w=["sq"])
        p.op("pool", lambda e, cw=cw: e.tensor_tensor(out=yt[:, :, :cw], in0=yt[:, :, :cw], in1=ys[:, :, :cw], op=ALU.mult), r=["sq", "xt"], w=["sq"])
        p.op("act", lambda e, cw=cw: e.activation(out=yt[:, :, :cw], in_=yt[:, :, :cw], func=AF.Sigmoid, scale=GC), r=["sq"], w=["sq"])
        p.op("dve", lambda e, c0=c0, cw=cw: e.tensor_tensor(out=a[:, :, c0:c0 + cw], in0=yt[:, :, :cw], in1=ys[:, :, :cw], op=ALU.mult), r=["sq", "xt"], w=["a"])
    wf = [[p.sb("wf%d%d" % (i, j), [128, ND, 128]) for j in range(2)] for i in range(2)]
    wb = [[p.sb("wb%d%d" % (i, j), [128, ND, 128], BF16) for j in range(2)] for i in range(2)]
    xb_ = [p.sb("xb%d" % i, [128, TB]) for i in range(2)]
    ob_ = [p.sb("ob%d" % i, [128, TB]) for i in range(2)]
    sg = p.sb("sg", [128, TB]); ol = p.sb("ol", [128, TB])
    pz1 = [p.ps("pza%d" % i, [128, TB]) for i in range(2)]
    pz2 = [p.ps("pzb0", [128, TB])] * 2
    qi = 0
    for blk in range(ND):
        s = blk % 2
        for j, src in enumerate((w1, w2)):
            p.dma("sync" if j else "pool", wf[s][j][:], src[:, :, blk * 128:(blk + 1) * 128], w=[("wf", s, j)])
            if j:
                p.op("act", lambda e, s=s, j=j: e.activation(out=wb[s][j][:], in_=wf[s][j][:], func=AF.Copy), r=[("wf", s, j)], w=[("wb", s, j)])
            else:
                p.op("pool", lambda e, s=s, j=j: e.tensor_copy(out=wb[s][j][:], in_=wf[s][j][:]), r=[("wf", s, j)], w=[("wb", s, j)])
        for (c0, cw) in tiles(TL, TB):
            q = qi % 2
            qi += 1
            p.dma("sync", xb_[q][:, :cw], xT[:, blk, c0:c0 + cw], w=[("xb", q)])
            for k in range(ND):
                p.op("pe", lambda e, q=q, k=k, s=s, c0=c0, cw=cw: e.matmul(pz1[q][:, :cw], lhsT=wb[s][0][:, k, :], rhs=a[:, k, c0:c0 + cw],
                                                                          start=(k == 0), stop=(k == ND - 1)), r=[("wb", s, 0), "a"], w=[("pza", q)])
            for k in range(ND):
                p.op("pe", lambda e, q=q, k=k, s=s, c0=c0, cw=cw: e.matmul(pz2[q][:, :cw], lhsT=wb[s][1][:, k, :], rhs=a[:, k, c0:c0 + cw],
                                                                          start=(k == 0), stop=(k == ND - 1)), r=[("wb", s, 1), "a"], w=[("pzb", 0)])
            p.op("act", lambda e, q=q, blk=blk, cw=cw: e.activation(out=sg[:, :cw], in_=pz2[q][:, :cw], func=AF.Sigmoid, bias=vs_[:, 2, blk:blk + 1], scale=1.0),
                 r=[("pzb", 0), "vecs"], w=["sg"])
            p.op("dve", lambda e, q=q, blk=blk, cw=cw: e.scalar_tensor_tensor(out=ol[:, :cw], in0=pz1[q][:, :cw], scalar=vs_[:, 1, blk:blk + 1], in1=sg[:, :cw],
                                                                            op0=ALU.add, op1=ALU.mult), r=[("pza", q), "sg", "vecs"], w=["ol"])
            p.op("dve", lambda e, q=q, blk=blk, cw=cw: e.scalar_tensor_tensor(out=ob_[q][:, :cw], in0=ol[:, :cw], scalar=vs_[:, 3, blk:blk + 1], in1=xb_[q][:, :cw],
                                                                            op0=ALU.mult, op1=ALU.add), r=["ol", ("xb", q), "vecs"], w=[("ob", q)])
            p.dma("pool", xlT[:, blk, c0:c0 + cw], ob_[q][:, :cw], r=[("ob", q)], w=[("xlo", blk, c0)], grp=("xlst", q))
    for (c0, cw) in tiles(TL, TK):
        tb0 = (c0 // TB) * TB
        p.dma("sync", C.xl[:, :, :cw], xlT[:, :, c0:c0 + cw], r=[("xlo", blk, tb0) for blk in range(ND)], w=["xl"])
        emit_norm_router(p, cfg, C, cw, lambda k: m2[:, k:k + 1], lambda k: vs_[:, 6, k:k + 1], tokT[:, :, c0:c0 + cw],
                         lambda t0, tw, c0=c0: gates[c0 + t0:c0 + t0 + tw, :])
    return p.build()


def lat_layout(cfg, lat):
    return [lat[c // cfg.CPB, (c % cfg.CPB) * cfg.TL:(c % cfg.CPB + 1) * cfg.TL] for c in range(cfg.NCORE)]


def lat_unlayout(cfg, per):
    out = np.zeros((cfg.B, cfg.L, per[0].shape[1]), per[0].dtype)
    for c in range(cfg.NCORE):
        out[c // cfg.CPB, (c % cfg.CPB) * cfg.TL:(c % cfg.CPB + 1) * cfg.TL] = per[c]
    return out


def run_glu(cfg, I, mods, yf, yb, u_lat, xl_lat):
    import ml_dtypes
    bf = ml_dtypes.bfloat16
    ND, NC = cfg.ND, cfg.NCORE
    nc = build_glu(cfg)
    wr, br = router_inputs(cfg, I, 1)
    w1 = wfm(I["s5_w1"][0], ND); w2 = wfm(I["s5_w2"][0], ND)
    yfs, ybs, us, xs = lat_layout(cfg, yf), lat_layout(cfg, yb), lat_layout(cfg, u_lat), lat_layout(cfg, xl_lat)
    ims = []
    for c in range(NC):
        mvd = mod_vecs(cfg, mods[1], c // cfg.CPB)
        vecs = np.stack([vfm(np.asarray(v, np.float32), ND) for v in (I["s5_d"][0], I["s5_b1"][0], I["s5_b2"][0], mvd["gt_a"], I["norm_g"][1, 1],
                                                                        mvd["sc_f"], mvd["sh_f"])], axis=1)
        ims.append({"yfT": fm(yfs[c], ND).astype(bf), "ybT": fm(ybs[c], ND).astype(bf), "uT": fm(us[c], ND).astype(bf),
                    "xT": fm(xs[c].astype(np.float32), ND), "w1": w1, "w2": w2, "vecsD": np.ascontiguousarray(vecs),
                    "wrD": wr, "brD": br, "identD": np.eye(128, dtype=np.float32)})
    res = run(nc, ims)
    xl = lat_unlayout(cfg, [unfm(r["xlT"]) for r in res])
    tok = lat_unlayout(cfg, [unfm(np.asarray(r["tokT"])) for r in res])
    gates = lat_unlayout(cfg, [r["gates"] for r in res])
    return xl, tok, gates


def forward(cfg, I, CH=16, PG=8, CAP=1536, log=None):
    import time
    t0 = time.time()

    def lg(msg):
        if log:
            print("[fwd %.1fs] %s" % (time.time() - t0, msg), flush=True)
    D = cfg.D
    mods = run_mods(cfg, I); lg("mods")
    filt = run_filt(cfg, I); lg("filt")
    (v_l, v_c), (x0_l, x0_c) = run_h1(cfg, I, mods); lg("h1")
    y_l, y_c = run_h2(cfg, I, v_l, v_c, filt); lg("h2")
    xl, tok, gates = run_h3(cfg, I, mods, y_l, y_c, x0_l, x0_c); lg("h3")
    tok_all = np.concatenate([tok[0].reshape(-1, D), tok[1].reshape(-1, D)], 0)
    gates_all = np.concatenate([gates[0].reshape(-1, 32), gates[1].reshape(-1, 32)], 0)
    y01, g01 = run_moe(cfg, I, 0, tok_all, gates_all, CAP); lg("moe0")
    xl_lat, xl_ctx, u_lat, u_ctx = run_comb(cfg, I, mods, 0, xl[0], xl[1], y01, g01, False); lg("comb0")
    prep = run_s5prep(cfg, I, CH); lg("s5prep")
    mats = s5_matrices(cfg, prep, CH); lg("s5mats")
    yf, yb = run_s5(cfg, mats, u_lat, u_ctx, CH, PG); lg("s5")
    xl3, tok1, gates1 = run_glu(cfg, I, mods, yf, yb, u_lat, xl_lat); lg("glu")
    y01, g01 = run_moe(cfg, I, 1, tok1.reshape(-1, D), gates1.reshape(-1, 32), CAP); lg("moe1")
    _, _, out, _ = run_comb(cfg, I, mods, 1, xl3, None, y01, g01, True); lg("final")
    return np.ascontiguousarray(out.astype(np.float32))


def kernel(**inputs):
    I = {k: np.asarray(v) for k, v in inputs.items()}
    return forward(FULL, I, CH=16, PG=8, CAP=None, log=True)
```

```python
import contextlib
import numpy as np
import concourse.bass as bass
import concourse.mybir as mybir
from concourse.bass_utils import run_bass_kernel_spmd

F32 = mybir.dt.float32
BF16 = mybir.dt.bfloat16
I32 = mybir.dt.int32
ALU = mybir.AluOpType
AF = mybir.ActivationFunctionType
AX = mybir.AxisListType

COMPUTE = ("pe", "act", "dve", "pool")
SKIP_SAME = set()


class Prog:
    def __init__(self):
        self.nc = bass.Bass("TRN2", target_bir_lowering=False)
        self.stack = contextlib.ExitStack()
        self.ops = []
        self.lastw = {}
        self.readers = {}
        self.out_names = []
        self.n_sb = 0
        self.barrier_idx = None

    def din(self, name, shape, dt=F32):
        return self.nc.dram_tensor(name, list(shape), dt, kind="ExternalInput").ap()

    def dout(self, name, shape, dt=F32):
        self.out_names.append(name)
        return self.nc.dram_tensor(name, list(shape), dt, kind="ExternalOutput").ap()

    def dtmp(self, name, shape, dt=F32):
        return self.nc.dram_tensor(name, list(shape), dt, kind="Internal").ap()

    def sb(self, name, shape, dt=F32):
        return self.stack.enter_context(self.nc.sbuf_tensor(name, list(shape), dt))

    def ps(self, name, shape, dt=F32):
        return self.stack.enter_context(self.nc.psum_tensor(name, list(shape), dt))

    def barrier(self):
        deps = set()
        last = {}
        for i, o in enumerate(self.ops):
            key = ("dma", o["grp"]) if o["dma"] else ("eng", o["eng"])
            last[key] = i
        deps = set(last.values())
        idx = len(self.ops)
        d = self.sb("bar%d" % idx, [128, 1])
        self.ops.append(dict(eng="dve", fn=lambda e: e.memset(d[:], 0.0), deps=deps, dma=False))
        self.barrier_idx = idx

    def _deps(self, r, w):
        deps = set()
        if self.barrier_idx is not None:
            deps.add(self.barrier_idx)
        for k in r:
            if k in self.lastw:
                deps.add(self.lastw[k])
        for k in w:
            if k in self.lastw:
                deps.add(self.lastw[k])
            for o in self.readers.get(k, ()):
                deps.add(o)
        return deps

    def _commit(self, idx, r, w):
        for k in r:
            self.readers.setdefault(k, []).append(idx)
        for k in w:
            self.lastw[k] = idx
            self.readers[k] = []

    def op(self, eng, fn, r=(), w=()):
        assert eng in COMPUTE
        idx = len(self.ops)
        deps = self._deps(r, w)
        self.ops.append(dict(eng=eng, fn=fn, deps=deps, dma=False))
        self._commit(idx, r, w)
        return idx

    def dma(self, q, out, in_, r=(), w=(), grp=None, **kw):
        idx = len(self.ops)
        deps = self._deps(r, w)
        if grp is None:
            grp = ("g", tuple(w)[0] if len(w) else tuple(r)[0])
        self.ops.append(dict(eng=q, fn=lambda e: e.dma_start(out=out, in_=in_, **kw),
                             deps=deps, dma=True, grp=grp))
        self._commit(idx, r, w)
        return idx

    def build(self):
        nc = self.nc
        ops = self.ops
        cnt = {}
        semkeys = []
        for o in ops:
            key = ("dma", o["grp"]) if o["dma"] else ("eng", o["eng"])
            if key not in cnt:
                cnt[key] = 0
                semkeys.append(key)
            cnt[key] += 16 if o["dma"] else 1
            o["sem"] = key
            o["val"] = cnt[key]
        sems = {}
        for i, key in enumerate(semkeys):
            sems[key] = self.stack.enter_context(nc.semaphore("s%d" % i))
        streams = {}
        for i, o in enumerate(ops):
            streams.setdefault(o["eng"], []).append(i)
        final_waits = [(sems[k], cnt[k]) for k in semkeys if k[0] == "dma"]
        blk = self.stack.enter_context(nc.Block())

        def emit_stream(eng_name, e, last=False):
            waited = {}
            for i in streams.get(eng_name, []):
                o = ops[i]
                need = {}
                for d in o["deps"]:
                    od = ops[d]
                    if od["eng"] == "pe" and o["eng"] == "pe" and not od["dma"] and not o["dma"]:
                        continue
                    if od["eng"] == o["eng"] and o["eng"] in SKIP_SAME and not od["dma"] and not o["dma"]:
                        continue
                    k = od["sem"]
                    need[k] = max(need.get(k, 0), od["val"])
                for k, v in need.items():
                    if waited.get(k, 0) < v:
                        e.wait_ge(sems[k], v)
                        waited[k] = v
                ins = o["fn"](e)
                ins.then_inc(sems[o["sem"]], 16 if o["dma"] else 1)
            if last:
                for s, v in final_waits:
                    e.wait_ge(s, v)

        @blk.tensor
        def _(e):
            emit_stream("pe", e)

        @blk.scalar
        def _(e):
            emit_stream("act", e)

        @blk.vector
        def _(e):
            emit_stream("dve", e)

        @blk.gpsimd
        def _(e):
            emit_stream("pool", e)

        @blk.sync
        def _(e):
            emit_stream("sync", e, last=True)

        self.stack.close()
        return nc


def run(prog_nc, in_maps, n=8):
    res = run_bass_kernel_spmd(prog_nc, in_maps, core_ids=list(range(n)))
    return res.results


class Cfg:
    def __init__(self, D=2048, B=4, L=4096, LC=256):
        self.D, self.B, self.L, self.LC = D, B, L, LC
        self.NCORE = 8
        self.ND = D // 128
        self.G = D // 16
        self.DE = D // 2
        self.CPB = self.NCORE // B
        self.TL = L // self.CPB
        self.TC = LC // self.CPB
        self.Cc = D // self.NCORE
        self.NE = 32
        self.EPC = self.NE // self.NCORE


FULL = Cfg()
EPS = 1e-6
MAGIC = 12582912.0
TWO_PI = 6.283185307179586
PI_LO = 3.1415925


def fm(a, ND):
    T, D = a.shape
    return np.ascontiguousarray(a.T.reshape(ND, 128, T).transpose(1, 0, 2))


def unfm(a):
    P, ND, T = a.shape
    return np.ascontiguousarray(a.transpose(1, 0, 2).reshape(ND * P, T).T)


def vfm(v, ND):
    return np.ascontiguousarray(v.reshape(ND, 128).T)


def tiles(n, t):
    return [(s, min(t, n - s)) for s in range(0, n, t)]


def build_mods(cfg):
    p = Prog()
    ND, NB1 = cfg.ND, cfg.B + 1
    W = 6 * cfg.D // cfg.NCORE
    ccT = p.din("ccT", [128, ND, NB1])
    aw = p.din("aw", [2, 128, ND, W])
    ab = p.din("ab", [2, 1, W])
    out = p.dout("mods", [2, NB1, W])
    cs = p.sb("cs", [128, ND, 128])
    p.op("dve", lambda e: e.memset(cs[:], 0.0), w=["cs"])
    p.dma("sync", cs[:, :, :NB1], ccT[:, :, :], w=["cs"])
    p.op("act", lambda e: e.activation(out=cs[:, :, :NB1], in_=cs[:, :, :NB1], func=AF.Silu), r=["cs"], w=["cs"])
    WT = 512
    wt_sb = [p.sb("wt%d" % i, [128, ND, WT]) for i in range(2)]
    bt = [p.sb("bt%d" % i, [NB1, WT]) for i in range(2)]
    ot = [p.sb("ot%d" % i, [NB1, WT]) for i in range(2)]
    pm = [p.ps("pm%d" % i, [128, WT]) for i in range(2)]
    it = 0
    for l in range(2):
        for (c0, cw) in tiles(W, WT):
            s = it % 2
            it += 1
            p.dma("sync", wt_sb[s][:, :, :cw], aw[l, :, :, c0:c0 + cw], w=[("wt", s)])
            p.dma("pool", bt[s][:, :cw], ab[l, 0:1, c0:c0 + cw].broadcast_to([NB1, cw]), w=[("bt", s)])
            for k in range(ND):
                p.op("pe", lambda e, s=s, k=k, cw=cw: e.matmul(pm[s][:, :cw], lhsT=cs[:, k, :], rhs=wt_sb[s][:, k, :cw],
                                                               start=(k == 0), stop=(k == ND - 1)),
                     r=["cs", ("wt", s)], w=[("pm", s)])
            p.op("dve", lambda e, s=s, cw=cw: e.tensor_tensor(out=ot[s][:, :cw], in0=pm[s][:NB1, :cw], in1=bt[s][:, :cw], op=ALU.add),
                 r=[("pm", s), ("bt", s)], w=[("ot", s)])
            p.dma("sync", out[l, :, c0:c0 + cw], ot[s][:, :cw], r=[("ot", s)], w=[("out", l, c0)], grp=("st", s))
    return p.build()


def run_mods(cfg, I):
    ND, NC = cfg.ND, cfg.NCORE
    W = 6 * cfg.D // NC
    cc = np.concatenate([I["c"], I["c_ctx"][None]], 0).astype(np.float32)
    ccT = fm(cc, ND)
    nc = build_mods(cfg)
    ims = []
    for c in range(NC):
        aw = I["ada_w"][:, :, c * W:(c + 1) * W]
        aw = np.ascontiguousarray(aw.reshape(2, ND, 128, W).transpose(0, 2, 1, 3))
        ab = np.ascontiguousarray(I["ada_b"][:, None, c * W:(c + 1) * W])
        ims.append({"ccT": ccT, "aw": aw, "ab": ab})
    res = run(nc, ims)
    mods = np.concatenate([r["mods"] for r in res], axis=2)
    return mods


def emit_rstd(p, cfg, xt, xkey, ncols, ones, sq, sqkey, pss, psskey, rstd, rkey):
    ND = cfg.ND
    p.op("pool", lambda e: e.tensor_tensor(out=sq[:, :, :ncols], in0=xt[:, :, :ncols], in1=xt[:, :, :ncols], op=ALU.mult),
         r=[xkey], w=[sqkey])
    for k in range(ND):
        p.op("pe", lambda e, k=k: e.matmul(pss[:, :ncols], lhsT=ones[:], rhs=sq[:, k, :ncols], start=(k == 0), stop=(k == ND - 1)),
             r=[sqkey, "ones"], w=[psskey])
    p.op("act", lambda e: e.activation(out=rstd[:, :ncols], in_=pss[:, :ncols], func=AF.Sqrt, bias=EPS, scale=1.0 / cfg.D),
         r=[psskey], w=[rkey])
    p.op("dve", lambda e: e.reciprocal(out=rstd[:, :ncols], in_=rstd[:, :ncols]), r=[rkey], w=[rkey])


def build_h1(cfg):
    p = Prog()
    ND, D, TL, TC = cfg.ND, cfg.D, cfg.TL, cfg.TC
    TT = TL + TC + 4
    NT = TL + TC
    xT = p.din("xT", [128, ND, TT])
    hm = p.din("hm", [128, 4])
    mv = p.din("mv", [128, 5, ND])
    w_in = p.din("w_in", [128, ND, 3 * D])
    fv = p.din("fv", [128, 5, 3 * ND])
    vT = p.dout("vT", [128, ND, NT], BF16)
    x0T = p.dout("x0T", [128, ND, NT], BF16)

    ones = p.sb("ones", [128, 128])
    p.op("dve", lambda e: e.memset(ones[:], 1.0), w=["ones"])
    hms = p.sb("hms", [128, 4]); p.dma("sync", hms[:], hm[:, :], w=["hms"])
    mvs = p.sb("mvs", [128, 5, ND]); p.dma("sync", mvs[:], mv[:, :, :], w=["mvs"])
    fvs = p.sb("fvs", [128, 5, 3 * ND]); p.dma("sync", fvs[:], fv[:, :, :], w=["fvs"])
    ml = p.sb("ml", [128, 2, ND])
    for i, j in ((0, 1), (1, 3)):
        p.op("dve", lambda e, i=i, j=j: e.scalar_tensor_tensor(out=ml[:, i, :], in0=mvs[:, j, :], scalar=1.0, in1=mvs[:, 0, :],
                                                               op0=ALU.add, op1=ALU.mult), r=["mvs"], w=["ml"])
    u = p.sb("u", [128, ND, TT], BF16)
    TK = 256
    xt = p.sb("xt", [128, ND, TK]); sq = p.sb("sq", [128, ND, TK]); rstd = p.sb("rstd", [128, TK])
    pss = p.ps("pss", [128, TK])
    segs = [(0, TL + 2, 0), (TL + 2, TC + 2, 1)]
    for (s0, sl, mi) in segs:
        for (c0, cw) in tiles(sl, TK):
            a = s0 + c0
            p.dma("sync", xt[:, :, :cw], xT[:, :, a:a + cw], w=["xt"])
            emit_rstd(p, cfg, xt, "xt", cw, ones, sq, "sq", pss, "pss", rstd, "rstd")
            for k in range(ND):
                p.op("dve", lambda e, k=k, cw=cw: e.tensor_tensor(out=sq[:, k, :cw], in0=xt[:, k, :cw], in1=rstd[:, :cw], op=ALU.mult),
                     r=["xt", "rstd"], w=["sq"])
                p.op("dve", lambda e, k=k, cw=cw, a=a, mi=mi: e.tensor_scalar(
                    out=u[:, k, a:a + cw], in0=sq[:, k, :cw], scalar1=ml[:, mi, k:k + 1], scalar2=mvs[:, 2 + 2 * mi, k:k + 1],
                    op0=ALU.mult, op1=ALU.add), r=["sq", "ml", "mvs"], w=["u"])
    wf = [p.sb("wf%d" % i, [128, ND, 128]) for i in range(2)]
    wb = [p.sb("wb%d" % i, [128, ND, 128], BF16) for i in range(2)]
    z = p.sb("z", [128, TT])
    zc = [p.sb("zc%d" % i, [128, TT]) for i in range(3)]
    ob = [p.sb("ob%d" % i, [128, NT], BF16) for i in range(2)]
    pz = [p.ps("pz%d" % i, [128, 512]) for i in range(2)]
    wi = 0
    pi = 0
    for c in range(ND):
        for which, blk in ((1, ND + c), (2, 2 * ND + c), (0, c)):
            s = wi % 2
            wi += 1
            p.dma("sync", wf[s][:], w_in[:, :, blk * 128:(blk + 1) * 128], w=[("wf", s)])
            p.op("pool", lambda e, s=s: e.tensor_copy(out=wb[s][:], in_=wf[s][:]), r=[("wf", s)], w=[("wb", s)])
            for (c0, cw) in tiles(TT, 512):
                q = pi % 2
                pi += 1
                for k in range(ND):
                    p.op("pe", lambda e, s=s, q=q, k=k, c0=c0, cw=cw: e.matmul(
                        pz[q][:, :cw], lhsT=wb[s][:, k, :], rhs=u[:, k, c0:c0 + cw], start=(k == 0), stop=(k == ND - 1)),
                        r=[("wb", s), "u"], w=[("pz", q)])
                p.op("act", lambda e, q=q, c0=c0, cw=cw, blk=blk: e.activation(
                    out=z[:, c0:c0 + cw], in_=pz[q][:, :cw], func=AF.Identity, bias=fvs[:, 0, blk:blk + 1], scale=1.0),
                    r=[("pz", q), "fvs"], w=["z"])
            for hi, col in enumerate((0, TL + 1, TL + 2, TL + TC + 3)):
                p.op("dve", lambda e, hi=hi, col=col: e.tensor_scalar(out=z[:, col:col + 1], in0=z[:, col:col + 1],
                                                                     scalar1=hms[:, hi:hi + 1], scalar2=None, op0=ALU.mult),
                     r=["z", "hms"], w=["z"])
            zo = zc[which]
            zk = ("zc", which)
            for (a, n) in ((1, TL), (TL + 3, TC)):
                p.op("dve", lambda e, a=a, n=n, blk=blk, zo=zo: e.tensor_scalar(
                    out=zo[:, a:a + n], in0=z[:, a - 1:a - 1 + n], scalar1=fvs[:, 1, blk:blk + 1], scalar2=fvs[:, 4, blk:blk + 1],
                    op0=ALU.mult, op1=ALU.add), r=["z", "fvs"], w=[zk])
                p.op("dve", lambda e, a=a, n=n, blk=blk, zo=zo: e.scalar_tensor_tensor(
                    out=zo[:, a:a + n], in0=z[:, a:a + n], scalar=fvs[:, 2, blk:blk + 1], in1=zo[:, a:a + n],
                    op0=ALU.mult, op1=ALU.add), r=["z", "fvs", zk], w=[zk])
                p.op("dve", lambda e, a=a, n=n, blk=blk, zo=zo: e.scalar_tensor_tensor(
                    out=zo[:, a:a + n], in0=z[:, a + 1:a + 1 + n], scalar=fvs[:, 3, blk:blk + 1], in1=zo[:, a:a + n],
                    op0=ALU.mult, op1=ALU.add), r=["z", "fvs", zk], w=[zk])
            if which == 2:
                for (a, n, o0) in ((1, TL, 0), (TL + 3, TC, TL)):
                    p.op("pool", lambda e, a=a, n=n, o0=o0: e.tensor_tensor(out=ob[0][:, o0:o0 + n], in0=zc[2][:, a:a + n],
                                                                           in1=zc[1][:, a:a + n], op=ALU.mult),
                         r=[("zc", 1), ("zc", 2)], w=[("ob", 0)])
                p.dma("pool", vT[:, c, :], ob[0][:], r=[("ob", 0)], w=[("vT", c)], grp=("st", 0))
            if which == 0:
                for (a, n, o0) in ((1, TL, 0), (TL + 3, TC, TL)):
                    p.op("pool", lambda e, a=a, n=n, o0=o0: e.tensor_copy(out=ob[1][:, o0:o0 + n], in_=zc[0][:, a:a + n]),
                         r=[("zc", 0)], w=[("ob", 1)])
                p.dma("pool", x0T[:, c, :], ob[1][:], r=[("ob", 1)], w=[("x0T", c)], grp=("st", 1))
    return p.build()


def tok_layout(cfg, lat, ctx):
    outs = []
    for c in range(cfg.NCORE):
        b, h = c // cfg.CPB, c % cfg.CPB
        outs.append(np.concatenate([lat[b, h * cfg.TL:(h + 1) * cfg.TL], ctx[b, h * cfg.TC:(h + 1) * cfg.TC]], 0))
    return outs


def tok_unlayout(cfg, per_core):
    Dd = per_core[0].shape[1]
    lat = np.zeros((cfg.B, cfg.L, Dd), per_core[0].dtype)
    ctx = np.zeros((cfg.B, cfg.LC, Dd), per_core[0].dtype)
    for c in range(cfg.NCORE):
        b, h = c // cfg.CPB, c % cfg.CPB
        lat[b, h * cfg.TL:(h + 1) * cfg.TL] = per_core[c][:cfg.TL]
        ctx[b, h * cfg.TC:(h + 1) * cfg.TC] = per_core[c][cfg.TL:]
    return lat, ctx


def mod_vecs(cfg, mods_l, b):
    D = cfg.D
    names = ["sh_a", "sc_a", "gt_a", "sh_f", "sc_f", "gt_f"]
    out = {}
    for i, n in enumerate(names):
        out[n] = mods_l[b, i * D:(i + 1) * D]
        out["c" + n] = mods_l[cfg.B, i * D:(i + 1) * D]
    return out


def run_h1(cfg, I, mods):
    ND, D, TL, TC, NC = cfg.ND, cfg.D, cfg.TL, cfg.TC, cfg.NCORE
    nc = build_h1(cfg)
    x, ctx = I["x"], I["ctx"]
    w_in = np.ascontiguousarray(I["hy_w_in"][0].reshape(ND, 128, 3 * D).transpose(1, 0, 2))
    fvec = np.stack([vfm(v, 3 * ND) for v in (I["hy_b_in"][0], I["hy_conv_w"][0, 0], I["hy_conv_w"][0, 1],
                                               I["hy_conv_w"][0, 2], I["hy_conv_b"][0])], axis=1)
    fvec = np.ascontiguousarray(fvec)
    ims = []
    zrow = np.zeros((1, D), np.float32)
    for c in range(NC):
        b, h = c // cfg.CPB, c % cfg.CPB
        l0, l1 = h * TL, (h + 1) * TL
        c0, c1 = h * TC, (h + 1) * TC
        hl = x[b, l0 - 1:l0] if l0 > 0 else zrow
        hr = x[b, l1:l1 + 1] if l1 < cfg.L else zrow
        chl = ctx[b, c0 - 1:c0] if c0 > 0 else zrow
        chr_ = ctx[b, c1:c1 + 1] if c1 < cfg.LC else zrow
        cols = np.concatenate([hl, x[b, l0:l1], hr, chl, ctx[b, c0:c1], chr_], 0)
        hm = np.array([l0 > 0, l1 < cfg.L, c0 > 0, c1 < cfg.LC], np.float32)
        mvd = mod_vecs(cfg, mods[0], b)
        mv = np.stack([vfm(v, ND) for v in (I["norm_g"][0, 0], mvd["sc_a"], mvd["sh_a"], mvd["csc_a"], mvd["csh_a"])], axis=1)
        ims.append({"xT": fm(cols, ND), "hm": np.ascontiguousarray(np.broadcast_to(hm, (128, 4))),
                    "mv": np.ascontiguousarray(mv), "w_in": w_in, "fv": fvec})
    res = run(nc, ims)
    v = [unfm(np.asarray(r["vT"]).astype(np.float32)) for r in res]
    x0 = [unfm(np.asarray(r["x0T"]).astype(np.float32)) for r in res]
    return tok_unlayout(cfg, v), tok_unlayout(cfg, x0)


def hy_consts(Lx, D):
    f32 = np.float32
    t = np.linspace(0.0, 1.0, Lx, dtype=f32)[:, None]
    w = (f32(2.0 * np.pi / Lx) * np.arange(Lx, dtype=f32))[:, None]
    bands = np.linspace(1e-4, 15, 16, dtype=f32)[None, :]
    z = np.concatenate([t, np.cos(bands * w), -np.sin(bands * w)], axis=-1).astype(f32)
    max_decay = np.log(1e-2) / 0.3
    min_decay = np.log(1e-2) / 1.5
    deltas = np.abs(np.linspace(min_decay, max_decay, D, dtype=f32))
    win = np.exp(-t * deltas[None, :]).astype(f32)
    return z, win


def emit_sin(p, e_out, okey, src, skey, ncols, tmp, tkey, scale_ap, bias_ap, extra_r=()):
    t0, t1 = tmp
    p.op("dve", lambda e: e.tensor_scalar(out=t0[:, :ncols], in0=src, scalar1=scale_ap, scalar2=bias_ap, op0=ALU.mult, op1=ALU.add),
         r=[skey] + list(extra_r), w=[(tkey, 0)])
    p.op("dve", lambda e: e.tensor_scalar(out=t1[:, :ncols], in0=t0[:, :ncols], scalar1=1.0 / TWO_PI, scalar2=MAGIC, op0=ALU.mult, op1=ALU.add),
         r=[(tkey, 0)], w=[(tkey, 1)])
    p.op("dve", lambda e: e.tensor_scalar(out=t1[:, :ncols], in0=t1[:, :ncols], scalar1=MAGIC, scalar2=-TWO_PI, op0=ALU.subtract, op1=ALU.mult),
         r=[(tkey, 1)], w=[(tkey, 1)])
    p.op("dve", lambda e: e.tensor_tensor(out=t0[:, :ncols], in0=t0[:, :ncols], in1=t1[:, :ncols], op=ALU.add),
         r=[(tkey, 0), (tkey, 1)], w=[(tkey, 0)])
    p.op("dve", lambda e: e.tensor_scalar(out=t0[:, :ncols], in0=t0[:, :ncols], scalar1=PI_LO, scalar2=-PI_LO, op0=ALU.min, op1=ALU.max),
         r=[(tkey, 0)], w=[(tkey, 0)])
    p.op("act", lambda e: e.activation(out=e_out, in_=t0[:, :ncols], func=AF.Sin), r=[(tkey, 0)], w=[okey])


def build_filt(cfg):
    p = Prog()
    Cc = cfg.Cc
    CP = min(Cc, 128)
    NCH = Cc // CP
    fw1 = p.din("fw1", [128, 128]); fw2 = p.din("fw2", [128, 128]); fw3 = p.din("fw3", [128, 2, NCH, 128])
    pv = p.din("pv", [128, 3])
    w1s = p.sb("w1s", [128, 128]); w2s = p.sb("w2s", [128, 128]); w3s = p.sb("w3s", [128, 2, NCH, 128]); pvs = p.sb("pvs", [128, 3])
    p.dma("sync", w1s[:], fw1[:, :], w=["w1s"]); p.dma("sync", w2s[:], fw2[:, :], w=["w2s"])
    p.dma("sync", w3s[:], fw3[:, :, :, :], w=["w3s"]); p.dma("sync", pvs[:], pv[:, :], w=["pvs"])
    fb = p.sb("fb", [128, 2])
    p.op("dve", lambda e: e.tensor_scalar(out=fb[:, 0:2], in0=pvs[:, 0:2], scalar1=pvs[:, 2:3], scalar2=None, op0=ALU.mult),
         r=["pvs"], w=["fb"])
    Lmax = max(cfg.L, cfg.LC)
    CT = 512
    zt = p.sb("zt", [128, CT]); h1 = p.sb("h1", [128, CT]); h2 = p.sb("h2", [128, Lmax])
    tmp = [p.sb("tmpa", [128, CT]), p.sb("tmpb", [128, CT])]
    hw = [p.sb("hw%d" % i, [128, Lmax]) for i in range(2)]
    wn = p.sb("wn", [128, Lmax])
    ab = p.sb("ab", [128, Lmax]); nr = p.sb("nr", [128, 2]); rn = p.sb("rn", [128, 1])
    ho = [p.sb("ho%d" % i, [128, Lmax], BF16) for i in range(2)]
    pa = p.ps("pa", [128, CT]); pb = p.ps("pb", [128, CT]); pc = p.ps("pc", [128, CT])
    for li, Lx in enumerate((cfg.L, cfg.LC)):
        zT = p.din("zT%d" % li, [128, Lx]); winT = p.din("winT%d" % li, [CP, NCH, Lx])
        hsT = p.dout("hsT%d" % li, [CP, NCH, Lx], BF16); hdT = p.dout("hdT%d" % li, [CP, NCH, Lx], BF16)
        for (c0, cw) in tiles(Lx, CT):
            p.dma("sync", zt[:, :cw], zT[:, c0:c0 + cw], w=["zt"])
            p.op("pe", lambda e, cw=cw: e.matmul(pa[:, :cw], lhsT=w1s[:], rhs=zt[:, :cw], start=True, stop=True), r=["w1s", "zt"], w=["pa"])
            emit_sin(p, h1[:, :cw], "h1", pa[:, :cw], "pa", cw, tmp, "tmp", pvs[:, 2:3], fb[:, 0:1], extra_r=["pvs", "fb"])
            p.op("pe", lambda e, cw=cw: e.matmul(pb[:, :cw], lhsT=w2s[:], rhs=h1[:, :cw], start=True, stop=True), r=["w2s", "h1"], w=["pb"])
            emit_sin(p, h2[:, c0:c0 + cw], "h2", pb[:, :cw], "pb", cw, tmp, "tmp", pvs[:, 2:3], fb[:, 1:2], extra_r=["pvs", "fb"])
        for ch in range(NCH):
            p.dma("sync", wn[:CP, :Lx], winT[:, ch, :], w=["wn"])
            for d in range(2):
                for (c0, cw) in tiles(Lx, CT):
                    p.op("pe", lambda e, d=d, ch=ch, c0=c0, cw=cw: e.matmul(pc[:, :cw], lhsT=w3s[:, d, ch, :], rhs=h2[:, c0:c0 + cw], start=True, stop=True),
                         r=["w3s", "h2"], w=["pc"])
                    p.op("dve", lambda e, d=d, c0=c0, cw=cw: e.tensor_tensor(out=hw[d][:CP, c0:c0 + cw], in0=pc[:CP, :cw], in1=wn[:CP, c0:c0 + cw], op=ALU.mult),
                         r=["pc", "wn"], w=[("hw", d)])
            p.op("dve", lambda e: e.memset(hw[1][:CP, 0:1], 0.0), r=[("hw", 1)], w=[("hw", 1)])
            for d in range(2):
                p.op("dve", lambda e, d=d, Lx=Lx: e.scalar_tensor_tensor(out=ab[:CP, :Lx], in0=hw[d][:CP, :Lx], scalar=-1.0, in1=hw[d][:CP, :Lx], op0=ALU.mult, op1=ALU.max),
                     r=[("hw", d)], w=["ab"])
                p.op("dve", lambda e, d=d, Lx=Lx: e.tensor_reduce(out=nr[:CP, d:d + 1], in_=ab[:CP, :Lx], axis=AX.X, op=ALU.add),
                     r=["ab"], w=["nr"])
            p.op("dve", lambda e: e.tensor_tensor(out=rn[:CP, :], in0=nr[:CP, 0:1], in1=nr[:CP, 1:2], op=ALU.add), r=["nr"], w=["rn"])
            p.op("dve", lambda e: e.reciprocal(out=rn[:CP, :], in_=rn[:CP, :]), r=["rn"], w=["rn"])
            p.op("dve", lambda e, Lx=Lx: e.tensor_tensor(out=ab[:CP, :Lx], in0=hw[0][:CP, :Lx], in1=hw[1][:CP, :Lx], op=ALU.add),
                 r=[("hw", 0), ("hw", 1)], w=["ab"])
            p.op("dve", lambda e, Lx=Lx: e.tensor_scalar(out=ho[0][:CP, :Lx], in0=ab[:CP, :Lx], scalar1=rn[:CP, 0:1], scalar2=None, op0=ALU.mult),
                 r=["ab", "rn"], w=[("ho", 0)])
            p.op("dve", lambda e, Lx=Lx: e.tensor_tensor(out=ab[:CP, :Lx], in0=hw[0][:CP, :Lx], in1=hw[1][:CP, :Lx], op=ALU.subtract),
                 r=[("hw", 0), ("hw", 1), ("ho", 0)], w=["ab"])
            p.op("dve", lambda e, Lx=Lx: e.tensor_scalar(out=ho[1][:CP, :Lx], in0=ab[:CP, :Lx], scalar1=rn[:CP, 0:1], scalar2=None, op0=ALU.mult),
                 r=["ab", "rn"], w=[("ho", 1)])
            p.dma("pool", hsT[:, ch, :], ho[0][:CP, :Lx], r=[("ho", 0)], w=[("hs", li, ch)], grp=("st", 0))
            p.dma("pool", hdT[:, ch, :], ho[1][:CP, :Lx], r=[("ho", 1)], w=[("hd", li, ch)], grp=("st", 1))
    return p.build()


def pad128(a):
    out = np.zeros((128,) + a.shape[1:], np.float32)
    out[:a.shape[0]] = a
    return out


def run_filt(cfg, I):
    Cc, D, NC = cfg.Cc, cfg.D, cfg.NCORE
    CP = min(Cc, 128); NCH = Cc // CP
    nc = build_filt(cfg)
    fw1 = np.zeros((128, 128), np.float32); fw1[:33, :64] = I["hy_fw1"][0]
    fw2 = np.zeros((128, 128), np.float32); fw2[:64, :64] = I["hy_fw2"][0]
    pv = np.zeros((128, 3), np.float32)
    pv[:64, 0] = I["hy_fb1"][0]; pv[:64, 1] = I["hy_fb2"][0]; pv[:64, 2] = I["hy_freq"][0]
    consts = [hy_consts(Lx, D) for Lx in (cfg.L, cfg.LC)]
    ims = []
    for c in range(NC):
        fw3 = np.zeros((128, 2, NCH, 128), np.float32)
        for d in range(2):
            blk = I["hy_fw3"][0][:, d * D + c * Cc: d * D + (c + 1) * Cc]
            fw3[:64, d, :, :CP] = blk.reshape(64, NCH, CP)
        m = {"fw1": fw1, "fw2": fw2, "fw3": fw3, "pv": pv}
        for li, (z, win) in enumerate(consts):
            m["zT%d" % li] = pad128(np.ascontiguousarray(z.T))
            wc = win[:, c * Cc:(c + 1) * Cc].T
            m["winT%d" % li] = np.ascontiguousarray(wc.reshape(NCH, CP, -1).transpose(1, 0, 2))
        ims.append(m)
    res = run(nc, ims)
    outs = []
    for li, Lx in enumerate((cfg.L, cfg.LC)):
        hs = np.zeros((Lx, D), np.float32); hd = np.zeros((Lx, D), np.float32)
        for c in range(NC):
            a = np.asarray(res[c]["hsT%d" % li]).astype(np.float32).transpose(1, 0, 2).reshape(Cc, Lx)
            b = np.asarray(res[c]["hdT%d" % li]).astype(np.float32).transpose(1, 0, 2).reshape(Cc, Lx)
            hs[:, c * Cc:(c + 1) * Cc] = a.T
            hd[:, c * Cc:(c + 1) * Cc] = b.T
        outs.append((hs, hd))
    return outs


def dft_tables(Lx):
    NS = Lx // 128
    M = 4 * Lx
    p = np.arange(128)[:, None, None]
    i = np.arange(NS)[None, :, None]
    q = np.arange(128)[None, None, :]

    def cis(k):
        ang = -2.0 * np.pi * (np.asarray(k, np.int64) % M).astype(np.float64) / M
        return np.stack([np.cos(ang), np.sin(ang)]).astype(np.float32)

    B2 = cis((2 * q + 1) * (128 * i + p))
    j = np.arange(NS)[None, :, None]
    ii = np.arange(NS)[None, None, :]
    A2 = cis(256 * j * (128 * ii + np.arange(128)[:, None, None]))
    qq = np.arange(128)[:, None, None]
    jj = np.arange(NS)[None, :, None]
    pp = np.arange(128)[None, None, :]
    B3 = cis((2 * (128 * jj + qq) + 1) * pp)
    i3 = np.arange(NS)[None, :, None]
    j3 = np.arange(NS)[None, None, :]
    A3 = cis((2 * (128 * j3 + qq) + 1) * 128 * i3)
    return [np.ascontiguousarray(t) for t in (A2, B2, A3, B3)]


def build_h2(cfg):
    p = Prog()
    B, Cc = cfg.B, cfg.Cc
    ncol = B * Cc
    CW = min(getattr(cfg, 'H2_CW', 512), ncol)
    nbp = CW // Cc
    NSmax = max(cfg.L, cfg.LC) // 128
    RAWN = 4 * NSmax * 128
    raw = p.sb("raw", [128, RAWN])
    es = [[p.sb("es%d%d" % (a, b), [128, NSmax, 128], BF16) for b in range(2)] for a in range(2)]
    a_s = p.sb("a_s", [128, 2, NSmax, NSmax])
    vs = p.sb("vs", [128, NSmax, CW], BF16)
    hsd = p.sb("hsd", [128, 2, NSmax, Cc], BF16)
    skb = p.sb("skb", [128, ncol])
    kk = p.sb("kk", [128, 2, Cc])
    tt = [p.sb("tt%d" % i, [128, CW]) for i in range(3)]
    yo = p.sb("yo", [128, CW], BF16)
    pV = [p.ps("pV%d" % i, [128, 512]) for i in range(2)]
    pK = [p.ps("pK%d" % i, [128, 512]) for i in range(2)]
    pY = p.ps("pY", [128, 512])
    def conv_li(li, Lx, skip):
        NS = Lx // 128
        NF = NS
        Mv = NS * 128
        v = p.din("v%d" % li, [128, NS, ncol], BF16)
        hsdi = p.din("hsd%d" % li, [128, 2, NS, Cc], BF16)
        A2 = p.din("A2_%d" % li, [2, 128, NF, NS]); B2 = p.din("B2_%d" % li, [2, 128, NS, 128])
        A3 = p.din("A3_%d" % li, [2, 128, NS, NF]); B3 = p.din("B3_%d" % li, [2, 128, NF, 128])
        if li == 0:
            skip = p.din("skip0", [128, ncol])
        y = p.dout("y%d" % li, [128, NS, ncol], BF16)
        Es = p.dtmp("Es%d" % li, [NF, 2, 128, NS, 128], BF16)
        Gs = p.dtmp("Gs%d" % li, [NS, 2, 128, NF, 128], BF16)
        Ks = p.dtmp("Ks%d" % li, [NF, 128, 2, Cc])
        p.barrier()
        bre = raw[:, 0:Mv].rearrange("p (i q) -> p i q", q=128)
        bim = raw[:, Mv:2 * Mv].rearrange("p (i q) -> p i q", q=128)
        t1 = raw[:, 2 * Mv:3 * Mv].rearrange("p (i q) -> p i q", q=128)
        t2 = raw[:, 3 * Mv:4 * Mv].rearrange("p (i q) -> p i q", q=128)
        for (At, Bt, dst, nout) in ((A2, B2, Es, NF), (A3, B3, Gs, NS)):
            p.dma("sync", raw[:, 0:Mv], Bt[0].rearrange("p i q -> p (i q)"), w=["bre"])
            p.dma("sync", raw[:, Mv:2 * Mv], Bt[1].rearrange("p i q -> p (i q)"), w=["bim"])
            p.dma("sync", a_s[:, 0, :nout, :NS], At[0], w=["a_s0"])
            p.dma("sync", a_s[:, 1, :nout, :NS], At[1], w=["a_s1"])
            for j in range(nout):
                s = j % 2
                are = a_s[:, 0, j, :NS].unsqueeze(2).broadcast_to([128, NS, 128])
                aim = a_s[:, 1, j, :NS].unsqueeze(2).broadcast_to([128, NS, 128])
                ere = es[s][0][:, :NS, :]
                eim = es[s][1][:, :NS, :]
                p.op("dve", lambda e, are=are: e.tensor_tensor(out=t1, in0=bre, in1=are, op=ALU.mult), r=["bre", "a_s0"], w=["t1"])
                p.op("pool", lambda e, aim=aim: e.tensor_tensor(out=t2, in0=bim, in1=aim, op=ALU.mult), r=["bim", "a_s1"], w=["t2"])
                p.op("dve", lambda e, ere=ere: e.tensor_tensor(out=ere, in0=t1, in1=t2, op=ALU.subtract), r=["t1", "t2"], w=[("es", s, 0)])
                p.op("dve", lambda e, are=are: e.tensor_tensor(out=t1, in0=bim, in1=are, op=ALU.mult), r=["bim", "a_s0"], w=["t1"])
                p.op("pool", lambda e, aim=aim: e.tensor_tensor(out=t2, in0=bre, in1=aim, op=ALU.mult), r=["bre", "a_s1"], w=["t2"])
                p.op("dve", lambda e, eim=eim: e.tensor_tensor(out=eim, in0=t1, in1=t2, op=ALU.add), r=["t1", "t2"], w=[("es", s, 1)])
                p.dma("sync", dst[j, 0], ere, r=[("es", s, 0)], w=[("tab", li, j, 0)], grp=("tst", s, 0))
                p.dma("sync", dst[j, 1], eim, r=[("es", s, 1)], w=[("tab", li, j, 1)], grp=("tst", s, 1))
        p.barrier()
        Yv = raw[:, 0:NF * CW].bitcast(BF16).rearrange("p (c j w) -> p c j w", c=2, j=NF)
        p.dma("sync", hsd[:, :, :NS, :], hsdi[:, :, :, :], w=["hsd"])
        if li == 0:
            p.dma("sync", skb[:], skip[:, :], w=["skb"])
        for c0 in range(0, ncol, CW):
            p.dma("sync", vs[:, :NS, :], v[:, :, c0:c0 + CW], w=["vs"])
            for j in range(NF):
                s = j % 2
                for c in range(2):
                    p.dma("pool" if c else "sync", es[s][c][:, :NS, :], Es[j, c], w=[("es", s, c)])
                first_pass = (c0 == 0)
                for i in range(NS):
                    fl = dict(start=(i == 0), stop=(i == NS - 1))
                    p.op("pe", lambda e, s=s, i=i, fl=fl: e.matmul(pV[0][:, :CW], lhsT=es[s][0][:, i, :], rhs=vs[:, i, :], **fl),
                         r=[("es", s, 0), "vs"], w=["pV0"])
                    p.op("pe", lambda e, s=s, i=i, fl=fl: e.matmul(pV[1][:, :CW], lhsT=es[s][1][:, i, :], rhs=vs[:, i, :], **fl),
                         r=[("es", s, 1), "vs"], w=["pV1"])
                    if first_pass:
                        p.op("pe", lambda e, s=s, i=i, fl=fl: e.matmul(pK[0][:, :Cc], lhsT=es[s][0][:, i, :], rhs=hsd[:, 0, i, :], **fl),
                             r=[("es", s, 0), "hsd"], w=["pK0"])
                        p.op("pe", lambda e, s=s, i=i, fl=fl: e.matmul(pK[1][:, :Cc], lhsT=es[s][1][:, i, :], rhs=hsd[:, 1, i, :], **fl),
                             r=[("es", s, 1), "hsd"], w=["pK1"])
                if first_pass:
                    for c in range(2):
                        p.op("act", lambda e, c=c: e.activation(out=kk[:, c, :], in_=pK[c][:, :Cc], func=AF.Copy), r=["pK%d" % c], w=[("kk", c)])
                    if ncol > CW:
                        p.dma("pool", Ks[j], kk[:, :, :], r=[("kk", 0), ("kk", 1)], w=[("Ks", li, j)], grp=("kst",))
                else:
                    p.dma("pool", kk[:, :, :], Ks[j], r=[("Ks", li, j)], w=[("kk", 0), ("kk", 1)], grp=("kld",))
                kre = kk[:, 0, :].unsqueeze(1).broadcast_to([128, nbp, Cc])
                kim = kk[:, 1, :].unsqueeze(1).broadcast_to([128, nbp, Cc])
                vre = pV[0][:, :CW].rearrange("p (b c) -> p b c", c=Cc)
                vim = pV[1][:, :CW].rearrange("p (b c) -> p b c", c=Cc)
                t3 = [t[:, :].rearrange("p (b c) -> p b c", c=Cc) for t in tt]
                yre = Yv[:, 0, j, :].rearrange("p (b c) -> p b c", c=Cc)
                yim = Yv[:, 1, j, :].rearrange("p (b c) -> p b c", c=Cc)
                p.op("dve", lambda e, vre=vre, kre=kre, t3=t3: e.tensor_tensor(out=t3[0], in0=vre, in1=kre, op=ALU.mult), r=["pV0", ("kk", 0)], w=["tt0"])
                p.op("dve", lambda e, vim=vim, kim=kim, t3=t3: e.tensor_tensor(out=t3[1], in0=vim, in1=kim, op=ALU.mult), r=["pV1", ("kk", 1)], w=["tt1"])
                p.op("pool", lambda e, yre=yre, t3=t3: e.tensor_tensor(out=yre, in0=t3[0], in1=t3[1], op=ALU.subtract), r=["tt0", "tt1"], w=[("Y", j)])
                p.op("dve", lambda e, vre=vre, kim=kim, t3=t3: e.tensor_tensor(out=t3[2], in0=vre, in1=kim, op=ALU.mult), r=["pV0", ("kk", 1)], w=["tt2"])
                p.op("dve", lambda e, vim=vim, kre=kre, t3=t3: e.tensor_tensor(out=t3[0], in0=vim, in1=kre, op=ALU.mult), r=["pV1", ("kk", 0), "tt0"], w=["tt0"])
                p.op("pool", lambda e, yim=yim, t3=t3: e.tensor_tensor(out=yim, in0=t3[2], in1=t3[0], op=ALU.add), r=["tt0", "tt2"], w=[("Y", j)])
            for i in range(NS):
                s = i % 2
                for c in range(2):
                    p.dma("pool" if c else "sync", es[s][c][:, :NF, :], Gs[i, c], w=[("es", s, c)])
                n = 0
                for j in range(NF):
                    for c in range(2):
                        p.op("pe", lambda e, s=s, j=j, c=c, n=n: e.matmul(pY[:, :CW], lhsT=es[s][c][:, j, :], rhs=Yv[:, c, j, :],
                                                                         start=(n == 0), stop=(n == 2 * NF - 1)),
                             r=[("es", s, c), ("Y", j)], w=["pY"])
                        n += 1
                p.op("pool", lambda e, i=i, c0=c0: e.tensor_tensor(out=tt[0][:, :], in0=vs[:, i, :], in1=skb[:, c0:c0 + CW], op=ALU.mult),
                     r=["vs", "skb"], w=["tt0"])
                p.op("dve", lambda e, Lx=Lx: e.scalar_tensor_tensor(out=yo[:, :], in0=pY[:, :CW], scalar=1.0 / Lx, in1=tt[0][:, :],
                                                                   op0=ALU.mult, op1=ALU.add), r=["pY", "tt0"], w=["yo"])
                p.dma("sync", y[:, i, c0:c0 + CW], yo[:, :], r=["yo"], w=[("y", li, i, c0)], grp=("yst",))
        return skip

    skip = None
    for li, Lx in enumerate((cfg.L, cfg.LC)):
        skip = conv_li(li, Lx, skip)
    return p.build()


def run_h2(cfg, I, v_lat, v_ctx, filt):
    import ml_dtypes
    bf = ml_dtypes.bfloat16
    B, Cc, D, NC = cfg.B, cfg.Cc, cfg.D, cfg.NCORE
    ncol = B * Cc
    nc = build_h2(cfg)
    tabs = [dft_tables(Lx) for Lx in (cfg.L, cfg.LC)]
    ims = []
    for c in range(NC):
        m = {}
        for li, (Lx, vv) in enumerate(((cfg.L, v_lat), (cfg.LC, v_ctx))):
            NS = Lx // 128
            vc = vv[:, :, c * Cc:(c + 1) * Cc].transpose(1, 0, 2).reshape(Lx, ncol)
            m["v%d" % li] = np.ascontiguousarray(vc.reshape(NS, 128, ncol).transpose(1, 0, 2)).astype(bf)
            hs, hd = filt[li]
            hh = np.stack([hs[:, c * Cc:(c + 1) * Cc], hd[:, c * Cc:(c + 1) * Cc]])
            m["hsd%d" % li] = np.ascontiguousarray(hh.reshape(2, NS, 128, Cc).transpose(2, 0, 1, 3)).astype(bf)
            A2, B2, A3, B3 = tabs[li]
            m["A2_%d" % li], m["B2_%d" % li], m["A3_%d" % li], m["B3_%d" % li] = A2, B2, A3, B3
        sk = np.tile(I["hy_skip"][0][c * Cc:(c + 1) * Cc], B)
        m["skip0"] = np.ascontiguousarray(np.broadcast_to(sk[None, :], (128, ncol))).astype(np.float32)
        ims.append(m)
    res = run(nc, ims)
    outs = []
    for li, Lx in enumerate((cfg.L, cfg.LC)):
        NS = Lx // 128
        yy = np.zeros((B, Lx, D), bf)
        for c in range(NC):
            a = np.asarray(res[c]["y%d" % li]).transpose(1, 0, 2).reshape(Lx, B, Cc)
            yy[:, :, c * Cc:(c + 1) * Cc] = a.transpose(1, 0, 2)
        outs.append(yy)
    return outs


class PostCtx:
    def __init__(self, p, cfg, TK):
        ND = cfg.ND
        self.TK = TK
        self.ones = p.sb("ones", [128, 128]); p.op("dve", lambda e: e.memset(self.ones[:], 1.0), w=["ones"])
        self.ident = p.sb("ident", [128, 128])
        self.xt = p.sb("xt", [128, ND, TK]); self.xl = p.sb("xl", [128, ND, TK]); self.sq = p.sb("sq", [128, ND, TK])
        self.tokf = p.sb("tokf", [128, ND, TK]); self.tokb = p.sb("tokb", [128, ND, TK], BF16)
        self.rstd = p.sb("rstd", [128, TK]); self.olt = p.sb("olt", [128, TK])
        self.wr = p.sb("wr", [128, ND, 128]); self.br = p.sb("br", [128, 1])
        self.lgT = p.sb("lgT", [128, TK]); self.lg = p.sb("lg", [128, 128])
        self.sm = p.sb("sm", [128, 16]); self.pen = p.sb("pen", [128, 4]); self.lem = p.sb("lem", [128, 32]); self.lem2 = p.sb("lem2", [128, 32])
        self.mk = p.sb("mk", [128, 2, 32]); self.gt = p.sb("gt", [128, 32])
        self.pss = p.ps("pss", [128, TK]); self.pz = [p.ps("pz%d" % i, [128, TK]) for i in range(2)]
        self.plg = p.ps("plg", [128, TK]); self.ptr = p.ps("ptr", [128, 128])
        self.pzi = 0


def emit_post(p, cfg, C, cw, a_tile, akey, Wb, wkey, bvec_ap_fn, gt_fn, m2_fn, sh_fn, xl_out_ap, tok_out_ap, gates_out_fn, want_router=True):
    ND = cfg.ND
    for blk in range(ND):
        q = C.pzi % 2
        C.pzi += 1
        for k in range(ND):
            p.op("pe", lambda e, q=q, k=k, blk=blk: e.matmul(C.pz[q][:, :cw], lhsT=Wb[:, k, blk * 128:(blk + 1) * 128], rhs=a_tile[:, k, :cw],
                                                           start=(k == 0), stop=(k == ND - 1)), r=[wkey, akey], w=[("pz", q)])
        p.op("act", lambda e, q=q, blk=blk: e.activation(out=C.olt[:, :cw], in_=C.pz[q][:, :cw], func=AF.Identity, bias=bvec_ap_fn(blk), scale=1.0),
             r=[("pz", q), "vecs"], w=["olt"])
        p.op("dve", lambda e, blk=blk: e.scalar_tensor_tensor(out=C.xl[:, blk, :cw], in0=C.olt[:, :cw], scalar=gt_fn(blk), in1=C.xt[:, blk, :cw],
                                                            op0=ALU.mult, op1=ALU.add), r=["olt", "xt", "vecs"], w=["xl"])
    p.dma("sync", xl_out_ap, C.xl[:, :, :cw], r=["xl"], w=[("xlo", id(xl_out_ap))], grp=("xlst",))
    emit_norm_router(p, cfg, C, cw, m2_fn, sh_fn, tok_out_ap, gates_out_fn, want_router)


def emit_norm_router(p, cfg, C, cw, m2_fn, sh_fn, tok_out_ap, gates_out_fn, want_router=True):
    ND = cfg.ND
    emit_rstd(p, cfg, C.xl, "xl", cw, C.ones, C.sq, "sq", C.pss, "pss", C.rstd, "rstd")
    for k in range(ND):
        p.op("dve", lambda e, k=k: e.tensor_tensor(out=C.sq[:, k, :cw], in0=C.xl[:, k, :cw], in1=C.rstd[:, :cw], op=ALU.mult),
             r=["xl", "rstd"], w=["sq"])
        p.op("dve", lambda e, k=k: e.tensor_scalar(out=C.tokf[:, k, :cw], in0=C.sq[:, k, :cw], scalar1=m2_fn(k), scalar2=sh_fn(k),
                                                  op0=ALU.mult, op1=ALU.add), r=["sq", "vecs"], w=["tokf"])
    p.op("pool", lambda e: e.tensor_copy(out=C.tokb[:, :, :cw], in_=C.tokf[:, :, :cw]), r=["tokf"], w=["tokb"])
    p.dma("sync", tok_out_ap, C.tokb[:, :, :cw], r=["tokb"], w=[("toko", id(tok_out_ap))], grp=("tokst",))
    if not want_router:
        return
    for k in range(ND):
        p.op("pe", lambda e, k=k: e.matmul(C.plg[:, :cw], lhsT=C.wr[:, k, :], rhs=C.tokf[:, k, :cw], start=(k == 0), stop=(k == ND - 1)),
             r=["wr", "tokf"], w=["plg"])
    p.op("act", lambda e: e.activation(out=C.lgT[:, :cw], in_=C.plg[:, :cw], func=AF.Identity, bias=C.br[:, 0:1], scale=1.0),
         r=["plg", "br"], w=["lgT"])
    for (t0, tw) in tiles(cw, 128):
        p.op("pe", lambda e, t0=t0, tw=tw: e.transpose(out=C.ptr[:tw, :], in_=C.lgT[:, t0:t0 + tw], identity=C.ident[:]),
             r=["lgT", "ident"], w=["ptr"])
        p.op("act", lambda e, tw=tw: e.activation(out=C.lg[:tw, :], in_=C.ptr[:tw, :], func=AF.Copy), r=["ptr"], w=["lg"])
        lg, sm = C.lg, C.sm
        R = lambda *k: list(k)
        p.op("dve", lambda e, tw=tw: e.tensor_reduce(out=sm[:tw, 0:1], in_=lg[:tw, 0:4], axis=AX.X, op=ALU.max), r=["lg"], w=["sm0"])
        p.op("dve", lambda e, tw=tw: e.tensor_scalar(out=sm[:tw, 4:8], in0=lg[:tw, 0:4], scalar1=sm[:tw, 0:1], scalar2=None, op0=ALU.subtract),
             r=["lg", "sm0"], w=["sm4"])
        p.op("act", lambda e, tw=tw: e.activation(out=sm[:tw, 4:8], in_=sm[:tw, 4:8], func=AF.Exp), r=["sm4"], w=["sm4"])
        p.op("dve", lambda e, tw=tw: e.tensor_reduce(out=sm[:tw, 1:2], in_=sm[:tw, 4:8], axis=AX.X, op=ALU.add), r=["sm4"], w=["sm1"])
        p.op("dve", lambda e, tw=tw: e.reciprocal(out=sm[:tw, 1:2], in_=sm[:tw, 1:2]), r=["sm1"], w=["sm1"])
        p.op("dve", lambda e, tw=tw: e.tensor_scalar(out=C.pen[:tw, :], in0=lg[:tw, 0:4], scalar1=sm[:tw, 0:1], scalar2=None, op0=ALU.is_equal),
             r=["lg", "sm0"], w=["pen"])
        p.op("dve", lambda e, tw=tw: e.tensor_scalar(out=C.pen[:tw, :], in0=C.pen[:tw, :], scalar1=-1.0, scalar2=1e30, op0=ALU.add, op1=ALU.mult),
             r=["pen"], w=["pen"])
        le = lg[:tw, 4:36].rearrange("p (g e) -> p g e", e=8)
        penb = C.pen[:tw, :].unsqueeze(2).broadcast_to([tw, 4, 8])
        lem3 = C.lem[:tw, :].rearrange("p (g e) -> p g e", e=8)
        p.op("dve", lambda e, le=le, penb=penb, lem3=lem3: e.tensor_tensor(out=lem3, in0=le, in1=penb, op=ALU.add), r=["lg", "pen"], w=["lem"])
        p.op("dve", lambda e, tw=tw: e.tensor_reduce(out=sm[:tw, 2:3], in_=C.lem[:tw, :], axis=AX.X, op=ALU.max), r=["lem"], w=["sm2"])
        p.op("dve", lambda e, tw=tw: e.tensor_scalar(out=C.mk[:tw, 0, :], in0=C.lem[:tw, :], scalar1=sm[:tw, 2:3], scalar2=None, op0=ALU.is_equal),
             r=["lem", "sm2"], w=["mk0"])
        p.op("dve", lambda e, tw=tw: e.scalar_tensor_tensor(out=C.lem2[:tw, :], in0=C.mk[:tw, 0, :], scalar=-1e30, in1=C.lem[:tw, :],
                                                           op0=ALU.mult, op1=ALU.add), r=["mk0", "lem"], w=["lem2"])
        p.op("dve", lambda e, tw=tw: e.tensor_reduce(out=sm[:tw, 3:4], in_=C.lem2[:tw, :], axis=AX.X, op=ALU.max), r=["lem2"], w=["sm3"])
        p.op("dve", lambda e, tw=tw: e.tensor_scalar(out=C.mk[:tw, 1, :], in0=C.lem2[:tw, :], scalar1=sm[:tw, 3:4], scalar2=None, op0=ALU.is_equal),
             r=["lem2", "sm3"], w=["mk1"])
        p.op("dve", lambda e, tw=tw: e.tensor_tensor(out=sm[:tw, 8:9], in0=sm[:tw, 3:4], in1=sm[:tw, 2:3], op=ALU.subtract), r=["sm2", "sm3"], w=["sm8"])
        p.op("act", lambda e, tw=tw: e.activation(out=sm[:tw, 8:9], in_=sm[:tw, 8:9], func=AF.Exp), r=["sm8"], w=["sm8"])
        p.op("dve", lambda e, tw=tw: e.tensor_scalar(out=sm[:tw, 9:10], in0=sm[:tw, 8:9], scalar1=1.0, scalar2=None, op0=ALU.add), r=["sm8"], w=["sm9"])
        p.op("dve", lambda e, tw=tw: e.reciprocal(out=sm[:tw, 9:10], in_=sm[:tw, 9:10]), r=["sm9"], w=["sm9"])
        p.op("dve", lambda e, tw=tw: e.tensor_tensor(out=sm[:tw, 10:11], in0=sm[:tw, 8:9], in1=sm[:tw, 9:10], op=ALU.mult), r=["sm8", "sm9"], w=["sm10"])
        p.op("dve", lambda e, tw=tw: e.tensor_scalar(out=sm[:tw, 9:11], in0=sm[:tw, 9:11], scalar1=sm[:tw, 1:2], scalar2=None, op0=ALU.mult),
             r=["sm9", "sm10", "sm1"], w=["sm9", "sm10"])
        p.op("dve", lambda e, tw=tw: e.tensor_scalar(out=C.gt[:tw, :], in0=C.mk[:tw, 0, :], scalar1=sm[:tw, 9:10], scalar2=None, op0=ALU.mult),
             r=["mk0", "sm9"], w=["gt"])
        p.op("dve", lambda e, tw=tw: e.scalar_tensor_tensor(out=C.gt[:tw, :], in0=C.mk[:tw, 1, :], scalar=sm[:tw, 10:11], in1=C.gt[:tw, :],
                                                           op0=ALU.mult, op1=ALU.add), r=["mk1", "sm10", "gt"], w=["gt"])
        go = gates_out_fn(t0, tw)
        p.dma("sync", go, C.gt[:tw, :], r=["gt"], w=[("go", id(go))], grp=("gst",))


def build_h3(cfg):
    p = Prog()
    ND, D, TL, TC = cfg.ND, cfg.D, cfg.TL, cfg.TC
    NT = TL + TC
    TK = 256
    ycT = p.din("ycT", [128, ND, NT], BF16); x0T = p.din("x0T", [128, ND, NT], BF16); xT = p.din("xT", [128, ND, NT])
    w_out = p.din("w_out", [128, ND, D]); vecs = p.din("vecsD", [128, 8, ND])
    wr_d = p.din("wrD", [128, ND, 128]); br_d = p.din("brD", [128, 1]); ident_d = p.din("identD", [128, 128])
    xlT = p.dout("xlT", [128, ND, NT]); tokT = p.dout("tokT", [128, ND, NT], BF16); gates = p.dout("gates", [NT, 32])
    C = PostCtx(p, cfg, TK)
    p.dma("sync", C.ident[:], ident_d[:, :], w=["ident"]); p.dma("sync", C.wr[:], wr_d[:, :, :], w=["wr"]); p.dma("sync", C.br[:], br_d[:, :], w=["br"])
    vs_ = p.sb("vecs", [128, 8, ND]); p.dma("sync", vs_[:], vecs[:, :, :], w=["vecs"])
    m2 = p.sb("m2", [128, 2, ND])
    for i, j in ((0, 3), (1, 6)):
        p.op("dve", lambda e, i=i, j=j: e.scalar_tensor_tensor(out=m2[:, i, :], in0=vs_[:, j, :], scalar=1.0, in1=vs_[:, 1, :], op0=ALU.add, op1=ALU.mult),
             r=["vecs"], w=["vecs"])
    Wb = p.sb("Wb", [128, ND, D], BF16)
    wf = [p.sb("wf%d" % i, [128, ND, 128]) for i in range(2)]
    for blk in range(ND):
        s = blk % 2
        p.dma("sync", wf[s][:], w_out[:, :, blk * 128:(blk + 1) * 128], w=[("wf", s)])
        p.op("pool", lambda e, s=s, blk=blk: e.tensor_copy(out=Wb[:, :, blk * 128:(blk + 1) * 128], in_=wf[s][:]), r=[("wf", s)], w=["Wb"])
    yc = p.sb("yc", [128, ND, TK], BF16); x0 = p.sb("x0", [128, ND, TK], BF16); a = p.sb("a", [128, ND, TK], BF16)
    for (s0, sl, mi) in ((0, TL, 0), (TL, TC, 1)):
        for (c0, cw) in tiles(sl, TK):
            o = s0 + c0
            p.dma("sync", yc[:, :, :cw], ycT[:, :, o:o + cw], w=["yc"])
            p.dma("pool", x0[:, :, :cw], x0T[:, :, o:o + cw], w=["x0"])
            p.dma("sync", C.xt[:, :, :cw], xT[:, :, o:o + cw], w=["xt"])
            p.op("pool", lambda e, cw=cw: e.tensor_tensor(out=a[:, :, :cw], in0=yc[:, :, :cw], in1=x0[:, :, :cw], op=ALU.mult), r=["yc", "x0"], w=["a"])
            emit_post(p, cfg, C, cw, a, "a", Wb, "Wb",
                      lambda blk: vs_[:, 0, blk:blk + 1],
                      lambda blk, mi=mi: vs_[:, 2 + 3 * mi, blk:blk + 1],
                      lambda k, mi=mi: m2[:, mi, k:k + 1],
                      lambda k, mi=mi: vs_[:, 4 + 3 * mi, k:k + 1],
                      xlT[:, :, o:o + cw], tokT[:, :, o:o + cw],
                      lambda t0, tw, o=o: gates[o + t0:o + t0 + tw, :])
    return p.build()


def router_inputs(cfg, I, layer):
    ND = cfg.ND
    wr = np.zeros((cfg.D, 128), np.float32)
    wr[:, 0:4] = I["moe_wg"][layer]; wr[:, 4:36] = I["moe_we"][layer]
    br = np.zeros((128, 1), np.float32)
    br[0:4, 0] = I["moe_bg"][layer]; br[4:36, 0] = I["moe_be"][layer]
    wr = np.ascontiguousarray(wr.reshape(ND, 128, 128).transpose(1, 0, 2))
    return wr, br


def wfm(w, ND):
    return np.ascontiguousarray(w.reshape(ND, 128, w.shape[1]).transpose(1, 0, 2))


def run_h3(cfg, I, mods, y_lat, y_ctx, x0_lat, x0_ctx):
    import ml_dtypes
    bf = ml_dtypes.bfloat16
    ND, NC = cfg.ND, cfg.NCORE
    nc = build_h3(cfg)
    yc = tok_layout(cfg, y_lat, y_ctx); x0 = tok_layout(cfg, x0_lat, x0_ctx); xx = tok_layout(cfg, I["x"], I["ctx"])
    wr, br = router_inputs(cfg, I, 0)
    w_out = wfm(I["hy_w_out"][0], ND)
    ims = []
    for c in range(NC):
        b = c // cfg.CPB
        mvd = mod_vecs(cfg, mods[0], b)
        vecs = np.stack([vfm(v, ND) for v in (I["hy_b_out"][0], I["norm_g"][0, 1], mvd["gt_a"], mvd["sc_f"], mvd["sh_f"],
                                              mvd["cgt_a"], mvd["csc_f"], mvd["csh_f"])], axis=1)
        ims.append({"ycT": fm(yc[c], ND).astype(bf), "x0T": fm(x0[c], ND).astype(bf), "xT": fm(xx[c].astype(np.float32), ND),
                    "w_out": w_out, "vecsD": np.ascontiguousarray(vecs), "wrD": wr, "brD": br, "identD": np.eye(128, dtype=np.float32)})
    res = run(nc, ims)
    xl = tok_unlayout(cfg, [unfm(r["xlT"]) for r in res])
    tok = tok_unlayout(cfg, [unfm(np.asarray(r["tokT"])) for r in res])
    gates = tok_unlayout(cfg, [r["gates"] for r in res])
    return xl, tok, gates


def build_moe(cfg, caps):
    p = Prog()
    ND, D, DE, EPC = cfg.ND, cfg.D, cfg.DE, cfg.EPC
    NDE = DE // 128
    CAP = max(caps)
    xes = [p.din("xe%d" % j, [128, ND, caps[j]], BF16) for j in range(EPC)]
    wg = p.din("wg", [EPC, 128, ND, DE]); wu = p.din("wu", [EPC, 128, ND, DE]); wd = p.din("wd", [EPC, 128, NDE, D])
    yes_ = [p.dout("ye%d" % j, [128, ND, caps[j]], BF16) for j in range(EPC)]
    Wg = p.sb("Wg", [128, ND, DE], BF16); Wu = p.sb("Wu", [128, ND, DE], BF16); Wd = p.sb("Wd", [128, NDE, D], BF16)
    SW = 512
    st = [p.sb("st%d" % i, [128, max(ND, NDE), SW]) for i in range(2)]
    CT = min(512, CAP)
    xs = p.sb("xs", [128, ND, CT], BF16); h = p.sb("h", [128, NDE, CT], BF16); yo = p.sb("yo", [128, ND, CT], BF16)
    tg = p.sb("tg", [128, CT])
    pg = p.ps("pg", [128, CT]); pu = p.ps("pu", [128, CT]); py = [p.ps("py%d" % i, [128, CT]) for i in range(2)]
    si = 0
    for ex in range(EPC):
        for (src, dst, key, nk, ncols) in ((wg, Wg, "Wg", ND, DE), (wu, Wu, "Wu", ND, DE), (wd, Wd, "Wd", NDE, D)):
            for (c0, cw) in tiles(ncols, SW):
                s = si % 2
                si += 1
                p.dma("sync" if s else "pool", st[s][:, :nk, :cw], src[ex, :, :, c0:c0 + cw], w=[("st", s)])
                p.op("dve" if s else "act", (lambda e, s=s, dst=dst, nk=nk, c0=c0, cw=cw: e.tensor_copy(out=dst[:, :, c0:c0 + cw], in_=st[s][:, :nk, :cw]))
                     if s else (lambda e, s=s, dst=dst, nk=nk, c0=c0, cw=cw: e.activation(out=dst[:, :, c0:c0 + cw], in_=st[s][:, :nk, :cw], func=AF.Copy)),
                     r=[("st", s)], w=[key])
        for (t0, tw) in tiles(caps[ex], CT):
            p.dma("sync", xs[:, :, :tw], xes[ex][:, :, t0:t0 + tw], w=["xs"])
            for fb in range(NDE):
                for k in range(ND):
                    p.op("pe", lambda e, fb=fb, k=k, tw=tw: e.matmul(pg[:, :tw], lhsT=Wg[:, k, fb * 128:(fb + 1) * 128], rhs=xs[:, k, :tw],
                                                                    start=(k == 0), stop=(k == ND - 1)), r=["Wg", "xs"], w=["pg"])
                for k in range(ND):
                    p.op("pe", lambda e, fb=fb, k=k, tw=tw: e.matmul(pu[:, :tw], lhsT=Wu[:, k, fb * 128:(fb + 1) * 128], rhs=xs[:, k, :tw],
                                                                    start=(k == 0), stop=(k == ND - 1)), r=["Wu", "xs"], w=["pu"])
                p.op("act", lambda e, tw=tw: e.activation(out=tg[:, :tw], in_=pg[:, :tw], func=AF.Silu), r=["pg"], w=["tg"])
                p.op("dve", lambda e, fb=fb, tw=tw: e.tensor_tensor(out=h[:, fb, :tw], in0=pu[:, :tw], in1=tg[:, :tw], op=ALU.mult),
                     r=["pu", "tg"], w=["h"])
            for ob in range(ND):
                q = ob % 2
                for f in range(NDE):
                    p.op("pe", lambda e, ob=ob, f=f, q=q, tw=tw: e.matmul(py[q][:, :tw], lhsT=Wd[:, f, ob * 128:(ob + 1) * 128], rhs=h[:, f, :tw],
                                                                         start=(f == 0), stop=(f == NDE - 1)), r=["Wd", "h"], w=[("py", q)])
                if q:
                    p.op("act", lambda e, ob=ob, q=q, tw=tw: e.activation(out=yo[:, ob, :tw], in_=py[q][:, :tw], func=AF.Copy), r=[("py", q)], w=["yo"])
                else:
                    p.op("dve", lambda e, ob=ob, q=q, tw=tw: e.tensor_copy(out=yo[:, ob, :tw], in_=py[q][:, :tw]), r=[("py", q)], w=["yo"])
            p.dma("sync", yes_[ex][:, :, t0:t0 + tw], yo[:, :, :tw], r=["yo"], w=[("ye", ex, t0)], grp=("yst",))
    return p.build()


_MOE_NC = {}


def run_moe(cfg, I, layer, tok, gates, CAP=None):
    import ml_dtypes
    bf = ml_dtypes.bfloat16
    ND, NC, EPC, D, DE = cfg.ND, cfg.NCORE, cfg.EPC, cfg.D, cfg.DE
    NDE = DE // 128
    T = tok.shape[0]
    sel = gates > 0
    idx = [np.nonzero(sel[:, e])[0] for e in range(cfg.NE)]
    cnt = np.array([len(ix) for ix in idx])
    order = np.argsort(-cnt, kind="stable")
    assign = [[int(order[j * NC + c]) for j in range(EPC)] for c in range(NC)]
    caps = tuple(int(max(128, ((cnt[order[j * NC:(j + 1) * NC]].max() + 127) // 128) * 128)) for j in range(EPC))
    key = (cfg.D, caps)
    if key not in _MOE_NC:
        _MOE_NC[key] = build_moe(cfg, caps)
    nc = _MOE_NC[key]
    print("[moe] counts min/mean/max", cnt.min(), T * 2 // cfg.NE, cnt.max(), "caps", caps, flush=True)
    slot = np.cumsum(sel, axis=1) - 1
    y01 = np.zeros((2, T, D), bf)
    g01 = np.zeros((2, T), np.float32)
    ims = []
    for c in range(NC):
        m = {}
        for j in range(EPC):
            e = assign[c][j]
            xe = np.zeros((128, ND, caps[j]), bf)
            if len(idx[e]):
                xe[:, :, :len(idx[e])] = fm(tok[idx[e]], ND)
            m["xe%d" % j] = xe
        m["wg"] = np.stack([wfm(I["moe_w_gate"][layer][e], ND) for e in assign[c]])
        m["wu"] = np.stack([wfm(I["moe_w_up"][layer][e], ND) for e in assign[c]])
        m["wd"] = np.stack([wfm(I["moe_w_down"][layer][e], NDE) for e in assign[c]])
        ims.append(m)
    res = run(nc, ims)
    for c in range(NC):
        for j in range(EPC):
            e = assign[c][j]
            ix = idx[e]
            if len(ix):
                yy = unfm(np.asarray(res[c]["ye%d" % j])[:, :, :len(ix)])
                sl = slot[ix, e]
                for s_ in (0, 1):
                    msk = sl == s_
                    y01[s_, ix[msk]] = yy[msk]
                    g01[s_, ix[msk]] = gates[ix[msk], e]
    return y01, g01


def build_comb(cfg, final):
    p = Prog()
    ND, TL, TC = cfg.ND, cfg.TL, cfg.TC
    NT = TL if final else TL + TC
    TK = 128
    xlT = p.din("xlT", [128, ND, NT]); y0T = p.din("y0T", [128, ND, NT], BF16); y1T = p.din("y1T", [128, ND, NT], BF16)
    gb = p.din("gb", [128, 2, NT]); vecs = p.din("vecsD", [128, 7, ND])
    xoT = p.dout("xoT", [128, ND, NT]); uT = p.dout("uT", [128, ND, NT], F32 if final else BF16)
    vs_ = p.sb("vecs", [128, 7, ND]); p.dma("sync", vs_[:], vecs[:, :, :], w=["vecs"])
    m2 = p.sb("m2", [128, 2, ND])
    for i, j in ((0, 3), (1, 5)):
        p.op("dve", lambda e, i=i, j=j: e.scalar_tensor_tensor(out=m2[:, i, :], in0=vs_[:, j, :], scalar=1.0, in1=vs_[:, 2, :], op0=ALU.add, op1=ALU.mult),
             r=["vecs"], w=["vecs"])
    ones = p.sb("ones", [128, 128]); p.op("dve", lambda e: e.memset(ones[:], 1.0), w=["ones"])
    NB_ = 2
    mk = lambda nm, dt=F32, shp=None: [p.sb("%s_%d" % (nm, i), shp or [128, ND, TK], dt) for i in range(NB_)]
    xt_, y0_, y1_ = mk("xt"), mk("y0", BF16), mk("y1", BF16)
    gs_ = mk("gs", F32, [128, 2, TK]); mo_, mo2_, xl_, sq_ = mk("mo"), mk("mo2"), mk("xl"), mk("sq")
    rstd_ = mk("rstd", F32, [128, TK]); uo_ = mk("uo", F32 if final else BF16)
    pss_ = [p.ps("pss%d" % i, [128, TK]) for i in range(NB_)]
    segs = ((0, TL, 0),) if final else ((0, TL, 0), (TL, TC, 1))
    ti = 0
    for (s0, sl, mi) in segs:
        for (c0, cw) in tiles(sl, TK):
            o = s0 + c0
            z = ti % NB_
            ti += 1
            xt, y0, y1, gs, mo, mo2, xl, sq, rstd, uo, pss = xt_[z], y0_[z], y1_[z], gs_[z], mo_[z], mo2_[z], xl_[z], sq_[z], rstd_[z], uo_[z], pss_[z]
            K_ = lambda n, z=z: (n, z)
            p.dma("sync", xt[:, :, :cw], xlT[:, :, o:o + cw], w=[K_("xt")])
            p.dma("pool", y0[:, :, :cw], y0T[:, :, o:o + cw], w=[K_("y0")])
            p.dma("pool", y1[:, :, :cw], y1T[:, :, o:o + cw], w=[K_("y1")])
            p.dma("sync", gs[:, :, :cw], gb[:, :, o:o + cw], w=[K_("gs")])
            g0 = gs[:, 0, :cw].unsqueeze(1).broadcast_to([128, ND, cw])
            g1 = gs[:, 1, :cw].unsqueeze(1).broadcast_to([128, ND, cw])
            p.op("dve", lambda e, cw=cw, g0=g0, mo=mo, y0=y0: e.tensor_tensor(out=mo[:, :, :cw], in0=y0[:, :, :cw], in1=g0, op=ALU.mult), r=[K_("y0"), K_("gs")], w=[K_("mo")])
            p.op("pool", lambda e, cw=cw, g1=g1, mo2=mo2, y1=y1: e.tensor_tensor(out=mo2[:, :, :cw], in0=y1[:, :, :cw], in1=g1, op=ALU.mult), r=[K_("y1"), K_("gs")], w=[K_("mo2")])
            p.op("dve", lambda e, cw=cw, mo=mo, mo2=mo2: e.tensor_tensor(out=mo[:, :, :cw], in0=mo[:, :, :cw], in1=mo2[:, :, :cw], op=ALU.add), r=[K_("mo"), K_("mo2")], w=[K_("mo")])
            gtb = vs_[:, mi, :].unsqueeze(2).broadcast_to([128, ND, cw])
            p.op("pool", lambda e, cw=cw, mo=mo, gtb=gtb: e.tensor_tensor(out=mo[:, :, :cw], in0=mo[:, :, :cw], in1=gtb, op=ALU.mult), r=[K_("mo"), "vecs"], w=[K_("mo")])
            p.op("dve", lambda e, cw=cw, mo=mo, xt=xt, xl=xl: e.tensor_tensor(out=xl[:, :, :cw], in0=mo[:, :, :cw], in1=xt[:, :, :cw], op=ALU.add), r=[K_("mo"), K_("xt")], w=[K_("xl")])
            p.dma("sync", xoT[:, :, o:o + cw], xl[:, :, :cw], r=[K_("xl")], w=[("xo", o)], grp=("xst", z))
            emit_rstd(p, cfg, xl, K_("xl"), cw, ones, sq, K_("sq"), pss, K_("pss"), rstd, K_("rstd"))
            rb = rstd[:, :cw].unsqueeze(1).broadcast_to([128, ND, cw])
            p.op("dve", lambda e, cw=cw, sq=sq, xl=xl, rb=rb: e.tensor_tensor(out=sq[:, :, :cw], in0=xl[:, :, :cw], in1=rb, op=ALU.mult), r=[K_("xl"), K_("rstd")], w=[K_("sq")])
            if final:
                gb_ = vs_[:, 2, :].unsqueeze(2).broadcast_to([128, ND, cw])
                p.op("pool", lambda e, cw=cw, sq=sq, uo=uo, gb_=gb_: e.tensor_tensor(out=uo[:, :, :cw], in0=sq[:, :, :cw], in1=gb_, op=ALU.mult), r=[K_("sq"), "vecs"], w=[K_("uo")])
            else:
                mb_ = m2[:, mi, :].unsqueeze(2).broadcast_to([128, ND, cw])
                sb_ = vs_[:, 4 + 2 * mi, :].unsqueeze(2).broadcast_to([128, ND, cw])
                p.op("pool", lambda e, cw=cw, sq=sq, mb_=mb_: e.tensor_tensor(out=sq[:, :, :cw], in0=sq[:, :, :cw], in1=mb_, op=ALU.mult), r=[K_("sq"), "vecs"], w=[K_("sq")])
                p.op("dve", lambda e, cw=cw, sq=sq, uo=uo, sb_=sb_: e.tensor_tensor(out=uo[:, :, :cw], in0=sq[:, :, :cw], in1=sb_, op=ALU.add), r=[K_("sq"), "vecs"], w=[K_("uo")])
            p.dma("pool", uT[:, :, o:o + cw], uo[:, :, :cw], r=[K_("uo")], w=[("uo_", o)], grp=("ust", z))
    return p.build()


def run_comb(cfg, I, mods, layer, xl_lat, xl_ctx, y01, g01, final):
    import ml_dtypes
    bf = ml_dtypes.bfloat16
    ND, NC, B, L, LC, D = cfg.ND, cfg.NCORE, cfg.B, cfg.L, cfg.LC, cfg.D
    nc = build_comb(cfg, final)
    nl = B * L
    def split(a, last):
        lat = a[:nl].reshape((B, L) + last)
        ctx = a[nl:].reshape((B, LC) + last) if not final else np.zeros((B, LC) + last, a.dtype)
        return lat, ctx
    y0 = split(y01[0], (D,)); y1 = split(y01[1], (D,))
    g0 = split(g01[0][:, None], (1,)); g1 = split(g01[1][:, None], (1,))
    xs = tok_layout(cfg, xl_lat, xl_ctx if xl_ctx is not None else np.zeros((B, LC, D), np.float32))
    y0s = tok_layout(cfg, *y0); y1s = tok_layout(cfg, *y1); g0s = tok_layout(cfg, *g0); g1s = tok_layout(cfg, *g1)
    NT = cfg.TL if final else cfg.TL + cfg.TC
    ims = []
    for c in range(NC):
        b = c // cfg.CPB
        mvd = mod_vecs(cfg, mods[layer], b)
        if final:
            z = np.zeros(D, np.float32)
            vl = (mvd["gt_f"], z, I["final_g"], z, z, z, z)
        else:
            nm = mod_vecs(cfg, mods[layer + 1], b)
            vl = (mvd["gt_f"], mvd["cgt_f"], I["norm_g"][layer + 1, 0], nm["sc_a"], nm["sh_a"], nm["csc_a"], nm["csh_a"])
        vecs = np.ascontiguousarray(np.stack([vfm(np.asarray(v, np.float32), ND) for v in vl], axis=1))
        gbv = np.stack([g0s[c][:NT, 0], g1s[c][:NT, 0]])
        ims.append({"xlT": fm(xs[c][:NT].astype(np.float32), ND), "y0T": fm(y0s[c][:NT], ND).astype(bf), "y1T": fm(y1s[c][:NT], ND).astype(bf),
                    "gb": np.ascontiguousarray(np.broadcast_to(gbv[None], (128, 2, NT))).astype(np.float32), "vecsD": vecs})
    res = run(nc, ims)
    def un(name):
        per = []
        for r in res:
            a = unfm(np.asarray(r[name]))
            if final:
                a = np.concatenate([a, np.zeros((cfg.TC, D), a.dtype)], 0)
            per.append(a)
        return tok_unlayout(cfg, per)
    xo = un("xoT"); u = un("uT")
    return xo[0], xo[1], u[0], u[1]


S5P, S5H = 64, 16


def emit_exp_poly(p, out_ap, okey, in_ap, ikey, shape, center, degree, name):
    import math
    t = p.sb(name + "_t", shape); r = p.sb(name + "_r", shape)
    p.op("dve", lambda e: e.tensor_scalar(out=t[:], in0=in_ap, scalar1=-center, scalar2=None, op0=ALU.add), r=[ikey], w=[name + "t"])
    p.op("dve", lambda e: e.memset(r[:], 1.0 / math.factorial(degree)), w=[name + "r"])
    for n in range(degree - 1, -1, -1):
        p.op("dve", lambda e: e.tensor_tensor(out=r[:], in0=r[:], in1=t[:], op=ALU.mult), r=[name + "t", name + "r"], w=[name + "r"])
        p.op("dve", lambda e, n=n: e.tensor_scalar(out=r[:], in0=r[:], scalar1=1.0 / math.factorial(n), scalar2=None, op0=ALU.add), r=[name + "r"], w=[name + "r"])
    p.op("dve", lambda e: e.tensor_scalar(out=out_ap, in0=r[:], scalar1=float(math.exp(center)), scalar2=None, op0=ALU.mult), r=[name + "r"], w=[okey])

def build_s5prep(cfg, CH):
    p = Prog()
    G = cfg.G
    P_, H = S5P, S5H
    NLc = max(1, CH // cfg.NCORE)
    NPW = CH + 1
    a_d = p.din("a_d", [G, 2, 2, P_])
    ls_d = p.din("ls_d", [G, 2])
    b_d = p.din("b_d", [G, 2, 2, P_, H])
    c_d = p.din("c_d", [G, 2, 2, H, P_])
    lsel = p.din("lsel", [G, NLc, NPW])
    XB = p.dout("XB", [G, NLc, 2, 2, P_, H])
    OC = p.dout("OC", [G, NLc, 2, 2, H, P_])
    Mo = p.dout("Mo", [G, NLc, 2, H, H])
    PC = p.dout("PC", [G, 3, 2, P_])
    DP = 2 * P_
    a = p.sb("a", [G, 2, DP]); ls = p.sb("ls", [G, 2]); b = p.sb("b", [G, 2, 2 * P_ * H]); c = p.sb("c", [G, 2, 2 * H * P_])
    p.dma("sync", a[:], a_d.rearrange("g r d p -> g r (d p)"), w=["a"]); p.dma("sync", ls[:], ls_d[:, :], w=["ls"])
    p.dma("sync", b[:], b_d.rearrange("g r d p h -> g r (d p h)"), w=["b"]); p.dma("pool", c[:], c_d.rearrange("g r d h p -> g r (d h p)"), w=["c"])
    sel = p.sb("sel", [G, NLc, NPW]); p.dma("sync", sel[:], lsel[:, :, :], w=["sel"])
    st = p.sb("stp", [G, 2])
    emit_exp_poly(p, st[:], "st", ls[:], "ls", [G, 2], float(np.log(0.01)), 20, "ep1")
    stb = st[:, :].unsqueeze(2).broadcast_to([G, 2, P_])
    lr = p.sb("lr", [G, DP]); li = p.sb("li", [G, DP]); mag = p.sb("mag", [G, DP]); cs = p.sb("cs", [G, 2, DP])
    v3 = lambda t: t.rearrange("g (d p) -> g d p", p=P_)
    p.op("dve", lambda e: e.tensor_tensor(out=v3(lr[:, :]), in0=v3(a[:, 0, :]), in1=stb, op=ALU.mult), r=["a", "st"], w=["lr"])
    p.op("dve", lambda e: e.tensor_tensor(out=v3(li[:, :]), in0=v3(a[:, 1, :]), in1=stb, op=ALU.mult), r=["a", "st"], w=["li"])
    emit_exp_poly(p, mag[:], "mag", lr[:], "lr", [G, DP], 0.0, 7, "ep2")
    tmp = [p.sb("tmpa", [G, DP]), p.sb("tmpb", [G, DP])]
    one = p.sb("one1", [G, 1]); p.op("dve", lambda e: e.memset(one[:], 1.0), w=["one1"])
    hp = p.sb("hpi", [G, 1]); p.op("dve", lambda e: e.memset(hp[:], float(np.pi / 2)), w=["hpi"])
    zr = p.sb("zr", [G, 1]); p.op("dve", lambda e: e.memset(zr[:], 0.0), w=["zr"])
    emit_sin(p, cs[:, 1, :], ("cs", 1), li[:, :], "li", DP, tmp, "tmp", one[:, 0:1], zr[:, 0:1], extra_r=["one1", "zr"])
    emit_sin(p, cs[:, 0, :], ("cs", 0), li[:, :], "li", DP, tmp, "tmp", one[:, 0:1], hp[:, 0:1], extra_r=["one1", "hpi"])
    pw = p.sb("pw", [G, NPW, 2, DP])
    p.op("dve", lambda e: e.memset(pw[:, 0, 0, :], 1.0), w=["pw"])
    p.op("dve", lambda e: e.memset(pw[:, 0, 1, :], 0.0), w=["pw"])
    p.op("dve", lambda e: e.tensor_tensor(out=pw[:, 1, 0, :], in0=mag[:], in1=cs[:, 0, :], op=ALU.mult), r=["mag", ("cs", 0)], w=["pw"])
    p.op("dve", lambda e: e.tensor_tensor(out=pw[:, 1, 1, :], in0=mag[:], in1=cs[:, 1, :], op=ALU.mult), r=["mag", ("cs", 1)], w=["pw"])
    t0, t1 = tmp

    def cmul(out_re, out_im, are, aim, bre, bim, rk, wk, neg_im_out=None):
        raise NotImplementedError

    for l in range(1, CH):
        p.op("dve", lambda e, l=l: e.tensor_tensor(out=t0[:], in0=pw[:, l, 0, :], in1=pw[:, 1, 0, :], op=ALU.mult), r=["pw"], w=[("tmp", 0)])
        p.op("dve", lambda e, l=l: e.tensor_tensor(out=t1[:], in0=pw[:, l, 1, :], in1=pw[:, 1, 1, :], op=ALU.mult), r=["pw"], w=[("tmp", 1)])
        p.op("dve", lambda e, l=l: e.tensor_tensor(out=pw[:, l + 1, 0, :], in0=t0[:], in1=t1[:], op=ALU.subtract), r=[("tmp", 0), ("tmp", 1)], w=["pw"])
        p.op("dve", lambda e, l=l: e.tensor_tensor(out=t0[:], in0=pw[:, l, 0, :], in1=pw[:, 1, 1, :], op=ALU.mult), r=["pw"], w=[("tmp", 0)])
        p.op("dve", lambda e, l=l: e.tensor_tensor(out=t1[:], in0=pw[:, l, 1, :], in1=pw[:, 1, 0, :], op=ALU.mult), r=["pw"], w=[("tmp", 1)])
        p.op("dve", lambda e, l=l: e.tensor_tensor(out=pw[:, l + 1, 1, :], in0=t0[:], in1=t1[:], op=ALU.add), r=[("tmp", 0), ("tmp", 1)], w=["pw"])
    pc = p.sb("pc", [G, 3, DP])
    p.op("dve", lambda e: e.tensor_copy(out=pc[:, 0:2, :], in_=pw[:, CH, :, :]), r=["pw"], w=["pc"])
    p.op("dve", lambda e: e.tensor_scalar(out=pc[:, 2, :], in0=pw[:, CH, 1, :], scalar1=-1.0, scalar2=None, op0=ALU.mult), r=["pw"], w=["pc"])
    p.dma("sync", PC.rearrange("g t d p -> g t (d p)"), pc[:], r=["pc"], w=["PC"])
    q = p.sb("q", [G, 2, DP]); dd = p.sb("dd", [G, DP]); nr = p.sb("nr", [G, DP])
    p.op("dve", lambda e: e.tensor_scalar(out=nr[:], in0=pw[:, 1, 0, :], scalar1=-1.0, scalar2=None, op0=ALU.add), r=["pw"], w=["nr"])
    p.op("dve", lambda e: e.tensor_tensor(out=t0[:], in0=a[:, 0, :], in1=a[:, 0, :], op=ALU.mult), r=["a"], w=[("tmp", 0)])
    p.op("dve", lambda e: e.tensor_tensor(out=t1[:], in0=a[:, 1, :], in1=a[:, 1, :], op=ALU.mult), r=["a"], w=[("tmp", 1)])
    p.op("dve", lambda e: e.tensor_tensor(out=dd[:], in0=t0[:], in1=t1[:], op=ALU.add), r=[("tmp", 0), ("tmp", 1)], w=["dd"])
    p.op("dve", lambda e: e.reciprocal(out=dd[:], in_=dd[:]), r=["dd"], w=["dd"])
    p.op("dve", lambda e: e.tensor_tensor(out=t0[:], in0=nr[:], in1=a[:, 0, :], op=ALU.mult), r=["nr", "a"], w=[("tmp", 0)])
    p.op("dve", lambda e: e.tensor_tensor(out=t1[:], in0=pw[:, 1, 1, :], in1=a[:, 1, :], op=ALU.mult), r=["pw", "a"], w=[("tmp", 1)])
    p.op("dve", lambda e: e.tensor_tensor(out=t0[:], in0=t0[:], in1=t1[:], op=ALU.add), r=[("tmp", 0), ("tmp", 1)], w=[("tmp", 0)])
    p.op("dve", lambda e: e.tensor_tensor(out=q[:, 0, :], in0=t0[:], in1=dd[:], op=ALU.mult), r=[("tmp", 0), "dd"], w=["q"])
    p.op("dve", lambda e: e.tensor_tensor(out=t0[:], in0=pw[:, 1, 1, :], in1=a[:, 0, :], op=ALU.mult), r=["pw", "a"], w=[("tmp", 0)])
    p.op("dve", lambda e: e.tensor_tensor(out=t1[:], in0=nr[:], in1=a[:, 1, :], op=ALU.mult), r=["nr", "a"], w=[("tmp", 1)])
    p.op("dve", lambda e: e.tensor_tensor(out=t0[:], in0=t0[:], in1=t1[:], op=ALU.subtract), r=[("tmp", 0), ("tmp", 1)], w=[("tmp", 0)])
    p.op("dve", lambda e: e.tensor_tensor(out=q[:, 1, :], in0=t0[:], in1=dd[:], op=ALU.mult), r=[("tmp", 0), "dd"], w=["q"])
    NB_ = 2 * P_ * H
    bb = p.sb("bb", [G, 2, NB_]); w0 = p.sb("w0", [G, NB_]); w1 = p.sb("w1", [G, NB_])
    v_ph = lambda t: t.rearrange("g (dp h) -> g dp h", h=H)
    bc_h = lambda t: t.unsqueeze(2).broadcast_to([G, DP, H])

    def cmul_ph(out_re, out_im, sre, sim, xre, xim, rk, wk, neg_im=False):
        p.op("dve", lambda e: e.tensor_tensor(out=v_ph(w0[:, :]), in0=v_ph(xre), in1=bc_h(sre), op=ALU.mult), r=rk, w=["w0"])
        p.op("pool", lambda e: e.tensor_tensor(out=v_ph(w1[:, :]), in0=v_ph(xim), in1=bc_h(sim), op=ALU.mult), r=rk, w=["w1"])
        p.op("dve", lambda e: e.tensor_tensor(out=out_re, in0=w0[:, :], in1=w1[:, :], op=ALU.subtract), r=["w0", "w1"], w=wk)
        p.op("dve", lambda e: e.tensor_tensor(out=v_ph(w0[:, :]), in0=v_ph(xim), in1=bc_h(sre), op=ALU.mult), r=rk, w=["w0"])
        p.op("pool", lambda e: e.tensor_tensor(out=v_ph(w1[:, :]), in0=v_ph(xre), in1=bc_h(sim), op=ALU.mult), r=rk, w=["w1"])
        if neg_im:
            p.op("dve", lambda e: e.scalar_tensor_tensor(out=out_im, in0=w0[:, :], scalar=-1.0, in1=w1[:, :], op0=ALU.mult, op1=ALU.subtract),
                 r=["w0", "w1"], w=wk)
        else:
            p.op("dve", lambda e: e.tensor_tensor(out=out_im, in0=w0[:, :], in1=w1[:, :], op=ALU.add), r=["w0", "w1"], w=wk)

    cmul_ph(bb[:, 0, :], bb[:, 1, :], q[:, 0, :], q[:, 1, :], b[:, 0, :], b[:, 1, :], ["q", "b"], ["bb"])
    pl = p.sb("pl", [G, 2, DP]); pl1 = p.sb("pl1", [G, 2, DP])
    xb = p.sb("xb", [G, 2, NB_]); oc = p.sb("oc", [G, 2, NB_]); mo = p.sb("mo", [G, 2, H * H])
    HH = H * H
    big0 = p.sb("big0", [G, (H // 2) * H * P_]); big1 = p.sb("big1", [G, (H // 2) * H * P_])
    for n in range(NLc):
        for (dst, off, key) in ((pl, 0, "pl"), (pl1, 1, "pl1")):
            for comp in range(2):
                first = True
                for l in range(CH):
                    src = pw[:, l + off, comp, :]
                    if first:
                        p.op("dve", lambda e, dst=dst, comp=comp, src=src, n=n, l=l: e.tensor_scalar(
                            out=dst[:, comp, :], in0=src, scalar1=sel[:, n, l:l + 1], scalar2=None, op0=ALU.mult), r=["pw", "sel"], w=[key])
                        first = False
                    else:
                        p.op("dve", lambda e, dst=dst, comp=comp, src=src, n=n, l=l: e.scalar_tensor_tensor(
                            out=dst[:, comp, :], in0=src, scalar=sel[:, n, l:l + 1], in1=dst[:, comp, :], op0=ALU.mult, op1=ALU.add),
                            r=["pw", "sel", key], w=[key])
        cmul_ph(xb[:, 0, :], xb[:, 1, :], pl[:, 0, :], pl[:, 1, :], bb[:, 0, :], bb[:, 1, :], ["pl", "bb"], ["xb"])
        p.dma("sync", XB[:, n].rearrange("g r d p h -> g r (d p h)"), xb[:], r=["xb"], w=[("XB", n)], grp=("xbst",))
        cv = lambda t: t.rearrange("g (d h p) -> g d h p", d=2, h=H)
        bc_hp = lambda t: t.rearrange("g (d p) -> g d p", d=2).unsqueeze(2).broadcast_to([G, 2, H, P_])
        NC_ = 2 * H * P_
        p.op("dve", lambda e: e.tensor_tensor(out=cv(w0[:, :NC_]), in0=cv(c[:, 0, :]), in1=bc_hp(pl1[:, 0, :]), op=ALU.mult), r=["c", "pl1"], w=["w0"])
        p.op("pool", lambda e: e.tensor_tensor(out=cv(w1[:, :NC_]), in0=cv(c[:, 1, :]), in1=bc_hp(pl1[:, 1, :]), op=ALU.mult), r=["c", "pl1"], w=["w1"])
        p.op("dve", lambda e: e.tensor_tensor(out=oc[:, 0, :], in0=w0[:, :NC_], in1=w1[:, :NC_], op=ALU.subtract), r=["w0", "w1"], w=["oc"])
        p.op("dve", lambda e: e.tensor_tensor(out=cv(w0[:, :NC_]), in0=cv(c[:, 1, :]), in1=bc_hp(pl1[:, 0, :]), op=ALU.mult), r=["c", "pl1"], w=["w0"])
        p.op("pool", lambda e: e.tensor_tensor(out=cv(w1[:, :NC_]), in0=cv(c[:, 0, :]), in1=bc_hp(pl1[:, 1, :]), op=ALU.mult), r=["c", "pl1"], w=["w1"])
        p.op("dve", lambda e: e.scalar_tensor_tensor(out=oc[:, 1, :], in0=w0[:, :NC_], scalar=-1.0, in1=w1[:, :NC_], op0=ALU.mult, op1=ALU.subtract),
             r=["w0", "w1"], w=["oc"])
        p.dma("sync", OC[:, n].rearrange("g r d h p -> g r (d h p)"), oc[:], r=["oc"], w=[("OC", n)], grp=("ocst",))
        for d in range(2):
            for hh in range(2):
                h0 = hh * (H // 2)
                def cview(comp, d=d, h0=h0):
                    t = c[:, comp, d * H * P_:(d + 1) * H * P_].rearrange("g (h p) -> g h p", p=P_)[:, h0:h0 + H // 2, :]
                    return t.unsqueeze(2).broadcast_to([G, H // 2, H, P_])
                def xview(comp, d=d):
                    t = xb[:, comp, d * P_ * H:(d + 1) * P_ * H].rearrange("g (p h) -> g h p", h=H)
                    return t.unsqueeze(1).broadcast_to([G, H // 2, H, P_])
                b0v = big0[:, :].rearrange("g (a h p) -> g a h p", a=H // 2, h=H)
                b1v = big1[:, :].rearrange("g (a h p) -> g a h p", a=H // 2, h=H)
                p.op("dve", lambda e, cview=cview, xview=xview, b0v=b0v: e.tensor_tensor(out=b0v, in0=cview(0), in1=xview(0), op=ALU.mult), r=["c", "xb"], w=["big0"])
                p.op("pool", lambda e, cview=cview, xview=xview, b1v=b1v: e.tensor_tensor(out=b1v, in0=cview(1), in1=xview(1), op=ALU.mult), r=["c", "xb"], w=["big1"])
                p.op("dve", lambda e: e.tensor_tensor(out=big0[:, :], in0=big0[:, :], in1=big1[:, :], op=ALU.subtract), r=["big0", "big1"], w=["big0"])
                p.op("dve", lambda e, d=d, h0=h0: e.tensor_reduce(out=mo[:, d, h0 * H:(h0 + H // 2) * H],
                                                                 in_=big0[:, :].rearrange("g (a p) -> g a p", p=P_), axis=AX.X, op=ALU.add),
                     r=["big0"], w=["mo"])
        p.dma("sync", Mo[:, n].rearrange("g d a b -> g d (a b)"), mo[:], r=["mo"], w=[("Mo", n)], grp=("most",))
    return p.build()


def run_s5prep(cfg, I, CH):
    G, NC = cfg.G, cfg.NCORE
    P_, H = S5P, S5H
    NLc = max(1, CH // NC)
    nc = build_s5prep(cfg, CH)
    a_d = np.ascontiguousarray(np.stack([I["s5_a_re"][0], I["s5_a_im"][0]]).transpose(2, 0, 1, 3)).astype(np.float32)
    ls_d = np.ascontiguousarray(I["s5_log_step"][0].T).astype(np.float32)
    b_d = np.ascontiguousarray(np.stack([I["s5_b_re"][0], I["s5_b_im"][0]]).transpose(2, 0, 1, 3, 4)).astype(np.float32)
    c_d = np.ascontiguousarray(np.stack([I["s5_c_re"][0], I["s5_c_im"][0]]).transpose(2, 0, 1, 3, 4)).astype(np.float32)
    ims = []
    lags = []
    for c in range(NC):
        ls = [c + NC * n for n in range(NLc)] if CH >= NC else [c % CH]
        lags.append(ls)
        sel = np.zeros((G, NLc, CH + 1), np.float32)
        for n, l in enumerate(ls):
            sel[:, n, l] = 1.0
        ims.append({"a_d": a_d, "ls_d": ls_d, "b_d": b_d, "c_d": c_d, "lsel": sel})
    res = run(nc, ims)
    XB = np.zeros((CH, G, 2, 2, P_, H), np.float32); OC = np.zeros((CH, G, 2, 2, H, P_), np.float32); Mo = np.zeros((CH, G, 2, H, H), np.float32)
    for c in range(NC):
        for n, l in enumerate(lags[c]):
            XB[l] = res[c]["XB"][:, n]; OC[l] = res[c]["OC"][:, n]; Mo[l] = res[c]["Mo"][:, n]
    PC = res[0]["PC"]
    return dict(XB=XB, OC=OC, Mo=Mo, PC=PC)


def build_s5(cfg, CH, PG):
    p = Prog()
    B = cfg.B
    GPC = cfg.G // cfg.NCORE
    NPr = 2 * GPC
    KR = CH * S5H
    KP = KR // 128
    LT = cfg.LC + cfg.L
    NCK = LT // CH
    NCOL = NCK * B
    CL0 = (cfg.LC // CH) * B
    NCOLL = NCOL - CL0
    U = p.din("U", [NPr, 128, KP, NCOL], BF16)
    Tm = p.din("Tm", [NPr, 128, KP, KR]); Xm = p.din("Xm", [NPr, 128, 2, KP, 128]); Om = p.din("Om", [NPr, 128, KR])
    CAd = p.din("CA", [128, NPr]); CBd = p.din("CB", [128, NPr])
    Y = p.dout("Y", [NPr, 128, KP, NCOLL], BF16)
    ca = p.sb("ca", [128, NPr]); cb = p.sb("cb", [128, NPr])
    p.dma("sync", ca[:], CAd[:, :], w=["ca"]); p.dma("sync", cb[:], CBd[:, :], w=["cb"])
    SA = p.sb("SA", [128, PG, NCK, B]); SW = p.sb("SW", [128, PG, NCK, B]); SAb = p.sb("SAb", [128, PG, NCK, B], BF16)
    Ub = p.sb("Ub", [128, PG, KP, NCOL], BF16)
    Tb = p.sb("Tb", [128, PG, KP, KR], BF16); Xb = p.sb("Xb", [128, PG, 2, KP, 128], BF16); Ob = p.sb("Ob", [128, PG, KR], BF16)
    stT = p.sb("stT", [128, KP, KR]); stX = p.sb("stX", [128, 2, KP, 128]); stO = p.sb("stO", [128, KR])
    tq = [p.sb("tq%d" % i, [128, PG, B]) for i in range(4)]
    yo = [p.sb("yo%d" % i, [128, 512], BF16) for i in range(2)]
    px = [p.ps("px%d" % i, [128, 512]) for i in range(2)]
    py = [p.ps("py%d" % i, [128, 512]) for i in range(2)]
    yi = 0
    for pg0 in range(0, NPr, PG):
        for pi in range(PG):
            pr = pg0 + pi
            p.dma("sync", Ub[:, pi], U[pr], w=[("Ub", pi)])
            p.dma("pool", stT[:], Tm[pr], w=["stT"]); p.dma("pool", stX[:], Xm[pr], w=["stX"]); p.dma("pool", stO[:], Om[pr], w=["stO"])
            p.op("dve", lambda e, pi=pi: e.tensor_copy(out=Tb[:, pi], in_=stT[:]), r=["stT"], w=[("Tb", pi)])
            p.op("act", lambda e, pi=pi: e.activation(out=Xb[:, pi], in_=stX[:], func=AF.Copy), r=["stX"], w=[("Xb", pi)])
            p.op("dve", lambda e, pi=pi: e.tensor_copy(out=Ob[:, pi], in_=stO[:]), r=["stO"], w=[("Ob", pi)])
            SAf = SA[:, pi].rearrange("p k b -> p (k b)")
            SWf = SW[:, pi].rearrange("p k b -> p (k b)")
            for (c0, cw) in tiles(NCOL, 512):
                for w_, dstf, key in ((0, SAf, "SA"), (1, SWf, "SW")):
                    for qk in range(KP):
                        p.op("pe", lambda e, pi=pi, w_=w_, qk=qk, c0=c0, cw=cw: e.matmul(px[w_][:, :cw], lhsT=Xb[:, pi, w_, qk, :], rhs=Ub[:, pi, qk, c0:c0 + cw],
                                                                                     start=(qk == 0), stop=(qk == KP - 1)),
                             r=[("Xb", pi), ("Ub", pi)], w=[("px", w_)])
                    if w_ == 0:
                        p.op("act", lambda e, dstf=dstf, c0=c0, cw=cw: e.activation(out=dstf[:, c0:c0 + cw], in_=px[0][:, :cw], func=AF.Copy),
                             r=[("px", 0)], w=[("SA", pi)])
                    else:
                        p.op("dve", lambda e, dstf=dstf, c0=c0, cw=cw: e.tensor_copy(out=dstf[:, c0:c0 + cw], in_=px[1][:, :cw]),
                             r=[("px", 1)], w=[("SW", pi)])
        cab = ca[:, pg0:pg0 + PG].unsqueeze(2).broadcast_to([128, PG, B])
        cbb = cb[:, pg0:pg0 + PG].unsqueeze(2).broadcast_to([128, PG, B])
        allSA = [("SA", pi) for pi in range(PG)]
        allSW = [("SW", pi) for pi in range(PG)]
        for k in range(1, NCK):
            rA = allSA if k == 1 else [("SAk", k - 1)]
            rW = allSW if k == 1 else [("SWk", k - 1)]
            wA = (allSA if k == 1 else []) + [("SAk", k)]
            wW = (allSW if k == 1 else []) + [("SWk", k)]
            Sp = SA[:, :, k - 1, :]; Wp = SW[:, :, k - 1, :]; Sk = SA[:, :, k, :]; Wk = SW[:, :, k, :]
            p.op("dve", lambda e, Sp=Sp, cab=cab: e.tensor_tensor(out=tq[0][:], in0=Sp, in1=cab, op=ALU.mult), r=rA + ["ca"], w=["tq0"])
            p.op("dve", lambda e, Wp=Wp, cbb=cbb: e.tensor_tensor(out=tq[1][:], in0=Wp, in1=cbb, op=ALU.mult), r=rW + ["cb"], w=["tq1"])
            p.op("dve", lambda e: e.tensor_tensor(out=tq[0][:], in0=tq[0][:], in1=tq[1][:], op=ALU.add), r=["tq0", "tq1"], w=["tq0"])
            p.op("dve", lambda e, Sk=Sk: e.tensor_tensor(out=Sk, in0=Sk, in1=tq[0][:], op=ALU.add), r=["tq0"] + rA, w=wA)
            p.op("pool", lambda e, Wp=Wp, cab=cab: e.tensor_tensor(out=tq[2][:], in0=Wp, in1=cab, op=ALU.mult), r=rW + ["ca"], w=["tq2"])
            p.op("pool", lambda e, Sp=Sp, cbb=cbb: e.tensor_tensor(out=tq[3][:], in0=Sp, in1=cbb, op=ALU.mult), r=rA + ["cb"], w=["tq3"])
            p.op("pool", lambda e: e.tensor_tensor(out=tq[2][:], in0=tq[2][:], in1=tq[3][:], op=ALU.subtract), r=["tq2", "tq3"], w=["tq2"])
            p.op("pool", lambda e, Wk=Wk: e.tensor_tensor(out=Wk, in0=Wk, in1=tq[2][:], op=ALU.add), r=["tq2"] + rW, w=wW)
        fin = [("SAk", NCK - 1), ("SWk", NCK - 1)] + allSA + allSW
        p.op("dve", lambda e: e.memset(SAb[:, :, 0, :], 0.0), r=fin, w=["SAb"])
        p.op("act", lambda e: e.activation(out=SAb[:, :, 1:NCK, :], in_=SA[:, :, 0:NCK - 1, :], func=AF.Copy), r=fin + [("SAk", k) for k in range(1, NCK)], w=["SAb"])
        for pi in range(PG):
            pr = pg0 + pi
            SAbf = SAb[:, pi].rearrange("p k b -> p (k b)")
            for mb in range(KP):
                for (c0, cw) in tiles(NCOLL, 512):
                    q = yi % 2
                    yi += 1
                    a0 = CL0 + c0
                    for qk in range(mb + 1):
                        p.op("pe", lambda e, pi=pi, mb=mb, qk=qk, q=q, a0=a0, cw=cw: e.matmul(
                            py[q][:, :cw], lhsT=Tb[:, pi, qk, mb * 128:(mb + 1) * 128], rhs=Ub[:, pi, qk, a0:a0 + cw], start=(qk == 0), stop=False),
                            r=[("Tb", pi), ("Ub", pi)], w=[("py", q)])
                    p.op("pe", lambda e, pi=pi, mb=mb, q=q, a0=a0, cw=cw, SAbf=SAbf: e.matmul(
                        py[q][:, :cw], lhsT=Ob[:, pi, mb * 128:(mb + 1) * 128], rhs=SAbf[:, a0:a0 + cw], start=False, stop=True),
                        r=[("Ob", pi), "SAb"], w=[("py", q)])
                    if q:
                        p.op("act", lambda e, q=q, cw=cw: e.activation(out=yo[q][:, :cw], in_=py[q][:, :cw], func=AF.Copy), r=[("py", q)], w=[("yo", q)])
                    else:
                        p.op("dve", lambda e, q=q, cw=cw: e.tensor_copy(out=yo[q][:, :cw], in_=py[q][:, :cw]), r=[("py", q)], w=[("yo", q)])
                    p.dma("sync", Y[pr, :, mb, c0:c0 + cw], yo[q][:, :cw], r=[("yo", q)], w=[("Y", pr, mb, c0)], grp=("yst", q))
        for k in range(1, NCK):
            for nm in ("SAk", "SWk"):
                pass
        p.barrier()
    return p.build()


def s5_matrices(cfg, prep, CH):
    G = cfg.G
    P_, H = S5P, S5H
    KR = CH * H
    KP = KR // 128
    XB, OC, Mo, PC = prep["XB"], prep["OC"], prep["Mo"], prep["PC"]
    NPall = G * 2
    Tm = np.zeros((NPall, KR, KR), np.float32); Xm = np.zeros((NPall, 2, KR, 128), np.float32); Om = np.zeros((NPall, 128, KR), np.float32)
    CA = np.zeros((128, NPall), np.float32); CB = np.zeros((128, NPall), np.float32)
    for g in range(G):
        for d in range(2):
            pr = g * 2 + d
            for j in range(CH):
                for j2 in range(j, CH):
                    Tm[pr, j * H:(j + 1) * H, j2 * H:(j2 + 1) * H] = Mo[j2 - j][g, d].T
                xb = XB[CH - 1 - j][g, :, d]
                Xm[pr, 0, j * H:(j + 1) * H, 0:P_] = xb[0].T; Xm[pr, 0, j * H:(j + 1) * H, P_:] = xb[1].T
                Xm[pr, 1, j * H:(j + 1) * H, 0:P_] = xb[1].T; Xm[pr, 1, j * H:(j + 1) * H, P_:] = xb[0].T
                oc = OC[j][g, :, d]
                Om[pr, 0:P_, j * H:(j + 1) * H] = oc[0].T; Om[pr, P_:, j * H:(j + 1) * H] = oc[1].T
            CA[0:P_, pr] = PC[g, 0, d]; CA[P_:, pr] = PC[g, 0, d]
            CB[0:P_, pr] = PC[g, 2, d]; CB[P_:, pr] = PC[g, 1, d]
    Tm = np.ascontiguousarray(Tm.reshape(NPall, KP, 128, KR).transpose(0, 2, 1, 3))
    Xm = np.ascontiguousarray(Xm.reshape(NPall, 2, KP, 128, 128).transpose(0, 3, 1, 2, 4))
    return Tm, Xm, Om, CA, CB


def run_s5(cfg, mats, u_lat, u_ctx, CH, PG):
    import ml_dtypes
    bf = ml_dtypes.bfloat16
    B, L, LC, D, NC = cfg.B, cfg.L, cfg.LC, cfg.D, cfg.NCORE
    GPC = cfg.G // NC
    NPr = 2 * GPC
    H = S5H
    KR = CH * H; KP = KR // 128
    LT = LC + L; NCK = LT // CH; NCOL = NCK * B
    CL0 = (LC // CH) * B
    Tm, Xm, Om, CA, CB = mats
    nc = build_s5(cfg, CH, PG)
    seqs = [np.concatenate([u_ctx, u_lat], 1), np.concatenate([u_ctx[:, ::-1], u_lat[:, ::-1]], 1)]
    ims = []
    for c in range(NC):
        U = np.zeros((NPr, 128, KP, NCOL), bf)
        for gi in range(GPC):
            g = c * GPC + gi
            for d in range(2):
                s = seqs[d][:, :, g * H:(g + 1) * H]
                s = s.reshape(B, NCK, CH, H).transpose(2, 3, 1, 0)
                U[gi * 2 + d] = s.reshape(KP, 128, NCOL).transpose(1, 0, 2)
        sl = slice(c * NPr, (c + 1) * NPr)
        ims.append({"U": U, "Tm": Tm[sl], "Xm": Xm[sl], "Om": Om[sl], "CA": np.ascontiguousarray(CA[:, sl]), "CB": np.ascontiguousarray(CB[:, sl])})
    res = run(nc, ims)
    ys = [np.zeros((B, L, D), bf), np.zeros((B, L, D), bf)]
    for c in range(NC):
        Yc = np.asarray(res[c]["Y"])
        for gi in range(GPC):
            g = c * GPC + gi
            for d in range(2):
                a = Yc[gi * 2 + d].transpose(1, 0, 2).reshape(CH, H, L // CH, B)
                a = a.transpose(3, 2, 0, 1).reshape(B, L, H)
                if d == 1:
                    a = a[:, ::-1]
                ys[d][:, :, g * H:(g + 1) * H] = a
    return ys


def build_glu(cfg):
    p = Prog()
    ND, D, TL = cfg.ND, cfg.D, cfg.TL
    TK = 128
    TB = 512
    yfT = p.din("yfT", [128, ND, TL], BF16); ybT = p.din("ybT", [128, ND, TL], BF16); uT = p.din("uT", [128, ND, TL], BF16)
    xT = p.din("xT", [128, ND, TL]); w1 = p.din("w1", [128, ND, D]); w2 = p.din("w2", [128, ND, D]); vecs = p.din("vecsD", [128, 7, ND])
    wr_d = p.din("wrD", [128, ND, 128]); br_d = p.din("brD", [128, 1]); ident_d = p.din("identD", [128, 128])
    xlT = p.dout("xlT", [128, ND, TL]); tokT = p.dout("tokT", [128, ND, TL], BF16); gates = p.dout("gates", [TL, 32])
    C = PostCtx(p, cfg, TK)
    p.dma("sync", C.ident[:], ident_d[:, :], w=["ident"]); p.dma("sync", C.wr[:], wr_d[:, :, :], w=["wr"]); p.dma("sync", C.br[:], br_d[:, :], w=["br"])
    vs_ = p.sb("vecs", [128, 7, ND]); p.dma("sync", vs_[:], vecs[:, :, :], w=["vecs"])
    m2 = p.sb("m2", [128, ND])
    p.op("dve", lambda e: e.scalar_tensor_tensor(out=m2[:, :], in0=vs_[:, 5, :], scalar=1.0, in1=vs_[:, 4, :], op0=ALU.add, op1=ALU.mult), r=["vecs"], w=["vecs"])
    a = p.sb("a_all", [128, ND, TL], BF16)
    yf = p.sb("yf", [128, ND, TK], BF16); yb = p.sb("yb", [128, ND, TK], BF16); uu = p.sb("uu", [128, ND, TK], BF16)
    ys, yt = C.xt, C.sq
    GC = 2.0 * float(np.sqrt(2.0 / np.pi))
    for (c0, cw) in tiles(TL, TK):
        p.dma("sync", yf[:, :, :cw], yfT[:, :, c0:c0 + cw], w=["yf"])
        p.dma("pool", yb[:, :, :cw], ybT[:, :, c0:c0 + cw], w=["yb"])
        p.dma("sync", uu[:, :, :cw], uT[:, :, c0:c0 + cw], w=["uu"])
        p.op("dve", lambda e, cw=cw: e.tensor_tensor(out=ys[:, :, :cw], in0=yf[:, :, :cw], in1=yb[:, :, :cw], op=ALU.add), r=["yf", "yb"], w=["xt"])
        for k in range(ND):
            p.op("dve", lambda e, k=k, cw=cw: e.scalar_tensor_tensor(out=ys[:, k, :cw], in0=uu[:, k, :cw], scalar=vs_[:, 0, k:k + 1], in1=ys[:, k, :cw],
                                                                   op0=ALU.mult, op1=ALU.add), r=["uu", "xt", "vecs"], w=["xt"])
        p.op("pool", lambda e, cw=cw: e.tensor_tensor(out=yt[:, :, :cw], in0=ys[:, :, :cw], in1=ys[:, :, :cw], op=ALU.mult), r=["xt"], w=["sq"])
        p.op("dve", lambda e, cw=cw: e.tensor_scalar(out=yt[:, :, :cw], in0=yt[:, :, :cw], scalar1=0.044715, scalar2=1.0, op0=ALU.mult, op1=ALU.add), r=["sq"], w=["sq"])
        p.op("pool", lambda e, cw=cw: e.tensor_tensor(out=yt[:, :, :cw], in0=yt[:, :, :cw], in1=ys[:, :, :cw], op=ALU.mult), r=["sq", "xt"], w=["sq"])
        p.op("act", lambda e, cw=cw: e.activation(out=yt[:, :, :cw], in_=yt[:, :, :cw], func=AF.Sigmoid, scale=GC), r=["sq"], w=["sq"])
        p.op("dve", lambda e, c0=c0, cw=cw: e.tensor_tensor(out=a[:, :, c0:c0 + cw], in0=yt[:, :, :cw], in1=ys[:, :, :cw], op=ALU.mult), r=["sq", "xt"], w=["a"])
    wf = [[p.sb("wf%d%d" % (i, j), [128, ND, 128]) for j in range(2)] for i in range(2)]
    wb = [[p.sb("wb%d%d" % (i, j), [128, ND, 128], BF16) for j in range(2)] for i in range(2)]
    xb_ = [p.sb("xb%d" % i, [128, TB]) for i in range(2)]
    ob_ = [p.sb("ob%d" % i, [128, TB]) for i in range(2)]
    sg = p.sb("sg", [128, TB]); ol = p.sb("ol", [128, TB])
    pz1 = [p.ps("pza%d" % i, [128, TB]) for i in range(2)]
    pz2 = [p.ps("pzb0", [128, TB])] * 2
    qi = 0
    for blk in range(ND):
        s = blk % 2
        for j, src in enumerate((w1, w2)):
            p.dma("sync" if j else "pool", wf[s][j][:], src[:, :, blk * 128:(blk + 1) * 128], w=[("wf", s, j)])
            if j:
                p.op("act", lambda e, s=s, j=j: e.activation(out=wb[s][j][:], in_=wf[s][j][:], func=AF.Copy), r=[("wf", s, j)], w=[("wb", s, j)])
            else:
                p.op("pool", lambda e, s=s, j=j: e.tensor_copy(out=wb[s][j][:], in_=wf[s][j][:]), r=[("wf", s, j)], w=[("wb", s, j)])
        for (c0, cw) in tiles(TL, TB):
            q = qi % 2
            qi += 1
            p.dma("sync", xb_[q][:, :cw], xT[:, blk, c0:c0 + cw], w=[("xb", q)])
            for k in range(ND):
                p.op("pe", lambda e, q=q, k=k, s=s, c0=c0, cw=cw: e.matmul(pz1[q][:, :cw], lhsT=wb[s][0][:, k, :], rhs=a[:, k, c0:c0 + cw],
                                                                          start=(k == 0), stop=(k == ND - 1)), r=[("wb", s, 0), "a"], w=[("pza", q)])
            for k in range(ND):
                p.op("pe", lambda e, q=q, k=k, s=s, c0=c0, cw=cw: e.matmul(pz2[q][:, :cw], lhsT=wb[s][1][:, k, :], rhs=a[:, k, c0:c0 + cw],
                                                                          start=(k == 0), stop=(k == ND - 1)), r=[("wb", s, 1), "a"], w=[("pzb", 0)])
            p.op("act", lambda e, q=q, blk=blk, cw=cw: e.activation(out=sg[:, :cw], in_=pz2[q][:, :cw], func=AF.Sigmoid, bias=vs_[:, 2, blk:blk + 1], scale=1.0),
                 r=[("pzb", 0), "vecs"], w=["sg"])
            p.op("dve", lambda e, q=q, blk=blk, cw=cw: e.scalar_tensor_tensor(out=ol[:, :cw], in0=pz1[q][:, :cw], scalar=vs_[:, 1, blk:blk + 1], in1=sg[:, :cw],
                                                                            op0=ALU.add, op1=ALU.mult), r=[("pza", q), "sg", "vecs"], w=["ol"])
            p.op("dve", lambda e, q=q, blk=blk, cw=cw: e.scalar_tensor_tensor(out=ob_[q][:, :cw], in0=ol[:, :cw], scalar=vs_[:, 3, blk:blk + 1], in1=xb_[q][:, :cw],
                                                                            op0=ALU.mult, op1=ALU.add), r=["ol", ("xb", q), "vecs"], w=[("ob", q)])
            p.dma("pool", xlT[:, blk, c0:c0 + cw], ob_[q][:, :cw], r=[("ob", q)], w=[("xlo", blk, c0)], grp=("xlst", q))
    for (c0, cw) in tiles(TL, TK):
        tb0 = (c0 // TB) * TB
        p.dma("sync", C.xl[:, :, :cw], xlT[:, :, c0:c0 + cw], r=[("xlo", blk, tb0) for blk in range(ND)], w=["xl"])
        emit_norm_router(p, cfg, C, cw, lambda k: m2[:, k:k + 1], lambda k: vs_[:, 6, k:k + 1], tokT[:, :, c0:c0 + cw],
                         lambda t0, tw, c0=c0: gates[c0 + t0:c0 + t0 + tw, :])
    return p.build()


def lat_layout(cfg, lat):
    return [lat[c // cfg.CPB, (c % cfg.CPB) * cfg.TL:(c % cfg.CPB + 1) * cfg.TL] for c in range(cfg.NCORE)]


def lat_unlayout(cfg, per):
    out = np.zeros((cfg.B, cfg.L, per[0].shape[1]), per[0].dtype)
    for c in range(cfg.NCORE):
        out[c // cfg.CPB, (c % cfg.CPB) * cfg.TL:(c % cfg.CPB + 1) * cfg.TL] = per[c]
    return out


def run_glu(cfg, I, mods, yf, yb, u_lat, xl_lat):
    import ml_dtypes
    bf = ml_dtypes.bfloat16
    ND, NC = cfg.ND, cfg.NCORE
    nc = build_glu(cfg)
    wr, br = router_inputs(cfg, I, 1)
    w1 = wfm(I["s5_w1"][0], ND); w2 = wfm(I["s5_w2"][0], ND)
    yfs, ybs, us, xs = lat_layout(cfg, yf), lat_layout(cfg, yb), lat_layout(cfg, u_lat), lat_layout(cfg, xl_lat)
    ims = []
    for c in range(NC):
        mvd = mod_vecs(cfg, mods[1], c // cfg.CPB)
        vecs = np.stack([vfm(np.asarray(v, np.float32), ND) for v in (I["s5_d"][0], I["s5_b1"][0], I["s5_b2"][0], mvd["gt_a"], I["norm_g"][1, 1],
                                                                        mvd["sc_f"], mvd["sh_f"])], axis=1)
        ims.append({"yfT": fm(yfs[c], ND).astype(bf), "ybT": fm(ybs[c], ND).astype(bf), "uT": fm(us[c], ND).astype(bf),
                    "xT": fm(xs[c].astype(np.float32), ND), "w1": w1, "w2": w2, "vecsD": np.ascontiguousarray(vecs),
                    "wrD": wr, "brD": br, "identD": np.eye(128, dtype=np.float32)})
    res = run(nc, ims)
    xl = lat_unlayout(cfg, [unfm(r["xlT"]) for r in res])
    tok = lat_unlayout(cfg, [unfm(np.asarray(r["tokT"])) for r in res])
    gates = lat_unlayout(cfg, [r["gates"] for r in res])
    return xl, tok, gates


def forward(cfg, I, CH=16, PG=8, CAP=1536, log=None):
    import time
    t0 = time.time()

    def lg(msg):
        if log:
            print("[fwd %.1fs] %s" % (time.time() - t0, msg), flush=True)
    D = cfg.D
    mods = run_mods(cfg, I); lg("mods")
    filt = run_filt(cfg, I); lg("filt")
    (v_l, v_c), (x0_l, x0_c) = run_h1(cfg, I, mods); lg("h1")
    y_l, y_c = run_h2r(cfg, I, v_l, v_c, filt); lg("h2r")
    xl, tok, gates = run_h3(cfg, I, mods, y_l, y_c, x0_l, x0_c); lg("h3")
    tok_all = np.concatenate([tok[0].reshape(-1, D), tok[1].reshape(-1, D)], 0)
    gates_all = np.concatenate([gates[0].reshape(-1, 32), gates[1].reshape(-1, 32)], 0)
    y01, g01 = run_moe(cfg, I, 0, tok_all, gates_all, CAP); lg("moe0")
    xl_lat, xl_ctx, u_lat, u_ctx = run_comb(cfg, I, mods, 0, xl[0], xl[1], y01, g01, False); lg("comb0")
    prep = run_s5prep(cfg, I, CH); lg("s5prep")
    mats = s5_matrices(cfg, prep, CH); lg("s5mats")
    yf, yb = run_s5(cfg, mats, u_lat, u_ctx, CH, PG); lg("s5")
    xl3, tok1, gates1 = run_glu(cfg, I, mods, yf, yb, u_lat, xl_lat); lg("glu")
    y01, g01 = run_moe(cfg, I, 1, tok1.reshape(-1, D), gates1.reshape(-1, 32), CAP); lg("moe1")
    _, _, out, _ = run_comb(cfg, I, mods, 1, xl3, None, y01, g01, True); lg("final")
    return np.ascontiguousarray(out.astype(np.float32))


def kernel(**inputs):
    I = {k: np.asarray(v) for k, v in inputs.items()}
    return forward(FULL, I, CH=16, PG=8, CAP=None, log=True)


def dft_tables_r2(Lx):
    M = 4 * Lx
    NH = max(1, Lx // 256)
    half = Lx // 2

    def cis(k, mask=None):
        ang = -2.0 * np.pi * (np.asarray(k, np.int64) % M).astype(np.float64) / M
        t = np.stack([np.cos(ang), np.sin(ang)]).astype(np.float32)
        if mask is not None:
            t = t * mask[None].astype(np.float32)
        return t

    p = np.arange(128)[:, None, None]
    i = np.arange(NH)[None, :, None]
    q = np.arange(128)[None, None, :]
    T = []; TI = []
    for r in range(2):
        m = ((128 * i + p) < half) & (q < half) & np.ones((128, NH, 128), bool)
        T.append(cis((2 * q + 1) * (256 * i + 2 * p + r), m))
        qq = np.arange(128)[:, None, None]; jj = np.arange(NH)[None, :, None]; pp = np.arange(128)[None, None, :]
        mi = ((128 * jj + qq) < half) & (pp < half) & np.ones((128, NH, 128), bool)
        TI.append(cis((256 * jj + 2 * qq + 1) * (2 * pp + r), mi))
    pj = np.arange(128)[:, None, None]; j2 = np.arange(NH)[None, :, None]; r2 = np.arange(2)[None, None, :]
    AL = cis(256 * j2 * (2 * pj + r2))
    qi = np.arange(128)[:, None]; i2 = np.arange(NH)[None, :]
    ALI = cis((2 * qi + 1) * 256 * i2)
    return (np.ascontiguousarray(np.stack(T)), np.ascontiguousarray(np.stack(TI)), np.ascontiguousarray(AL), np.ascontiguousarray(ALI))


def build_h2r(cfg):
    p = Prog()
    B, Cc = cfg.B, cfg.Cc
    ncol = B * Cc
    CW = min(getattr(cfg, 'H2_CW', 512), ncol)
    nbp = CW // Cc
    NHmax = max(1, max(cfg.L, cfg.LC) // 256)
    RAWN = max(2 * NHmax * CW, 2 * NHmax * 128)
    raw = p.sb("raw", [128, RAWN])
    es = [[[p.sb("es%d%d%d" % (a, r, c), [128, NHmax, 128], BF16) for c in range(2)] for r in range(2)] for a in range(2)]
    vs = p.sb("vs", [128, 2, NHmax, CW], BF16)
    hsd = p.sb("hsd", [128, 2, 2, NHmax, Cc], BF16)
    skb = p.sb("skb", [128, ncol])
    kk = p.sb("kk", [128, 2, 2, Cc])
    p1 = p.sb("p1", [128, 2, CW])
    V = p.sb("V", [128, 2, 2, CW])
    Yt = p.sb("Yt", [128, 2, 2, CW])
    tt = [p.sb("tt%d" % i, [128, CW]) for i in range(2)]
    yo = p.sb("yo", [128, CW], BF16)
    pk1 = p.sb("pk1", [128, Cc])
    pV = [[p.ps("pV%d%d" % (r, c), [128, 512]) for c in range(2)] for r in range(2)]
    pK = [p.ps("pK%d" % r, [128, 512]) for r in range(2)]
    pY = p.ps("pY", [128, 512])

    def conv_li(li, Lx, skip):
        NH = max(1, Lx // 256)
        v = p.din("v%d" % li, [128, 2, NH, ncol], BF16)
        hsdi = p.din("hsd%d" % li, [128, 2, 2, NH, Cc], BF16)
        T = p.din("T_%d" % li, [2, 2, 128, NH, 128]); TI = p.din("TI_%d" % li, [2, 2, 128, NH, 128])
        AL = p.din("AL_%d" % li, [128, 2, NH, 2]); ALI = p.din("ALI_%d" % li, [128, 2, NH])
        al = p.sb("al%d" % li, [128, 2, NH, 2]); ali = p.sb("ali%d" % li, [128, 2, NH])
        if li == 0:
            skip = p.din("skip0", [128, ncol])
        y = p.dout("y%d" % li, [128, 2, NH, ncol], BF16)
        Es = p.dtmp("Es%d" % li, [2, NH, 2, 128, NH, 128], BF16)
        Gs = p.dtmp("Gs%d" % li, [2, NH, 2, 128, NH, 128], BF16)
        Ks = p.dtmp("Ks%d" % li, [NH, 128, 2, 2, Cc])
        p.barrier()
        Mv = NH * 128
        bre = raw[:, 0:Mv]; bim = raw[:, Mv:2 * Mv]
        t1 = p_t1[:, :Mv]; t2 = p_t2[:, :Mv]
        p.dma("sync", al[:], AL[:, :, :, :], w=["al"])
        p.dma("sync", ali[:], ALI[:, :, :], w=["ali"])
        bi = 0
        for (Tt, dst, inv) in ((T, Es, False), (TI, Gs, True)):
            for r in range(2):
                p.dma("sync", bre, Tt[r, 0].rearrange("p i q -> p (i q)"), w=["bre"])
                p.dma("pool", bim, Tt[r, 1].rearrange("p i q -> p (i q)"), w=["bim"])
                for j in range(NH):
                    s = bi % 2
                    bi += 1
                    are = ali[:, 0, j:j + 1] if inv else al[:, 0, j, r:r + 1]
                    aim = ali[:, 1, j:j + 1] if inv else al[:, 1, j, r:r + 1]
                    ere = es[s][0][0][:, :NH, :].rearrange("p i q -> p (i q)")
                    eim = es[s][0][1][:, :NH, :].rearrange("p i q -> p (i q)")
                    ak = "ali" if inv else "al"
                    p.op("act", lambda e, aim=aim: e.activation(out=t1, in_=bim, func=AF.Copy, scale=aim), r=["bim", ak], w=["t1"])
                    p.op("dve", lambda e, are=are, ere=ere: e.scalar_tensor_tensor(out=ere, in0=bre, scalar=are, in1=t1, op0=ALU.mult, op1=ALU.subtract),
                         r=["bre", "t1", ak], w=[("es", s, 0, 0)])
                    p.op("act", lambda e, aim=aim: e.activation(out=t2, in_=bre, func=AF.Copy, scale=aim), r=["bre", ak], w=["t2"])
                    p.op("dve", lambda e, are=are, eim=eim: e.scalar_tensor_tensor(out=eim, in0=bim, scalar=are, in1=t2, op0=ALU.mult, op1=ALU.add),
                         r=["bim", "t2", ak], w=[("es", s, 0, 1)])
                    p.dma("sync", dst[r, j, 0], es[s][0][0][:, :NH, :], r=[("es", s, 0, 0)], w=[("tab", li, inv, r, j, 0)], grp=("tst", s, 0))
                    p.dma("sync", dst[r, j, 1], es[s][0][1][:, :NH, :], r=[("es", s, 0, 1)], w=[("tab", li, inv, r, j, 1)], grp=("tst", s, 1))
        p.barrier()
        Zv = raw[:, 0:2 * NH * CW].bitcast(BF16).rearrange("p (r c j w) -> p r c j w", r=2, c=2, j=NH)
        p.dma("sync", hsd[:, :, :, :NH, :], hsdi[:, :, :, :, :], w=["hsd"])
        if li == 0:
            p.dma("sync", skb[:], skip[:, :], w=["skb"])
        for c0 in range(0, ncol, CW):
            first_pass = (c0 == 0)
            p.dma("sync", vs[:, :, :NH, :], v[:, :, :, c0:c0 + CW], w=["vs"])
            for j in range(NH):
                s = j % 2
                for r in range(2):
                    for c in range(2):
                        p.dma("pool" if c else "sync", es[s][r][c][:, :NH, :], Es[r, j, c], w=[("es", s, r, c)])
                for r in range(2):
                    for c in range(2):
                        for i in range(NH):
                            p.op("pe", lambda e, s=s, r=r, c=c, i=i: e.matmul(pV[r][c][:, :CW], lhsT=es[s][r][c][:, i, :], rhs=vs[:, r, i, :],
                                                                             start=(i == 0), stop=(i == NH - 1)),
                                 r=[("es", s, r, c), "vs"], w=[("pV", r, c)])
                if first_pass:
                    for c in range(2):
                        for r in range(2):
                            for i in range(NH):
                                p.op("pe", lambda e, s=s, r=r, c=c, i=i: e.matmul(pK[r][:, :Cc], lhsT=es[s][r][c][:, i, :], rhs=hsd[:, c, r, i, :],
                                                                                 start=(i == 0), stop=(i == NH - 1)),
                                     r=[("es", s, r, c), "hsd"], w=[("pK", r)])
                        p.op("act", lambda e: e.activation(out=pk1[:, :], in_=pK[1][:, :Cc], func=AF.Copy), r=[("pK", 1)], w=["pk1"])
                        p.op("dve", lambda e, c=c: e.tensor_tensor(out=kk[:, 0, c, :], in0=pK[0][:, :Cc], in1=pk1[:, :], op=ALU.add), r=[("pK", 0), "pk1"], w=[("kk", 0, c)])
                        if c == 0:
                            p.op("dve", lambda e, c=c: e.tensor_tensor(out=kk[:, 1, c, :], in0=pK[0][:, :Cc], in1=pk1[:, :], op=ALU.subtract), r=[("pK", 0), "pk1"], w=[("kk", 1, c)])
                        else:
                            p.op("dve", lambda e, c=c: e.scalar_tensor_tensor(out=kk[:, 1, c, :], in0=pK[0][:, :Cc], scalar=-1.0, in1=pk1[:, :], op0=ALU.mult, op1=ALU.add),
                                 r=[("pK", 0), "pk1"], w=[("kk", 1, c)])
                    kkeys = [("kk", h, c) for h in range(2) for c in range(2)]
                    if ncol > CW:
                        p.dma("pool", Ks[j], kk[:, :, :, :], r=kkeys, w=[("Ks", li, j)], grp=("kst",))
                else:
                    kkeys = [("kk", h, c) for h in range(2) for c in range(2)]
                    p.dma("pool", kk[:, :, :, :], Ks[j], r=[("Ks", li, j)], w=kkeys, grp=("kld",))
                for c in range(2):
                    p.op("act", lambda e, c=c: e.activation(out=p1[:, c, :], in_=pV[1][c][:, :CW], func=AF.Copy), r=[("pV", 1, c)], w=[("p1", c)])
                    p.op("dve", lambda e, c=c: e.tensor_tensor(out=V[:, 0, c, :], in0=pV[0][c][:, :CW], in1=p1[:, c, :], op=ALU.add),
                         r=[("pV", 0, c), ("p1", c)], w=[("V", 0, c)])
                    if c == 0:
                        p.op("dve", lambda e, c=c: e.tensor_tensor(out=V[:, 1, c, :], in0=pV[0][c][:, :CW], in1=p1[:, c, :], op=ALU.subtract),
                             r=[("pV", 0, c), ("p1", c)], w=[("V", 1, c)])
                    else:
                        p.op("dve", lambda e, c=c: e.scalar_tensor_tensor(out=V[:, 1, c, :], in0=pV[0][c][:, :CW], scalar=-1.0, in1=p1[:, c, :], op0=ALU.mult, op1=ALU.add),
                             r=[("pV", 0, c), ("p1", c)], w=[("V", 1, c)])
                for h in range(2):
                    kre = kk[:, h, 0, :].unsqueeze(1).broadcast_to([128, nbp, Cc])
                    kim = kk[:, h, 1, :].unsqueeze(1).broadcast_to([128, nbp, Cc])
                    vre = V[:, h, 0, :].rearrange("p (b c) -> p b c", c=Cc)
                    vim = V[:, h, 1, :].rearrange("p (b c) -> p b c", c=Cc)
                    yre = Yt[:, h, 0, :].rearrange("p (b c) -> p b c", c=Cc)
                    yim = Yt[:, h, 1, :].rearrange("p (b c) -> p b c", c=Cc)
                    t3 = [t[:, :].rearrange("p (b c) -> p b c", c=Cc) for t in tt]
                    eng = "dve" if h == 0 else "pool"
                    tk = ["tt0", "tt1"]
                    p.op(eng, lambda e, vre=vre, kre=kre, t3=t3: e.tensor_tensor(out=t3[0], in0=vre, in1=kre, op=ALU.mult), r=[("V", h, 0), ("kk", h, 0)], w=["tt0"])
                    p.op(eng, lambda e, vim=vim, kim=kim, t3=t3: e.tensor_tensor(out=t3[1], in0=vim, in1=kim, op=ALU.mult), r=[("V", h, 1), ("kk", h, 1)], w=["tt1"])
                    p.op(eng, lambda e, yre=yre, t3=t3: e.tensor_tensor(out=yre, in0=t3[0], in1=t3[1], op=ALU.subtract), r=["tt0", "tt1"], w=[("Yt", h, 0)])
                    p.op(eng, lambda e, vre=vre, kim=kim, t3=t3: e.tensor_tensor(out=t3[0], in0=vre, in1=kim, op=ALU.mult), r=[("V", h, 0), ("kk", h, 1)], w=["tt0"])
                    p.op(eng, lambda e, vim=vim, kre=kre, t3=t3: e.tensor_tensor(out=t3[1], in0=vim, in1=kre, op=ALU.mult), r=[("V", h, 1), ("kk", h, 0)], w=["tt1"])
                    p.op(eng, lambda e, yim=yim, t3=t3: e.tensor_tensor(out=yim, in0=t3[0], in1=t3[1], op=ALU.add), r=["tt0", "tt1"], w=[("Yt", h, 1)])
                yk = [("Yt", h, c) for h in range(2) for c in range(2)]
                p.op("dve", lambda e, j=j: e.tensor_tensor(out=Zv[:, 0, 0, j, :], in0=Yt[:, 0, 0, :], in1=Yt[:, 1, 0, :], op=ALU.add), r=yk, w=[("Z", j)])
                p.op("pool", lambda e, j=j: e.tensor_tensor(out=Zv[:, 0, 1, j, :], in0=Yt[:, 0, 1, :], in1=Yt[:, 1, 1, :], op=ALU.subtract), r=yk, w=[("Z", j, 1)])
                p.op("dve", lambda e, j=j: e.tensor_tensor(out=Zv[:, 1, 0, j, :], in0=Yt[:, 0, 0, :], in1=Yt[:, 1, 0, :], op=ALU.subtract), r=yk, w=[("Z", j, 2)])
                p.op("pool", lambda e, j=j: e.tensor_tensor(out=Zv[:, 1, 1, j, :], in0=Yt[:, 0, 1, :], in1=Yt[:, 1, 1, :], op=ALU.add), r=yk, w=[("Z", j, 3)])
            bi2 = 0
            for r in range(2):
                for i in range(NH):
                    s = bi2 % 2
                    bi2 += 1
                    for c in range(2):
                        p.dma("pool" if c else "sync", es[s][0][c][:, :NH, :], Gs[r, i, c], w=[("es", s, 0, c)])
                    n = 0
                    for j in range(NH):
                        for c in range(2):
                            p.op("pe", lambda e, s=s, j=j, c=c, n=n, r=r: e.matmul(pY[:, :CW], lhsT=es[s][0][c][:, j, :], rhs=Zv[:, r, c, j, :],
                                                                                  start=(n == 0), stop=(n == 2 * NH - 1)),
                                 r=[("es", s, 0, c), ("Z", j), ("Z", j, 1), ("Z", j, 2), ("Z", j, 3)], w=["pY"])
                            n += 1
                    p.op("pool", lambda e, i=i, r=r, c0=c0: e.tensor_tensor(out=tt[0][:, :], in0=vs[:, r, i, :], in1=skb[:, c0:c0 + CW], op=ALU.mult),
                         r=["vs", "skb"], w=["tt0"])
                    p.op("dve", lambda e, Lx=Lx: e.scalar_tensor_tensor(out=yo[:, :], in0=pY[:, :CW], scalar=1.0 / Lx, in1=tt[0][:, :],
                                                                       op0=ALU.mult, op1=ALU.add), r=["pY", "tt0"], w=["yo"])
                    p.dma("sync", y[:, r, i, c0:c0 + CW], yo[:, :], r=["yo"], w=[("y", li, r, i, c0)], grp=("yst",))
        return skip

    assert 4 * CW >= NHmax * 128
    p_t1 = V[:, :, :, :].rearrange("p a b w -> p (a b w)"); p_t2 = Yt[:, :, :, :].rearrange("p a b w -> p (a b w)")
    skip = None
    for li, Lx in enumerate((cfg.L, cfg.LC)):
        skip = conv_li(li, Lx, skip)
    return p.build()


def parity_layout(a, Lx, NH):
    X = a.shape[1]
    half = Lx // 2
    out = np.zeros((2, NH * 128, X), a.dtype)
    out[:, :half] = a.reshape(half, 2, X).transpose(1, 0, 2)
    return np.ascontiguousarray(out.reshape(2, NH, 128, X).transpose(2, 0, 1, 3))


def run_h2r(cfg, I, v_lat, v_ctx, filt):
    import ml_dtypes
    bf = ml_dtypes.bfloat16
    B, Cc, D, NC = cfg.B, cfg.Cc, cfg.D, cfg.NCORE
    ncol = B * Cc
    nc = build_h2r(cfg)
    tabs = [dft_tables_r2(Lx) for Lx in (cfg.L, cfg.LC)]
    ims = []
    for c in range(NC):
        m = {}
        for li, (Lx, vv) in enumerate(((cfg.L, v_lat), (cfg.LC, v_ctx))):
            NH = max(1, Lx // 256)
            vc = np.asarray(vv[:, :, c * Cc:(c + 1) * Cc]).transpose(1, 0, 2).reshape(Lx, ncol)
            m["v%d" % li] = parity_layout(vc.astype(bf), Lx, NH)
            hs, hd = filt[li]
            m["hsd%d" % li] = np.ascontiguousarray(np.stack([parity_layout(hs[:, c * Cc:(c + 1) * Cc].astype(bf), Lx, NH),
                                                             parity_layout(hd[:, c * Cc:(c + 1) * Cc].astype(bf), Lx, NH)], axis=1))
            T, TI, AL, ALI = tabs[li]
            m["T_%d" % li], m["TI_%d" % li] = T, TI
            m["AL_%d" % li] = np.ascontiguousarray(AL.transpose(1, 0, 2, 3)); m["ALI_%d" % li] = np.ascontiguousarray(ALI.transpose(1, 0, 2))
        sk = np.tile(I["hy_skip"][0][c * Cc:(c + 1) * Cc], B)
        m["skip0"] = np.ascontiguousarray(np.broadcast_to(sk[None, :], (128, ncol))).astype(np.float32)
        ims.append(m)
    res = run(nc, ims)
    outs = []
    for li, Lx in enumerate((cfg.L, cfg.LC)):
        NH = max(1, Lx // 256)
        half = Lx // 2
        yy = np.zeros((B, Lx, D), bf)
        for c in range(NC):
            a = np.asarray(res[c]["y%d" % li])
            a = a.transpose(1, 2, 0, 3).reshape(2, NH * 128, ncol)[:, :half]
            a = a.transpose(1, 0, 2).reshape(Lx, B, Cc)
            yy[:, :, c * Cc:(c + 1) * Cc] = a.transpose(1, 0, 2)
        outs.append(yy)
    return outs
```

```python
import contextlib
import numpy as np
import concourse.bass as bass
import concourse.mybir as mybir
from concourse.bass_utils import run_bass_kernel_spmd

F32 = mybir.dt.float32
BF16 = mybir.dt.bfloat16
I32 = mybir.dt.int32
ALU = mybir.AluOpType
AF = mybir.ActivationFunctionType
AX = mybir.AxisListType

COMPUTE = ("pe", "act", "dve", "pool")
SKIP_SAME = set()


class Prog:
    def __init__(self):
        self.nc = bass.Bass("TRN2", target_bir_lowering=False)
        self.stack = contextlib.ExitStack()
        self.ops = []
        self.lastw = {}
        self.readers = {}
        self.out_names = []
        self.n_sb = 0
        self.barrier_idx = None

    def din(self, name, shape, dt=F32):
        return self.nc.dram_tensor(name, list(shape), dt, kind="ExternalInput").ap()

    def dout(self, name, shape, dt=F32):
        self.out_names.append(name)
        return self.nc.dram_tensor(name, list(shape), dt, kind="ExternalOutput").ap()

    def dtmp(self, name, shape, dt=F32):
        return self.nc.dram_tensor(name, list(shape), dt, kind="Internal").ap()

    def sb(self, name, shape, dt=F32):
        return self.stack.enter_context(self.nc.sbuf_tensor(name, list(shape), dt))

    def ps(self, name, shape, dt=F32):
        return self.stack.enter_context(self.nc.psum_tensor(name, list(shape), dt))

    def barrier(self):
        deps = set()
        last = {}
        for i, o in enumerate(self.ops):
            key = ("dma", o["grp"]) if o["dma"] else ("eng", o["eng"])
            last[key] = i
        deps = set(last.values())
        idx = len(self.ops)
        d = self.sb("bar%d" % idx, [128, 1])
        self.ops.append(dict(eng="dve", fn=lambda e: e.memset(d[:], 0.0), deps=deps, dma=False))
        self.barrier_idx = idx

    def _deps(self, r, w):
        deps = set()
        if self.barrier_idx is not None:
            deps.add(self.barrier_idx)
        for k in r:
            if k in self.lastw:
                deps.add(self.lastw[k])
        for k in w:
            if k in self.lastw:
                deps.add(self.lastw[k])
            for o in self.readers.get(k, ()):
                deps.add(o)
        return deps

    def _commit(self, idx, r, w):
        for k in r:
            self.readers.setdefault(k, []).append(idx)
        for k in w:
            self.lastw[k] = idx
            self.readers[k] = []

    def op(self, eng, fn, r=(), w=()):
        assert eng in COMPUTE
        idx = len(self.ops)
        deps = self._deps(r, w)
        self.ops.append(dict(eng=eng, fn=fn, deps=deps, dma=False))
        self._commit(idx, r, w)
        return idx

    def dma(self, q, out, in_, r=(), w=(), grp=None, **kw):
        idx = len(self.ops)
        deps = self._deps(r, w)
        if grp is None:
            grp = ("g", tuple(w)[0] if len(w) else tuple(r)[0])
        self.ops.append(dict(eng=q, fn=lambda e: e.dma_start(out=out, in_=in_, **kw),
                             deps=deps, dma=True, grp=grp))
        self._commit(idx, r, w)
        return idx

    def build(self):
        nc = self.nc
        ops = self.ops
        cnt = {}
        semkeys = []
        for o in ops:
            key = ("dma", o["grp"]) if o["dma"] else ("eng", o["eng"])
            if key not in cnt:
                cnt[key] = 0
                semkeys.append(key)
            cnt[key] += 16 if o["dma"] else 1
            o["sem"] = key
            o["val"] = cnt[key]
        sems = {}
        for i, key in enumerate(semkeys):
            sems[key] = self.stack.enter_context(nc.semaphore("s%d" % i))
        streams = {}
        for i, o in enumerate(ops):
            streams.setdefault(o["eng"], []).append(i)
        final_waits = [(sems[k], cnt[k]) for k in semkeys if k[0] == "dma"]
        blk = self.stack.enter_context(nc.Block())

        def emit_stream(eng_name, e, last=False):
            waited = {}
            for i in streams.get(eng_name, []):
                o = ops[i]
                need = {}
                for d in o["deps"]:
                    od = ops[d]
                    if od["eng"] == "pe" and o["eng"] == "pe" and not od["dma"] and not o["dma"]:
                        continue
                    if od["eng"] == o["eng"] and o["eng"] in SKIP_SAME and not od["dma"] and not o["dma"]:
                        continue
                    k = od["sem"]
                    need[k] = max(need.get(k, 0), od["val"])
                for k, v in need.items():
                    if waited.get(k, 0) < v:
                        e.wait_ge(sems[k], v)
                        waited[k] = v
                ins = o["fn"](e)
                ins.then_inc(sems[o["sem"]], 16 if o["dma"] else 1)
            if last:
                for s, v in final_waits:
                    e.wait_ge(s, v)

        @blk.tensor
        def _(e):
            emit_stream("pe", e)

        @blk.scalar
        def _(e):
            emit_stream("act", e)

        @blk.vector
        def _(e):
            emit_stream("dve", e)

        @blk.gpsimd
        def _(e):
            emit_stream("pool", e)

        @blk.sync
        def _(e):
            emit_stream("sync", e, last=True)

        self.stack.close()
        return nc


def run(prog_nc, in_maps, n=8):
    res = run_bass_kernel_spmd(prog_nc, in_maps, core_ids=list(range(n)))
    return res.results


class Cfg:
    def __init__(self, D=2048, B=4, L=4096, LC=256):
        self.D, self.B, self.L, self.LC = D, B, L, LC
        self.NCORE = 8
        self.ND = D // 128
        self.G = D // 16
        self.DE = D // 2
        self.CPB = self.NCORE // B
        self.TL = L // self.CPB
        self.TC = LC // self.CPB
        self.Cc = D // self.NCORE
        self.NE = 32
        self.EPC = self.NE // self.NCORE


FULL = Cfg()
EPS = 1e-6
MAGIC = 12582912.0
TWO_PI = 6.283185307179586
PI_LO = 3.1415925


def fm(a, ND):
    T, D = a.shape
    return np.ascontiguousarray(a.T.reshape(ND, 128, T).transpose(1, 0, 2))


def unfm(a):
    P, ND, T = a.shape
    return np.ascontiguousarray(a.transpose(1, 0, 2).reshape(ND * P, T).T)


def vfm(v, ND):
    return np.ascontiguousarray(v.reshape(ND, 128).T)


def tiles(n, t):
    return [(s, min(t, n - s)) for s in range(0, n, t)]


def build_mods(cfg):
    p = Prog()
    ND, NB1 = cfg.ND, cfg.B + 1
    W = 6 * cfg.D // cfg.NCORE
    ccT = p.din("ccT", [128, ND, NB1])
    aw = p.din("aw", [2, 128, ND, W])
    ab = p.din("ab", [2, 1, W])
    out = p.dout("mods", [2, NB1, W])
    cs = p.sb("cs", [128, ND, 128])
    p.op("dve", lambda e: e.memset(cs[:], 0.0), w=["cs"])
    p.dma("sync", cs[:, :, :NB1], ccT[:, :, :], w=["cs"])
    p.op("act", lambda e: e.activation(out=cs[:, :, :NB1], in_=cs[:, :, :NB1], func=AF.Silu), r=["cs"], w=["cs"])
    WT = 512
    wt_sb = [p.sb("wt%d" % i, [128, ND, WT]) for i in range(2)]
    bt = [p.sb("bt%d" % i, [NB1, WT]) for i in range(2)]
    ot = [p.sb("ot%d" % i, [NB1, WT]) for i in range(2)]
    pm = [p.ps("pm%d" % i, [128, WT]) for i in range(2)]
    it = 0
    for l in range(2):
        for (c0, cw) in tiles(W, WT):
            s = it % 2
            it += 1
            p.dma("sync", wt_sb[s][:, :, :cw], aw[l, :, :, c0:c0 + cw], w=[("wt", s)])
            p.dma("pool", bt[s][:, :cw], ab[l, 0:1, c0:c0 + cw].broadcast_to([NB1, cw]), w=[("bt", s)])
            for k in range(ND):
                p.op("pe", lambda e, s=s, k=k, cw=cw: e.matmul(pm[s][:, :cw], lhsT=cs[:, k, :], rhs=wt_sb[s][:, k, :cw],
                                                               start=(k == 0), stop=(k == ND - 1)),
                     r=["cs", ("wt", s)], w=[("pm", s)])
            p.op("dve", lambda e, s=s, cw=cw: e.tensor_tensor(out=ot[s][:, :cw], in0=pm[s][:NB1, :cw], in1=bt[s][:, :cw], op=ALU.add),
                 r=[("pm", s), ("bt", s)], w=[("ot", s)])
            p.dma("sync", out[l, :, c0:c0 + cw], ot[s][:, :cw], r=[("ot", s)], w=[("out", l, c0)], grp=("st", s))
    return p.build()


def run_mods(cfg, I):
    ND, NC = cfg.ND, cfg.NCORE
    W = 6 * cfg.D // NC
    cc = np.concatenate([I["c"], I["c_ctx"][None]], 0).astype(np.float32)
    ccT = fm(cc, ND)
    nc = build_mods(cfg)
    ims = []
    for c in range(NC):
        aw = I["ada_w"][:, :, c * W:(c + 1) * W]
        aw = np.ascontiguousarray(aw.reshape(2, ND, 128, W).transpose(0, 2, 1, 3))
        ab = np.ascontiguousarray(I["ada_b"][:, None, c * W:(c + 1) * W])
        ims.append({"ccT": ccT, "aw": aw, "ab": ab})
    res = run(nc, ims)
    mods = np.concatenate([r["mods"] for r in res], axis=2)
    return mods


def emit_rstd(p, cfg, xt, xkey, ncols, ones, sq, sqkey, pss, psskey, rstd, rkey):
    ND = cfg.ND
    p.op("pool", lambda e: e.tensor_tensor(out=sq[:, :, :ncols], in0=xt[:, :, :ncols], in1=xt[:, :, :ncols], op=ALU.mult),
         r=[xkey], w=[sqkey])
    for k in range(ND):
        p.op("pe", lambda e, k=k: e.matmul(pss[:, :ncols], lhsT=ones[:], rhs=sq[:, k, :ncols], start=(k == 0), stop=(k == ND - 1)),
             r=[sqkey, "ones"], w=[psskey])
    p.op("act", lambda e: e.activation(out=rstd[:, :ncols], in_=pss[:, :ncols], func=AF.Sqrt, bias=EPS, scale=1.0 / cfg.D),
         r=[psskey], w=[rkey])
    p.op("dve", lambda e: e.reciprocal(out=rstd[:, :ncols], in_=rstd[:, :ncols]), r=[rkey], w=[rkey])


def build_h1(cfg):
    p = Prog()
    ND, D, TL, TC = cfg.ND, cfg.D, cfg.TL, cfg.TC
    TT = TL + TC + 4
    NT = TL + TC
    xT = p.din("xT", [128, ND, TT])
    hm = p.din("hm", [128, 4])
    mv = p.din("mv", [128, 5, ND])
    w_in = p.din("w_in", [128, ND, 3 * D])
    fv = p.din("fv", [128, 5, 3 * ND])
    vT = p.dout("vT", [128, ND, NT], BF16)
    x0T = p.dout("x0T", [128, ND, NT], BF16)

    ones = p.sb("ones", [128, 128])
    p.op("dve", lambda e: e.memset(ones[:], 1.0), w=["ones"])
    hms = p.sb("hms", [128, 4]); p.dma("sync", hms[:], hm[:, :], w=["hms"])
    mvs = p.sb("mvs", [128, 5, ND]); p.dma("sync", mvs[:], mv[:, :, :], w=["mvs"])
    fvs = p.sb("fvs", [128, 5, 3 * ND]); p.dma("sync", fvs[:], fv[:, :, :], w=["fvs"])
    ml = p.sb("ml", [128, 2, ND])
    for i, j in ((0, 1), (1, 3)):
        p.op("dve", lambda e, i=i, j=j: e.scalar_tensor_tensor(out=ml[:, i, :], in0=mvs[:, j, :], scalar=1.0, in1=mvs[:, 0, :],
                                                               op0=ALU.add, op1=ALU.mult), r=["mvs"], w=["ml"])
    u = p.sb("u", [128, ND, TT], BF16)
    TK = 256
    xt = p.sb("xt", [128, ND, TK]); sq = p.sb("sq", [128, ND, TK]); rstd = p.sb("rstd", [128, TK])
    pss = p.ps("pss", [128, TK])
    segs = [(0, TL + 2, 0), (TL + 2, TC + 2, 1)]
    for (s0, sl, mi) in segs:
        for (c0, cw) in tiles(sl, TK):
            a = s0 + c0
            p.dma("sync", xt[:, :, :cw], xT[:, :, a:a + cw], w=["xt"])
            emit_rstd(p, cfg, xt, "xt", cw, ones, sq, "sq", pss, "pss", rstd, "rstd")
            for k in range(ND):
                p.op("dve", lambda e, k=k, cw=cw: e.tensor_tensor(out=sq[:, k, :cw], in0=xt[:, k, :cw], in1=rstd[:, :cw], op=ALU.mult),
                     r=["xt", "rstd"], w=["sq"])
                p.op("dve", lambda e, k=k, cw=cw, a=a, mi=mi: e.tensor_scalar(
                    out=u[:, k, a:a + cw], in0=sq[:, k, :cw], scalar1=ml[:, mi, k:k + 1], scalar2=mvs[:, 2 + 2 * mi, k:k + 1],
                    op0=ALU.mult, op1=ALU.add), r=["sq", "ml", "mvs"], w=["u"])
    wf = [p.sb("wf%d" % i, [128, ND, 128]) for i in range(2)]
    wb = [p.sb("wb%d" % i, [128, ND, 128], BF16) for i in range(2)]
    z = p.sb("z", [128, TT])
    zc = [p.sb("zc%d" % i, [128, TT]) for i in range(3)]
    ob = [p.sb("ob%d" % i, [128, NT], BF16) for i in range(2)]
    pz = [p.ps("pz%d" % i, [128, 512]) for i in range(2)]
    wi = 0
    pi = 0
    for c in range(ND):
        for which, blk in ((1, ND + c), (2, 2 * ND + c), (0, c)):
            s = wi % 2
            wi += 1
            p.dma("sync", wf[s][:], w_in[:, :, blk * 128:(blk + 1) * 128], w=[("wf", s)])
            p.op("pool", lambda e, s=s: e.tensor_copy(out=wb[s][:], in_=wf[s][:]), r=[("wf", s)], w=[("wb", s)])
            for (c0, cw) in tiles(TT, 512):
                q = pi % 2
                pi += 1
                for k in range(ND):
                    p.op("pe", lambda e, s=s, q=q, k=k, c0=c0, cw=cw: e.matmul(
                        pz[q][:, :cw], lhsT=wb[s][:, k, :], rhs=u[:, k, c0:c0 + cw], start=(k == 0), stop=(k == ND - 1)),
                        r=[("wb", s), "u"], w=[("pz", q)])
                p.op("act", lambda e, q=q, c0=c0, cw=cw, blk=blk: e.activation(
                    out=z[:, c0:c0 + cw], in_=pz[q][:, :cw], func=AF.Identity, bias=fvs[:, 0, blk:blk + 1], scale=1.0),
                    r=[("pz", q), "fvs"], w=["z"])
            for hi, col in enumerate((0, TL + 1, TL + 2, TL + TC + 3)):
                p.op("dve", lambda e, hi=hi, col=col: e.tensor_scalar(out=z[:, col:col + 1], in0=z[:, col:col + 1],
                                                                     scalar1=hms[:, hi:hi + 1], scalar2=None, op0=ALU.mult),
                     r=["z", "hms"], w=["z"])
            zo = zc[which]
            zk = ("zc", which)
            for (a, n) in ((1, TL), (TL + 3, TC)):
                p.op("dve", lambda e, a=a, n=n, blk=blk, zo=zo: e.tensor_scalar(
                    out=zo[:, a:a + n], in0=z[:, a - 1:a - 1 + n], scalar1=fvs[:, 1, blk:blk + 1], scalar2=fvs[:, 4, blk:blk + 1],
                    op0=ALU.mult, op1=ALU.add), r=["z", "fvs"], w=[zk])
                p.op("dve", lambda e, a=a, n=n, blk=blk, zo=zo: e.scalar_tensor_tensor(
                    out=zo[:, a:a + n], in0=z[:, a:a + n], scalar=fvs[:, 2, blk:blk + 1], in1=zo[:, a:a + n],
                    op0=ALU.mult, op1=ALU.add), r=["z", "fvs", zk], w=[zk])
                p.op("dve", lambda e, a=a, n=n, blk=blk, zo=zo: e.scalar_tensor_tensor(
                    out=zo[:, a:a + n], in0=z[:, a + 1:a + 1 + n], scalar=fvs[:, 3, blk:blk + 1], in1=zo[:, a:a + n],
                    op0=ALU.mult, op1=ALU.add), r=["z", "fvs", zk], w=[zk])
            if which == 2:
                for (a, n, o0) in ((1, TL, 0), (TL + 3, TC, TL)):
                    p.op("pool", lambda e, a=a, n=n, o0=o0: e.tensor_tensor(out=ob[0][:, o0:o0 + n], in0=zc[2][:, a:a + n],
                                                                           in1=zc[1][:, a:a + n], op=ALU.mult),
                         r=[("zc", 1), ("zc", 2)], w=[("ob", 0)])
                p.dma("pool", vT[:, c, :], ob[0][:], r=[("ob", 0)], w=[("vT", c)], grp=("st", 0))
            if which == 0:
                for (a, n, o0) in ((1, TL, 0), (TL + 3, TC, TL)):
                    p.op("pool", lambda e, a=a, n=n, o0=o0: e.tensor_copy(out=ob[1][:, o0:o0 + n], in_=zc[0][:, a:a + n]),
                         r=[("zc", 0)], w=[("ob", 1)])
                p.dma("pool", x0T[:, c, :], ob[1][:], r=[("ob", 1)], w=[("x0T", c)], grp=("st", 1))
    return p.build()


def tok_layout(cfg, lat, ctx):
    outs = []
    for c in range(cfg.NCORE):
        b, h = c // cfg.CPB, c % cfg.CPB
        outs.append(np.concatenate([lat[b, h * cfg.TL:(h + 1) * cfg.TL], ctx[b, h * cfg.TC:(h + 1) * cfg.TC]], 0))
    return outs


def tok_unlayout(cfg, per_core):
    Dd = per_core[0].shape[1]
    lat = np.zeros((cfg.B, cfg.L, Dd), per_core[0].dtype)
    ctx = np.zeros((cfg.B, cfg.LC, Dd), per_core[0].dtype)
    for c in range(cfg.NCORE):
        b, h = c // cfg.CPB, c % cfg.CPB
        lat[b, h * cfg.TL:(h + 1) * cfg.TL] = per_core[c][:cfg.TL]
        ctx[b, h * cfg.TC:(h + 1) * cfg.TC] = per_core[c][cfg.TL:]
    return lat, ctx


def mod_vecs(cfg, mods_l, b):
    D = cfg.D
    names = ["sh_a", "sc_a", "gt_a", "sh_f", "sc_f", "gt_f"]
    out = {}
    for i, n in enumerate(names):
        out[n] = mods_l[b, i * D:(i + 1) * D]
        out["c" + n] = mods_l[cfg.B, i * D:(i + 1) * D]
    return out


def run_h1(cfg, I, mods):
    ND, D, TL, TC, NC = cfg.ND, cfg.D, cfg.TL, cfg.TC, cfg.NCORE
    nc = build_h1(cfg)
    x, ctx = I["x"], I["ctx"]
    w_in = np.ascontiguousarray(I["hy_w_in"][0].reshape(ND, 128, 3 * D).transpose(1, 0, 2))
    fvec = np.stack([vfm(v, 3 * ND) for v in (I["hy_b_in"][0], I["hy_conv_w"][0, 0], I["hy_conv_w"][0, 1],
                                               I["hy_conv_w"][0, 2], I["hy_conv_b"][0])], axis=1)
    fvec = np.ascontiguousarray(fvec)
    ims = []
    zrow = np.zeros((1, D), np.float32)
    for c in range(NC):
        b, h = c // cfg.CPB, c % cfg.CPB
        l0, l1 = h * TL, (h + 1) * TL
        c0, c1 = h * TC, (h + 1) * TC
        hl = x[b, l0 - 1:l0] if l0 > 0 else zrow
        hr = x[b, l1:l1 + 1] if l1 < cfg.L else zrow
        chl = ctx[b, c0 - 1:c0] if c0 > 0 else zrow
        chr_ = ctx[b, c1:c1 + 1] if c1 < cfg.LC else zrow
        cols = np.concatenate([hl, x[b, l0:l1], hr, chl, ctx[b, c0:c1], chr_], 0)
        hm = np.array([l0 > 0, l1 < cfg.L, c0 > 0, c1 < cfg.LC], np.float32)
        mvd = mod_vecs(cfg, mods[0], b)
        mv = np.stack([vfm(v, ND) for v in (I["norm_g"][0, 0], mvd["sc_a"], mvd["sh_a"], mvd["csc_a"], mvd["csh_a"])], axis=1)
        ims.append({"xT": fm(cols, ND), "hm": np.ascontiguousarray(np.broadcast_to(hm, (128, 4))),
                    "mv": np.ascontiguousarray(mv), "w_in": w_in, "fv": fvec})
    res = run(nc, ims)
    v = [unfm(np.asarray(r["vT"]).astype(np.float32)) for r in res]
    x0 = [unfm(np.asarray(r["x0T"]).astype(np.float32)) for r in res]
    return tok_unlayout(cfg, v), tok_unlayout(cfg, x0)


def hy_consts(Lx, D):
    f32 = np.float32
    t = np.linspace(0.0, 1.0, Lx, dtype=f32)[:, None]
    w = (f32(2.0 * np.pi / Lx) * np.arange(Lx, dtype=f32))[:, None]
    bands = np.linspace(1e-4, 15, 16, dtype=f32)[None, :]
    z = np.concatenate([t, np.cos(bands * w), -np.sin(bands * w)], axis=-1).astype(f32)
    max_decay = np.log(1e-2) / 0.3
    min_decay = np.log(1e-2) / 1.5
    deltas = np.abs(np.linspace(min_decay, max_decay, D, dtype=f32))
    win = np.exp(-t * deltas[None, :]).astype(f32)
    return z, win


def emit_sin(p, e_out, okey, src, skey, ncols, tmp, tkey, scale_ap, bias_ap, extra_r=()):
    t0, t1 = tmp
    p.op("dve", lambda e: e.tensor_scalar(out=t0[:, :ncols], in0=src, scalar1=scale_ap, scalar2=bias_ap, op0=ALU.mult, op1=ALU.add),
         r=[skey] + list(extra_r), w=[(tkey, 0)])
    p.op("dve", lambda e: e.tensor_scalar(out=t1[:, :ncols], in0=t0[:, :ncols], scalar1=1.0 / TWO_PI, scalar2=MAGIC, op0=ALU.mult, op1=ALU.add),
         r=[(tkey, 0)], w=[(tkey, 1)])
    p.op("dve", lambda e: e.tensor_scalar(out=t1[:, :ncols], in0=t1[:, :ncols], scalar1=MAGIC, scalar2=-TWO_PI, op0=ALU.subtract, op1=ALU.mult),
         r=[(tkey, 1)], w=[(tkey, 1)])
    p.op("dve", lambda e: e.tensor_tensor(out=t0[:, :ncols], in0=t0[:, :ncols], in1=t1[:, :ncols], op=ALU.add),
         r=[(tkey, 0), (tkey, 1)], w=[(tkey, 0)])
    p.op("dve", lambda e: e.tensor_scalar(out=t0[:, :ncols], in0=t0[:, :ncols], scalar1=PI_LO, scalar2=-PI_LO, op0=ALU.min, op1=ALU.max),
         r=[(tkey, 0)], w=[(tkey, 0)])
    p.op("act", lambda e: e.activation(out=e_out, in_=t0[:, :ncols], func=AF.Sin), r=[(tkey, 0)], w=[okey])


def build_filt(cfg):
    p = Prog()
    Cc = cfg.Cc
    CP = min(Cc, 128)
    NCH = Cc // CP
    fw1 = p.din("fw1", [128, 128]); fw2 = p.din("fw2", [128, 128]); fw3 = p.din("fw3", [128, 2, NCH, 128])
    pv = p.din("pv", [128, 3])
    w1s = p.sb("w1s", [128, 128]); w2s = p.sb("w2s", [128, 128]); w3s = p.sb("w3s", [128, 2, NCH, 128]); pvs = p.sb("pvs", [128, 3])
    p.dma("sync", w1s[:], fw1[:, :], w=["w1s"]); p.dma("sync", w2s[:], fw2[:, :], w=["w2s"])
    p.dma("sync", w3s[:], fw3[:, :, :, :], w=["w3s"]); p.dma("sync", pvs[:], pv[:, :], w=["pvs"])
    fb = p.sb("fb", [128, 2])
    p.op("dve", lambda e: e.tensor_scalar(out=fb[:, 0:2], in0=pvs[:, 0:2], scalar1=pvs[:, 2:3], scalar2=None, op0=ALU.mult),
         r=["pvs"], w=["fb"])
    Lmax = max(cfg.L, cfg.LC)
    CT = 512
    zt = p.sb("zt", [128, CT]); h1 = p.sb("h1", [128, CT]); h2 = p.sb("h2", [128, Lmax])
    tmp = [p.sb("tmpa", [128, CT]), p.sb("tmpb", [128, CT])]
    hw = [p.sb("hw%d" % i, [128, Lmax]) for i in range(2)]
    wn = p.sb("wn", [128, Lmax])
    ab = p.sb("ab", [128, Lmax]); nr = p.sb("nr", [128, 2]); rn = p.sb("rn", [128, 1])
    ho = [p.sb("ho%d" % i, [128, Lmax], BF16) for i in range(2)]
    pa = p.ps("pa", [128, CT]); pb = p.ps("pb", [128, CT]); pc = p.ps("pc", [128, CT])
    for li, Lx in enumerate((cfg.L, cfg.LC)):
        zT = p.din("zT%d" % li, [128, Lx]); winT = p.din("winT%d" % li, [CP, NCH, Lx])
        hsT = p.dout("hsT%d" % li, [CP, NCH, Lx], BF16); hdT = p.dout("hdT%d" % li, [CP, NCH, Lx], BF16)
        for (c0, cw) in tiles(Lx, CT):
            p.dma("sync", zt[:, :cw], zT[:, c0:c0 + cw], w=["zt"])
            p.op("pe", lambda e, cw=cw: e.matmul(pa[:, :cw], lhsT=w1s[:], rhs=zt[:, :cw], start=True, stop=True), r=["w1s", "zt"], w=["pa"])
            emit_sin(p, h1[:, :cw], "h1", pa[:, :cw], "pa", cw, tmp, "tmp", pvs[:, 2:3], fb[:, 0:1], extra_r=["pvs", "fb"])
            p.op("pe", lambda e, cw=cw: e.matmul(pb[:, :cw], lhsT=w2s[:], rhs=h1[:, :cw], start=True, stop=True), r=["w2s", "h1"], w=["pb"])
            emit_sin(p, h2[:, c0:c0 + cw], "h2", pb[:, :cw], "pb", cw, tmp, "tmp", pvs[:, 2:3], fb[:, 1:2], extra_r=["pvs", "fb"])
        for ch in range(NCH):
            p.dma("sync", wn[:CP, :Lx], winT[:, ch, :], w=["wn"])
            for d in range(2):
                for (c0, cw) in tiles(Lx, CT):
                    p.op("pe", lambda e, d=d, ch=ch, c0=c0, cw=cw: e.matmul(pc[:, :cw], lhsT=w3s[:, d, ch, :], rhs=h2[:, c0:c0 + cw], start=True, stop=True),
                         r=["w3s", "h2"], w=["pc"])
                    p.op("dve", lambda e, d=d, c0=c0, cw=cw: e.tensor_tensor(out=hw[d][:CP, c0:c0 + cw], in0=pc[:CP, :cw], in1=wn[:CP, c0:c0 + cw], op=ALU.mult),
                         r=["pc", "wn"], w=[("hw", d)])
            p.op("dve", lambda e: e.memset(hw[1][:CP, 0:1], 0.0), r=[("hw", 1)], w=[("hw", 1)])
            for d in range(2):
                p.op("dve", lambda e, d=d, Lx=Lx: e.scalar_tensor_tensor(out=ab[:CP, :Lx], in0=hw[d][:CP, :Lx], scalar=-1.0, in1=hw[d][:CP, :Lx], op0=ALU.mult, op1=ALU.max),
                     r=[("hw", d)], w=["ab"])
                p.op("dve", lambda e, d=d, Lx=Lx: e.tensor_reduce(out=nr[:CP, d:d + 1], in_=ab[:CP, :Lx], axis=AX.X, op=ALU.add),
                     r=["ab"], w=["nr"])
            p.op("dve", lambda e: e.tensor_tensor(out=rn[:CP, :], in0=nr[:CP, 0:1], in1=nr[:CP, 1:2], op=ALU.add), r=["nr"], w=["rn"])
            p.op("dve", lambda e: e.reciprocal(out=rn[:CP, :], in_=rn[:CP, :]), r=["rn"], w=["rn"])
            p.op("dve", lambda e, Lx=Lx: e.tensor_tensor(out=ab[:CP, :Lx], in0=hw[0][:CP, :Lx], in1=hw[1][:CP, :Lx], op=ALU.add),
                 r=[("hw", 0), ("hw", 1)], w=["ab"])
            p.op("dve", lambda e, Lx=Lx: e.tensor_scalar(out=ho[0][:CP, :Lx], in0=ab[:CP, :Lx], scalar1=rn[:CP, 0:1], scalar2=None, op0=ALU.mult),
                 r=["ab", "rn"], w=[("ho", 0)])
            p.op("dve", lambda e, Lx=Lx: e.tensor_tensor(out=ab[:CP, :Lx], in0=hw[0][:CP, :Lx], in1=hw[1][:CP, :Lx], op=ALU.subtract),
                 r=[("hw", 0), ("hw", 1), ("ho", 0)], w=["ab"])
            p.op("dve", lambda e, Lx=Lx: e.tensor_scalar(out=ho[1][:CP, :Lx], in0=ab[:CP, :Lx], scalar1=rn[:CP, 0:1], scalar2=None, op0=ALU.mult),
                 r=["ab", "rn"], w=[("ho", 1)])
            p.dma("pool", hsT[:, ch, :], ho[0][:CP, :Lx], r=[("ho", 0)], w=[("hs", li, ch)], grp=("st", 0))
            p.dma("pool", hdT[:, ch, :], ho[1][:CP, :Lx], r=[("ho", 1)], w=[("hd", li, ch)], grp=("st", 1))
    return p.build()


def pad128(a):
    out = np.zeros((128,) + a.shape[1:], np.float32)
    out[:a.shape[0]] = a
    return out


def run_filt(cfg, I):
    Cc, D, NC = cfg.Cc, cfg.D, cfg.NCORE
    CP = min(Cc, 128); NCH = Cc // CP
    nc = build_filt(cfg)
    fw1 = np.zeros((128, 128), np.float32); fw1[:33, :64] = I["hy_fw1"][0]
    fw2 = np.zeros((128, 128), np.float32); fw2[:64, :64] = I["hy_fw2"][0]
    pv = np.zeros((128, 3), np.float32)
    pv[:64, 0] = I["hy_fb1"][0]; pv[:64, 1] = I["hy_fb2"][0]; pv[:64, 2] = I["hy_freq"][0]
    consts = [hy_consts(Lx, D) for Lx in (cfg.L, cfg.LC)]
    ims = []
    for c in range(NC):
        fw3 = np.zeros((128, 2, NCH, 128), np.float32)
        for d in range(2):
            blk = I["hy_fw3"][0][:, d * D + c * Cc: d * D + (c + 1) * Cc]
            fw3[:64, d, :, :CP] = blk.reshape(64, NCH, CP)
        m = {"fw1": fw1, "fw2": fw2, "fw3": fw3, "pv": pv}
        for li, (z, win) in enumerate(consts):
            m["zT%d" % li] = pad128(np.ascontiguousarray(z.T))
            wc = win[:, c * Cc:(c + 1) * Cc].T
            m["winT%d" % li] = np.ascontiguousarray(wc.reshape(NCH, CP, -1).transpose(1, 0, 2))
        ims.append(m)
    res = run(nc, ims)
    outs = []
    for li, Lx in enumerate((cfg.L, cfg.LC)):
        hs = np.zeros((Lx, D), np.float32); hd = np.zeros((Lx, D), np.float32)
        for c in range(NC):
            a = np.asarray(res[c]["hsT%d" % li]).astype(np.float32).transpose(1, 0, 2).reshape(Cc, Lx)
            b = np.asarray(res[c]["hdT%d" % li]).astype(np.float32).transpose(1, 0, 2).reshape(Cc, Lx)
            hs[:, c * Cc:(c + 1) * Cc] = a.T
            hd[:, c * Cc:(c + 1) * Cc] = b.T
        outs.append((hs, hd))
    return outs


def dft_tables(Lx):
    NS = Lx // 128
    M = 4 * Lx
    p = np.arange(128)[:, None, None]
    i = np.arange(NS)[None, :, None]
    q = np.arange(128)[None, None, :]

    def cis(k):
        ang = -2.0 * np.pi * (np.asarray(k, np.int64) % M).astype(np.float64) / M
        return np.stack([np.cos(ang), np.sin(ang)]).astype(np.float32)

    B2 = cis((2 * q + 1) * (128 * i + p))
    j = np.arange(NS)[None, :, None]
    ii = np.arange(NS)[None, None, :]
    A2 = cis(256 * j * (128 * ii + np.arange(128)[:, None, None]))
    qq = np.arange(128)[:, None, None]
    jj = np.arange(NS)[None, :, None]
    pp = np.arange(128)[None, None, :]
    B3 = cis((2 * (128 * jj + qq) + 1) * pp)
    i3 = np.arange(NS)[None, :, None]
    j3 = np.arange(NS)[None, None, :]
    A3 = cis((2 * (128 * j3 + qq) + 1) * 128 * i3)
    return [np.ascontiguousarray(t) for t in (A2, B2, A3, B3)]


def build_h2(cfg):
    p = Prog()
    B, Cc = cfg.B, cfg.Cc
    ncol = B * Cc
    CW = min(getattr(cfg, 'H2_CW', 512), ncol)
    nbp = CW // Cc
    NSmax = max(cfg.L, cfg.LC) // 128
    RAWN = 4 * NSmax * 128
    raw = p.sb("raw", [128, RAWN])
    es = [[p.sb("es%d%d" % (a, b), [128, NSmax, 128], BF16) for b in range(2)] for a in range(2)]
    a_s = p.sb("a_s", [128, 2, NSmax, NSmax])
    vs = p.sb("vs", [128, NSmax, CW], BF16)
    hsd = p.sb("hsd", [128, 2, NSmax, Cc], BF16)
    skb = p.sb("skb", [128, ncol])
    kk = p.sb("kk", [128, 2, Cc])
    tt = [p.sb("tt%d" % i, [128, CW]) for i in range(3)]
    yo = p.sb("yo", [128, CW], BF16)
    pV = [p.ps("pV%d" % i, [128, 512]) for i in range(2)]
    pK = [p.ps("pK%d" % i, [128, 512]) for i in range(2)]
    pY = p.ps("pY", [128, 512])
    def conv_li(li, Lx, skip):
        NS = Lx // 128
        NF = NS
        Mv = NS * 128
        v = p.din("v%d" % li, [128, NS, ncol], BF16)
        hsdi = p.din("hsd%d" % li, [128, 2, NS, Cc], BF16)
        A2 = p.din("A2_%d" % li, [2, 128, NF, NS]); B2 = p.din("B2_%d" % li, [2, 128, NS, 128])
        A3 = p.din("A3_%d" % li, [2, 128, NS, NF]); B3 = p.din("B3_%d" % li, [2, 128, NF, 128])
        if li == 0:
            skip = p.din("skip0", [128, ncol])
        y = p.dout("y%d" % li, [128, NS, ncol], BF16)
        Es = p.dtmp("Es%d" % li, [NF, 2, 128, NS, 128], BF16)
        Gs = p.dtmp("Gs%d" % li, [NS, 2, 128, NF, 128], BF16)
        Ks = p.dtmp("Ks%d" % li, [NF, 128, 2, Cc])
        p.barrier()
        bre = raw[:, 0:Mv].rearrange("p (i q) -> p i q", q=128)
        bim = raw[:, Mv:2 * Mv].rearrange("p (i q) -> p i q", q=128)
        t1 = raw[:, 2 * Mv:3 * Mv].rearrange("p (i q) -> p i q", q=128)
        t2 = raw[:, 3 * Mv:4 * Mv].rearrange("p (i q) -> p i q", q=128)
        for (At, Bt, dst, nout) in ((A2, B2, Es, NF), (A3, B3, Gs, NS)):
            p.dma("sync", raw[:, 0:Mv], Bt[0].rearrange("p i q -> p (i q)"), w=["bre"])
            p.dma("sync", raw[:, Mv:2 * Mv], Bt[1].rearrange("p i q -> p (i q)"), w=["bim"])
            p.dma("sync", a_s[:, 0, :nout, :NS], At[0], w=["a_s0"])
            p.dma("sync", a_s[:, 1, :nout, :NS], At[1], w=["a_s1"])
            for j in range(nout):
                s = j % 2
                are = a_s[:, 0, j, :NS].unsqueeze(2).broadcast_to([128, NS, 128])
                aim = a_s[:, 1, j, :NS].unsqueeze(2).broadcast_to([128, NS, 128])
                ere = es[s][0][:, :NS, :]
                eim = es[s][1][:, :NS, :]
                p.op("dve", lambda e, are=are: e.tensor_tensor(out=t1, in0=bre, in1=are, op=ALU.mult), r=["bre", "a_s0"], w=["t1"])
                p.op("pool", lambda e, aim=aim: e.tensor_tensor(out=t2, in0=bim, in1=aim, op=ALU.mult), r=["bim", "a_s1"], w=["t2"])
                p.op("dve", lambda e, ere=ere: e.tensor_tensor(out=ere, in0=t1, in1=t2, op=ALU.subtract), r=["t1", "t2"], w=[("es", s, 0)])
                p.op("dve", lambda e, are=are: e.tensor_tensor(out=t1, in0=bim, in1=are, op=ALU.mult), r=["bim", "a_s0"], w=["t1"])
                p.op("pool", lambda e, aim=aim: e.tensor_tensor(out=t2, in0=bre, in1=aim, op=ALU.mult), r=["bre", "a_s1"], w=["t2"])
                p.op("dve", lambda e, eim=eim: e.tensor_tensor(out=eim, in0=t1, in1=t2, op=ALU.add), r=["t1", "t2"], w=[("es", s, 1)])
                p.dma("sync", dst[j, 0], ere, r=[("es", s, 0)], w=[("tab", li, j, 0)], grp=("tst", s, 0))
                p.dma("sync", dst[j, 1], eim, r=[("es", s, 1)], w=[("tab", li, j, 1)], grp=("tst", s, 1))
        p.barrier()
        Yv = raw[:, 0:NF * CW].bitcast(BF16).rearrange("p (c j w) -> p c j w", c=2, j=NF)
        p.dma("sync", hsd[:, :, :NS, :], hsdi[:, :, :, :], w=["hsd"])
        if li == 0:
            p.dma("sync", skb[:], skip[:, :], w=["skb"])
        for c0 in range(0, ncol, CW):
            p.dma("sync", vs[:, :NS, :], v[:, :, c0:c0 + CW], w=["vs"])
            for j in range(NF):
                s = j % 2
                for c in range(2):
                    p.dma("pool" if c else "sync", es[s][c][:, :NS, :], Es[j, c], w=[("es", s, c)])
                first_pass = (c0 == 0)
                for i in range(NS):
                    fl = dict(start=(i == 0), stop=(i == NS - 1))
                    p.op("pe", lambda e, s=s, i=i, fl=fl: e.matmul(pV[0][:, :CW], lhsT=es[s][0][:, i, :], rhs=vs[:, i, :], **fl),
                         r=[("es", s, 0), "vs"], w=["pV0"])
                    p.op("pe", lambda e, s=s, i=i, fl=fl: e.matmul(pV[1][:, :CW], lhsT=es[s][1][:, i, :], rhs=vs[:, i, :], **fl),
                         r=[("es", s, 1), "vs"], w=["pV1"])
                    if first_pass:
                        p.op("pe", lambda e, s=s, i=i, fl=fl: e.matmul(pK[0][:, :Cc], lhsT=es[s][0][:, i, :], rhs=hsd[:, 0, i, :], **fl),
                             r=[("es", s, 0), "hsd"], w=["pK0"])
                        p.op("pe", lambda e, s=s, i=i, fl=fl: e.matmul(pK[1][:, :Cc], lhsT=es[s][1][:, i, :], rhs=hsd[:, 1, i, :], **fl),
                             r=[("es", s, 1), "hsd"], w=["pK1"])
                if first_pass:
                    for c in range(2):
                        p.op("act", lambda e, c=c: e.activation(out=kk[:, c, :], in_=pK[c][:, :Cc], func=AF.Copy), r=["pK%d" % c], w=[("kk", c)])
                    if ncol > CW:
                        p.dma("pool", Ks[j], kk[:, :, :], r=[("kk", 0), ("kk", 1)], w=[("Ks", li, j)], grp=("kst",))
                else:
                    p.dma("pool", kk[:, :, :], Ks[j], r=[("Ks", li, j)], w=[("kk", 0), ("kk", 1)], grp=("kld",))
                kre = kk[:, 0, :].unsqueeze(1).broadcast_to([128, nbp, Cc])
                kim = kk[:, 1, :].unsqueeze(1).broadcast_to([128, nbp, Cc])
                vre = pV[0][:, :CW].rearrange("p (b c) -> p b c", c=Cc)
                vim = pV[1][:, :CW].rearrange("p (b c) -> p b c", c=Cc)
                t3 = [t[:, :].rearrange("p (b c) -> p b c", c=Cc) for t in tt]
                yre = Yv[:, 0, j, :].rearrange("p (b c) -> p b c", c=Cc)
                yim = Yv[:, 1, j, :].rearrange("p (b c) -> p b c", c=Cc)
                p.op("dve", lambda e, vre=vre, kre=kre, t3=t3: e.tensor_tensor(out=t3[0], in0=vre, in1=kre, op=ALU.mult), r=["pV0", ("kk", 0)], w=["tt0"])
                p.op("dve", lambda e, vim=vim, kim=kim, t3=t3: e.tensor_tensor(out=t3[1], in0=vim, in1=kim, op=ALU.mult), r=["pV1", ("kk", 1)], w=["tt1"])
                p.op("pool", lambda e, yre=yre, t3=t3: e.tensor_tensor(out=yre, in0=t3[0], in1=t3[1], op=ALU.subtract), r=["tt0", "tt1"], w=[("Y", j)])
                p.op("dve", lambda e, vre=vre, kim=kim, t3=t3: e.tensor_tensor(out=t3[2], in0=vre, in1=kim, op=ALU.mult), r=["pV0", ("kk", 1)], w=["tt2"])
                p.op("dve", lambda e, vim=vim, kre=kre, t3=t3: e.tensor_tensor(out=t3[0], in0=vim, in1=kre, op=ALU.mult), r=["pV1", ("kk", 0), "tt0"], w=["tt0"])
                p.op("pool", lambda e, yim=yim, t3=t3: e.tensor_tensor(out=yim, in0=t3[2], in1=t3[0], op=ALU.add), r=["tt0", "tt2"], w=[("Y", j)])
            for i in range(NS):
                s = i % 2
                for c in range(2):
                    p.dma("pool" if c else "sync", es[s][c][:, :NF, :], Gs[i, c], w=[("es", s, c)])
                n = 0
                for j in range(NF):
                    for c in range(2):
                        p.op("pe", lambda e, s=s, j=j, c=c, n=n: e.matmul(pY[:, :CW], lhsT=es[s][c][:, j, :], rhs=Yv[:, c, j, :],
                                                                         start=(n == 0), stop=(n == 2 * NF - 1)),
                             r=[("es", s, c), ("Y", j)], w=["pY"])
                        n += 1
                p.op("pool", lambda e, i=i, c0=c0: e.tensor_tensor(out=tt[0][:, :], in0=vs[:, i, :], in1=skb[:, c0:c0 + CW], op=ALU.mult),
                     r=["vs", "skb"], w=["tt0"])
                p.op("dve", lambda e, Lx=Lx: e.scalar_tensor_tensor(out=yo[:, :], in0=pY[:, :CW], scalar=1.0 / Lx, in1=tt[0][:, :],
                                                                   op0=ALU.mult, op1=ALU.add), r=["pY", "tt0"], w=["yo"])
                p.dma("sync", y[:, i, c0:c0 + CW], yo[:, :], r=["yo"], w=[("y", li, i, c0)], grp=("yst",))
        return skip

    skip = None
    for li, Lx in enumerate((cfg.L, cfg.LC)):
        skip = conv_li(li, Lx, skip)
    return p.build()


def run_h2(cfg, I, v_lat, v_ctx, filt):
    import ml_dtypes
    bf = ml_dtypes.bfloat16
    B, Cc, D, NC = cfg.B, cfg.Cc, cfg.D, cfg.NCORE
    ncol = B * Cc
    nc = build_h2(cfg)
    tabs = [dft_tables(Lx) for Lx in (cfg.L, cfg.LC)]
    ims = []
    for c in range(NC):
        m = {}
        for li, (Lx, vv) in enumerate(((cfg.L, v_lat), (cfg.LC, v_ctx))):
            NS = Lx // 128
            vc = vv[:, :, c * Cc:(c + 1) * Cc].transpose(1, 0, 2).reshape(Lx, ncol)
            m["v%d" % li] = np.ascontiguousarray(vc.reshape(NS, 128, ncol).transpose(1, 0, 2)).astype(bf)
            hs, hd = filt[li]
            hh = np.stack([hs[:, c * Cc:(c + 1) * Cc], hd[:, c * Cc:(c + 1) * Cc]])
            m["hsd%d" % li] = np.ascontiguousarray(hh.reshape(2, NS, 128, Cc).transpose(2, 0, 1, 3)).astype(bf)
            A2, B2, A3, B3 = tabs[li]
            m["A2_%d" % li], m["B2_%d" % li], m["A3_%d" % li], m["B3_%d" % li] = A2, B2, A3, B3
        sk = np.tile(I["hy_skip"][0][c * Cc:(c + 1) * Cc], B)
        m["skip0"] = np.ascontiguousarray(np.broadcast_to(sk[None, :], (128, ncol))).astype(np.float32)
        ims.append(m)
    res = run(nc, ims)
    outs = []
    for li, Lx in enumerate((cfg.L, cfg.LC)):
        NS = Lx // 128
        yy = np.zeros((B, Lx, D), bf)
        for c in range(NC):
            a = np.asarray(res[c]["y%d" % li]).transpose(1, 0, 2).reshape(Lx, B, Cc)
            yy[:, :, c * Cc:(c + 1) * Cc] = a.transpose(1, 0, 2)
        outs.append(yy)
    return outs


class PostCtx:
    def __init__(self, p, cfg, TK):
        ND = cfg.ND
        self.TK = TK
        self.ones = p.sb("ones", [128, 128]); p.op("dve", lambda e: e.memset(self.ones[:], 1.0), w=["ones"])
        self.ident = p.sb("ident", [128, 128])
        self.xt = p.sb("xt", [128, ND, TK]); self.xl = p.sb("xl", [128, ND, TK]); self.sq = p.sb("sq", [128, ND, TK])
        self.tokf = p.sb("tokf", [128, ND, TK]); self.tokb = p.sb("tokb", [128, ND, TK], BF16)
        self.rstd = p.sb("rstd", [128, TK]); self.olt = p.sb("olt", [128, TK])
        self.wr = p.sb("wr", [128, ND, 128]); self.br = p.sb("br", [128, 1])
        self.lgT = p.sb("lgT", [128, TK]); self.lg = p.sb("lg", [128, 128])
        self.sm = p.sb("sm", [128, 16]); self.pen = p.sb("pen", [128, 4]); self.lem = p.sb("lem", [128, 32]); self.lem2 = p.sb("lem2", [128, 32])
        self.mk = p.sb("mk", [128, 2, 32]); self.gt = p.sb("gt", [128, 32])
        self.pss = p.ps("pss", [128, TK]); self.pz = [p.ps("pz%d" % i, [128, TK]) for i in range(2)]
        self.plg = p.ps("plg", [128, TK]); self.ptr = p.ps("ptr", [128, 128])
        self.pzi = 0


def emit_post(p, cfg, C, cw, a_tile, akey, Wb, wkey, bvec_ap_fn, gt_fn, m2_fn, sh_fn, xl_out_ap, tok_out_ap, gates_out_fn, want_router=True):
    ND = cfg.ND
    for blk in range(ND):
        q = C.pzi % 2
        C.pzi += 1
        for k in range(ND):
            p.op("pe", lambda e, q=q, k=k, blk=blk: e.matmul(C.pz[q][:, :cw], lhsT=Wb[:, k, blk * 128:(blk + 1) * 128], rhs=a_tile[:, k, :cw],
                                                           start=(k == 0), stop=(k == ND - 1)), r=[wkey, akey], w=[("pz", q)])
        p.op("act", lambda e, q=q, blk=blk: e.activation(out=C.olt[:, :cw], in_=C.pz[q][:, :cw], func=AF.Identity, bias=bvec_ap_fn(blk), scale=1.0),
             r=[("pz", q), "vecs"], w=["olt"])
        p.op("dve", lambda e, blk=blk: e.scalar_tensor_tensor(out=C.xl[:, blk, :cw], in0=C.olt[:, :cw], scalar=gt_fn(blk), in1=C.xt[:, blk, :cw],
                                                            op0=ALU.mult, op1=ALU.add), r=["olt", "xt", "vecs"], w=["xl"])
    p.dma("sync", xl_out_ap, C.xl[:, :, :cw], r=["xl"], w=[("xlo", id(xl_out_ap))], grp=("xlst",))
    emit_norm_router(p, cfg, C, cw, m2_fn, sh_fn, tok_out_ap, gates_out_fn, want_router)


def emit_norm_router(p, cfg, C, cw, m2_fn, sh_fn, tok_out_ap, gates_out_fn, want_router=True):
    ND = cfg.ND
    emit_rstd(p, cfg, C.xl, "xl", cw, C.ones, C.sq, "sq", C.pss, "pss", C.rstd, "rstd")
    for k in range(ND):
        p.op("dve", lambda e, k=k: e.tensor_tensor(out=C.sq[:, k, :cw], in0=C.xl[:, k, :cw], in1=C.rstd[:, :cw], op=ALU.mult),
             r=["xl", "rstd"], w=["sq"])
        p.op("dve", lambda e, k=k: e.tensor_scalar(out=C.tokf[:, k, :cw], in0=C.sq[:, k, :cw], scalar1=m2_fn(k), scalar2=sh_fn(k),
                                                  op0=ALU.mult, op1=ALU.add), r=["sq", "vecs"], w=["tokf"])
    p.op("pool", lambda e: e.tensor_copy(out=C.tokb[:, :, :cw], in_=C.tokf[:, :, :cw]), r=["tokf"], w=["tokb"])
    p.dma("sync", tok_out_ap, C.tokb[:, :, :cw], r=["tokb"], w=[("toko", id(tok_out_ap))], grp=("tokst",))
    if not want_router:
        return
    for k in range(ND):
        p.op("pe", lambda e, k=k: e.matmul(C.plg[:, :cw], lhsT=C.wr[:, k, :], rhs=C.tokf[:, k, :cw], start=(k == 0), stop=(k == ND - 1)),
             r=["wr", "tokf"], w=["plg"])
    p.op("act", lambda e: e.activation(out=C.lgT[:, :cw], in_=C.plg[:, :cw], func=AF.Identity, bias=C.br[:, 0:1], scale=1.0),
         r=["plg", "br"], w=["lgT"])
    for (t0, tw) in tiles(cw, 128):
        p.op("pe", lambda e, t0=t0, tw=tw: e.transpose(out=C.ptr[:tw, :], in_=C.lgT[:, t0:t0 + tw], identity=C.ident[:]),
             r=["lgT", "ident"], w=["ptr"])
        p.op("act", lambda e, tw=tw: e.activation(out=C.lg[:tw, :], in_=C.ptr[:tw, :], func=AF.Copy), r=["ptr"], w=["lg"])
        lg, sm = C.lg, C.sm
        R = lambda *k: list(k)
        p.op("dve", lambda e, tw=tw: e.tensor_reduce(out=sm[:tw, 0:1], in_=lg[:tw, 0:4], axis=AX.X, op=ALU.max), r=["lg"], w=["sm0"])
        p.op("dve", lambda e, tw=tw: e.tensor_scalar(out=sm[:tw, 4:8], in0=lg[:tw, 0:4], scalar1=sm[:tw, 0:1], scalar2=None, op0=ALU.subtract),
             r=["lg", "sm0"], w=["sm4"])
        p.op("act", lambda e, tw=tw: e.activation(out=sm[:tw, 4:8], in_=sm[:tw, 4:8], func=AF.Exp), r=["sm4"], w=["sm4"])
        p.op("dve", lambda e, tw=tw: e.tensor_reduce(out=sm[:tw, 1:2], in_=sm[:tw, 4:8], axis=AX.X, op=ALU.add), r=["sm4"], w=["sm1"])
        p.op("dve", lambda e, tw=tw: e.reciprocal(out=sm[:tw, 1:2], in_=sm[:tw, 1:2]), r=["sm1"], w=["sm1"])
        p.op("dve", lambda e, tw=tw: e.tensor_scalar(out=C.pen[:tw, :], in0=lg[:tw, 0:4], scalar1=sm[:tw, 0:1], scalar2=None, op0=ALU.is_equal),
             r=["lg", "sm0"], w=["pen"])
        p.op("dve", lambda e, tw=tw: e.tensor_scalar(out=C.pen[:tw, :], in0=C.pen[:tw, :], scalar1=-1.0, scalar2=1e30, op0=ALU.add, op1=ALU.mult),
             r=["pen"], w=["pen"])
        le = lg[:tw, 4:36].rearrange("p (g e) -> p g e", e=8)
        penb = C.pen[:tw, :].unsqueeze(2).broadcast_to([tw, 4, 8])
        lem3 = C.lem[:tw, :].rearrange("p (g e) -> p g e", e=8)
        p.op("dve", lambda e, le=le, penb=penb, lem3=lem3: e.tensor_tensor(out=lem3, in0=le, in1=penb, op=ALU.add), r=["lg", "pen"], w=["lem"])
        p.op("dve", lambda e, tw=tw: e.tensor_reduce(out=sm[:tw, 2:3], in_=C.lem[:tw, :], axis=AX.X, op=ALU.max), r=["lem"], w=["sm2"])
        p.op("dve", lambda e, tw=tw: e.tensor_scalar(out=C.mk[:tw, 0, :], in0=C.lem[:tw, :], scalar1=sm[:tw, 2:3], scalar2=None, op0=ALU.is_equal),
             r=["lem", "sm2"], w=["mk0"])
        p.op("dve", lambda e, tw=tw: e.scalar_tensor_tensor(out=C.lem2[:tw, :], in0=C.mk[:tw, 0, :], scalar=-1e30, in1=C.lem[:tw, :],
                                                           op0=ALU.mult, op1=ALU.add), r=["mk0", "lem"], w=["lem2"])
        p.op("dve", lambda e, tw=tw: e.tensor_reduce(out=sm[:tw, 3:4], in_=C.lem2[:tw, :], axis=AX.X, op=ALU.max), r=["lem2"], w=["sm3"])
        p.op("dve", lambda e, tw=tw: e.tensor_scalar(out=C.mk[:tw, 1, :], in0=C.lem2[:tw, :], scalar1=sm[:tw, 3:4], scalar2=None, op0=ALU.is_equal),
             r=["lem2", "sm3"], w=["mk1"])
        p.op("dve", lambda e, tw=tw: e.tensor_tensor(out=sm[:tw, 8:9], in0=sm[:tw, 3:4], in1=sm[:tw, 2:3], op=ALU.subtract), r=["sm2", "sm3"], w=["sm8"])
        p.op("act", lambda e, tw=tw: e.activation(out=sm[:tw, 8:9], in_=sm[:tw, 8:9], func=AF.Exp), r=["sm8"], w=["sm8"])
        p.op("dve", lambda e, tw=tw: e.tensor_scalar(out=sm[:tw, 9:10], in0=sm[:tw, 8:9], scalar1=1.0, scalar2=None, op0=ALU.add), r=["sm8"], w=["sm9"])
        p.op("dve", lambda e, tw=tw: e.reciprocal(out=sm[:tw, 9:10], in_=sm[:tw, 9:10]), r=["sm9"], w=["sm9"])
        p.op("dve", lambda e, tw=tw: e.tensor_tensor(out=sm[:tw, 10:11], in0=sm[:tw, 8:9], in1=sm[:tw, 9:10], op=ALU.mult), r=["sm8", "sm9"], w=["sm10"])
        p.op("dve", lambda e, tw=tw: e.tensor_scalar(out=sm[:tw, 9:11], in0=sm[:tw, 9:11], scalar1=sm[:tw, 1:2], scalar2=None, op0=ALU.mult),
             r=["sm9", "sm10", "sm1"], w=["sm9", "sm10"])
        p.op("dve", lambda e, tw=tw: e.tensor_scalar(out=C.gt[:tw, :], in0=C.mk[:tw, 0, :], scalar1=sm[:tw, 9:10], scalar2=None, op0=ALU.mult),
             r=["mk0", "sm9"], w=["gt"])
        p.op("dve", lambda e, tw=tw: e.scalar_tensor_tensor(out=C.gt[:tw, :], in0=C.mk[:tw, 1, :], scalar=sm[:tw, 10:11], in1=C.gt[:tw, :],
                                                           op0=ALU.mult, op1=ALU.add), r=["mk1", "sm10", "gt"], w=["gt"])
        go = gates_out_fn(t0, tw)
        p.dma("sync", go, C.gt[:tw, :], r=["gt"], w=[("go", id(go))], grp=("gst",))


def build_h3(cfg):
    p = Prog()
    ND, D, TL, TC = cfg.ND, cfg.D, cfg.TL, cfg.TC
    NT = TL + TC
    TK = 256
    ycT = p.din("ycT", [128, ND, NT], BF16); x0T = p.din("x0T", [128, ND, NT], BF16); xT = p.din("xT", [128, ND, NT])
    w_out = p.din("w_out", [128, ND, D]); vecs = p.din("vecsD", [128, 8, ND])
    wr_d = p.din("wrD", [128, ND, 128]); br_d = p.din("brD", [128, 1]); ident_d = p.din("identD", [128, 128])
    xlT = p.dout("xlT", [128, ND, NT]); tokT = p.dout("tokT", [128, ND, NT], BF16); gates = p.dout("gates", [NT, 32])
    C = PostCtx(p, cfg, TK)
    p.dma("sync", C.ident[:], ident_d[:, :], w=["ident"]); p.dma("sync", C.wr[:], wr_d[:, :, :], w=["wr"]); p.dma("sync", C.br[:], br_d[:, :], w=["br"])
    vs_ = p.sb("vecs", [128, 8, ND]); p.dma("sync", vs_[:], vecs[:, :, :], w=["vecs"])
    m2 = p.sb("m2", [128, 2, ND])
    for i, j in ((0, 3), (1, 6)):
        p.op("dve", lambda e, i=i, j=j: e.scalar_tensor_tensor(out=m2[:, i, :], in0=vs_[:, j, :], scalar=1.0, in1=vs_[:, 1, :], op0=ALU.add, op1=ALU.mult),
             r=["vecs"], w=["vecs"])
    Wb = p.sb("Wb", [128, ND, D], BF16)
    wf = [p.sb("wf%d" % i, [128, ND, 128]) for i in range(2)]
    for blk in range(ND):
        s = blk % 2
        p.dma("sync", wf[s][:], w_out[:, :, blk * 128:(blk + 1) * 128], w=[("wf", s)])
        p.op("pool", lambda e, s=s, blk=blk: e.tensor_copy(out=Wb[:, :, blk * 128:(blk + 1) * 128], in_=wf[s][:]), r=[("wf", s)], w=["Wb"])
    yc = p.sb("yc", [128, ND, TK], BF16); x0 = p.sb("x0", [128, ND, TK], BF16); a = p.sb("a", [128, ND, TK], BF16)
    for (s0, sl, mi) in ((0, TL, 0), (TL, TC, 1)):
        for (c0, cw) in tiles(sl, TK):
            o = s0 + c0
            p.dma("sync", yc[:, :, :cw], ycT[:, :, o:o + cw], w=["yc"])
            p.dma("pool", x0[:, :, :cw], x0T[:, :, o:o + cw], w=["x0"])
            p.dma("sync", C.xt[:, :, :cw], xT[:, :, o:o + cw], w=["xt"])
            p.op("pool", lambda e, cw=cw: e.tensor_tensor(out=a[:, :, :cw], in0=yc[:, :, :cw], in1=x0[:, :, :cw], op=ALU.mult), r=["yc", "x0"], w=["a"])
            emit_post(p, cfg, C, cw, a, "a", Wb, "Wb",
                      lambda blk: vs_[:, 0, blk:blk + 1],
                      lambda blk, mi=mi: vs_[:, 2 + 3 * mi, blk:blk + 1],
                      lambda k, mi=mi: m2[:, mi, k:k + 1],
                      lambda k, mi=mi: vs_[:, 4 + 3 * mi, k:k + 1],
                      xlT[:, :, o:o + cw], tokT[:, :, o:o + cw],
                      lambda t0, tw, o=o: gates[o + t0:o + t0 + tw, :])
    return p.build()


def router_inputs(cfg, I, layer):
    ND = cfg.ND
    wr = np.zeros((cfg.D, 128), np.float32)
    wr[:, 0:4] = I["moe_wg"][layer]; wr[:, 4:36] = I["moe_we"][layer]
    br = np.zeros((128, 1), np.float32)
    br[0:4, 0] = I["moe_bg"][layer]; br[4:36, 0] = I["moe_be"][layer]
    wr = np.ascontiguousarray(wr.reshape(ND, 128, 128).transpose(1, 0, 2))
    return wr, br


def wfm(w, ND):
    return np.ascontiguousarray(w.reshape(ND, 128, w.shape[1]).transpose(1, 0, 2))


def run_h3(cfg, I, mods, y_lat, y_ctx, x0_lat, x0_ctx):
    import ml_dtypes
    bf = ml_dtypes.bfloat16
    ND, NC = cfg.ND, cfg.NCORE
    nc = build_h3(cfg)
    yc = tok_layout(cfg, y_lat, y_ctx); x0 = tok_layout(cfg, x0_lat, x0_ctx); xx = tok_layout(cfg, I["x"], I["ctx"])
    wr, br = router_inputs(cfg, I, 0)
    w_out = wfm(I["hy_w_out"][0], ND)
    ims = []
    for c in range(NC):
        b = c // cfg.CPB
        mvd = mod_vecs(cfg, mods[0], b)
        vecs = np.stack([vfm(v, ND) for v in (I["hy_b_out"][0], I["norm_g"][0, 1], mvd["gt_a"], mvd["sc_f"], mvd["sh_f"],
                                              mvd["cgt_a"], mvd["csc_f"], mvd["csh_f"])], axis=1)
        ims.append({"ycT": fm(yc[c], ND).astype(bf), "x0T": fm(x0[c], ND).astype(bf), "xT": fm(xx[c].astype(np.float32), ND),
                    "w_out": w_out, "vecsD": np.ascontiguousarray(vecs), "wrD": wr, "brD": br, "identD": np.eye(128, dtype=np.float32)})
    res = run(nc, ims)
    xl = tok_unlayout(cfg, [unfm(r["xlT"]) for r in res])
    tok = tok_unlayout(cfg, [unfm(np.asarray(r["tokT"])) for r in res])
    gates = tok_unlayout(cfg, [r["gates"] for r in res])
    return xl, tok, gates


def build_moe(cfg, caps):
    p = Prog()
    ND, D, DE, EPC = cfg.ND, cfg.D, cfg.DE, cfg.EPC
    NDE = DE // 128
    CAP = max(caps)
    xes = [p.din("xe%d" % j, [128, ND, caps[j]], BF16) for j in range(EPC)]
    wg = p.din("wg", [EPC, 128, ND, DE]); wu = p.din("wu", [EPC, 128, ND, DE]); wd = p.din("wd", [EPC, 128, NDE, D])
    yes_ = [p.dout("ye%d" % j, [128, ND, caps[j]], BF16) for j in range(EPC)]
    Wg = p.sb("Wg", [128, ND, DE], BF16); Wu = p.sb("Wu", [128, ND, DE], BF16); Wd = p.sb("Wd", [128, NDE, D], BF16)
    SW = 512
    st = [p.sb("st%d" % i, [128, max(ND, NDE), SW]) for i in range(2)]
    CT = min(512, CAP)
    xs = p.sb("xs", [128, ND, CT], BF16); h = p.sb("h", [128, NDE, CT], BF16); yo = p.sb("yo", [128, ND, CT], BF16)
    tg = p.sb("tg", [128, CT])
    pg = p.ps("pg", [128, CT]); pu = p.ps("pu", [128, CT]); py = [p.ps("py%d" % i, [128, CT]) for i in range(2)]
    si = 0
    for ex in range(EPC):
        for (src, dst, key, nk, ncols) in ((wg, Wg, "Wg", ND, DE), (wu, Wu, "Wu", ND, DE), (wd, Wd, "Wd", NDE, D)):
            for (c0, cw) in tiles(ncols, SW):
                s = si % 2
                si += 1
                p.dma("sync" if s else "pool", st[s][:, :nk, :cw], src[ex, :, :, c0:c0 + cw], w=[("st", s)])
                p.op("dve" if s else "act", (lambda e, s=s, dst=dst, nk=nk, c0=c0, cw=cw: e.tensor_copy(out=dst[:, :, c0:c0 + cw], in_=st[s][:, :nk, :cw]))
                     if s else (lambda e, s=s, dst=dst, nk=nk, c0=c0, cw=cw: e.activation(out=dst[:, :, c0:c0 + cw], in_=st[s][:, :nk, :cw], func=AF.Copy)),
                     r=[("st", s)], w=[key])
        for (t0, tw) in tiles(caps[ex], CT):
            p.dma("sync", xs[:, :, :tw], xes[ex][:, :, t0:t0 + tw], w=["xs"])
            for fb in range(NDE):
                for k in range(ND):
                    p.op("pe", lambda e, fb=fb, k=k, tw=tw: e.matmul(pg[:, :tw], lhsT=Wg[:, k, fb * 128:(fb + 1) * 128], rhs=xs[:, k, :tw],
                                                                    start=(k == 0), stop=(k == ND - 1)), r=["Wg", "xs"], w=["pg"])
                for k in range(ND):
                    p.op("pe", lambda e, fb=fb, k=k, tw=tw: e.matmul(pu[:, :tw], lhsT=Wu[:, k, fb * 128:(fb + 1) * 128], rhs=xs[:, k, :tw],
                                                                    start=(k == 0), stop=(k == ND - 1)), r=["Wu", "xs"], w=["pu"])
                p.op("act", lambda e, tw=tw: e.activation(out=tg[:, :tw], in_=pg[:, :tw], func=AF.Silu), r=["pg"], w=["tg"])
                p.op("dve", lambda e, fb=fb, tw=tw: e.tensor_tensor(out=h[:, fb, :tw], in0=pu[:, :tw], in1=tg[:, :tw], op=ALU.mult),
                     r=["pu", "tg"], w=["h"])
            for ob in range(ND):
                q = ob % 2
                for f in range(NDE):
                    p.op("pe", lambda e, ob=ob, f=f, q=q, tw=tw: e.matmul(py[q][:, :tw], lhsT=Wd[:, f, ob * 128:(ob + 1) * 128], rhs=h[:, f, :tw],
                                                                         start=(f == 0), stop=(f == NDE - 1)), r=["Wd", "h"], w=[("py", q)])
                if q:
                    p.op("act", lambda e, ob=ob, q=q, tw=tw: e.activation(out=yo[:, ob, :tw], in_=py[q][:, :tw], func=AF.Copy), r=[("py", q)], w=["yo"])
                else:
                    p.op("dve", lambda e, ob=ob, q=q, tw=tw: e.tensor_copy(out=yo[:, ob, :tw], in_=py[q][:, :tw]), r=[("py", q)], w=["yo"])
            p.dma("sync", yes_[ex][:, :, t0:t0 + tw], yo[:, :, :tw], r=["yo"], w=[("ye", ex, t0)], grp=("yst",))
    return p.build()


_MOE_NC = {}


def run_moe(cfg, I, layer, tok, gates, CAP=None):
    import ml_dtypes
    bf = ml_dtypes.bfloat16
    ND, NC, EPC, D, DE = cfg.ND, cfg.NCORE, cfg.EPC, cfg.D, cfg.DE
    NDE = DE // 128
    T = tok.shape[0]
    sel = gates > 0
    idx = [np.nonzero(sel[:, e])[0] for e in range(cfg.NE)]
    cnt = np.array([len(ix) for ix in idx])
    order = np.argsort(-cnt, kind="stable")
    assign = [[int(order[j * NC + c]) for j in range(EPC)] for c in range(NC)]
    caps = tuple(int(max(128, ((cnt[order[j * NC:(j + 1) * NC]].max() + 127) // 128) * 128)) for j in range(EPC))
    key = (cfg.D, caps)
    if key not in _MOE_NC:
        _MOE_NC[key] = build_moe(cfg, caps)
    nc = _MOE_NC[key]
    print("[moe] counts min/mean/max", cnt.min(), T * 2 // cfg.NE, cnt.max(), "caps", caps, flush=True)
    slot = np.cumsum(sel, axis=1) - 1
    y01 = np.zeros((2, T, D), bf)
    g01 = np.zeros((2, T), np.float32)
    ims = []
    for c in range(NC):
        m = {}
        for j in range(EPC):
            e = assign[c][j]
            xe = np.zeros((128, ND, caps[j]), bf)
            if len(idx[e]):
                xe[:, :, :len(idx[e])] = fm(tok[idx[e]], ND)
            m["xe%d" % j] = xe
        m["wg"] = np.stack([wfm(I["moe_w_gate"][layer][e], ND) for e in assign[c]])
        m["wu"] = np.stack([wfm(I["moe_w_up"][layer][e], ND) for e in assign[c]])
        m["wd"] = np.stack([wfm(I["moe_w_down"][layer][e], NDE) for e in assign[c]])
        ims.append(m)
    res = run(nc, ims)
    for c in range(NC):
        for j in range(EPC):
            e = assign[c][j]
            ix = idx[e]
            if len(ix):
                yy = unfm(np.asarray(res[c]["ye%d" % j])[:, :, :len(ix)])
                sl = slot[ix, e]
                for s_ in (0, 1):
                    msk = sl == s_
                    y01[s_, ix[msk]] = yy[msk]
                    g01[s_, ix[msk]] = gates[ix[msk], e]
    return y01, g01


def build_comb(cfg, final):
    p = Prog()
    ND, TL, TC = cfg.ND, cfg.TL, cfg.TC
    NT = TL if final else TL + TC
    TK = 128
    xlT = p.din("xlT", [128, ND, NT]); y0T = p.din("y0T", [128, ND, NT], BF16); y1T = p.din("y1T", [128, ND, NT], BF16)
    gb = p.din("gb", [128, 2, NT]); vecs = p.din("vecsD", [128, 7, ND])
    xoT = p.dout("xoT", [128, ND, NT]); uT = p.dout("uT", [128, ND, NT], F32 if final else BF16)
    vs_ = p.sb("vecs", [128, 7, ND]); p.dma("sync", vs_[:], vecs[:, :, :], w=["vecs"])
    m2 = p.sb("m2", [128, 2, ND])
    for i, j in ((0, 3), (1, 5)):
        p.op("dve", lambda e, i=i, j=j: e.scalar_tensor_tensor(out=m2[:, i, :], in0=vs_[:, j, :], scalar=1.0, in1=vs_[:, 2, :], op0=ALU.add, op1=ALU.mult),
             r=["vecs"], w=["vecs"])
    ones = p.sb("ones", [128, 128]); p.op("dve", lambda e: e.memset(ones[:], 1.0), w=["ones"])
    NB_ = 2
    mk = lambda nm, dt=F32, shp=None: [p.sb("%s_%d" % (nm, i), shp or [128, ND, TK], dt) for i in range(NB_)]
    xt_, y0_, y1_ = mk("xt"), mk("y0", BF16), mk("y1", BF16)
    gs_ = mk("gs", F32, [128, 2, TK]); mo_, mo2_, xl_, sq_ = mk("mo"), mk("mo2"), mk("xl"), mk("sq")
    rstd_ = mk("rstd", F32, [128, TK]); uo_ = mk("uo", F32 if final else BF16)
    pss_ = [p.ps("pss%d" % i, [128, TK]) for i in range(NB_)]
    segs = ((0, TL, 0),) if final else ((0, TL, 0), (TL, TC, 1))
    ti = 0
    for (s0, sl, mi) in segs:
        for (c0, cw) in tiles(sl, TK):
            o = s0 + c0
            z = ti % NB_
            ti += 1
            xt, y0, y1, gs, mo, mo2, xl, sq, rstd, uo, pss = xt_[z], y0_[z], y1_[z], gs_[z], mo_[z], mo2_[z], xl_[z], sq_[z], rstd_[z], uo_[z], pss_[z]
            K_ = lambda n, z=z: (n, z)
            p.dma("sync", xt[:, :, :cw], xlT[:, :, o:o + cw], w=[K_("xt")])
            p.dma("pool", y0[:, :, :cw], y0T[:, :, o:o + cw], w=[K_("y0")])
            p.dma("pool", y1[:, :, :cw], y1T[:, :, o:o + cw], w=[K_("y1")])
            p.dma("sync", gs[:, :, :cw], gb[:, :, o:o + cw], w=[K_("gs")])
            g0 = gs[:, 0, :cw].unsqueeze(1).broadcast_to([128, ND, cw])
            g1 = gs[:, 1, :cw].unsqueeze(1).broadcast_to([128, ND, cw])
            p.op("dve", lambda e, cw=cw, g0=g0, mo=mo, y0=y0: e.tensor_tensor(out=mo[:, :, :cw], in0=y0[:, :, :cw], in1=g0, op=ALU.mult), r=[K_("y0"), K_("gs")], w=[K_("mo")])
            p.op("dve", lambda e, cw=cw, g1=g1, mo2=mo2, y1=y1: e.tensor_tensor(out=mo2[:, :, :cw], in0=y1[:, :, :cw], in1=g1, op=ALU.mult), r=[K_("y1"), K_("gs")], w=[K_("mo2")])
            p.op("dve", lambda e, cw=cw, mo=mo, mo2=mo2: e.tensor_tensor(out=mo[:, :, :cw], in0=mo[:, :, :cw], in1=mo2[:, :, :cw], op=ALU.add), r=[K_("mo"), K_("mo2")], w=[K_("mo")])
            gtb = vs_[:, mi, :].unsqueeze(2).broadcast_to([128, ND, cw])
            p.op("dve", lambda e, cw=cw, mo=mo, gtb=gtb: e.tensor_tensor(out=mo[:, :, :cw], in0=mo[:, :, :cw], in1=gtb, op=ALU.mult), r=[K_("mo"), "vecs"], w=[K_("mo")])
            p.op("dve", lambda e, cw=cw, mo=mo, xt=xt, xl=xl: e.tensor_tensor(out=xl[:, :, :cw], in0=mo[:, :, :cw], in1=xt[:, :, :cw], op=ALU.add), r=[K_("mo"), K_("xt")], w=[K_("xl")])
            p.dma("sync", xoT[:, :, o:o + cw], xl[:, :, :cw], r=[K_("xl")], w=[("xo", o)], grp=("xst", z))
            emit_rstd(p, cfg, xl, K_("xl"), cw, ones, sq, K_("sq"), pss, K_("pss"), rstd, K_("rstd"))
            rb = rstd[:, :cw].unsqueeze(1).broadcast_to([128, ND, cw])
            p.op("dve", lambda e, cw=cw, sq=sq, xl=xl, rb=rb: e.tensor_tensor(out=sq[:, :, :cw], in0=xl[:, :, :cw], in1=rb, op=ALU.mult), r=[K_("xl"), K_("rstd")], w=[K_("sq")])
            if final:
                gb_ = vs_[:, 2, :].unsqueeze(2).broadcast_to([128, ND, cw])
                p.op("dve", lambda e, cw=cw, sq=sq, uo=uo, gb_=gb_: e.tensor_tensor(out=uo[:, :, :cw], in0=sq[:, :, :cw], in1=gb_, op=ALU.mult), r=[K_("sq"), "vecs"], w=[K_("uo")])
            else:
                mb_ = m2[:, mi, :].unsqueeze(2).broadcast_to([128, ND, cw])
                sb_ = vs_[:, 4 + 2 * mi, :].unsqueeze(2).broadcast_to([128, ND, cw])
                p.op("dve", lambda e, cw=cw, sq=sq, mb_=mb_: e.tensor_tensor(out=sq[:, :, :cw], in0=sq[:, :, :cw], in1=mb_, op=ALU.mult), r=[K_("sq"), "vecs"], w=[K_("sq")])
                p.op("dve", lambda e, cw=cw, sq=sq, uo=uo, sb_=sb_: e.tensor_tensor(out=uo[:, :, :cw], in0=sq[:, :, :cw], in1=sb_, op=ALU.add), r=[K_("sq"), "vecs"], w=[K_("uo")])
            p.dma("pool", uT[:, :, o:o + cw], uo[:, :, :cw], r=[K_("uo")], w=[("uo_", o)], grp=("ust", z))
    return p.build()


def run_comb(cfg, I, mods, layer, xl_lat, xl_ctx, y01, g01, final):
    import ml_dtypes
    bf = ml_dtypes.bfloat16
    ND, NC, B, L, LC, D = cfg.ND, cfg.NCORE, cfg.B, cfg.L, cfg.LC, cfg.D
    nc = build_comb(cfg, final)
    nl = B * L
    def split(a, last):
        lat = a[:nl].reshape((B, L) + last)
        ctx = a[nl:].reshape((B, LC) + last) if not final else np.zeros((B, LC) + last, a.dtype)
        return lat, ctx
    y0 = split(y01[0], (D,)); y1 = split(y01[1], (D,))
    g0 = split(g01[0][:, None], (1,)); g1 = split(g01[1][:, None], (1,))
    xs = tok_layout(cfg, xl_lat, xl_ctx if xl_ctx is not None else np.zeros((B, LC, D), np.float32))
    y0s = tok_layout(cfg, *y0); y1s = tok_layout(cfg, *y1); g0s = tok_layout(cfg, *g0); g1s = tok_layout(cfg, *g1)
    NT = cfg.TL if final else cfg.TL + cfg.TC
    ims = []
    for c in range(NC):
        b = c // cfg.CPB
        mvd = mod_vecs(cfg, mods[layer], b)
        if final:
            z = np.zeros(D, np.float32)
            vl = (mvd["gt_f"], z, I["final_g"], z, z, z, z)
        else:
            nm = mod_vecs(cfg, mods[layer + 1], b)
            vl = (mvd["gt_f"], mvd["cgt_f"], I["norm_g"][layer + 1, 0], nm["sc_a"], nm["sh_a"], nm["csc_a"], nm["csh_a"])
        vecs = np.ascontiguousarray(np.stack([vfm(np.asarray(v, np.float32), ND) for v in vl], axis=1))
        gbv = np.stack([g0s[c][:NT, 0], g1s[c][:NT, 0]])
        ims.append({"xlT": fm(xs[c][:NT].astype(np.float32), ND), "y0T": fm(y0s[c][:NT], ND).astype(bf), "y1T": fm(y1s[c][:NT], ND).astype(bf),
                    "gb": np.ascontiguousarray(np.broadcast_to(gbv[None], (128, 2, NT))).astype(np.float32), "vecsD": vecs})
    res = run(nc, ims)
    def un(name):
        per = []
        for r in res:
            a = unfm(np.asarray(r[name]))
            if final:
                a = np.concatenate([a, np.zeros((cfg.TC, D), a.dtype)], 0)
            per.append(a)
        return tok_unlayout(cfg, per)
    xo = un("xoT"); u = un("uT")
    return xo[0], xo[1], u[0], u[1]


S5P, S5H = 64, 16
S5_WCHAIN_ENG = "dve"
S5_GROUP_BARRIER = True


def emit_exp_poly(p, out_ap, okey, in_ap, ikey, shape, center, degree, name):
    import math
    t = p.sb(name + "_t", shape); r = p.sb(name + "_r", shape)
    p.op("dve", lambda e: e.tensor_scalar(out=t[:], in0=in_ap, scalar1=-center, scalar2=None, op0=ALU.add), r=[ikey], w=[name + "t"])
    p.op("dve", lambda e: e.memset(r[:], 1.0 / math.factorial(degree)), w=[name + "r"])
    for n in range(degree - 1, -1, -1):
        p.op("dve", lambda e: e.tensor_tensor(out=r[:], in0=r[:], in1=t[:], op=ALU.mult), r=[name + "t", name + "r"], w=[name + "r"])
        p.op("dve", lambda e, n=n: e.tensor_scalar(out=r[:], in0=r[:], scalar1=1.0 / math.factorial(n), scalar2=None, op0=ALU.add), r=[name + "r"], w=[name + "r"])
    p.op("dve", lambda e: e.tensor_scalar(out=out_ap, in0=r[:], scalar1=float(math.exp(center)), scalar2=None, op0=ALU.mult), r=[name + "r"], w=[okey])

def build_s5prep(cfg, CH):
    p = Prog()
    G = cfg.G
    P_, H = S5P, S5H
    NLc = max(1, CH // cfg.NCORE)
    NPW = CH + 1
    a_d = p.din("a_d", [G, 2, 2, P_])
    ls_d = p.din("ls_d", [G, 2])
    b_d = p.din("b_d", [G, 2, 2, P_, H])
    c_d = p.din("c_d", [G, 2, 2, H, P_])
    lsel = p.din("lsel", [G, NLc, NPW])
    XB = p.dout("XB", [G, NLc, 2, 2, P_, H])
    OC = p.dout("OC", [G, NLc, 2, 2, H, P_])
    Mo = p.dout("Mo", [G, NLc, 2, H, H])
    PC = p.dout("PC", [G, 3, 2, P_])
    DP = 2 * P_
    a = p.sb("a", [G, 2, DP]); ls = p.sb("ls", [G, 2]); b = p.sb("b", [G, 2, 2 * P_ * H]); c = p.sb("c", [G, 2, 2 * H * P_])
    p.dma("sync", a[:], a_d.rearrange("g r d p -> g r (d p)"), w=["a"]); p.dma("sync", ls[:], ls_d[:, :], w=["ls"])
    p.dma("sync", b[:], b_d.rearrange("g r d p h -> g r (d p h)"), w=["b"]); p.dma("pool", c[:], c_d.rearrange("g r d h p -> g r (d h p)"), w=["c"])
    sel = p.sb("sel", [G, NLc, NPW]); p.dma("sync", sel[:], lsel[:, :, :], w=["sel"])
    st = p.sb("stp", [G, 2])
    emit_exp_poly(p, st[:], "st", ls[:], "ls", [G, 2], float(np.log(0.01)), 20, "ep1")
    stb = st[:, :].unsqueeze(2).broadcast_to([G, 2, P_])
    lr = p.sb("lr", [G, DP]); li = p.sb("li", [G, DP]); mag = p.sb("mag", [G, DP]); cs = p.sb("cs", [G, 2, DP])
    v3 = lambda t: t.rearrange("g (d p) -> g d p", p=P_)
    p.op("dve", lambda e: e.tensor_tensor(out=v3(lr[:, :]), in0=v3(a[:, 0, :]), in1=stb, op=ALU.mult), r=["a", "st"], w=["lr"])
    p.op("dve", lambda e: e.tensor_tensor(out=v3(li[:, :]), in0=v3(a[:, 1, :]), in1=stb, op=ALU.mult), r=["a", "st"], w=["li"])
    emit_exp_poly(p, mag[:], "mag", lr[:], "lr", [G, DP], 0.0, 7, "ep2")
    tmp = [p.sb("tmpa", [G, DP]), p.sb("tmpb", [G, DP])]
    one = p.sb("one1", [G, 1]); p.op("dve", lambda e: e.memset(one[:], 1.0), w=["one1"])
    hp = p.sb("hpi", [G, 1]); p.op("dve", lambda e: e.memset(hp[:], float(np.pi / 2)), w=["hpi"])
    zr = p.sb("zr", [G, 1]); p.op("dve", lambda e: e.memset(zr[:], 0.0), w=["zr"])
    emit_sin(p, cs[:, 1, :], ("cs", 1), li[:, :], "li", DP, tmp, "tmp", one[:, 0:1], zr[:, 0:1], extra_r=["one1", "zr"])
    emit_sin(p, cs[:, 0, :], ("cs", 0), li[:, :], "li", DP, tmp, "tmp", one[:, 0:1], hp[:, 0:1], extra_r=["one1", "hpi"])
    pw = p.sb("pw", [G, NPW, 2, DP])
    p.op("dve", lambda e: e.memset(pw[:, 0, 0, :], 1.0), w=["pw"])
    p.op("dve", lambda e: e.memset(pw[:, 0, 1, :], 0.0), w=["pw"])
    p.op("dve", lambda e: e.tensor_tensor(out=pw[:, 1, 0, :], in0=mag[:], in1=cs[:, 0, :], op=ALU.mult), r=["mag", ("cs", 0)], w=["pw"])
    p.op("dve", lambda e: e.tensor_tensor(out=pw[:, 1, 1, :], in0=mag[:], in1=cs[:, 1, :], op=ALU.mult), r=["mag", ("cs", 1)], w=["pw"])
    t0, t1 = tmp

    def cmul(out_re, out_im, are, aim, bre, bim, rk, wk, neg_im_out=None):
        raise NotImplementedError

    for l in range(1, CH):
        p.op("dve", lambda e, l=l: e.tensor_tensor(out=t0[:], in0=pw[:, l, 0, :], in1=pw[:, 1, 0, :], op=ALU.mult), r=["pw"], w=[("tmp", 0)])
        p.op("dve", lambda e, l=l: e.tensor_tensor(out=t1[:], in0=pw[:, l, 1, :], in1=pw[:, 1, 1, :], op=ALU.mult), r=["pw"], w=[("tmp", 1)])
        p.op("dve", lambda e, l=l: e.tensor_tensor(out=pw[:, l + 1, 0, :], in0=t0[:], in1=t1[:], op=ALU.subtract), r=[("tmp", 0), ("tmp", 1)], w=["pw"])
        p.op("dve", lambda e, l=l: e.tensor_tensor(out=t0[:], in0=pw[:, l, 0, :], in1=pw[:, 1, 1, :], op=ALU.mult), r=["pw"], w=[("tmp", 0)])
        p.op("dve", lambda e, l=l: e.tensor_tensor(out=t1[:], in0=pw[:, l, 1, :], in1=pw[:, 1, 0, :], op=ALU.mult), r=["pw"], w=[("tmp", 1)])
        p.op("dve", lambda e, l=l: e.tensor_tensor(out=pw[:, l + 1, 1, :], in0=t0[:], in1=t1[:], op=ALU.add), r=[("tmp", 0), ("tmp", 1)], w=["pw"])
    pc = p.sb("pc", [G, 3, DP])
    p.op("dve", lambda e: e.tensor_copy(out=pc[:, 0:2, :], in_=pw[:, CH, :, :]), r=["pw"], w=["pc"])
    p.op("dve", lambda e: e.tensor_scalar(out=pc[:, 2, :], in0=pw[:, CH, 1, :], scalar1=-1.0, scalar2=None, op0=ALU.mult), r=["pw"], w=["pc"])
    p.dma("sync", PC.rearrange("g t d p -> g t (d p)"), pc[:], r=["pc"], w=["PC"])
    q = p.sb("q", [G, 2, DP]); dd = p.sb("dd", [G, DP]); nr = p.sb("nr", [G, DP])
    p.op("dve", lambda e: e.tensor_scalar(out=nr[:], in0=pw[:, 1, 0, :], scalar1=-1.0, scalar2=None, op0=ALU.add), r=["pw"], w=["nr"])
    p.op("dve", lambda e: e.tensor_tensor(out=t0[:], in0=a[:, 0, :], in1=a[:, 0, :], op=ALU.mult), r=["a"], w=[("tmp", 0)])
    p.op("dve", lambda e: e.tensor_tensor(out=t1[:], in0=a[:, 1, :], in1=a[:, 1, :], op=ALU.mult), r=["a"], w=[("tmp", 1)])
    p.op("dve", lambda e: e.tensor_tensor(out=dd[:], in0=t0[:], in1=t1[:], op=ALU.add), r=[("tmp", 0), ("tmp", 1)], w=["dd"])
    p.op("dve", lambda e: e.reciprocal(out=dd[:], in_=dd[:]), r=["dd"], w=["dd"])
    p.op("dve", lambda e: e.tensor_tensor(out=t0[:], in0=nr[:], in1=a[:, 0, :], op=ALU.mult), r=["nr", "a"], w=[("tmp", 0)])
    p.op("dve", lambda e: e.tensor_tensor(out=t1[:], in0=pw[:, 1, 1, :], in1=a[:, 1, :], op=ALU.mult), r=["pw", "a"], w=[("tmp", 1)])
    p.op("dve", lambda e: e.tensor_tensor(out=t0[:], in0=t0[:], in1=t1[:], op=ALU.add), r=[("tmp", 0), ("tmp", 1)], w=[("tmp", 0)])
    p.op("dve", lambda e: e.tensor_tensor(out=q[:, 0, :], in0=t0[:], in1=dd[:], op=ALU.mult), r=[("tmp", 0), "dd"], w=["q"])
    p.op("dve", lambda e: e.tensor_tensor(out=t0[:], in0=pw[:, 1, 1, :], in1=a[:, 0, :], op=ALU.mult), r=["pw", "a"], w=[("tmp", 0)])
    p.op("dve", lambda e: e.tensor_tensor(out=t1[:], in0=nr[:], in1=a[:, 1, :], op=ALU.mult), r=["nr", "a"], w=[("tmp", 1)])
    p.op("dve", lambda e: e.tensor_tensor(out=t0[:], in0=t0[:], in1=t1[:], op=ALU.subtract), r=[("tmp", 0), ("tmp", 1)], w=[("tmp", 0)])
    p.op("dve", lambda e: e.tensor_tensor(out=q[:, 1, :], in0=t0[:], in1=dd[:], op=ALU.mult), r=[("tmp", 0), "dd"], w=["q"])
    NB_ = 2 * P_ * H
    bb = p.sb("bb", [G, 2, NB_]); w0 = p.sb("w0", [G, NB_]); w1 = p.sb("w1", [G, NB_])
    v_ph = lambda t: t.rearrange("g (dp h) -> g dp h", h=H)
    bc_h = lambda t: t.unsqueeze(2).broadcast_to([G, DP, H])

    def cmul_ph(out_re, out_im, sre, sim, xre, xim, rk, wk, neg_im=False):
        p.op("dve", lambda e: e.tensor_tensor(out=v_ph(w0[:, :]), in0=v_ph(xre), in1=bc_h(sre), op=ALU.mult), r=rk, w=["w0"])
        p.op("pool", lambda e: e.tensor_tensor(out=v_ph(w1[:, :]), in0=v_ph(xim), in1=bc_h(sim), op=ALU.mult), r=rk, w=["w1"])
        p.op("dve", lambda e: e.tensor_tensor(out=out_re, in0=w0[:, :], in1=w1[:, :], op=ALU.subtract), r=["w0", "w1"], w=wk)
        p.op("dve", lambda e: e.tensor_tensor(out=v_ph(w0[:, :]), in0=v_ph(xim), in1=bc_h(sre), op=ALU.mult), r=rk, w=["w0"])
        p.op("pool", lambda e: e.tensor_tensor(out=v_ph(w1[:, :]), in0=v_ph(xre), in1=bc_h(sim), op=ALU.mult), r=rk, w=["w1"])
        if neg_im:
            p.op("dve", lambda e: e.scalar_tensor_tensor(out=out_im, in0=w0[:, :], scalar=-1.0, in1=w1[:, :], op0=ALU.mult, op1=ALU.subtract),
                 r=["w0", "w1"], w=wk)
        else:
            p.op("dve", lambda e: e.tensor_tensor(out=out_im, in0=w0[:, :], in1=w1[:, :], op=ALU.add), r=["w0", "w1"], w=wk)

    cmul_ph(bb[:, 0, :], bb[:, 1, :], q[:, 0, :], q[:, 1, :], b[:, 0, :], b[:, 1, :], ["q", "b"], ["bb"])
    pl = p.sb("pl", [G, 2, DP]); pl1 = p.sb("pl1", [G, 2, DP])
    xb = p.sb("xb", [G, 2, NB_]); oc = p.sb("oc", [G, 2, NB_]); mo = p.sb("mo", [G, 2, H * H])
    HH = H * H
    big0 = p.sb("big0", [G, (H // 2) * H * P_]); big1 = p.sb("big1", [G, (H // 2) * H * P_])
    for n in range(NLc):
        for (dst, off, key) in ((pl, 0, "pl"), (pl1, 1, "pl1")):
            for comp in range(2):
                first = True
                for l in range(CH):
                    src = pw[:, l + off, comp, :]
                    if first:
                        p.op("dve", lambda e, dst=dst, comp=comp, src=src, n=n, l=l: e.tensor_scalar(
                            out=dst[:, comp, :], in0=src, scalar1=sel[:, n, l:l + 1], scalar2=None, op0=ALU.mult), r=["pw", "sel"], w=[key])
                        first = False
                    else:
                        p.op("dve", lambda e, dst=dst, comp=comp, src=src, n=n, l=l: e.scalar_tensor_tensor(
                            out=dst[:, comp, :], in0=src, scalar=sel[:, n, l:l + 1], in1=dst[:, comp, :], op0=ALU.mult, op1=ALU.add),
                            r=["pw", "sel", key], w=[key])
        cmul_ph(xb[:, 0, :], xb[:, 1, :], pl[:, 0, :], pl[:, 1, :], bb[:, 0, :], bb[:, 1, :], ["pl", "bb"], ["xb"])
        p.dma("sync", XB[:, n].rearrange("g r d p h -> g r (d p h)"), xb[:], r=["xb"], w=[("XB", n)], grp=("xbst",))
        cv = lambda t: t.rearrange("g (d h p) -> g d h p", d=2, h=H)
        bc_hp = lambda t: t.rearrange("g (d p) -> g d p", d=2).unsqueeze(2).broadcast_to([G, 2, H, P_])
        NC_ = 2 * H * P_
        p.op("dve", lambda e: e.tensor_tensor(out=cv(w0[:, :NC_]), in0=cv(c[:, 0, :]), in1=bc_hp(pl1[:, 0, :]), op=ALU.mult), r=["c", "pl1"], w=["w0"])
        p.op("pool", lambda e: e.tensor_tensor(out=cv(w1[:, :NC_]), in0=cv(c[:, 1, :]), in1=bc_hp(pl1[:, 1, :]), op=ALU.mult), r=["c", "pl1"], w=["w1"])
        p.op("dve", lambda e: e.tensor_tensor(out=oc[:, 0, :], in0=w0[:, :NC_], in1=w1[:, :NC_], op=ALU.subtract), r=["w0", "w1"], w=["oc"])
        p.op("dve", lambda e: e.tensor_tensor(out=cv(w0[:, :NC_]), in0=cv(c[:, 1, :]), in1=bc_hp(pl1[:, 0, :]), op=ALU.mult), r=["c", "pl1"], w=["w0"])
        p.op("pool", lambda e: e.tensor_tensor(out=cv(w1[:, :NC_]), in0=cv(c[:, 0, :]), in1=bc_hp(pl1[:, 1, :]), op=ALU.mult), r=["c", "pl1"], w=["w1"])
        p.op("dve", lambda e: e.scalar_tensor_tensor(out=oc[:, 1, :], in0=w0[:, :NC_], scalar=-1.0, in1=w1[:, :NC_], op0=ALU.mult, op1=ALU.subtract),
             r=["w0", "w1"], w=["oc"])
        p.dma("sync", OC[:, n].rearrange("g r d h p -> g r (d h p)"), oc[:], r=["oc"], w=[("OC", n)], grp=("ocst",))
        for d in range(2):
            for hh in range(2):
                h0 = hh * (H // 2)
                def cview(comp, d=d, h0=h0):
                    t = c[:, comp, d * H * P_:(d + 1) * H * P_].rearrange("g (h p) -> g h p", p=P_)[:, h0:h0 + H // 2, :]
                    return t.unsqueeze(2).broadcast_to([G, H // 2, H, P_])
                def xview(comp, d=d):
                    t = xb[:, comp, d * P_ * H:(d + 1) * P_ * H].rearrange("g (p h) -> g h p", h=H)
                    return t.unsqueeze(1).broadcast_to([G, H // 2, H, P_])
                b0v = big0[:, :].rearrange("g (a h p) -> g a h p", a=H // 2, h=H)
                b1v = big1[:, :].rearrange("g (a h p) -> g a h p", a=H // 2, h=H)
                p.op("dve", lambda e, cview=cview, xview=xview, b0v=b0v: e.tensor_tensor(out=b0v, in0=cview(0), in1=xview(0), op=ALU.mult), r=["c", "xb"], w=["big0"])
                p.op("pool", lambda e, cview=cview, xview=xview, b1v=b1v: e.tensor_tensor(out=b1v, in0=cview(1), in1=xview(1), op=ALU.mult), r=["c", "xb"], w=["big1"])
                p.op("dve", lambda e: e.tensor_tensor(out=big0[:, :], in0=big0[:, :], in1=big1[:, :], op=ALU.subtract), r=["big0", "big1"], w=["big0"])
                p.op("dve", lambda e, d=d, h0=h0: e.tensor_reduce(out=mo[:, d, h0 * H:(h0 + H // 2) * H],
                                                                 in_=big0[:, :].rearrange("g (a p) -> g a p", p=P_), axis=AX.X, op=ALU.add),
                     r=["big0"], w=["mo"])
        p.dma("sync", Mo[:, n].rearrange("g d a b -> g d (a b)"), mo[:], r=["mo"], w=[("Mo", n)], grp=("most",))
    return p.build()


def run_s5prep(cfg, I, CH):
    G, NC = cfg.G, cfg.NCORE
    P_, H = S5P, S5H
    NLc = max(1, CH // NC)
    nc = build_s5prep(cfg, CH)
    a_d = np.ascontiguousarray(np.stack([I["s5_a_re"][0], I["s5_a_im"][0]]).transpose(2, 0, 1, 3)).astype(np.float32)
    ls_d = np.ascontiguousarray(I["s5_log_step"][0].T).astype(np.float32)
    b_d = np.ascontiguousarray(np.stack([I["s5_b_re"][0], I["s5_b_im"][0]]).transpose(2, 0, 1, 3, 4)).astype(np.float32)
    c_d = np.ascontiguousarray(np.stack([I["s5_c_re"][0], I["s5_c_im"][0]]).transpose(2, 0, 1, 3, 4)).astype(np.float32)
    ims = []
    lags = []
    for c in range(NC):
        ls = [c + NC * n for n in range(NLc)] if CH >= NC else [c % CH]
        lags.append(ls)
        sel = np.zeros((G, NLc, CH + 1), np.float32)
        for n, l in enumerate(ls):
            sel[:, n, l] = 1.0
        ims.append({"a_d": a_d, "ls_d": ls_d, "b_d": b_d, "c_d": c_d, "lsel": sel})
    res = run(nc, ims)
    XB = np.zeros((CH, G, 2, 2, P_, H), np.float32); OC = np.zeros((CH, G, 2, 2, H, P_), np.float32); Mo = np.zeros((CH, G, 2, H, H), np.float32)
    for c in range(NC):
        for n, l in enumerate(lags[c]):
            XB[l] = res[c]["XB"][:, n]; OC[l] = res[c]["OC"][:, n]; Mo[l] = res[c]["Mo"][:, n]
    PC = res[0]["PC"]
    return dict(XB=XB, OC=OC, Mo=Mo, PC=PC)


def build_s5(cfg, CH, PG):
    p = Prog()
    B = cfg.B
    GPC = cfg.G // cfg.NCORE
    NPr = 2 * GPC
    KR = CH * S5H
    KP = KR // 128
    LT = cfg.LC + cfg.L
    NCK = LT // CH
    NCOL = NCK * B
    CL0 = (cfg.LC // CH) * B
    NCOLL = NCOL - CL0
    U = p.din("U", [NPr, 128, KP, NCOL], BF16)
    Tm = p.din("Tm", [NPr, 128, KP, KR]); Xm = p.din("Xm", [NPr, 128, 2, KP, 128]); Om = p.din("Om", [NPr, 128, KR])
    CAd = p.din("CA", [128, NPr]); CBd = p.din("CB", [128, NPr])
    Y = p.dout("Y", [NPr, 128, KP, NCOLL], BF16)
    ca = p.sb("ca", [128, NPr]); cb = p.sb("cb", [128, NPr])
    p.dma("sync", ca[:], CAd[:, :], w=["ca"]); p.dma("sync", cb[:], CBd[:, :], w=["cb"])
    SA = p.sb("SA", [128, PG, NCK, B]); SW = p.sb("SW", [128, PG, NCK, B]); SAb = p.sb("SAb", [128, PG, NCK, B], BF16)
    Ub = p.sb("Ub", [128, PG, KP, NCOL], BF16)
    Tb = p.sb("Tb", [128, PG, KP, KR], BF16); Xb = p.sb("Xb", [128, PG, 2, KP, 128], BF16); Ob = p.sb("Ob", [128, PG, KR], BF16)
    stT = p.sb("stT", [128, KP, KR]); stX = p.sb("stX", [128, 2, KP, 128]); stO = p.sb("stO", [128, KR])
    tq = [p.sb("tq%d" % i, [128, PG, B]) for i in range(4)]
    yo = [p.sb("yo%d" % i, [128, 512], BF16) for i in range(2)]
    px = [p.ps("px%d" % i, [128, 512]) for i in range(2)]
    py = [p.ps("py%d" % i, [128, 512]) for i in range(2)]
    yi = 0
    for pg0 in range(0, NPr, PG):
        for pi in range(PG):
            pr = pg0 + pi
            p.dma("sync", Ub[:, pi], U[pr], w=[("Ub", pi)])
            p.dma("pool", stT[:], Tm[pr], w=["stT"]); p.dma("pool", stX[:], Xm[pr], w=["stX"]); p.dma("pool", stO[:], Om[pr], w=["stO"])
            p.op("dve", lambda e, pi=pi: e.tensor_copy(out=Tb[:, pi], in_=stT[:]), r=["stT"], w=[("Tb", pi)])
            p.op("act", lambda e, pi=pi: e.activation(out=Xb[:, pi], in_=stX[:], func=AF.Copy), r=["stX"], w=[("Xb", pi)])
            p.op("dve", lambda e, pi=pi: e.tensor_copy(out=Ob[:, pi], in_=stO[:]), r=["stO"], w=[("Ob", pi)])
            SAf = SA[:, pi].rearrange("p k b -> p (k b)")
            SWf = SW[:, pi].rearrange("p k b -> p (k b)")
            for (c0, cw) in tiles(NCOL, 512):
                for w_, dstf, key in ((0, SAf, "SA"), (1, SWf, "SW")):
                    for qk in range(KP):
                        p.op("pe", lambda e, pi=pi, w_=w_, qk=qk, c0=c0, cw=cw: e.matmul(px[w_][:, :cw], lhsT=Xb[:, pi, w_, qk, :], rhs=Ub[:, pi, qk, c0:c0 + cw],
                                                                                     start=(qk == 0), stop=(qk == KP - 1)),
                             r=[("Xb", pi), ("Ub", pi)], w=[("px", w_)])
                    if w_ == 0:
                        p.op("act", lambda e, dstf=dstf, c0=c0, cw=cw: e.activation(out=dstf[:, c0:c0 + cw], in_=px[0][:, :cw], func=AF.Copy),
                             r=[("px", 0)], w=[("SA", pi)])
                    else:
                        p.op("dve", lambda e, dstf=dstf, c0=c0, cw=cw: e.tensor_copy(out=dstf[:, c0:c0 + cw], in_=px[1][:, :cw]),
                             r=[("px", 1)], w=[("SW", pi)])
        cab = ca[:, pg0:pg0 + PG].unsqueeze(2).broadcast_to([128, PG, B])
        cbb = cb[:, pg0:pg0 + PG].unsqueeze(2).broadcast_to([128, PG, B])
        allSA = [("SA", pi) for pi in range(PG)]
        allSW = [("SW", pi) for pi in range(PG)]
        for k in range(1, NCK):
            rA = allSA if k == 1 else [("SAk", k - 1)]
            rW = allSW if k == 1 else [("SWk", k - 1)]
            wA = (allSA if k == 1 else []) + [("SAk", k)]
            wW = (allSW if k == 1 else []) + [("SWk", k)]
            Sp = SA[:, :, k - 1, :]; Wp = SW[:, :, k - 1, :]; Sk = SA[:, :, k, :]; Wk = SW[:, :, k, :]
            WE = S5_WCHAIN_ENG
            opsA = [
                ("dve", lambda e, Sp=Sp, cab=cab: e.tensor_tensor(out=tq[0][:], in0=Sp, in1=cab, op=ALU.mult), rA + ["ca"], ["tq0"]),
                ("dve", lambda e, Wp=Wp, cbb=cbb: e.tensor_tensor(out=tq[1][:], in0=Wp, in1=cbb, op=ALU.mult), rW + ["cb"], ["tq1"]),
                ("dve", lambda e: e.tensor_tensor(out=tq[0][:], in0=tq[0][:], in1=tq[1][:], op=ALU.add), ["tq0", "tq1"], ["tq0"]),
                ("dve", lambda e, Sk=Sk: e.tensor_tensor(out=Sk, in0=Sk, in1=tq[0][:], op=ALU.add), ["tq0"] + rA, wA),
            ]
            opsW = [
                (WE, lambda e, Wp=Wp, cab=cab: e.tensor_tensor(out=tq[2][:], in0=Wp, in1=cab, op=ALU.mult), rW + ["ca"], ["tq2"]),
                (WE, lambda e, Sp=Sp, cbb=cbb: e.tensor_tensor(out=tq[3][:], in0=Sp, in1=cbb, op=ALU.mult), rA + ["cb"], ["tq3"]),
                (WE, lambda e: e.tensor_tensor(out=tq[2][:], in0=tq[2][:], in1=tq[3][:], op=ALU.subtract), ["tq2", "tq3"], ["tq2"]),
                (WE, lambda e, Wk=Wk: e.tensor_tensor(out=Wk, in0=Wk, in1=tq[2][:], op=ALU.add), ["tq2"] + rW, wW),
            ]
            seq = [x for pair in zip(opsA, opsW) for x in pair] if WE == "dve" else opsA + opsW
            if WE == "dve":
                seq = [opsA[0], opsW[0], opsA[1], opsW[1], opsA[2], opsW[2], opsA[3], opsW[3]]
            for (eng_, fn_, r_, w_) in seq:
                p.op(eng_, fn_, r=r_, w=w_)
        fin = [("SAk", NCK - 1), ("SWk", NCK - 1)] + allSA + allSW
        p.op("dve", lambda e: e.memset(SAb[:, :, 0, :], 0.0), r=fin, w=["SAb"])
        p.op("act", lambda e: e.activation(out=SAb[:, :, 1:NCK, :], in_=SA[:, :, 0:NCK - 1, :], func=AF.Copy), r=fin + [("SAk", k) for k in range(1, NCK)], w=["SAb"])
        for pi in range(PG):
            pr = pg0 + pi
            SAbf = SAb[:, pi].rearrange("p k b -> p (k b)")
            for mb in range(KP):
                for (c0, cw) in tiles(NCOLL, 512):
                    q = yi % 2
                    yi += 1
                    a0 = CL0 + c0
                    for qk in range(mb + 1):
                        p.op("pe", lambda e, pi=pi, mb=mb, qk=qk, q=q, a0=a0, cw=cw: e.matmul(
                            py[q][:, :cw], lhsT=Tb[:, pi, qk, mb * 128:(mb + 1) * 128], rhs=Ub[:, pi, qk, a0:a0 + cw], start=(qk == 0), stop=False),
                            r=[("Tb", pi), ("Ub", pi)], w=[("py", q)])
                    p.op("pe", lambda e, pi=pi, mb=mb, q=q, a0=a0, cw=cw, SAbf=SAbf: e.matmul(
                        py[q][:, :cw], lhsT=Ob[:, pi, mb * 128:(mb + 1) * 128], rhs=SAbf[:, a0:a0 + cw], start=False, stop=True),
                        r=[("Ob", pi), "SAb"], w=[("py", q)])
                    if q:
                        p.op("act", lambda e, q=q, cw=cw: e.activation(out=yo[q][:, :cw], in_=py[q][:, :cw], func=AF.Copy), r=[("py", q)], w=[("yo", q)])
                    else:
                        p.op("dve", lambda e, q=q, cw=cw: e.tensor_copy(out=yo[q][:, :cw], in_=py[q][:, :cw]), r=[("py", q)], w=[("yo", q)])
                    p.dma("sync", Y[pr, :, mb, c0:c0 + cw], yo[q][:, :cw], r=[("yo", q)], w=[("Y", pr, mb, c0)], grp=("yst", q))
        if S5_GROUP_BARRIER:
            p.barrier()
    return p.build()


def s5_matrices(cfg, prep, CH):
    G = cfg.G
    P_, H = S5P, S5H
    KR = CH * H
    KP = KR // 128
    XB, OC, Mo, PC = prep["XB"], prep["OC"], prep["Mo"], prep["PC"]
    NPall = G * 2
    Tm = np.zeros((NPall, KR, KR), np.float32); Xm = np.zeros((NPall, 2, KR, 128), np.float32); Om = np.zeros((NPall, 128, KR), np.float32)
    CA = np.zeros((128, NPall), np.float32); CB = np.zeros((128, NPall), np.float32)
    for g in range(G):
        for d in range(2):
            pr = g * 2 + d
            for j in range(CH):
                for j2 in range(j, CH):
                    Tm[pr, j * H:(j + 1) * H, j2 * H:(j2 + 1) * H] = Mo[j2 - j][g, d].T
                xb = XB[CH - 1 - j][g, :, d]
                Xm[pr, 0, j * H:(j + 1) * H, 0:P_] = xb[0].T; Xm[pr, 0, j * H:(j + 1) * H, P_:] = xb[1].T
                Xm[pr, 1, j * H:(j + 1) * H, 0:P_] = xb[1].T; Xm[pr, 1, j * H:(j + 1) * H, P_:] = xb[0].T
                oc = OC[j][g, :, d]
                Om[pr, 0:P_, j * H:(j + 1) * H] = oc[0].T; Om[pr, P_:, j * H:(j + 1) * H] = oc[1].T
            CA[0:P_, pr] = PC[g, 0, d]; CA[P_:, pr] = PC[g, 0, d]
            CB[0:P_, pr] = PC[g, 2, d]; CB[P_:, pr] = PC[g, 1, d]
    Tm = np.ascontiguousarray(Tm.reshape(NPall, KP, 128, KR).transpose(0, 2, 1, 3))
    Xm = np.ascontiguousarray(Xm.reshape(NPall, 2, KP, 128, 128).transpose(0, 3, 1, 2, 4))
    return Tm, Xm, Om, CA, CB


def run_s5(cfg, mats, u_lat, u_ctx, CH, PG):
    import ml_dtypes
    bf = ml_dtypes.bfloat16
    B, L, LC, D, NC = cfg.B, cfg.L, cfg.LC, cfg.D, cfg.NCORE
    GPC = cfg.G // NC
    NPr = 2 * GPC
    H = S5H
    KR = CH * H; KP = KR // 128
    LT = LC + L; NCK = LT // CH; NCOL = NCK * B
    CL0 = (LC // CH) * B
    Tm, Xm, Om, CA, CB = mats
    nc = build_s5(cfg, CH, PG)
    seqs = [np.concatenate([u_ctx, u_lat], 1), np.concatenate([u_ctx[:, ::-1], u_lat[:, ::-1]], 1)]
    ims = []
    for c in range(NC):
        U = np.zeros((NPr, 128, KP, NCOL), bf)
        for gi in range(GPC):
            g = c * GPC + gi
            for d in range(2):
                s = seqs[d][:, :, g * H:(g + 1) * H]
                s = s.reshape(B, NCK, CH, H).transpose(2, 3, 1, 0)
                U[gi * 2 + d] = s.reshape(KP, 128, NCOL).transpose(1, 0, 2)
        sl = slice(c * NPr, (c + 1) * NPr)
        ims.append({"U": U, "Tm": Tm[sl], "Xm": Xm[sl], "Om": Om[sl], "CA": np.ascontiguousarray(CA[:, sl]), "CB": np.ascontiguousarray(CB[:, sl])})
    res = run(nc, ims)
    ys = [np.zeros((B, L, D), bf), np.zeros((B, L, D), bf)]
    for c in range(NC):
        Yc = np.asarray(res[c]["Y"])
        for gi in range(GPC):
            g = c * GPC + gi
            for d in range(2):
                a = Yc[gi * 2 + d].transpose(1, 0, 2).reshape(CH, H, L // CH, B)
                a = a.transpose(3, 2, 0, 1).reshape(B, L, H)
                if d == 1:
                    a = a[:, ::-1]
                ys[d][:, :, g * H:(g + 1) * H] = a
    return ys


def build_glu(cfg):
    p = Prog()
    ND, D, TL = cfg.ND, cfg.D, cfg.TL
    TK = 128
    TB = 512
    yfT = p.din("yfT", [128, ND, TL], BF16); ybT = p.din("ybT", [128, ND, TL], BF16); uT = p.din("uT", [128, ND, TL], BF16)
    xT = p.din("xT", [128, ND, TL]); w1 = p.din("w1", [128, ND, D]); w2 = p.din("w2", [128, ND, D]); vecs = p.din("vecsD", [128, 7, ND])
    wr_d = p.din("wrD", [128, ND, 128]); br_d = p.din("brD", [128, 1]); ident_d = p.din("identD", [128, 128])
    xlT = p.dout("xlT", [128, ND, TL]); tokT = p.dout("tokT", [128, ND, TL], BF16); gates = p.dout("gates", [TL, 32])
    C = PostCtx(p, cfg, TK)
    p.dma("sync", C.ident[:], ident_d[:, :], w=["ident"]); p.dma("sync", C.wr[:], wr_d[:, :, :], w=["wr"]); p.dma("sync", C.br[:], br_d[:, :], w=["br"])
    vs_ = p.sb("vecs", [128, 7, ND]); p.dma("sync", vs_[:], vecs[:, :, :], w=["vecs"])
    m2 = p.sb("m2", [128, ND])
    p.op("dve", lambda e: e.scalar_tensor_tensor(out=m2[:, :], in0=vs_[:, 5, :], scalar=1.0, in1=vs_[:, 4, :], op0=ALU.add, op1=ALU.mult), r=["vecs"], w=["vecs"])
    a = p.sb("a_all", [128, ND, TL], BF16)
    yf = p.sb("yf", [128, ND, TK], BF16); yb = p.sb("yb", [128, ND, TK], BF16); uu = p.sb("uu", [128, ND, TK], BF16)
    ys, yt = C.xt, C.sq
    GC = 2.0 * float(np.sqrt(2.0 / np.pi))
    for (c0, cw) in tiles(TL, TK):
        p.dma("sync", yf[:, :, :cw], yfT[:, :, c0:c0 + cw], w=["yf"])
        p.dma("pool", yb[:, :, :cw], ybT[:, :, c0:c0 + cw], w=["yb"])
        p.dma("sync", uu[:, :, :cw], uT[:, :, c0:c0 + cw], w=["uu"])
        p.op("dve", lambda e, cw=cw: e.tensor_tensor(out=ys[:, :, :cw], in0=yf[:, :, :cw], in1=yb[:, :, :cw], op=ALU.add), r=["yf", "yb"], w=["xt"])
        for k in range(ND):
            p.op("dve", lambda e, k=k, cw=cw: e.scalar_tensor_tensor(out=ys[:, k, :cw], in0=uu[:, k, :cw], scalar=vs_[:, 0, k:k + 1], in1=ys[:, k, :cw],
                                                                   op0=ALU.mult, op1=ALU.add), r=["uu", "xt", "vecs"], w=["xt"])
        p.op("pool", lambda e, cw=cw: e.tensor_tensor(out=yt[:, :, :cw], in0=ys[:, :, :cw], in1=ys[:, :, :cw], op=ALU.mult), r=["xt"], w=["sq"])
        p.op("dve", lambda e, cw=cw: e.tensor_scalar(out=yt[:, :, :cw], in0=yt[:, :, :cw], scalar1=0.044715, scalar2=1.0, op0=ALU.mult, op1=ALU.add), r=["sq"], w=["sq"])
        p.op("pool", lambda e, cw=cw: e.tensor_tensor(out=yt[:, :, :cw], in0=yt[:, :, :cw], in1=ys[:, :, :cw], op=ALU.mult), r=["sq", "xt"], w=["sq"])
        p.op("act", lambda e, cw=cw: e.activation(out=yt[:, :, :cw], in_=yt[:, :, :cw], func=AF.Sigmoid, scale=GC), r=["sq"], w=["sq"])
        p.op("dve", lambda e, c0=c0, cw=cw: e.tensor_tensor(out=a[:, :, c0:c0 + cw], in0=yt[:, :, :cw], in1=ys[:, :, :cw], op=ALU.mult), r=["sq", "xt"], w=["a"])
    wf = [[p.sb("wf%d%d" % (i, j), [128, ND, 128]) for j in range(2)] for i in range(2)]
    wb = [[p.sb("wb%d%d" % (i, j), [128, ND, 128], BF16) for j in range(2)] for i in range(2)]
    xb_ = [p.sb("xb%d" % i, [128, TB]) for i in range(2)]
    ob_ = [p.sb("ob%d" % i, [128, TB]) for i in range(2)]
    sg = p.sb("sg", [128, TB]); ol = p.sb("ol", [128, TB])
    pz1 = [p.ps("pza%d" % i, [128, TB]) for i in range(2)]
    pz2 = [p.ps("pzb0", [128, TB])] * 2
    qi = 0
    for blk in range(ND):
        s = blk % 2
        for j, src in enumerate((w1, w2)):
            p.dma("sync" if j else "pool", wf[s][j][:], src[:, :, blk * 128:(blk + 1) * 128], w=[("wf", s, j)])
            if j:
                p.op("act", lambda e, s=s, j=j: e.activation(out=wb[s][j][:], in_=wf[s][j][:], func=AF.Copy), r=[("wf", s, j)], w=[("wb", s, j)])
            else:
                p.op("pool", lambda e, s=s, j=j: e.tensor_copy(out=wb[s][j][:], in_=wf[s][j][:]), r=[("wf", s, j)], w=[("wb", s, j)])
        for (c0, cw) in tiles(TL, TB):
            q = qi % 2
            qi += 1
            p.dma("sync", xb_[q][:, :cw], xT[:, blk, c0:c0 + cw], w=[("xb", q)])
            for k in range(ND):
                p.op("pe", lambda e, q=q, k=k, s=s, c0=c0, cw=cw: e.matmul(pz1[q][:, :cw], lhsT=wb[s][0][:, k, :], rhs=a[:, k, c0:c0 + cw],
                                                                          start=(k == 0), stop=(k == ND - 1)), r=[("wb", s, 0), "a"], w=[("pza", q)])
            for k in range(ND):
                p.op("pe", lambda e, q=q, k=k, s=s, c0=c0, cw=cw: e.matmul(pz2[q][:, :cw], lhsT=wb[s][1][:, k, :], rhs=a[:, k, c0:c0 + cw],
                                                                          start=(k == 0), stop=(k == ND - 1)), r=[("wb", s, 1), "a"], w=[("pzb", 0)])
            p.op("act", lambda e, q=q, blk=blk, cw=cw: e.activation(out=sg[:, :cw], in_=pz2[q][:, :cw], func=AF.Sigmoid, bias=vs_[:, 2, blk:blk + 1], scale=1.0),
                 r=[("pzb", 0), "vecs"], w=["sg"])
            p.op("dve", lambda e, q=q, blk=blk, cw=cw: e.scalar_tensor_tensor(out=ol[:, :cw], in0=pz1[q][:, :cw], scalar=vs_[:, 1, blk:blk + 1], in1=sg[:, :cw],
                                                                            op0=ALU.add, op1=ALU.mult), r=[("pza", q), "sg", "vecs"], w=["ol"])
            p.op("dve", lambda e, q=q, blk=blk, cw=cw: e.scalar_tensor_tensor(out=ob_[q][:, :cw], in0=ol[:, :cw], scalar=vs_[:, 3, blk:blk + 1], in1=xb_[q][:, :cw],
                                                                            op0=ALU.mult, op1=ALU.add), r=["ol", ("xb", q), "vecs"], w=[("ob", q)])
            p.dma("pool", xlT[:, blk, c0:c0 + cw], ob_[q][:, :cw], r=[("ob", q)], w=[("xlo", blk, c0)], grp=("xlst", q))
    for (c0, cw) in tiles(TL, TK):
        tb0 = (c0 // TB) * TB
        p.dma("sync", C.xl[:, :, :cw], xlT[:, :, c0:c0 + cw], r=[("xlo", blk, tb0) for blk in range(ND)], w=["xl"])
        emit_norm_router(p, cfg, C, cw, lambda k: m2[:, k:k + 1], lambda k: vs_[:, 6, k:k + 1], tokT[:, :, c0:c0 + cw],
                         lambda t0, tw, c0=c0: gates[c0 + t0:c0 + t0 + tw, :])
    return p.build()


def lat_layout(cfg, lat):
    return [lat[c // cfg.CPB, (c % cfg.CPB) * cfg.TL:(c % cfg.CPB + 1) * cfg.TL] for c in range(cfg.NCORE)]


def lat_unlayout(cfg, per):
    out = np.zeros((cfg.B, cfg.L, per[0].shape[1]), per[0].dtype)
    for c in range(cfg.NCORE):
        out[c // cfg.CPB, (c % cfg.CPB) * cfg.TL:(c % cfg.CPB + 1) * cfg.TL] = per[c]
    return out


def run_glu(cfg, I, mods, yf, yb, u_lat, xl_lat):
    import ml_dtypes
    bf = ml_dtypes.bfloat16
    ND, NC = cfg.ND, cfg.NCORE
    nc = build_glu(cfg)
    wr, br = router_inputs(cfg, I, 1)
    w1 = wfm(I["s5_w1"][0], ND); w2 = wfm(I["s5_w2"][0], ND)
    yfs, ybs, us, xs = lat_layout(cfg, yf), lat_layout(cfg, yb), lat_layout(cfg, u_lat), lat_layout(cfg, xl_lat)
    ims = []
    for c in range(NC):
        mvd = mod_vecs(cfg, mods[1], c // cfg.CPB)
        vecs = np.stack([vfm(np.asarray(v, np.float32), ND) for v in (I["s5_d"][0], I["s5_b1"][0], I["s5_b2"][0], mvd["gt_a"], I["norm_g"][1, 1],
                                                                        mvd["sc_f"], mvd["sh_f"])], axis=1)
        ims.append({"yfT": fm(yfs[c], ND).astype(bf), "ybT": fm(ybs[c], ND).astype(bf), "uT": fm(us[c], ND).astype(bf),
                    "xT": fm(xs[c].astype(np.float32), ND), "w1": w1, "w2": w2, "vecsD": np.ascontiguousarray(vecs),
                    "wrD": wr, "brD": br, "identD": np.eye(128, dtype=np.float32)})
    res = run(nc, ims)
    xl = lat_unlayout(cfg, [unfm(r["xlT"]) for r in res])
    tok = lat_unlayout(cfg, [unfm(np.asarray(r["tokT"])) for r in res])
    gates = lat_unlayout(cfg, [r["gates"] for r in res])
    return xl, tok, gates


def forward(cfg, I, CH=16, PG=8, CAP=1536, log=None):
    import time
    t0 = time.time()

    def lg(msg):
        if log:
            print("[fwd %.1fs] %s" % (time.time() - t0, msg), flush=True)
    D = cfg.D
    mods = run_mods(cfg, I); lg("mods")
    filt = run_filt(cfg, I); lg("filt")
    (v_l, v_c), (x0_l, x0_c) = run_h1(cfg, I, mods); lg("h1")
    y_l, y_c = run_h2r(cfg, I, v_l, v_c, filt); lg("h2r")
    xl, tok, gates = run_h3(cfg, I, mods, y_l, y_c, x0_l, x0_c); lg("h3")
    tok_all = np.concatenate([tok[0].reshape(-1, D), tok[1].reshape(-1, D)], 0)
    gates_all = np.concatenate([gates[0].reshape(-1, 32), gates[1].reshape(-1, 32)], 0)
    y01, g01 = run_moe(cfg, I, 0, tok_all, gates_all, CAP); lg("moe0")
    xl_lat, xl_ctx, u_lat, u_ctx = run_comb(cfg, I, mods, 0, xl[0], xl[1], y01, g01, False); lg("comb0")
    prep = run_s5prep(cfg, I, CH); lg("s5prep")
    mats = s5_matrices(cfg, prep, CH); lg("s5mats")
    yf, yb = run_s5(cfg, mats, u_lat, u_ctx, CH, PG); lg("s5")
    xl3, tok1, gates1 = run_glu(cfg, I, mods, yf, yb, u_lat, xl_lat); lg("glu")
    y01, g01 = run_moe(cfg, I, 1, tok1.reshape(-1, D), gates1.reshape(-1, 32), CAP); lg("moe1")
    _, _, out, _ = run_comb(cfg, I, mods, 1, xl3, None, y01, g01, True); lg("final")
    return np.ascontiguousarray(out.astype(np.float32))


def kernel(**inputs):
    I = {k: np.asarray(v) for k, v in inputs.items()}
    return forward(FULL, I, CH=16, PG=8, CAP=None, log=True)


def dft_tables_r2(Lx):
    M = 4 * Lx
    NH = max(1, Lx // 256)
    half = Lx // 2

    def cis(k, mask=None):
        ang = -2.0 * np.pi * (np.asarray(k, np.int64) % M).astype(np.float64) / M
        t = np.stack([np.cos(ang), np.sin(ang)]).astype(np.float32)
        if mask is not None:
            t = t * mask[None].astype(np.float32)
        return t

    p = np.arange(128)[:, None, None]
    i = np.arange(NH)[None, :, None]
    q = np.arange(128)[None, None, :]
    T = []; TI = []
    for r in range(2):
        m = ((128 * i + p) < half) & (q < half) & np.ones((128, NH, 128), bool)
        T.append(cis((2 * q + 1) * (256 * i + 2 * p + r), m))
        qq = np.arange(128)[:, None, None]; jj = np.arange(NH)[None, :, None]; pp = np.arange(128)[None, None, :]
        mi = ((128 * jj + qq) < half) & (pp < half) & np.ones((128, NH, 128), bool)
        TI.append(cis((256 * jj + 2 * qq + 1) * (2 * pp + r), mi))
    pj = np.arange(128)[:, None, None]; j2 = np.arange(NH)[None, :, None]; r2 = np.arange(2)[None, None, :]
    AL = cis(256 * j2 * (2 * pj + r2))
    qi = np.arange(128)[:, None]; i2 = np.arange(NH)[None, :]
    ALI = cis((2 * qi + 1) * 256 * i2)
    return (np.ascontiguousarray(np.stack(T)), np.ascontiguousarray(np.stack(TI)), np.ascontiguousarray(AL), np.ascontiguousarray(ALI))


def build_h2r(cfg):
    p = Prog()
    B, Cc = cfg.B, cfg.Cc
    ncol = B * Cc
    CW = min(getattr(cfg, 'H2_CW', 512), ncol)
    nbp = CW // Cc
    NHmax = max(1, max(cfg.L, cfg.LC) // 256)
    RAWN = max(2 * NHmax * CW, 2 * NHmax * 128)
    raw = p.sb("raw", [128, RAWN])
    es = [[[p.sb("es%d%d%d" % (a, r, c), [128, NHmax, 128], BF16) for c in range(2)] for r in range(2)] for a in range(2)]
    vs = p.sb("vs", [128, 2, NHmax, CW], BF16)
    hsd = p.sb("hsd", [128, 2, 2, NHmax, Cc], BF16)
    skb = p.sb("skb", [128, ncol])
    kk = p.sb("kk", [128, 2, 2, Cc])
    p1 = p.sb("p1", [128, 2, CW])
    V = p.sb("V", [128, 2, 2, CW])
    Yt = p.sb("Yt", [128, 2, 2, CW])
    tt = [p.sb("tt%d" % i, [128, CW]) for i in range(2)]
    yo = p.sb("yo", [128, CW], BF16)
    pk1 = p.sb("pk1", [128, Cc])
    pV = [[p.ps("pV%d%d" % (r, c), [128, 512]) for c in range(2)] for r in range(2)]
    pK = [p.ps("pK%d" % r, [128, 512]) for r in range(2)]
    pY = p.ps("pY", [128, 512])

    def conv_li(li, Lx, skip):
        NH = max(1, Lx // 256)
        v = p.din("v%d" % li, [128, 2, NH, ncol], BF16)
        hsdi = p.din("hsd%d" % li, [128, 2, 2, NH, Cc], BF16)
        T = p.din("T_%d" % li, [2, 2, 128, NH, 128]); TI = p.din("TI_%d" % li, [2, 2, 128, NH, 128])
        AL = p.din("AL_%d" % li, [128, 2, NH, 2]); ALI = p.din("ALI_%d" % li, [128, 2, NH])
        al = p.sb("al%d" % li, [128, 2, NH, 2]); ali = p.sb("ali%d" % li, [128, 2, NH])
        if li == 0:
            skip = p.din("skip0", [128, ncol])
        y = p.dout("y%d" % li, [128, 2, NH, ncol], BF16)
        Es = p.dtmp("Es%d" % li, [2, NH, 2, 128, NH, 128], BF16)
        Gs = p.dtmp("Gs%d" % li, [2, NH, 2, 128, NH, 128], BF16)
        Ks = p.dtmp("Ks%d" % li, [NH, 128, 2, 2, Cc])
        p.barrier()
        Mv = NH * 128
        bre = raw[:, 0:Mv]; bim = raw[:, Mv:2 * Mv]
        t1 = p_t1[:, :Mv]; t2 = p_t2[:, :Mv]
        p.dma("sync", al[:], AL[:, :, :, :], w=["al"])
        p.dma("sync", ali[:], ALI[:, :, :], w=["ali"])
        bi = 0
        for (Tt, dst, inv) in ((T, Es, False), (TI, Gs, True)):
            for r in range(2):
                p.dma("sync", bre, Tt[r, 0].rearrange("p i q -> p (i q)"), w=["bre"])
                p.dma("pool", bim, Tt[r, 1].rearrange("p i q -> p (i q)"), w=["bim"])
                for j in range(NH):
                    s = bi % 2
                    bi += 1
                    are = ali[:, 0, j:j + 1] if inv else al[:, 0, j, r:r + 1]
                    aim = ali[:, 1, j:j + 1] if inv else al[:, 1, j, r:r + 1]
                    ere = es[s][0][0][:, :NH, :].rearrange("p i q -> p (i q)")
                    eim = es[s][0][1][:, :NH, :].rearrange("p i q -> p (i q)")
                    ak = "ali" if inv else "al"
                    p.op("act", lambda e, aim=aim: e.activation(out=t1, in_=bim, func=AF.Copy, scale=aim), r=["bim", ak], w=["t1"])
                    p.op("dve", lambda e, are=are, ere=ere: e.scalar_tensor_tensor(out=ere, in0=bre, scalar=are, in1=t1, op0=ALU.mult, op1=ALU.subtract),
                         r=["bre", "t1", ak], w=[("es", s, 0, 0)])
                    p.op("act", lambda e, aim=aim: e.activation(out=t2, in_=bre, func=AF.Copy, scale=aim), r=["bre", ak], w=["t2"])
                    p.op("dve", lambda e, are=are, eim=eim: e.scalar_tensor_tensor(out=eim, in0=bim, scalar=are, in1=t2, op0=ALU.mult, op1=ALU.add),
                         r=["bim", "t2", ak], w=[("es", s, 0, 1)])
                    p.dma("sync", dst[r, j, 0], es[s][0][0][:, :NH, :], r=[("es", s, 0, 0)], w=[("tab", li, inv, r, j, 0)], grp=("tst", s, 0))
                    p.dma("sync", dst[r, j, 1], es[s][0][1][:, :NH, :], r=[("es", s, 0, 1)], w=[("tab", li, inv, r, j, 1)], grp=("tst", s, 1))
        p.barrier()
        Zv = raw[:, 0:2 * NH * CW].bitcast(BF16).rearrange("p (r c j w) -> p r c j w", r=2, c=2, j=NH)
        p.dma("sync", hsd[:, :, :, :NH, :], hsdi[:, :, :, :, :], w=["hsd"])
        if li == 0:
            p.dma("sync", skb[:], skip[:, :], w=["skb"])
        for c0 in range(0, ncol, CW):
            first_pass = (c0 == 0)
            p.dma("sync", vs[:, :, :NH, :], v[:, :, :, c0:c0 + CW], w=["vs"])
            for j in range(NH):
                s = j % 2
                for r in range(2):
                    for c in range(2):
                        p.dma("pool" if c else "sync", es[s][r][c][:, :NH, :], Es[r, j, c], w=[("es", s, r, c)])
                for r in range(2):
                    for c in range(2):
                        for i in range(NH):
                            p.op("pe", lambda e, s=s, r=r, c=c, i=i: e.matmul(pV[r][c][:, :CW], lhsT=es[s][r][c][:, i, :], rhs=vs[:, r, i, :],
                                                                             start=(i == 0), stop=(i == NH - 1)),
                                 r=[("es", s, r, c), "vs"], w=[("pV", r, c)])
                if first_pass:
                    for c in range(2):
                        for r in range(2):
                            for i in range(NH):
                                p.op("pe", lambda e, s=s, r=r, c=c, i=i: e.matmul(pK[r][:, :Cc], lhsT=es[s][r][c][:, i, :], rhs=hsd[:, c, r, i, :],
                                                                                 start=(i == 0), stop=(i == NH - 1)),
                                     r=[("es", s, r, c), "hsd"], w=[("pK", r)])
                        p.op("act", lambda e: e.activation(out=pk1[:, :], in_=pK[1][:, :Cc], func=AF.Copy), r=[("pK", 1)], w=["pk1"])
                        p.op("dve", lambda e, c=c: e.tensor_tensor(out=kk[:, 0, c, :], in0=pK[0][:, :Cc], in1=pk1[:, :], op=ALU.add), r=[("pK", 0), "pk1"], w=[("kk", 0, c)])
                        if c == 0:
                            p.op("dve", lambda e, c=c: e.tensor_tensor(out=kk[:, 1, c, :], in0=pK[0][:, :Cc], in1=pk1[:, :], op=ALU.subtract), r=[("pK", 0), "pk1"], w=[("kk", 1, c)])
                        else:
                            p.op("dve", lambda e, c=c: e.scalar_tensor_tensor(out=kk[:, 1, c, :], in0=pK[0][:, :Cc], scalar=-1.0, in1=pk1[:, :], op0=ALU.mult, op1=ALU.add),
                                 r=[("pK", 0), "pk1"], w=[("kk", 1, c)])
                    kkeys = [("kk", h, c) for h in range(2) for c in range(2)]
                    if ncol > CW:
                        p.dma("pool", Ks[j], kk[:, :, :, :], r=kkeys, w=[("Ks", li, j)], grp=("kst",))
                else:
                    kkeys = [("kk", h, c) for h in range(2) for c in range(2)]
                    p.dma("pool", kk[:, :, :, :], Ks[j], r=[("Ks", li, j)], w=kkeys, grp=("kld",))
                for c in range(2):
                    p.op("act", lambda e, c=c: e.activation(out=p1[:, c, :], in_=pV[1][c][:, :CW], func=AF.Copy), r=[("pV", 1, c)], w=[("p1", c)])
                    p.op("dve", lambda e, c=c: e.tensor_tensor(out=V[:, 0, c, :], in0=pV[0][c][:, :CW], in1=p1[:, c, :], op=ALU.add),
                         r=[("pV", 0, c), ("p1", c)], w=[("V", 0, c)])
                    if c == 0:
                        p.op("dve", lambda e, c=c: e.tensor_tensor(out=V[:, 1, c, :], in0=pV[0][c][:, :CW], in1=p1[:, c, :], op=ALU.subtract),
                             r=[("pV", 0, c), ("p1", c)], w=[("V", 1, c)])
                    else:
                        p.op("dve", lambda e, c=c: e.scalar_tensor_tensor(out=V[:, 1, c, :], in0=pV[0][c][:, :CW], scalar=-1.0, in1=p1[:, c, :], op0=ALU.mult, op1=ALU.add),
                             r=[("pV", 0, c), ("p1", c)], w=[("V", 1, c)])
                for h in range(2):
                    kre = kk[:, h, 0, :].unsqueeze(1).broadcast_to([128, nbp, Cc])
                    kim = kk[:, h, 1, :].unsqueeze(1).broadcast_to([128, nbp, Cc])
                    vre = V[:, h, 0, :].rearrange("p (b c) -> p b c", c=Cc)
                    vim = V[:, h, 1, :].rearrange("p (b c) -> p b c", c=Cc)
                    yre = Yt[:, h, 0, :].rearrange("p (b c) -> p b c", c=Cc)
                    yim = Yt[:, h, 1, :].rearrange("p (b c) -> p b c", c=Cc)
                    t3 = [t[:, :].rearrange("p (b c) -> p b c", c=Cc) for t in tt]
                    eng = "dve" if h == 0 else "pool"
                    tk = ["tt0", "tt1"]
                    p.op(eng, lambda e, vre=vre, kre=kre, t3=t3: e.tensor_tensor(out=t3[0], in0=vre, in1=kre, op=ALU.mult), r=[("V", h, 0), ("kk", h, 0)], w=["tt0"])
                    p.op(eng, lambda e, vim=vim, kim=kim, t3=t3: e.tensor_tensor(out=t3[1], in0=vim, in1=kim, op=ALU.mult), r=[("V", h, 1), ("kk", h, 1)], w=["tt1"])
                    p.op(eng, lambda e, yre=yre, t3=t3: e.tensor_tensor(out=yre, in0=t3[0], in1=t3[1], op=ALU.subtract), r=["tt0", "tt1"], w=[("Yt", h, 0)])
                    p.op(eng, lambda e, vre=vre, kim=kim, t3=t3: e.tensor_tensor(out=t3[0], in0=vre, in1=kim, op=ALU.mult), r=[("V", h, 0), ("kk", h, 1)], w=["tt0"])
                    p.op(eng, lambda e, vim=vim, kre=kre, t3=t3: e.tensor_tensor(out=t3[1], in0=vim, in1=kre, op=ALU.mult), r=[("V", h, 1), ("kk", h, 0)], w=["tt1"])
                    p.op(eng, lambda e, yim=yim, t3=t3: e.tensor_tensor(out=yim, in0=t3[0], in1=t3[1], op=ALU.add), r=["tt0", "tt1"], w=[("Yt", h, 1)])
                yk = [("Yt", h, c) for h in range(2) for c in range(2)]
                p.op("dve", lambda e, j=j: e.tensor_tensor(out=Zv[:, 0, 0, j, :], in0=Yt[:, 0, 0, :], in1=Yt[:, 1, 0, :], op=ALU.add), r=yk, w=[("Z", j)])
                p.op("pool", lambda e, j=j: e.tensor_tensor(out=Zv[:, 0, 1, j, :], in0=Yt[:, 0, 1, :], in1=Yt[:, 1, 1, :], op=ALU.subtract), r=yk, w=[("Z", j, 1)])
                p.op("dve", lambda e, j=j: e.tensor_tensor(out=Zv[:, 1, 0, j, :], in0=Yt[:, 0, 0, :], in1=Yt[:, 1, 0, :], op=ALU.subtract), r=yk, w=[("Z", j, 2)])
                p.op("pool", lambda e, j=j: e.tensor_tensor(out=Zv[:, 1, 1, j, :], in0=Yt[:, 0, 1, :], in1=Yt[:, 1, 1, :], op=ALU.add), r=yk, w=[("Z", j, 3)])
            bi2 = 0
            for r in range(2):
                for i in range(NH):
                    s = bi2 % 2
                    bi2 += 1
                    for c in range(2):
                        p.dma("pool" if c else "sync", es[s][0][c][:, :NH, :], Gs[r, i, c], w=[("es", s, 0, c)])
                    n = 0
                    for j in range(NH):
                        for c in range(2):
                            p.op("pe", lambda e, s=s, j=j, c=c, n=n, r=r: e.matmul(pY[:, :CW], lhsT=es[s][0][c][:, j, :], rhs=Zv[:, r, c, j, :],
                                                                                  start=(n == 0), stop=(n == 2 * NH - 1)),
                                 r=[("es", s, 0, c), ("Z", j), ("Z", j, 1), ("Z", j, 2), ("Z", j, 3)], w=["pY"])
                            n += 1
                    p.op("pool", lambda e, i=i, r=r, c0=c0: e.tensor_tensor(out=tt[0][:, :], in0=vs[:, r, i, :], in1=skb[:, c0:c0 + CW], op=ALU.mult),
                         r=["vs", "skb"], w=["tt0"])
                    p.op("dve", lambda e, Lx=Lx: e.scalar_tensor_tensor(out=yo[:, :], in0=pY[:, :CW], scalar=1.0 / Lx, in1=tt[0][:, :],
                                                                       op0=ALU.mult, op1=ALU.add), r=["pY", "tt0"], w=["yo"])
                    p.dma("sync", y[:, r, i, c0:c0 + CW], yo[:, :], r=["yo"], w=[("y", li, r, i, c0)], grp=("yst",))
        return skip

    assert 4 * CW >= NHmax * 128
    p_t1 = V[:, :, :, :].rearrange("p a b w -> p (a b w)"); p_t2 = Yt[:, :, :, :].rearrange("p a b w -> p (a b w)")
    skip = None
    for li, Lx in enumerate((cfg.L, cfg.LC)):
        skip = conv_li(li, Lx, skip)
    return p.build()


def parity_layout(a, Lx, NH):
    X = a.shape[1]
    half = Lx // 2
    out = np.zeros((2, NH * 128, X), a.dtype)
    out[:, :half] = a.reshape(half, 2, X).transpose(1, 0, 2)
    return np.ascontiguousarray(out.reshape(2, NH, 128, X).transpose(2, 0, 1, 3))


def run_h2r(cfg, I, v_lat, v_ctx, filt):
    import ml_dtypes
    bf = ml_dtypes.bfloat16
    B, Cc, D, NC = cfg.B, cfg.Cc, cfg.D, cfg.NCORE
    ncol = B * Cc
    nc = build_h2r(cfg)
    tabs = [dft_tables_r2(Lx) for Lx in (cfg.L, cfg.LC)]
    ims = []
    for c in range(NC):
        m = {}
        for li, (Lx, vv) in enumerate(((cfg.L, v_lat), (cfg.LC, v_ctx))):
            NH = max(1, Lx // 256)
            vc = np.asarray(vv[:, :, c * Cc:(c + 1) * Cc]).transpose(1, 0, 2).reshape(Lx, ncol)
            m["v%d" % li] = parity_layout(vc.astype(bf), Lx, NH)
            hs, hd = filt[li]
            m["hsd%d" % li] = np.ascontiguousarray(np.stack([parity_layout(hs[:, c * Cc:(c + 1) * Cc].astype(bf), Lx, NH),
                                                             parity_layout(hd[:, c * Cc:(c + 1) * Cc].astype(bf), Lx, NH)], axis=1))
            T, TI, AL, ALI = tabs[li]
            m["T_%d" % li], m["TI_%d" % li] = T, TI
            m["AL_%d" % li] = np.ascontiguousarray(AL.transpose(1, 0, 2, 3)); m["ALI_%d" % li] = np.ascontiguousarray(ALI.transpose(1, 0, 2))
        sk = np.tile(I["hy_skip"][0][c * Cc:(c + 1) * Cc], B)
        m["skip0"] = np.ascontiguousarray(np.broadcast_to(sk[None, :], (128, ncol))).astype(np.float32)
        ims.append(m)
    res = run(nc, ims)
    outs = []
    for li, Lx in enumerate((cfg.L, cfg.LC)):
        NH = max(1, Lx // 256)
        half = Lx // 2
        yy = np.zeros((B, Lx, D), bf)
        for c in range(NC):
            a = np.asarray(res[c]["y%d" % li])
            a = a.transpose(1, 2, 0, 3).reshape(2, NH * 128, ncol)[:, :half]
            a = a.transpose(1, 0, 2).reshape(Lx, B, Cc)
            yy[:, :, c * Cc:(c + 1) * Cc] = a.transpose(1, 0, 2)
        outs.append(yy)
    return outs
```
